# Optimizing a Trainium2 kernel written in Bass

```python
import jax, jax.numpy as jnp
from jax import lax
import numpy as np

D_MODEL = 1024
BATCH = 2
SEQ = 8192
DEPTH = 1

GRID_W = 64
CTX_LEN = 256
N_MOD = 6
EPS = 1e-6
HGRN_HEADS = 8
HGRN_DK = 128
HGRN_DV = 128
HGRN_W = HGRN_HEADS * HGRN_DK
HGRN_CHUNK = 64
ATT_HEADS = 8
ATT_KV_HEADS = 2
ATT_GROUPS = ATT_HEADS // ATT_KV_HEADS
HEAD_DIM = 128
ATT_W = ATT_HEADS * HEAD_DIM
KV_W = ATT_KV_HEADS * HEAD_DIM
ROPE_AXIS_DIM = HEAD_DIM // 2
ROPE_THETA = 10000.0
Q_BLOCK = 128
N_BRANCH = 2
N_EXPERTS = 32
TOP_K = 4
D_EXPERT = 1024
SWIGLU_LIMIT = 7.0
SWIGLU_ALPHA = 1.702
IN_SIZES = (HGRN_W, HGRN_W, HGRN_W, HGRN_W, HGRN_W, ATT_W, KV_W, KV_W, D_MODEL, D_MODEL)
IN_COLS = sum(IN_SIZES)

kernel_name = "hybrid_hgrn2_gqa_moe_dit_layer"


def rmsnorm(x, g):
    xf = x.astype(jnp.float32)
    y = xf * lax.rsqrt(jnp.mean(xf * xf, axis=-1, keepdims=True) + EPS)
    return (y * g.astype(jnp.float32)).astype(x.dtype)


def modulation(cv, w, b):
    m = jax.nn.silu(cv) @ w + b
    return [a[:, None, :] for a in jnp.split(m, N_MOD, axis=-1)]


def modulate(xn, shift, scale):
    return xn * (1.0 + scale) + shift


def split_cols(p):
    return jnp.split(p, np.cumsum(IN_SIZES)[:-1].tolist(), axis=-1)


def axial_rope(x, rows, cols):
    inv = ROPE_THETA ** (-jnp.arange(0, ROPE_AXIS_DIM, 2, dtype=jnp.float32) / ROPE_AXIS_DIM)
    bshape = (x.shape[1],) + (1,) * (x.ndim - 3) + (ROPE_AXIS_DIM // 2,)

    def rot(xa, pos):
        ang = (pos.astype(jnp.float32)[:, None] * inv[None, :]).reshape(bshape)
        cos, sin = jnp.cos(ang), jnp.sin(ang)
        xf = xa.astype(jnp.float32)
        x1, x2 = xf[..., : ROPE_AXIS_DIM // 2], xf[..., ROPE_AXIS_DIM // 2:]
        return jnp.concatenate([x1 * cos - x2 * sin, x2 * cos + x1 * sin], axis=-1).astype(xa.dtype)

    return jnp.concatenate([rot(x[..., :ROPE_AXIS_DIM], rows), rot(x[..., ROPE_AXIS_DIM:], cols)], axis=-1)


def hgrn_gates(q_raw, i_raw, ff_raw, fb_raw, lb):
    B, L, _ = q_raw.shape

    def heads(a):
        return a.reshape(B, L, HGRN_HEADS, HGRN_DK).transpose(0, 2, 1, 3).astype(jnp.float32)

    def both(a_fwd, a_bwd):
        return jnp.stack([a_fwd, a_bwd[:, :, ::-1]], axis=0)

    q, v = heads(q_raw), heads(i_raw)
    lbh = lb.reshape(2, 1, HGRN_HEADS, 1, HGRN_DK)
    f = lbh + (1.0 - lbh) * jax.nn.sigmoid(both(heads(ff_raw), heads(fb_raw)))
    return both(q, q), 1.0 - f, both(v, v), jnp.log(f)


def gla_scan(q, k, v, log_f, s0):
    N, B, H, L, K = q.shape
    V = v.shape[-1]
    C = HGRN_CHUNK
    nc = L // C
    mask = jnp.tril(jnp.ones((C, C), dtype=bool))

    def to_chunks(a):
        return jnp.moveaxis(a.reshape(a.shape[:3] + (nc, C, a.shape[-1])), 3, 0)

    def step(S, inp):
        qc, kc, vc, gc = inp
        b = jnp.cumsum(gc, axis=-2)
        diff = b[..., :, None, :] - b[..., None, :, :]
        decay = jnp.exp(jnp.where(mask[:, :, None], diff, -jnp.inf))
        att = jnp.einsum('nbhtk,nbhtsk,nbhsk->nbhts', qc, decay, kc)
        o = jnp.einsum('nbhts,nbhsv->nbhtv', att, vc) + jnp.einsum('nbhtk,nbhkv->nbhtv', qc * jnp.exp(b), S)
        b_end = b[..., -1:, :]
        S = jnp.exp(b_end)[..., 0, :, None] * S + jnp.einsum('nbhsk,nbhsv->nbhkv', kc * jnp.exp(b_end - b), vc)
        return S, o

    s_fin, o = lax.scan(step, s0, (to_chunks(q), to_chunks(k), to_chunks(v), to_chunks(log_f)))
    return jnp.moveaxis(o, 0, 3).reshape(N, B, H, L, V), s_fin


def hgrn_readout(o_d, g_raw, norm_g):
    o = o_d[0] + o_d[1][:, :, ::-1]
    B, _, L, _ = o.shape
    o = rmsnorm(o, norm_g).transpose(0, 2, 1, 3).reshape(B, L, HGRN_W)
    return o.astype(g_raw.dtype) * jax.nn.silu(g_raw)


def attn_qkv(q_raw, k_raw, v_raw, qk_norm_g):
    B, L, _ = q_raw.shape
    q = rmsnorm(q_raw.reshape(B, L, ATT_KV_HEADS, ATT_GROUPS, HEAD_DIM), qk_norm_g[0])
    k = rmsnorm(k_raw.reshape(B, L, ATT_KV_HEADS, HEAD_DIM), qk_norm_g[1])
    v = v_raw.reshape(B, L, ATT_KV_HEADS, HEAD_DIM)
    return q, k, v


def attend(qblk, k, v):
    s = jnp.einsum('bqkgd,bskd->bkgqs', qblk, k).astype(jnp.float32) * (HEAD_DIM ** -0.5)
    p = jax.nn.softmax(s, axis=-1).astype(v.dtype)
    return jnp.einsum('bkgqs,bskd->bqkgd', p, v)


def blocked_attention(q, k_all, v_all):
    B, L = q.shape[:2]
    nb = L // Q_BLOCK
    qb = jnp.moveaxis(q.reshape(B, nb, Q_BLOCK, ATT_KV_HEADS, ATT_GROUPS, HEAD_DIM), 1, 0)
    o = lax.map(lambda qblk: attend(qblk, k_all, v_all), qb)
    return jnp.moveaxis(o, 0, 1).reshape(B, L, ATT_W)


def merge(o_h, o_a, gate_h_raw, gate_a_raw, w_branch, w_out):
    y = jax.nn.sigmoid(gate_h_raw) * (o_h @ w_branch[0]) + jax.nn.sigmoid(gate_a_raw) * (o_a @ w_branch[1])
    return y @ w_out


def token_mixer(u_lat, u_ctx, rows, cols, w_in, lb, hgrn_norm_g, qk_norm_g, w_branch, w_out, need_ctx):
    pl = split_cols(u_lat @ w_in)
    pc = split_cols(u_ctx @ w_in)
    qd, kd, vd, fd = hgrn_gates(pc[0], pc[1], pc[2], pc[3], lb)
    s0 = jnp.zeros((2, u_ctx.shape[0], HGRN_HEADS, HGRN_DK, HGRN_DV), jnp.float32)
    o_ctx_d, s_ctx = gla_scan(qd, kd, vd, fd, s0)
    ql, kl, vl, fl = hgrn_gates(pl[0], pl[1], pl[2], pl[3], lb)
    o_lat_d, _ = gla_scan(ql, kl, vl, fl, s_ctx)
    o_h = hgrn_readout(o_lat_d, pl[4], hgrn_norm_g)
    q, k, v = attn_qkv(pl[5], pl[6], pl[7], qk_norm_g)
    q, k = axial_rope(q, rows, cols), axial_rope(k, rows, cols)
    qc, kc, vc = attn_qkv(pc[5], pc[6], pc[7], qk_norm_g)
    k_all = jnp.concatenate([k, kc], axis=1)
    v_all = jnp.concatenate([v, vc], axis=1)
    o_a = blocked_attention(q, k_all, v_all)
    y_lat = merge(o_h, o_a, pl[8], pl[9], w_branch, w_out)
    y_ctx = None
    if need_ctx:
        oh_c = hgrn_readout(o_ctx_d, pc[4], hgrn_norm_g)
        oa_c = attend(qc, kc, vc).reshape(u_ctx.shape[0], u_ctx.shape[1], ATT_W)
        y_ctx = merge(oh_c, oa_c, pc[8], pc[9], w_branch, w_out)
    return y_lat, y_ctx


def moe(u, router_w, router_b, w_up, b_up, w_down, b_down):
    B, L, D = u.shape
    t = u.reshape(B * L, D)
    logits = (t @ router_w + router_b).astype(jnp.float32)
    top_v, top_i = lax.top_k(logits, TOP_K)
    w = jax.nn.softmax(top_v, axis=-1)
    combine = jnp.sum(jax.nn.one_hot(top_i, N_EXPERTS, dtype=jnp.float32) * w[..., None], axis=1)
    out = jnp.zeros((B * L, D), jnp.float32)
    for e in range(N_EXPERTS):
        h = t @ w_up[e] + b_up[e]
        glu, lin = h[:, :D_EXPERT], h[:, D_EXPERT:]
        glu = jnp.minimum(glu, SWIGLU_LIMIT)
        lin = jnp.clip(lin, -SWIGLU_LIMIT, SWIGLU_LIMIT)
        a = glu * jax.nn.sigmoid(SWIGLU_ALPHA * glu) * (lin + 1.0)
        out = out + combine[:, e:e + 1] * (a @ w_down[e] + b_down[e])
    return out.astype(u.dtype).reshape(B, L, D)


def setup_inputs(seed: int = 0) -> dict:
    key = jax.random.key(seed)
    ks = jax.random.split(key, 24)
    f32 = jnp.float32
    D = D_MODEL

    def nrm(k, shape, scale):
        return jax.random.normal(k, shape, f32) * scale

    return {
        "x": nrm(ks[0], (BATCH, SEQ, D), 1.0),
        "c": nrm(ks[1], (BATCH, D), 1.0),
        "ctx": nrm(ks[2], (BATCH, CTX_LEN, D), 1.0),
        "c_ctx": nrm(ks[3], (D,), 1.0),
        "w_mod": nrm(ks[4], (DEPTH, D, N_MOD * D), 0.5 * D ** -0.5),
        "b_mod": nrm(ks[5], (DEPTH, N_MOD * D), 0.02),
        "norm_g": 1.0 + nrm(ks[6], (DEPTH, 4, D), 0.05),
        "w_in": nrm(ks[7], (DEPTH, D, IN_COLS), D ** -0.5),
        "hgrn_lb": nrm(ks[8], (2, DEPTH + 1, HGRN_W), 0.1) + jnp.arange(DEPTH + 1, dtype=f32)[None, :, None],
        "hgrn_norm_g": 1.0 + nrm(ks[9], (DEPTH, HGRN_DV), 0.05),
        "qk_norm_g": 1.0 + nrm(ks[10], (DEPTH, 2, HEAD_DIM), 0.05),
        "w_branch": nrm(ks[11], (DEPTH, N_BRANCH, HGRN_W, D), HGRN_W ** -0.5),
        "w_out": nrm(ks[12], (DEPTH, D, D), D ** -0.5),
        "router_w": nrm(ks[13], (DEPTH, D, N_EXPERTS), D ** -0.5),
        "router_b": nrm(ks[14], (DEPTH, N_EXPERTS), 0.01),
        "w_up": nrm(ks[15], (DEPTH, N_EXPERTS, D, 2 * D_EXPERT), D ** -0.5),
        "b_up": nrm(ks[16], (DEPTH, N_EXPERTS, 2 * D_EXPERT), 0.02),
        "w_down": nrm(ks[17], (DEPTH, N_EXPERTS, D_EXPERT, D), D_EXPERT ** -0.5),
        "b_down": nrm(ks[18], (DEPTH, N_EXPERTS, D), 0.02),
    }


def reference(x, c, ctx, c_ctx, w_mod, b_mod, norm_g, w_in, hgrn_lb, hgrn_norm_g, qk_norm_g,
              w_branch, w_out, router_w, router_b, w_up, b_up, w_down, b_down):
    B, L, _ = x.shape
    n_rows = L // GRID_W
    rows = jnp.repeat(jnp.arange(n_rows, dtype=jnp.int32), GRID_W)
    cols = jnp.tile(jnp.arange(GRID_W, dtype=jnp.int32), n_rows)
    lb_all = jnp.cumsum(jax.nn.softmax(hgrn_lb.astype(jnp.float32), axis=1), axis=1)
    h = ctx
    for l in range(DEPTH):
        last = l == DEPTH - 1
        g = norm_g[l]
        m_lat = modulation(c, w_mod[l], b_mod[l])
        m_ctx = modulation(c_ctx[None], w_mod[l], b_mod[l])
        u_lat = modulate(rmsnorm(x, g[0]), m_lat[0], m_lat[1])
        u_ctx = modulate(rmsnorm(h, g[0]), m_ctx[0], m_ctx[1])
        y_lat, y_ctx = token_mixer(u_lat, u_ctx, rows, cols, w_in[l], lb_all[:, l], hgrn_norm_g[l],
                                   qk_norm_g[l], w_branch[l], w_out[l], not last)
        x = x + m_lat[2] * rmsnorm(y_lat, g[1])
        f_lat = moe(modulate(rmsnorm(x, g[2]), m_lat[3], m_lat[4]),
                    router_w[l], router_b[l], w_up[l], b_up[l], w_down[l], b_down[l])
        x = x + m_lat[5] * rmsnorm(f_lat, g[3])
        if not last:
            h = h + m_ctx[2] * rmsnorm(y_ctx, g[1])
            f_ctx = moe(modulate(rmsnorm(h, g[2]), m_ctx[3], m_ctx[4]),
                        router_w[l], router_b[l], w_up[l], b_up[l], w_down[l], b_down[l])
            h = h + m_ctx[5] * rmsnorm(f_ctx, g[3])
    return x
```

```python
from contextlib import ExitStack
import numpy as np
import concourse.bass as bass
import concourse.mybir as mybir
from concourse.bass_utils import run_bass_kernel_spmd

F32 = mybir.dt.float32
BF16 = mybir.dt.bfloat16
ALU = mybir.AluOpType
AF = mybir.ActivationFunctionType
AX = mybir.AxisListType

ENGS = ("pe", "act", "dve", "pool", "sp")
EPOCH = 16000
EPS = 1e-6


def _freeze(fn, memo=None):
    import types
    if memo is None:
        memo = {}
    if not isinstance(fn, types.FunctionType) or fn.__closure__ is None:
        return fn
    if id(fn) in memo:
        return memo[id(fn)]
    cells = []
    for c in fn.__closure__:
        try:
            v = c.cell_contents
        except ValueError:
            cells.append(c)
            continue
        if isinstance(v, types.FunctionType) and v.__closure__ is not None and v is not fn:
            v = _freeze(v, memo)
        cells.append(types.CellType(v))
    new = types.FunctionType(fn.__code__, fn.__globals__, fn.__name__, fn.__defaults__, tuple(cells))
    new.__kwdefaults__ = fn.__kwdefaults__
    memo[id(fn)] = new
    return new


class Prog:
    def __init__(self, nc, same_engine_sync=True):
        self.nc = nc
        self.es = ExitStack()
        self.q = {e: [] for e in ENGS}
        self.cnt = {e: 0 for e in ENGS}
        self.waited = {}
        self.buf = {}
        self.sems = {}
        self.dma_cnt = {}
        self.same_engine_sync = same_engine_sync
        self.n_sem = 0

    def sem(self, key):
        if key not in self.sems:
            self.n_sem += 1
            self.sems[key] = self.es.enter_context(self.nc.semaphore("s%d" % self.n_sem))
        return self.sems[key]

    def sbuf(self, name, shape, dtype):
        return self.es.enter_context(self.nc.sbuf_tensor(name, list(shape), dtype))

    def psum(self, name, shape, dtype=F32):
        return self.es.enter_context(self.nc.psum_tensor(name, list(shape), dtype))

    def _semkey_for(self, prod):
        kind, name, count = prod
        if kind == "e":
            ep = (count - 1) // EPOCH
            return ("e", name, ep), count - ep * EPOCH
        return ("d", name), count

    def _need(self, eng, prod, waits):
        if prod is None:
            return
        kind, name, count = prod
        if kind == "e" and name == eng and (eng in ("pe", "sp") or not self.same_engine_sync):
            return
        sk, val = self._semkey_for(prod)
        wk = (eng, kind, name)
        if self.waited.get(wk, 0) >= count:
            return
        self.waited[wk] = count
        waits.append((sk, val))

    def _deps(self, eng, reads, writes):
        waits = []
        for k in reads:
            b = self.buf.get(k)
            if b is not None:
                self._need(eng, b["w"], waits)
        for k in writes:
            b = self.buf.get(k)
            if b is not None:
                self._need(eng, b["w"], waits)
                for r in b["r"].values():
                    self._need(eng, r, waits)
        return waits

    def _record(self, prod, reads, writes):
        for k in reads:
            b = self.buf.setdefault(k, {"w": None, "r": {}})
            b["r"][(prod[0], prod[1])] = prod
        for k in writes:
            self.buf[k] = {"w": prod, "r": {}}

    def op(self, eng, fn, reads=(), writes=()):
        fn = _freeze(fn)
        waits = self._deps(eng, reads, writes)
        self.cnt[eng] += 1
        prod = ("e", eng, self.cnt[eng])
        sk, _ = self._semkey_for(prod)
        self.q[eng].append((fn, waits, (sk, 1)))
        self._record(prod, reads, writes)
        return prod

    def dma(self, eng, out, in_, semname, reads=(), writes=(), **kw):
        if writes:
            semname = semname + ":" + writes[0]
        waits = self._deps(eng, reads, writes)
        self.dma_cnt[semname] = self.dma_cnt.get(semname, 0) + 16
        prod = ("d", semname, self.dma_cnt[semname])
        self.q[eng].append((lambda e: e.dma_start(out=out, in_=in_, **kw), waits, (("d", semname), 16)))
        self._record(prod, reads, writes)
        return prod

    def custom(self, eng, fn, semname, inc, reads=(), writes=()):
        fn = _freeze(fn)
        waits = self._deps(eng, reads, writes)
        self.dma_cnt[semname] = self.dma_cnt.get(semname, 0) + inc
        prod = ("d", semname, self.dma_cnt[semname])
        self.q[eng].append((fn, waits, (("d", semname), inc)))
        self._record(prod, reads, writes)
        return prod

    def wait_all(self, eng):
        waits = []
        for e in ENGS:
            if self.cnt[e] > 0 and e != eng:
                self._need(eng, ("e", e, self.cnt[e]), waits)
        for name, c in self.dma_cnt.items():
            self._need(eng, ("d", name, c), waits)
        self.q[eng].append((None, waits, None))

    def barrier(self, keep=()):
        for e in ENGS:
            self.wait_all(e)
        self.buf = {k: v for k, v in self.buf.items() if k in keep}

    def build(self):
        nc = self.nc
        keys = []
        for e in ENGS:
            for (_, w, inc) in self.q[e]:
                for x in w:
                    keys.append(x[0])
                if inc:
                    keys.append(inc[0])
        for sk in dict.fromkeys(keys):
            self.sem(sk)
        engmap = {"pe": "tensor", "act": "scalar", "dve": "vector", "pool": "gpsimd", "sp": "sync"}
        with nc.Block() as block:
            for e in ENGS:
                items = self.q[e]

                def body(eng, items=items):
                    for fn, waits, inc in items:
                        for sk, val in waits:
                            eng.wait_ge(self.sems[sk], val)
                        if fn is not None:
                            ins = fn(eng)
                            if inc is not None:
                                ins.then_inc(self.sems[inc[0]], inc[1])

                getattr(block, engmap[e])(body)

    def close(self):
        self.es.close()


class Arena:
    def __init__(self, P, nbytes):
        self.t = P.sbuf("arena", [128, nbytes // 2], BF16)
        self.nbytes = nbytes
        self.off = 0

    def alloc(self, free_shape, dtype):
        n = int(np.prod(free_shape))
        size = n * (4 if dtype == F32 else 2)
        size = (size + 63) // 64 * 64
        assert self.off + size <= self.nbytes, ("SBUF arena overflow", self.off, size)
        v = self.t[:, self.off // 2:(self.off + size) // 2]
        if dtype == F32:
            v = v.bitcast(F32)
        v = v[:, 0:n]
        self.off += size
        if len(free_shape) == 2:
            v = v.rearrange("p (a b) -> p a b", b=free_shape[1])
        elif len(free_shape) == 3:
            v = v.rearrange("p (a b c) -> p a b c", b=free_shape[1], c=free_shape[2])
        elif len(free_shape) == 4:
            v = v.rearrange("p (a b c d) -> p a b c d", b=free_shape[1], c=free_shape[2], d=free_shape[3])
        return v

    def mark(self):
        return self.off

    def release(self, m):
        self.off = m


def bc(ap, axis, shape):
    return ap.unsqueeze(axis).to_broadcast(list(shape))


NT = 2048
NTT = 16
NCTX = 256
NALL = NT + NCTX
D = 1024
NE = 32
LAST_PHASE = 99


def build(upto=LAST_PHASE, debug=False):
    nc = bass.Bass("TRN2", target_bir_lowering=False)

    def din(name, shape, dt=F32):
        return nc.dram_tensor(name, list(shape), dt, kind="ExternalInput").ap()

    x_d = din("x", [NT, D])
    ctx_d = din("ctx", [NCTX, D])
    cvec_d = din("cvec", [128, 16])
    wmod_d = din("w_mod", [D, 6 * D])
    bmod_d = din("b_mod", [1, 6 * D])
    ng_d = din("norm_g", [1, 4 * D])
    win_d = din("w_in", [D, 8704])
    lbv_d = din("lbv", [128, 32])
    hng_d = din("hng", [128, 1])
    qkg_d = din("qkg", [1, 256])
    wbr_d = din("w_branch", [2, D, D])
    wout_d = din("w_out", [D, D])
    rw_d = din("router_w", [D, NE])
    rb_d = din("router_b", [1, NE])
    NEW = NE if upto >= 7 else 1
    wup_d = din("w_up", [NEW, D, 2 * D])
    bupT_d = din("b_upT", [128, NE * 16])
    wdn_d = din("w_down", [NEW, D, D])
    bdn_d = din("b_down", [NE, D])
    rope_d = din("rope", [NT, 256])
    sel_d = din("sel", [128, 8])
    out_d = nc.dram_tensor("out", [NT, D], F32, kind="ExternalOutput").ap()

    def dscr(name, shape, dt):
        if debug:
            return nc.dram_tensor(name, list(shape), dt, kind="ExternalOutput").ap()
        return nc.dram_tensor(name, list(shape), dt).ap()

    dm_mod = dscr("dm_mod", [128, 6 * D], F32)
    dm_ut = dscr("dm_ut", [128, 8, NALL], BF16)
    ag1_ins = [nc.dram_tensor("ag1_in%d" % q, [128, 2048], BF16).ap() for q in range(4)]
    ag1_outs = [nc.dram_tensor("ag1_out%d" % q, [4 * 128, 2048], BF16).ap() for q in range(4)]
    dm_kvctx = dscr("dm_kvctx", [512, NCTX], BF16)
    dm_oloc = dscr("dm_oloc", [8, 128, NT], F32)
    dm_qb = dscr("dm_qb", [2, 8, 128, NT], BF16)
    dm_gs = dscr("dm_gs", [8, 128, NT], BF16)
    ag2_ins = [nc.dram_tensor("ag2_in%d" % d, [128, 1032], F32).ap() for d in range(2)]
    ag2_outs = [nc.dram_tensor("ag2_out%d" % d, [4 * 128, 1032], F32).ap() for d in range(2)]
    dm_oth = dscr("dm_oth", [8, 128, NT], BF16)
    dm_ota = dscr("dm_ota", [8, 128, NT], BF16)
    dm_x1 = dscr("dm_x1", [NT, D], F32)
    dm_u2t = dscr("dm_u2t", [128, 8, NT], BF16)
    dm_comb = dscr("dm_comb", [128, NTT, NE], F32)
    dm_combT = dscr("dm_combT", [NE, NT], F32)

    P = Prog(nc)
    A = Arena(P, 207 * 1024)
    banks = [P.psum("bank%d" % i, [128, 512], F32) for i in range(8)]

    def bank_bf(i):
        return banks[i][:, :].bitcast(BF16)

    ident_f = A.alloc((128,), F32)
    ident_b = A.alloc((128,), BF16)
    ones_b = A.alloc((128,), BF16)
    ones_f = A.alloc((128,), F32)
    P.op("pool", lambda e: e.memset(ident_f, 0.0), writes=["ident_f"])
    P.op("pool", lambda e: e.affine_select(out=ident_f, in_=ident_f, pattern=[[-1, 128]], compare_op=ALU.not_equal,
                                           fill=1.0, base=0, channel_multiplier=1), reads=["ident_f"], writes=["ident_f"])
    P.op("dve", lambda e: e.tensor_copy(out=ident_b, in_=ident_f), reads=["ident_f"], writes=["ident_b"])
    P.op("pool", lambda e: e.memset(ones_f, 1.0), writes=["ones_f"])
    P.op("dve", lambda e: e.tensor_copy(out=ones_b, in_=ones_f), reads=["ones_f"], writes=["ones_b"])

    m_pre_ut = A.mark()
    uT = A.alloc((8, NALL), BF16)

    def rstd_from_ss(ss_ap, n, out_ap, tmp_ap, rk, wk):
        P.op("act", lambda e: e.activation(out=tmp_ap, in_=ss_ap, func=AF.Ln, scale=1.0 / n, bias=EPS), reads=rk, writes=[wk + "_ln"])
        P.op("act", lambda e: e.activation(out=out_ap, in_=tmp_ap, func=AF.Exp, scale=-0.5), reads=[wk + "_ln"], writes=[wk])

    m0 = A.mark()
    cv = A.alloc((16,), F32)
    scv = A.alloc((16,), F32)
    scb = A.alloc((16, 128), F32)
    bmod = A.alloc((6 * D,), F32)
    ng = A.alloc((4, D), F32)
    modl = A.alloc((6 * D,), F32)
    modc = A.alloc((2 * D,), F32)
    wm = [A.alloc((8, 512), F32) for _ in range(2)]
    P.dma("sp", cv, cvec_d, "ld_c", writes=["cv"])
    P.dma("sp", bmod, bmod_d[0].partition_broadcast(128), "ld_c", writes=["bmod"])
    P.dma("sp", ng, ng_d[0].partition_broadcast(128).rearrange("p (a b) -> p a b", b=D), "ld_c", writes=["ng"])
    P.op("act", lambda e: e.activation(out=scv, in_=cv, func=AF.Silu), reads=["cv"], writes=["scv"])
    for k in range(16):
        P.op("dve", lambda e, k=k: e.tensor_copy(out=scb[:, k, :], in_=scv[:, k:k + 1].to_broadcast([128, 128])),
             reads=["scv"], writes=["scb"])
    for s in range(12):
        w = wm[s % 2]
        wk = "wm%d" % (s % 2)
        P.dma("sp", w, wmod_d[:, s * 512:(s + 1) * 512].rearrange("(kt p) n -> p kt n", p=128), "ld_" + wk, writes=[wk])

        def mm(e, w=w, off=0, bk=0):
            for kt in range(8):
                r = e.matmul(banks[bk][:, :], lhsT=scb[:, off + kt, :], rhs=w[:, kt, :], start=(kt == 0), stop=(kt == 7))
            return r
        P.op("pe", lambda e, w=w: mm(e, w, 0, 0), reads=["scb", wk], writes=["bank0"])
        P.op("dve", lambda e, s=s: e.tensor_tensor(out=modl[:, s * 512:(s + 1) * 512], in0=banks[0][:, :], in1=bmod[:, s * 512:(s + 1) * 512], op=ALU.add),
             reads=["bank0", "bmod"], writes=["modl"])
        if s < 4:
            P.op("pe", lambda e, w=w: mm(e, w, 8, 1), reads=["scb", wk], writes=["bank1"])
            P.op("dve", lambda e, s=s: e.tensor_tensor(out=modc[:, s * 512:(s + 1) * 512], in0=banks[1][:, :], in1=bmod[:, s * 512:(s + 1) * 512], op=ALU.add),
                 reads=["bank1", "bmod"], writes=["modc"])
    P.op("dve", lambda e: e.scalar_tensor_tensor(out=modl[:, D:2 * D], in0=modl[:, D:2 * D], scalar=1.0, in1=ng[:, 0, :], op0=ALU.add, op1=ALU.mult),
         reads=["modl", "ng"], writes=["modl"])
    P.op("dve", lambda e: e.scalar_tensor_tensor(out=modc[:, D:2 * D], in0=modc[:, D:2 * D], scalar=1.0, in1=ng[:, 0, :], op0=ALU.add, op1=ALU.mult),
         reads=["modc", "ng"], writes=["modc"])
    P.op("dve", lambda e: e.tensor_tensor(out=modl[:, 2 * D:3 * D], in0=modl[:, 2 * D:3 * D], in1=ng[:, 1, :], op=ALU.mult), reads=["modl", "ng"], writes=["modl"])
    P.op("dve", lambda e: e.scalar_tensor_tensor(out=modl[:, 4 * D:5 * D], in0=modl[:, 4 * D:5 * D], scalar=1.0, in1=ng[:, 2, :], op0=ALU.add, op1=ALU.mult),
         reads=["modl", "ng"], writes=["modl"])
    P.op("dve", lambda e: e.tensor_tensor(out=modl[:, 5 * D:6 * D], in0=modl[:, 5 * D:6 * D], in1=ng[:, 3, :], op=ALU.mult), reads=["modl", "ng"], writes=["modl"])
    P.dma("sp", dm_mod, modl, "st_mod", reads=["modl"], writes=["dm_mod"])

    xt = [A.alloc((D,), F32) for _ in range(2)]
    junk = A.alloc((D,), BF16)
    tmpf = A.alloc((D,), F32)
    ub = [A.alloc((D,), BF16) for _ in range(2)]
    ss1 = A.alloc((18,), F32)
    ln1 = A.alloc((18,), F32)
    rs1 = A.alloc((18,), F32)
    P.op("pool", lambda e: e.memset(ss1, 0.0), writes=["ss1"])
    for ti in range(18):
        s = ti % 2
        src = x_d[ti * 128:(ti + 1) * 128, :] if ti < NTT else ctx_d[(ti - NTT) * 128:(ti - NTT + 1) * 128, :]
        Am = modl if ti < NTT else modc
        amk = "modl" if ti < NTT else "modc"
        P.dma("sp", xt[s], src, "ld_xt%d" % s, writes=["xt%d" % s])
        P.op("act", lambda e, s=s, ti=ti: e.activation(out=junk, in_=xt[s], func=AF.Square, accum_out=ss1[:, ti:ti + 1]),
             reads=["xt%d" % s, "ss1"], writes=["junk", "ss1_%d" % ti])
        rstd_from_ss(ss1[:, ti:ti + 1], D, rs1[:, ti:ti + 1], ln1[:, ti:ti + 1], ["ss1_%d" % ti], "rs1_%d" % ti)
        P.op("dve", lambda e, s=s, ti=ti, Am=Am: e.scalar_tensor_tensor(out=tmpf, in0=xt[s], scalar=rs1[:, ti:ti + 1], in1=Am[:, D:2 * D], op0=ALU.mult, op1=ALU.mult),
             reads=["xt%d" % s, "rs1_%d" % ti, amk], writes=["tmpf"])
        P.op("dve", lambda e, s=s, Am=Am: e.tensor_tensor(out=ub[s], in0=tmpf, in1=Am[:, 0:D], op=ALU.add), reads=["tmpf", amk], writes=["ub%d" % s])
        bk = 2 + s

        def tr(e, s=s, bk=bk):
            for kt in range(8):
                r = e.transpose(bank_bf(bk)[:, kt * 128:(kt + 1) * 128], ub[s][:, kt * 128:(kt + 1) * 128], ident_b)
            return r
        P.op("pe", tr, reads=["ub%d" % s, "ident_b"], writes=["bank%d" % bk])
        P.op("act", lambda e, ti=ti, bk=bk: e.copy(out=uT[:, :, ti * 128:(ti + 1) * 128], in_=bank_bf(bk).rearrange("p (a b) -> p a b", b=128)),
             reads=["bank%d" % bk], writes=["uT_%d" % ti])
    UT_KEYS = ["uT_%d" % ti for ti in range(18)]
    if debug:
        P.dma("sp", dm_ut, uT, "st_dbg", reads=UT_KEYS, writes=["dm_ut"])
    P.barrier()
    A.release(m0)
    if upto <= 1:
        return finish(P, nc)


    m2 = A.mark()
    wkv = A.alloc((8, 512), BF16)
    P.dma("pool", wkv, win_d[:, 6144:6656].rearrange("(kt p) n -> p kt n", p=128), "ld_wkv", writes=["wkv"])
    ropeT = A.alloc((NTT, 256), F32)
    P.dma("sp", ropeT, rope_d.rearrange("(t p) n -> p t n", p=128), "ld_rope", writes=["ropeT"])
    gqk = A.alloc((256,), F32)
    P.dma("sp", gqk, qkg_d[0].partition_broadcast(128), "ld_c", writes=["gqk"])
    KTl = A.alloc((2, NALL), BF16)
    Vl = A.alloc((18, 256), BF16)
    ssk = A.alloc((18, 2), F32)
    lnk = A.alloc((18, 2), F32)
    rsk = A.alloc((18, 2), F32)
    knb = [A.alloc((2, 128), F32) for _ in range(2)]
    t1b = A.alloc((2, 128), F32)
    t2b = A.alloc((2, 128), F32)
    kbf = [A.alloc((256,), BF16) for _ in range(2)]
    junk2 = A.alloc((128,), BF16)
    P.op("pool", lambda e: e.memset(ssk, 0.0), writes=["ssk"])

    def rope_apply(xn, nh, ti, outbf, pfx, eng2="dve"):
        cosv = ropeT[:, ti, 0:128]
        sinv = ropeT[:, ti, 128:256].rearrange("p (r x d) -> p r x d", r=2, x=2, d=32)
        t1 = t1b if nh == 2 else t1q
        t2 = t2b if nh == 2 else t2q
        P.op("dve", lambda e: e.tensor_tensor(out=t1, in0=xn, in1=bc(cosv, 1, [128, nh, 128]), op=ALU.mult),
             reads=[pfx + "xn", "ropeT"], writes=[pfx + "t1"])
        x6 = xn.rearrange("p h (r x d) -> p h r x d", r=2, x=2, d=32)
        t6 = t2.rearrange("p h (r x d) -> p h r x d", r=2, x=2, d=32)
        for xo in range(2):
            P.op(eng2, lambda e, xo=xo: e.tensor_tensor(out=t6[:, :, :, xo, :], in0=x6[:, :, :, 1 - xo, :],
                                                        in1=bc(sinv[:, :, xo, :], 1, [128, nh, 2, 32]), op=ALU.mult),
                 reads=[pfx + "xn", "ropeT"], writes=[pfx + "t2_%d" % xo])
        P.op("dve", lambda e: e.tensor_tensor(out=outbf.rearrange("p (h d) -> p h d", d=128), in0=t1, in1=t2, op=ALU.add),
             reads=[pfx + "t1", pfx + "t2_0", pfx + "t2_1"], writes=[pfx + "bf"])

    import os
    BIS = int(os.environ.get("BIS", "99"))
    for ti in range(18):
        s = ti % 2
        bk = s
        kn = knb[s]

        def mmkv(e, ti=ti, bk=bk):
            for kt in range(8):
                r = e.matmul(banks[bk][:, :], lhsT=uT[:, kt, ti * 128:(ti + 1) * 128], rhs=wkv[:, kt, :], start=(kt == 0), stop=(kt == 7))
            return r
        P.op("pe", mmkv, reads=["uT_%d" % ti, "wkv"], writes=["bank%d" % bk])
        kps = banks[bk][:, 0:256].rearrange("p (h d) -> p h d", d=128)
        P.op("act", lambda e, ti=ti, bk=bk: e.copy(out=Vl[:, ti, :], in_=banks[bk][:, 256:512]), reads=["bank%d" % bk], writes=["Vl_%d" % ti])
        if BIS < 2:
            continue
        for h in range(2):
            P.op("act", lambda e, h=h, ti=ti, kps=kps: e.activation(out=junk2, in_=kps[:, h, :], func=AF.Square, accum_out=ssk[:, ti, h:h + 1]),
                 reads=["bank%d" % bk, "ssk"], writes=["junk2", "ssk_%d_%d" % (ti, h)])
        rstd_from_ss(ssk[:, ti, :], 128, rsk[:, ti, :], lnk[:, ti, :], ["ssk_%d_0" % ti, "ssk_%d_1" % ti], "rsk_%d" % ti)
        P.op("dve", lambda e, kn=kn, kps=kps, ti=ti: e.tensor_tensor(out=kn, in0=kps, in1=bc(rsk[:, ti, :], 2, [128, 2, 128]), op=ALU.mult),
             reads=["bank%d" % bk, "rsk_%d" % ti], writes=["k%dxn" % s])
        P.op("dve", lambda e, kn=kn: e.tensor_tensor(out=kn, in0=kn, in1=bc(gqk[:, 128:256], 1, [128, 2, 128]), op=ALU.mult),
             reads=["k%dxn" % s, "gqk"], writes=["k%dxn" % s])
        if BIS < 3:
            continue
        if ti < NTT and BIS != 3:
            rope_apply(kn, 2, ti, kbf[s], "k%d" % s)
        else:
            P.op("dve", lambda e, kn=kn, s=s: e.tensor_copy(out=kbf[s].rearrange("p (h d) -> p h d", d=128), in_=kn), reads=["k%dxn" % s], writes=["k%dbf" % s])
        bk2 = 2 + s
        if BIS < 5:
            continue

        def trk(e, s=s, bk2=bk2):
            for h in range(2):
                r = e.transpose(bank_bf(bk2)[:, h * 128:(h + 1) * 128], kbf[s][:, h * 128:(h + 1) * 128], ident_b)
            return r
        P.op("pe", trk, reads=["k%dbf" % s, "ident_b"], writes=["bank%d" % bk2])
        P.op("act", lambda e, ti=ti, bk2=bk2: e.copy(out=KTl[:, :, ti * 128:(ti + 1) * 128], in_=bank_bf(bk2)[:, 0:256].rearrange("p (a b) -> p a b", b=128)),
             reads=["bank%d" % bk2], writes=["KTl_%d" % ti])
    for h in range(2 if BIS >= 6 else 0):
        P.dma("sp", ag1_ins[h], KTl[:, h, 0:NT], "st_ag1", reads=["KTl_%d" % t for t in range(16)], writes=["ag1_in"])
        P.dma("sp", ag1_ins[2 + h].rearrange("p (ti c) -> p ti c", c=256), Vl[:, 8 * h:8 * h + 8, :], "st_ag1", reads=["Vl_%d" % t for t in range(16)], writes=["ag1_in"])
    if BIS >= 6:
        P.dma("sp", dm_kvctx[0:256, :].rearrange("(h p) t -> p h t", p=128), KTl[:, :, NT:NALL], "st_kvc", reads=["KTl_16", "KTl_17"], writes=["dm_kvctx"])
        P.dma("sp", dm_kvctx[256:512, :].rearrange("(ti p) c -> p ti c", p=128), Vl[:, 16:18, :], "st_kvc", reads=["Vl_16", "Vl_17"], writes=["dm_kvctx"])
    for q in range(4 if BIS >= 7 else 0):
        P.custom("pool", lambda e, q=q: e.collective_compute("AllGather", ALU.bypass, replica_groups=[[0, 1, 2, 3], [4, 5, 6, 7]], ins=[ag1_ins[q]], outs=[ag1_outs[q]]),
                 "cc1", 1, reads=["ag1_in"] + (["ag1_out"] if q > 0 else []), writes=["ag1_out"])
    P.barrier(keep=["ag1_out", "dm_kvctx", "dm_mod"] + UT_KEYS)
    A.release(m2)
    if upto <= 2:
        return finish(P, nc)

    m3 = A.mark()
    rst = A.alloc((NALL,), F32)
    maskF = A.alloc((128,), F32)
    maskB = A.alloc((128,), F32)
    lbt = A.alloc((2, 2, 8), F32)
    lbd = A.alloc((2, 8), F32)
    lb = A.alloc((2, 8), F32)
    oml = A.alloc((2, 8), F32)
    hng = A.alloc((1,), F32)
    stage = A.alloc((2, 8, 128), F32)
    sctx = A.alloc((2, 8, 128), F32)
    Dv = A.alloc((2, 8), F32)
    m3h = A.mark()
    whb = [A.alloc((8, 5, 128), BF16)] * 2
    qT = A.alloc((NT,), F32)
    gsT = A.alloc((NT,), BF16)
    v_tm = A.alloc((18, 128), BF16)
    fa = A.alloc((NALL,), F32)
    lf = A.alloc((NALL,), F32)
    kk = A.alloc((NALL,), F32)
    bb = A.alloc((NALL,), F32)
    xx = A.alloc((NALL,), F32)
    ee = A.alloc((NALL,), F32)
    gtmp = ee[:, 0:NT]
    qe = [A.alloc((NT,), BF16) for _ in range(2)]
    ke = [A.alloc((NT,), BF16) for _ in range(2)]
    kd = [A.alloc((NALL,), BF16) for _ in range(2)]
    kd_tm = [A.alloc((18, 128), BF16) for _ in range(2)]
    qB = [A.alloc((NT,), BF16) for _ in range(2)]
    tot = [A.alloc((36,), F32) for _ in range(2)]
    etot = [A.alloc((36,), F32) for _ in range(2)]
    ipf = [A.alloc((32,), F32) for _ in range(2)]
    gg = [A.alloc((32,), F32) for _ in range(2)]
    eg = [A.alloc((32,), F32) for _ in range(2)]
    attm = [A.alloc((4, 128), BF16) for _ in range(2)]
    Sst = [A.alloc((128,), F32) for _ in range(2)]
    Sbf = [A.alloc((128,), BF16) for _ in range(2)]
    Scx = [A.alloc((128,), F32) for _ in range(2)]
    o_acc = A.alloc((NT,), F32)

    P.op("pool", lambda e: e.memset(rst, 1.0), writes=["rst"])
    P.op("pool", lambda e: e.memset(rst.rearrange("p (c t) -> p c t", t=64)[:, :, 0:1], 0.0), reads=["rst"], writes=["rst"])
    P.op("pool", lambda e: e.memset(maskF, 1.0), writes=["maskF"])
    P.op("pool", lambda e: e.affine_select(out=maskF, in_=maskF, pattern=[[1, 128]], compare_op=ALU.is_ge, fill=0.0, base=0, channel_multiplier=-1),
         reads=["maskF"], writes=["maskF"])
    P.op("pool", lambda e: e.memset(maskF[0:64, 64:128], 0.0), reads=["maskF"], writes=["maskF"])
    P.op("pool", lambda e: e.memset(maskB, 1.0), writes=["maskB"])
    P.op("pool", lambda e: e.affine_select(out=maskB, in_=maskB, pattern=[[-1, 128]], compare_op=ALU.is_ge, fill=0.0, base=0, channel_multiplier=1),
         reads=["maskB"], writes=["maskB"])
    P.op("pool", lambda e: e.memset(maskB[64:128, 0:64], 0.0), reads=["maskB"], writes=["maskB"])
    masks = [maskF, maskB]
    P.dma("sp", lbt, lbv_d.rearrange("p (a b c) -> p a b c", a=2, b=2), "ld_c", writes=["lbt"])
    P.dma("sp", hng, hng_d, "ld_c", writes=["hng"])
    P.op("dve", lambda e: e.tensor_tensor(out=lbd, in0=lbt[:, :, 0, :], in1=lbt[:, :, 1, :], op=ALU.subtract), reads=["lbt"], writes=["lbd"])
    P.op("act", lambda e: e.activation(out=lb, in_=lbd, func=AF.Sigmoid), reads=["lbd"], writes=["lb"])
    P.op("dve", lambda e: e.tensor_scalar(out=oml, in0=lb, scalar1=-1.0, scalar2=1.0, op0=ALU.mult, op1=ALU.add), reads=["lb"], writes=["oml"])

    win_v = win_d.rearrange("(kt p) (s n) -> p kt s n", p=128, n=128)
    BLK5 = [(0, 512), (512, 512), (1024, 512), (1536, 512), (2048, 256)]
    PB = 6
    pbc = [0]

    def proj_fm(wh, whk, sidx, c0, n, evac):
        bk = PB + (pbc[0] % 2)
        pbc[0] += 1

        def mm(e):
            for kt in range(8):
                r = e.matmul(banks[bk][:, 0:n], lhsT=wh[:, kt, sidx, :], rhs=uT[:, kt, c0:c0 + n], start=(kt == 0), stop=(kt == 7))
            return r
        P.op("pe", mm, reads=[whk] + ["uT_%d" % t for t in range(c0 // 128, (c0 + n) // 128)], writes=["bank%d" % bk])
        evac(banks[bk][:, 0:n], "bank%d" % bk)

    def hgrn_dir_prep(h, d):
        wh = whb[h % 2]
        whk = "wh0"
        dk = "d%d" % d
        for (c0, n) in BLK5:
            proj_fm(wh, whk, 2 + d, c0, n, lambda bap, bkey, c0=c0, n=n: P.op(
                "act", lambda e: e.activation(out=fa[:, c0:c0 + n], in_=bap, func=AF.Sigmoid), reads=[bkey], writes=["fa"]))
        fak = ["fa"]
        P.op("dve", lambda e: e.tensor_scalar(out=fa, in0=fa, scalar1=oml[:, d, h:h + 1], scalar2=lb[:, d, h:h + 1], op0=ALU.mult, op1=ALU.add),
             reads=fak + ["oml", "lb"], writes=["fa"])
        P.op("act", lambda e: e.activation(out=lf, in_=fa, func=AF.Ln), reads=["fa"], writes=["lf"])
        P.op("dve", lambda e: e.tensor_scalar(out=kk, in0=fa, scalar1=-1.0, scalar2=1.0, op0=ALU.mult, op1=ALU.add), reads=["fa"], writes=["kk"])
        P.op("dve", lambda e: e.tensor_tensor_scan(out=bb, data0=rst, data1=lf, initial=0.0, op0=ALU.mult, op1=ALU.add), reads=["rst", "lf"], writes=["bb"])
        b3 = bb.rearrange("p (c t) -> p c t", t=64)
        P.op("dve", lambda e: e.tensor_copy(out=tot[d], in_=b3[:, :, 63]), reads=["bb"], writes=["tot" + dk])
        P.op("act", lambda e: e.activation(out=etot[d], in_=tot[d], func=AF.Exp), reads=["tot" + dk], writes=["etot" + dk])
        x3 = xx.rearrange("p (c t) -> p c t", t=64)
        P.op("dve", lambda e: e.tensor_tensor(out=x3, in0=bc(tot[d], 2, [128, 36, 64]), in1=b3, op=ALU.subtract), reads=["tot" + dk, "bb"], writes=["xx"])
        if d == 0:
            bu, dd = bb, xx
            bk_, ddk = "bb", "xx"
        else:
            P.op("dve", lambda e: e.tensor_tensor(out=xx, in0=xx, in1=lf, op=ALU.add), reads=["xx", "lf"], writes=["xx"])
            P.op("dve", lambda e: e.tensor_tensor(out=bb, in0=bb, in1=lf, op=ALU.subtract), reads=["bb", "lf"], writes=["bb"])
            bu, dd = xx, bb
            bk_, ddk = "xx", "bb"
        P.op("act", lambda e: e.activation(out=ee[:, 0:NT], in_=bu[:, 0:NT], func=AF.Exp), reads=[bk_], writes=["ee"])
        P.op("dve", lambda e: e.tensor_tensor(out=qe[d], in0=qT, in1=ee[:, 0:NT], op=ALU.mult), reads=["ee", "qT"], writes=["qe" + dk])
        P.op("act", lambda e: e.activation(out=ee[:, 0:NT], in_=bu[:, 0:NT], func=AF.Exp, scale=-1.0), reads=[bk_, "ee"], writes=["ee"])
        P.op("dve", lambda e: e.tensor_tensor(out=ke[d], in0=kk[:, 0:NT], in1=ee[:, 0:NT], op=ALU.mult), reads=["ee", "kk"], writes=["ke" + dk])
        P.op("act", lambda e: e.activation(out=ee, in_=dd, func=AF.Exp), reads=[ddk, "ee"], writes=["ee"])
        P.op("dve", lambda e: e.tensor_tensor(out=kd[d], in0=kk, in1=ee, op=ALU.mult), reads=["ee", "kk"], writes=["kd" + dk])
        P.op("dve", lambda e: e.tensor_tensor_scan(out=ipf[d], data0=ones_f[:, 0:32], data1=tot[d][:, 0:32], initial=0.0, op0=ALU.mult, op1=ALU.add),
             reads=["tot" + dk, "ones_f"], writes=["ipf" + dk])
        if d == 0:
            P.op("dve", lambda e: e.tensor_tensor(out=gg[d], in0=ipf[d], in1=tot[d][:, 0:32], op=ALU.subtract), reads=["ipf" + dk, "tot" + dk], writes=["gg" + dk])
        else:
            P.op("dve", lambda e: e.tensor_tensor(out=gg[d], in0=ipf[d][:, 31:32].to_broadcast([128, 32]), in1=ipf[d], op=ALU.subtract),
                 reads=["ipf" + dk], writes=["gg" + dk])
        P.op("act", lambda e: e.activation(out=eg[d], in_=gg[d], func=AF.Exp), reads=["gg" + dk], writes=["eg" + dk])
        P.op("act", lambda e: e.activation(out=Dv[:, d, h:h + 1], in_=ipf[d][:, 31:32], func=AF.Exp), reads=["ipf" + dk], writes=["Dv_%d_%d" % (d, h)])
        P.op("dve", lambda e: e.tensor_tensor(out=qB[d].rearrange("p (c t) -> p c t", t=64), in0=qe[d].rearrange("p (c t) -> p c t", t=64),
                                               in1=bc(eg[d], 2, [128, 32, 64]), op=ALU.mult), reads=["qe" + dk, "eg" + dk], writes=["qB" + dk])
        P.dma("sp", dm_qb[d, h], qB[d], "st_qb", reads=["qB" + dk], writes=["dm_qb"])
        for g3 in range(3):
            bk = PB + (pbc[0] % 2)
            pbc[0] += 1

            def trd(e, g3=g3, bk=bk):
                for i in range(6):
                    ti = g3 * 6 + i
                    r = e.transpose(bank_bf(bk)[:, i * 128:(i + 1) * 128], kd[d][:, ti * 128:(ti + 1) * 128], ident_b)
                return r
            P.op("pe", trd, reads=["kd" + dk, "ident_b"], writes=["bank%d" % bk])
            P.op("act", lambda e, g3=g3, bk=bk: e.copy(out=kd_tm[d][:, g3 * 6:(g3 + 1) * 6, :], in_=bank_bf(bk)[:, 0:768].rearrange("p (a b) -> p a b", b=128)),
                 reads=["bank%d" % bk], writes=["kdtm%s_%d" % (dk, g3)])

    def hgrn_dir_scan(h, d):
        dk = "d%d" % d
        kdk = ["kdtm%s_%d" % (dk, g3) for g3 in range(3)]
        bA, bO, bS = 3 * d, 3 * d + 1, 3 * d + 2
        S, Sb, Sc = Sst[d], Sbf[d], Scx[d]

        def state_step(c, St, stk, first):
            ti, half = c // 2, c % 2
            ps = slice(half * 64, half * 64 + 64)
            P.op("pe", lambda e: e.matmul(banks[bS][:, 0:128], lhsT=kd_tm[d][ps, ti, :], rhs=v_tm[ps, ti, :], start=True, stop=True),
                 reads=kdk + ["v_tm"], writes=["bank%d" % bS])
            if first:
                P.op("dve", lambda e: e.tensor_copy(out=St, in_=banks[bS][:, 0:128]), reads=["bank%d" % bS], writes=[stk])
            else:
                P.op("dve", lambda e: e.scalar_tensor_tensor(out=St, in0=St, scalar=etot[d][:, c:c + 1], in1=banks[bS][:, 0:128], op0=ALU.mult, op1=ALU.add),
                     reads=["bank%d" % bS, stk, "etot" + dk], writes=[stk])
        corder = [32, 33, 34, 35] if d == 0 else [35, 34, 33, 32]
        for i, c in enumerate(corder):
            state_step(c, Sc, "Sc" + dk, i == 0)
            yield
        P.op("dve", lambda e: e.tensor_copy(out=sctx[:, d, h, :], in_=Sc), reads=["Sc" + dk], writes=["sctx_%d_%d" % (d, h)])
        P.op("pool", lambda e: e.memset(Sb, 0.0), reads=["Sb" + dk], writes=["Sb" + dk])
        groups = [0, 1, 2, 3] if d == 0 else [3, 2, 1, 0]
        first_state = True
        for g in groups:
            def att(e, g=g):
                for pi in range(4):
                    p = g * 4 + pi
                    r = e.matmul(banks[bA][:, pi * 128:(pi + 1) * 128], lhsT=ke[d][:, p * 128:(p + 1) * 128], rhs=qe[d][:, p * 128:(p + 1) * 128], start=True, stop=True)
                return r
            P.op("pe", att, reads=["ke" + dk, "qe" + dk], writes=["bank%d" % bA])
            P.op("dve", lambda e: e.tensor_tensor(out=attm[d], in0=banks[bA][:, :].rearrange("p (a b) -> p a b", b=128), in1=bc(masks[d], 1, [128, 4, 128]), op=ALU.mult),
                 reads=["bank%d" % bA, "mask"], writes=["attm" + dk])
            pis = [0, 1, 2, 3] if d == 0 else [3, 2, 1, 0]
            for pi in pis:
                p = g * 4 + pi
                P.op("pe", lambda e, pi=pi, p=p: e.matmul(banks[bO][:, pi * 128:(pi + 1) * 128], lhsT=v_tm[:, p, :], rhs=attm[d][:, pi, :], start=True, stop=False),
                     reads=["v_tm", "attm" + dk], writes=["bank%d" % bO])
                chunks = [2 * p, 2 * p + 1] if d == 0 else [2 * p + 1, 2 * p]
                for ci, c in enumerate(chunks):
                    col = pi * 128 + (c % 2) * 64
                    P.op("pe", lambda e, c=c, col=col, ci=ci: e.matmul(banks[bO][:, col:col + 64], lhsT=Sb, rhs=qe[d][:, c * 64:(c + 1) * 64], start=False, stop=(ci == 1)),
                         reads=["Sb" + dk, "qe" + dk], writes=["bank%d" % bO])
                    state_step(c, S, "S" + dk, first_state)
                    first_state = False
                    P.op("act", lambda e: e.copy(out=Sb, in_=S), reads=["S" + dk], writes=["Sb" + dk])
                    yield
            cs = slice(g * 512, (g + 1) * 512)
            if (d == 0 and g < 2) or (d == 1 and g >= 2):
                P.op("act", lambda e, cs=cs: e.copy(out=o_acc[:, cs], in_=banks[bO][:, :]), reads=["bank%d" % bO, "oacc_%d" % g], writes=["oacc_%d" % g])
            else:
                P.op("dve", lambda e, cs=cs: e.tensor_tensor(out=o_acc[:, cs], in0=o_acc[:, cs], in1=banks[bO][:, :], op=ALU.add),
                     reads=["bank%d" % bO, "oacc_%d" % g], writes=["oacc_%d" % g])
        P.op("dve", lambda e: e.tensor_copy(out=stage[:, d, h, :], in_=S), reads=["S" + dk], writes=["stage_%d_%d" % (d, h)])

    P.buf["mask"] = {"w": ("e", "pool", P.cnt["pool"]), "r": {}}
    for h in range(8):
        wh = whb[h % 2]
        whk = "wh0"
        for s5 in range(5):
            P.dma("pool", wh[:, :, s5, :], win_v[:, :, h + 8 * s5, :], "ld_" + whk, writes=[whk])
        for blk in range(4):
            proj_fm(wh, whk, 0, blk * 512, 512, lambda bap, bkey, blk=blk: P.op(
                "act", lambda e: e.copy(out=qT[:, blk * 512:(blk + 1) * 512], in_=bap), reads=[bkey, "qT"], writes=["qT"]))
        for blk in range(4):
            proj_fm(wh, whk, 4, blk * 512, 512, lambda bap, bkey, blk=blk: P.op(
                "act", lambda e: e.activation(out=gtmp[:, blk * 512:(blk + 1) * 512], in_=bap, func=AF.Silu), reads=[bkey, "ee"], writes=["ee"]))
        P.op("dve", lambda e: e.tensor_scalar(out=gsT, in0=gtmp, scalar1=hng[:, 0:1], scalar2=None, op0=ALU.mult), reads=["ee", "hng"], writes=["gsT"])
        P.dma("sp", dm_gs[h], gsT, "st_gs", reads=["gsT"], writes=["dm_gs"])
        for g4 in range(5):
            tiles = list(range(g4 * 4, min(18, g4 * 4 + 4)))
            bk = PB + (pbc[0] % 2)
            pbc[0] += 1

            def mmv(e, tiles=tiles, bk=bk, wh=wh):
                for i, ti in enumerate(tiles):
                    for kt in range(8):
                        r = e.matmul(banks[bk][:, i * 128:(i + 1) * 128], lhsT=uT[:, kt, ti * 128:(ti + 1) * 128], rhs=wh[:, kt, 1, :], start=(kt == 0), stop=(kt == 7))
                return r
            P.op("pe", mmv, reads=[whk] + ["uT_%d" % t for t in tiles], writes=["bank%d" % bk])
            nt_ = len(tiles)
            P.op("act", lambda e, tiles=tiles, bk=bk, nt_=nt_: e.copy(out=v_tm[:, tiles[0]:tiles[0] + nt_, :], in_=banks[bk][:, 0:nt_ * 128].rearrange("p (a b) -> p a b", b=128)),
                 reads=["bank%d" % bk, "v_tm"], writes=["v_tm"])
        hgrn_dir_prep(h, 0)
        hgrn_dir_prep(h, 1)
        gens = [hgrn_dir_scan(h, 0), hgrn_dir_scan(h, 1)]
        alive = [True, True]
        while any(alive):
            for i in range(2):
                if alive[i]:
                    try:
                        next(gens[i])
                    except StopIteration:
                        alive[i] = False
        P.dma("sp", dm_oloc[h], o_acc, "st_oloc", reads=["oacc_%d" % g for g in range(4)], writes=["dm_oloc"])
    for d in range(2):
        P.dma("sp", ag2_ins[d][:, 0:1024], stage[:, d, :, :].rearrange("p b c -> p (b c)"), "st_ag2", reads=["stage_%d_%d" % (d, h) for h in range(8)], writes=["ag2_in"])
        P.dma("sp", ag2_ins[d][:, 1024:1032], Dv[:, d, :], "st_ag2", reads=["Dv_%d_%d" % (d, h) for h in range(8)], writes=["ag2_in"])
    for d in range(2):
        P.custom("pool", lambda e, d=d: e.collective_compute("AllGather", ALU.bypass, replica_groups=[[0, 1, 2, 3], [4, 5, 6, 7]], ins=[ag2_ins[d]], outs=[ag2_outs[d]]),
                 "cc2", 1, reads=["ag2_in"] + (["ag2_out"] if d > 0 else []), writes=["ag2_out"])
    P.barrier(keep=["ag1_out", "dm_kvctx", "dm_mod", "ag2_out", "dm_oloc", "dm_qb", "dm_gs"] + UT_KEYS)
    A.release(m3h)
    gath = A.alloc((2, 4, 1032), F32)
    Rr = A.alloc((2, 8, 128), F32)
    Tt = A.alloc((8, 128), F32)
    selv = A.alloc((8,), F32)
    Sin = A.alloc((2, 8, 128), BF16)
    for d in range(2):
        P.dma("sp", gath[:, d, :, :], ag2_outs[d].rearrange("(r p) n -> p r n", p=128), "ld_gath", reads=["ag2_out"], writes=["gath"])
    P.dma("sp", selv, sel_d, "ld_c", writes=["selv"])
    SCK = ["sctx_%d_%d" % (d, h) for d in range(2) for h in range(8)]
    P.op("dve", lambda e: e.tensor_copy(out=Rr.rearrange("p a b c -> p (a b c)"), in_=sctx.rearrange("p a b c -> p (a b c)")), writes=["Rr"])
    for d in range(2):
        order = [0, 1, 2, 3] if d == 0 else [3, 2, 1, 0]
        for r in order:
            Sl = gath[:, d, r, 0:1024].rearrange("p (h v) -> p h v", v=128)
            Dr = gath[:, d, r, 1024:1032]
            P.op("dve", lambda e, d=d, Dr=Dr: e.tensor_tensor(out=Tt, in0=Rr[:, d, :, :], in1=bc(Dr, 2, [128, 8, 128]), op=ALU.mult), reads=["Rr", "gath"], writes=["Tt"])
            P.op("dve", lambda e, Sl=Sl: e.tensor_tensor(out=Tt, in0=Tt, in1=Sl, op=ALU.add), reads=["Tt", "gath"], writes=["Tt"])
            P.op("dve", lambda e, d=d: e.tensor_tensor(out=Tt, in0=Tt, in1=Rr[:, d, :, :], op=ALU.subtract), reads=["Tt", "Rr"], writes=["Tt"])
            P.op("dve", lambda e, d=d, r=r: e.scalar_tensor_tensor(out=Rr[:, d, :, :], in0=Tt, scalar=selv[:, d * 4 + r:d * 4 + r + 1], in1=Rr[:, d, :, :], op0=ALU.mult, op1=ALU.add),
                 reads=["Tt", "Rr", "selv"], writes=["Rr"])
    P.op("act", lambda e: e.copy(out=Sin.rearrange("p a b c -> p (a b c)"), in_=Rr.rearrange("p a b c -> p (a b c)")), reads=["Rr"], writes=["Sin"])
    ol = A.alloc((NT,), F32)
    qbf_ = A.alloc((NT,), BF16)
    qbb_ = A.alloc((NT,), BF16)
    gsl = A.alloc((NT,), BF16)
    sqb = A.alloc((NT,), BF16)
    lnr = A.alloc((NT,), F32)
    rsr = A.alloc((NT,), F32)
    for h in range(8):
        P.dma("sp", ol, dm_oloc[h], "ld_ol", reads=["dm_oloc"], writes=["ol"] + ["ol_%d" % b_ for b_ in range(4)])
        P.dma("sp", qbf_, dm_qb[0, h], "ld_qb0", reads=["dm_qb"], writes=["qbf_"])
        P.dma("sp", qbb_, dm_qb[1, h], "ld_qb1", reads=["dm_qb"], writes=["qbb_"])
        P.dma("sp", gsl, dm_gs[h], "ld_gs", reads=["dm_gs"], writes=["gsl"])
        for blk in range(4):
            cs = slice(blk * 512, (blk + 1) * 512)
            bk = blk % 2

            def corr(e, h=h, cs=cs, bk=bk):
                e.matmul(banks[bk][:, :], lhsT=Sin[:, 0, h, :], rhs=qbf_[:, cs], start=True, stop=False)
                return e.matmul(banks[bk][:, :], lhsT=Sin[:, 1, h, :], rhs=qbb_[:, cs], start=False, stop=True)
            P.op("pe", corr, reads=["Sin", "qbf_", "qbb_"], writes=["bank%d" % bk])
            P.op("dve", lambda e, cs=cs, bk=bk: e.tensor_tensor(out=ol[:, cs], in0=ol[:, cs], in1=banks[bk][:, :], op=ALU.add), reads=["bank%d" % bk, "ol", "ol_%d" % blk], writes=["ol_%d" % blk])
            P.op("act", lambda e, cs=cs: e.activation(out=sqb[:, cs], in_=ol[:, cs], func=AF.Square), reads=["ol_%d" % blk], writes=["sqb_%d" % blk])
            bk2 = 2 + blk % 2
            P.op("pe", lambda e, cs=cs, bk2=bk2: e.matmul(banks[bk2][:, :], lhsT=ones_b, rhs=sqb[:, cs], start=True, stop=True), reads=["sqb_%d" % blk, "ones_b"], writes=["bank%d" % bk2])
            P.op("act", lambda e, cs=cs, bk2=bk2: e.activation(out=lnr[:, cs], in_=banks[bk2][:, :], func=AF.Ln, scale=1.0 / 128, bias=EPS), reads=["bank%d" % bk2], writes=["lnr_%d" % blk])
            P.op("act", lambda e, cs=cs: e.activation(out=rsr[:, cs], in_=lnr[:, cs], func=AF.Exp, scale=-0.5), reads=["lnr_%d" % blk], writes=["rsr_%d" % blk])
            P.op("dve", lambda e, cs=cs: e.tensor_tensor(out=ol[:, cs], in0=ol[:, cs], in1=rsr[:, cs], op=ALU.mult), reads=["ol_%d" % blk, "rsr_%d" % blk], writes=["ol_%d" % blk])
            P.op("dve", lambda e, cs=cs: e.tensor_tensor(out=sqb[:, cs], in0=ol[:, cs], in1=gsl[:, cs], op=ALU.mult), reads=["ol_%d" % blk, "gsl"], writes=["sqb_%d" % blk])
        P.dma("sp", dm_oth[h], sqb, "st_oth", reads=["sqb_%d" % b_ for b_ in range(4)], writes=["dm_oth"])
        for b_ in range(4):
            for nm in ("ol_%d", "sqb_%d", "lnr_%d", "rsr_%d", "oth_%d"):
                pass
    P.barrier(keep=["ag1_out", "dm_kvctx", "dm_mod", "dm_oth"] + UT_KEYS)
    A.release(m3)
    if upto <= 3:
        return finish(P, nc)


    m4 = A.mark()
    QT = A.alloc((8, NT), BF16)
    KTa = A.alloc((2, 8448), BF16)
    Va = A.alloc((66, 256), BF16)
    for r in range(4):
        for h in range(2):
            P.dma("sp", KTa[:, h, r * NT:(r + 1) * NT], ag1_outs[h][r * 128:(r + 1) * 128, :], "ld_kta", reads=["ag1_out"], writes=["KTa"])
            P.dma("sp", Va[:, r * 16 + 8 * h:r * 16 + 8 * h + 8, :], ag1_outs[2 + h][r * 128:(r + 1) * 128, :].rearrange("p (ti c) -> p ti c", c=256), "ld_va", reads=["ag1_out"], writes=["Va"])
    P.dma("sp", KTa[:, :, 8192:8448], dm_kvctx[0:256, :].rearrange("(h p) t -> p h t", p=128), "ld_kta", reads=["dm_kvctx"], writes=["KTa"])
    P.dma("sp", Va[:, 64:66, :], dm_kvctx[256:512, :].rearrange("(ti p) c -> p ti c", p=128), "ld_va", reads=["dm_kvctx"], writes=["Va"])
    m4b = A.mark()
    wq = A.alloc((8, 1024), BF16)
    P.dma("pool", wq, win_d[:, 5120:6144].rearrange("(kt p) n -> p kt n", p=128), "ld_wq", writes=["wq"])
    ropeT = A.alloc((NTT, 256), F32)
    P.dma("sp", ropeT, rope_d.rearrange("(t p) n -> p t n", p=128), "ld_rope", writes=["ropeT"])
    gqk = A.alloc((256,), F32)
    P.dma("sp", gqk, qkg_d[0].partition_broadcast(128), "ld_c", writes=["gqk"])
    ssq = A.alloc((16, 8), F32)
    lnq = A.alloc((16, 8), F32)
    rsq = A.alloc((16, 8), F32)
    qn = A.alloc((8, 128), F32)
    t1q = A.alloc((8, 128), F32)
    t2q = A.alloc((8, 128), F32)
    qbf = A.alloc((1024,), BF16)
    junk2 = A.alloc((128,), BF16)
    P.op("pool", lambda e: e.memset(ssq, 0.0), writes=["ssq"])
    for ti in range(NTT):
        s = ti % 2
        for half in range(2):
            def mmq(e, ti=ti, half=half):
                for kt in range(8):
                    r = e.matmul(banks[half][:, :], lhsT=uT[:, kt, ti * 128:(ti + 1) * 128], rhs=wq[:, kt, half * 512:(half + 1) * 512], start=(kt == 0), stop=(kt == 7))
                return r
            P.op("pe", mmq, reads=["uT_%d" % ti, "wq"], writes=["bank%d" % half])
        for h in range(8):
            P.op("act", lambda e, h=h, ti=ti: e.activation(out=junk2, in_=banks[h // 4][:, (h % 4) * 128:(h % 4 + 1) * 128], func=AF.Square, accum_out=ssq[:, ti, h:h + 1]),
                 reads=["bank%d" % (h // 4), "ssq"], writes=["junk2", "ssq_%d" % ti])
        rstd_from_ss(ssq[:, ti, :], 128, rsq[:, ti, :], lnq[:, ti, :], ["ssq_%d" % ti], "rsq_%d" % ti)
        for half in range(2):
            P.op("dve", lambda e, half=half, ti=ti: e.tensor_tensor(out=qn[:, half * 4:(half + 1) * 4, :], in0=banks[half][:, :].rearrange("p (h d) -> p h d", d=128),
                                                                in1=bc(rsq[:, ti, half * 4:(half + 1) * 4], 2, [128, 4, 128]), op=ALU.mult),
                 reads=["bank%d" % half, "rsq_%d" % ti, "qxn"], writes=["qxn"])
        P.op("dve", lambda e: e.tensor_tensor(out=qn, in0=qn, in1=bc(gqk[:, 0:128], 1, [128, 8, 128]), op=ALU.mult), reads=["qxn", "gqk"], writes=["qxn"])
        rope_apply(qn, 8, ti, qbf, "q")
        bk2 = 2 + s

        def trq(e, bk2=bk2):
            for h in range(8):
                r = e.transpose(bank_bf(bk2)[:, h * 128:(h + 1) * 128], qbf[:, h * 128:(h + 1) * 128], ident_b)
            return r
        P.op("pe", trq, reads=["qbf", "ident_b"], writes=["bank%d" % bk2])
        P.op("act", lambda e, ti=ti, bk2=bk2: e.copy(out=QT[:, :, ti * 128:(ti + 1) * 128], in_=bank_bf(bk2).rearrange("p (a b) -> p a b", b=128)),
             reads=["bank%d" % bk2], writes=["QT"])
    P.barrier(keep=["dm_mod", "dm_oth", "KTa", "Va"] + UT_KEYS)
    A.release(m4b)
    if upto <= 4:
        return finish(P, nc)

    pT = [A.alloc((512,), BF16) for _ in range(3)]
    rden = A.alloc((512,), F32)
    ob = [A.alloc((512,), BF16) for _ in range(2)]
    dacc = [A.alloc((512,), F32) for _ in range(2)]
    SCALE = float(128 ** -0.5)
    NKT = 66
    it = 0
    for kvh in range(2):
        for qb in range(4):
            for g in range(4):
                head = kvh * 4 + g
                bo, bd = 4 + it % 2, 6 + it % 2
                qs = slice(qb * 512, (qb + 1) * 512)

                def s_mm(kt, kvh=kvh, head=head, qs=qs):
                    P.op("pe", lambda e: e.matmul(banks[kt % 3][:, :], lhsT=KTa[:, kvh, kt * 128:(kt + 1) * 128], rhs=QT[:, head, qs], start=True, stop=True),
                         reads=["KTa", "QT"], writes=["bank%d" % (kt % 3)])
                s_mm(0)
                for kt in range(NKT):
                    if kt + 1 < NKT:
                        s_mm(kt + 1)
                    P.op("act", lambda e, kt=kt: e.activation(out=pT[kt % 3], in_=banks[kt % 3][:, :], func=AF.Exp, scale=SCALE),
                         reads=["bank%d" % (kt % 3)], writes=["pT%d" % (kt % 3)])

                    P.op("pe", lambda e, kt=kt, kvh=kvh, bo=bo: e.matmul(banks[bo][:, :], lhsT=Va[:, kt, kvh * 128:(kvh + 1) * 128], rhs=pT[kt % 3], start=(kt == 0), stop=(kt == NKT - 1)),
                         reads=["pT%d" % (kt % 3), "Va"], writes=["bank%d" % bo])
                    deng = "dve" if kt % 2 == 0 else "pool"
                    da = dacc[kt % 2]
                    dkey = "dacc%d" % (kt % 2)
                    if kt < 2:
                        P.op(deng, lambda e, kt=kt, da=da: e.tensor_copy(out=da, in_=pT[kt % 3]), reads=["pT%d" % (kt % 3)], writes=[dkey])
                    else:
                        P.op(deng, lambda e, kt=kt, da=da: e.tensor_tensor(out=da, in0=da, in1=pT[kt % 3], op=ALU.add), reads=["pT%d" % (kt % 3), dkey], writes=[dkey])

                def dsum(e, bd=bd):
                    e.matmul(banks[bd][:, :], lhsT=ones_f, rhs=dacc[0], start=True, stop=False)
                    return e.matmul(banks[bd][:, :], lhsT=ones_f, rhs=dacc[1], start=False, stop=True)
                P.op("pe", dsum, reads=["dacc0", "dacc1", "ones_f"], writes=["bank%d" % bd])
                P.op("dve", lambda e, bd=bd: e.reciprocal(out=rden, in_=banks[bd][:, :]), reads=["bank%d" % bd], writes=["rden"])
                P.op("dve", lambda e, bo=bo, it=it: e.tensor_tensor(out=ob[it % 2], in0=banks[bo][:, :], in1=rden, op=ALU.mult), reads=["bank%d" % bo, "rden"], writes=["ob%d" % (it % 2)])
                P.dma("sp", dm_ota[head][:, qs], ob[it % 2], "st_ota%d" % (it % 2), reads=["ob%d" % (it % 2)], writes=["dm_ota"])
                it += 1
    P.barrier(keep=["dm_mod", "dm_oth", "dm_ota"] + UT_KEYS)
    A.release(m4)
    if upto <= 5:
        return finish(P, nc)

    m6 = A.mark()
    wg = A.alloc((8, 2048), BF16)
    wb0 = A.alloc((8, 1024), BF16)
    wb1 = A.alloc((8, 1024), BF16)
    wo = A.alloc((8, 1024), BF16)
    P.dma("pool", wg, win_d[:, 6656:8704].rearrange("(kt p) n -> p kt n", p=128), "ld_w6", writes=["wg"])
    P.dma("pool", wb0, wbr_d[0].rearrange("(kt p) n -> p kt n", p=128), "ld_w6", writes=["wb0"])
    P.dma("pool", wb1, wbr_d[1].rearrange("(kt p) n -> p kt n", p=128), "ld_w6", writes=["wb1"])
    P.dma("pool", wo, wout_d.rearrange("(kt p) n -> p kt n", p=128), "ld_w6", writes=["wo"])
    G1 = A.alloc((D,), F32)
    A2 = A.alloc((D,), F32)
    B2 = A.alloc((D,), F32)
    P.dma("sp", G1, dm_mod[:, 2 * D:3 * D], "ld_c", reads=["dm_mod"], writes=["G1"])
    P.dma("sp", A2, dm_mod[:, 4 * D:5 * D], "ld_c", reads=["dm_mod"], writes=["A2"])
    P.dma("sp", B2, dm_mod[:, 3 * D:4 * D], "ld_c", reads=["dm_mod"], writes=["B2"])
    rwt = A.alloc((8, NE), F32)
    rbt = A.alloc((NE,), F32)
    P.dma("sp", rwt, rw_d.rearrange("(kt p) e -> p kt e", p=128), "ld_c", writes=["rwt"])
    P.dma("sp", rbt, rb_d[0].partition_broadcast(128), "ld_c", writes=["rbt"])
    othb = A.alloc((8, 512), BF16)
    otab = A.alloc((8, 512), BF16)
    y1T = A.alloc((8, 512), BF16)
    sgh = [A.alloc((512,), F32)] * 2
    sga = [A.alloc((512,), F32)] * 2
    tA = [A.alloc((512,), F32)] * 2
    tB = [A.alloc((512,), F32)] * 2
    xt6 = [A.alloc((D,), F32) for _ in range(2)]
    tmp6 = A.alloc((D,), F32)
    x1t = [A.alloc((D,), F32)] * 2
    u2f = A.alloc((D,), F32)
    u2b = A.alloc((D,), BF16)
    junk6 = A.alloc((D,), BF16)
    ssy = A.alloc((16, 2), F32)
    ssy1 = A.alloc((16,), F32)
    lny = A.alloc((16,), F32)
    rsy = A.alloc((16,), F32)
    ssx = A.alloc((16,), F32)
    lnx = A.alloc((16,), F32)
    rsx = A.alloc((16,), F32)
    u2Tf = A.alloc((8, 128), F32)
    u2Tb = [A.alloc((8, 128), BF16) for _ in range(2)]
    lg = A.alloc((NE,), F32)
    mx8 = A.alloc((8,), F32)
    msk = A.alloc((NE,), F32)
    em = A.alloc((NE,), F32)
    nmx = A.alloc((1,), F32)
    ssum = A.alloc((1,), F32)
    rsum = A.alloc((1,), F32)
    cmb = A.alloc((16, NE), F32)
    cT = A.alloc((128,), F32)
    P.op("pool", lambda e: e.memset(ssy, 0.0), writes=["ssy"])
    P.op("pool", lambda e: e.memset(ssx, 0.0), writes=["ssx"])
    oth_v = dm_oth.rearrange("h p t -> p h t")
    ota_v = dm_ota.rearrange("h p t -> p h t")
    for blk in range(4):
        cs = slice(blk * 512, (blk + 1) * 512)
        P.dma("sp", othb, oth_v[:, :, cs], "ld_oth", reads=["dm_oth"], writes=["othb"])
        P.dma("sp", otab, ota_v[:, :, cs], "ld_ota", reads=["dm_ota"], writes=["otab"])
        utk = ["uT_%d" % t for t in range(blk * 4, blk * 4 + 4)]
        for dt in range(8):
            s = 0
            ds = slice(dt * 128, (dt + 1) * 128)

            def mm4(e, ds=ds, dt=dt, cs=cs):
                for kt in range(8):
                    e.matmul(banks[0][:, :], lhsT=wg[:, kt, dt * 128:(dt + 1) * 128], rhs=uT[:, kt, cs], start=(kt == 0), stop=(kt == 7))
                for kt in range(8):
                    e.matmul(banks[1][:, :], lhsT=wg[:, kt, 1024 + dt * 128:1024 + (dt + 1) * 128], rhs=uT[:, kt, cs], start=(kt == 0), stop=(kt == 7))
                for kt in range(8):
                    e.matmul(banks[2][:, :], lhsT=wb0[:, kt, ds], rhs=othb[:, kt, :], start=(kt == 0), stop=(kt == 7))
                for kt in range(8):
                    r = e.matmul(banks[3][:, :], lhsT=wb1[:, kt, ds], rhs=otab[:, kt, :], start=(kt == 0), stop=(kt == 7))
                return r
            P.op("pe", mm4, reads=["wg", "wb0", "wb1", "othb", "otab"] + utk, writes=["bank0", "bank1", "bank2", "bank3"])
            P.op("act", lambda e, s=s: e.activation(out=sgh[s], in_=banks[0][:, :], func=AF.Sigmoid), reads=["bank0"], writes=["sgh%d" % s])
            P.op("act", lambda e, s=s: e.activation(out=sga[s], in_=banks[1][:, :], func=AF.Sigmoid), reads=["bank1"], writes=["sga%d" % s])
            P.op("dve", lambda e, s=s: e.tensor_tensor(out=tA[s], in0=sgh[s], in1=banks[2][:, :], op=ALU.mult), reads=["sgh%d" % s, "bank2"], writes=["tA%d" % s])
            P.op("dve", lambda e, s=s: e.tensor_tensor(out=tB[s], in0=sga[s], in1=banks[3][:, :], op=ALU.mult), reads=["sga%d" % s, "bank3"], writes=["tB%d" % s])
            P.op("dve", lambda e, s=s, dt=dt: e.tensor_tensor(out=y1T[:, dt, :], in0=tA[s], in1=tB[s], op=ALU.add), reads=["tA%d" % s, "tB%d" % s], writes=["y1T"])
        for tt in range(4):
            ti = blk * 4 + tt
            s = ti % 2
            ts_ = slice(tt * 128, (tt + 1) * 128)
            P.dma("sp", xt6[s], x_d[ti * 128:(ti + 1) * 128, :], "ld_x6%d" % s, writes=["xt6%d" % s])
            for half in range(2):
                def mmy(e, half=half, ts_=ts_):
                    for kt in range(8):
                        r = e.matmul(banks[4 + half][:, :], lhsT=y1T[:, kt, ts_], rhs=wo[:, kt, half * 512:(half + 1) * 512], start=(kt == 0), stop=(kt == 7))
                    return r
                P.op("pe", mmy, reads=["y1T", "wo"], writes=["bank%d" % (4 + half)])
                P.op("act", lambda e, half=half, ti=ti: e.activation(out=junk6[:, 0:512], in_=banks[4 + half][:, :], func=AF.Square, accum_out=ssy[:, ti, half:half + 1]),
                     reads=["bank%d" % (4 + half), "ssy"], writes=["junk6", "ssy_%d_%d" % (ti, half)])
            P.op("dve", lambda e, ti=ti: e.tensor_tensor(out=ssy1[:, ti:ti + 1], in0=ssy[:, ti, 0:1], in1=ssy[:, ti, 1:2], op=ALU.add),
                 reads=["ssy_%d_0" % ti, "ssy_%d_1" % ti], writes=["ssy1_%d" % ti])
            rstd_from_ss(ssy1[:, ti:ti + 1], D, rsy[:, ti:ti + 1], lny[:, ti:ti + 1], ["ssy1_%d" % ti], "rsy_%d" % ti)
            for half in range(2):
                hs = slice(half * 512, (half + 1) * 512)
                P.op("dve", lambda e, half=half, hs=hs, ti=ti: e.scalar_tensor_tensor(out=tmp6[:, hs], in0=banks[4 + half][:, :], scalar=rsy[:, ti:ti + 1], in1=G1[:, hs], op0=ALU.mult, op1=ALU.mult),
                     reads=["bank%d" % (4 + half), "rsy_%d" % ti, "G1", "tmp6"], writes=["tmp6"])
            P.op("dve", lambda e, s=s: e.tensor_tensor(out=x1t[s], in0=tmp6, in1=xt6[s], op=ALU.add), reads=["tmp6", "xt6%d" % s], writes=["x1t"])
            P.dma("sp", dm_x1[ti * 128:(ti + 1) * 128, :], x1t[s], "st_x1%d" % s, reads=["x1t"], writes=["dm_x1"])
            P.op("act", lambda e, s=s, ti=ti: e.activation(out=junk6, in_=x1t[s], func=AF.Square, accum_out=ssx[:, ti:ti + 1]), reads=["x1t", "ssx"], writes=["junk6", "ssx_%d" % ti])
            rstd_from_ss(ssx[:, ti:ti + 1], D, rsx[:, ti:ti + 1], lnx[:, ti:ti + 1], ["ssx_%d" % ti], "rsx_%d" % ti)
            P.op("dve", lambda e, s=s, ti=ti: e.scalar_tensor_tensor(out=tmp6, in0=x1t[s], scalar=rsx[:, ti:ti + 1], in1=A2, op0=ALU.mult, op1=ALU.mult),
                 reads=["x1t", "rsx_%d" % ti, "A2", "tmp6"], writes=["tmp6"])
            P.op("dve", lambda e: e.tensor_tensor(out=u2f, in0=tmp6, in1=B2, op=ALU.add), reads=["tmp6", "B2"], writes=["u2f"])
            P.op("act", lambda e: e.copy(out=u2b, in_=u2f), reads=["u2f"], writes=["u2b"])

            def tru(e):
                for kt in range(8):
                    r = e.transpose(bank_bf(6)[:, kt * 128:(kt + 1) * 128], u2b[:, kt * 128:(kt + 1) * 128], ident_b)
                return r
            P.op("pe", tru, reads=["u2b", "ident_b"], writes=["bank6"])
            P.op("act", lambda e, s=s: e.copy(out=u2Tb[s], in_=bank_bf(6).rearrange("p (a b) -> p a b", b=128)), reads=["bank6"], writes=["u2Tb%d" % s])
            P.dma("sp", dm_u2t[:, :, ti * 128:(ti + 1) * 128], u2Tb[s], "st_u2t%d" % s, reads=["u2Tb%d" % s], writes=["dm_u2t"])
            for g2 in range(2):
                def truf(e, g2=g2):
                    for i in range(4):
                        kt = g2 * 4 + i
                        r = e.transpose(banks[7][:, i * 128:(i + 1) * 128], u2f[:, kt * 128:(kt + 1) * 128], ident_f)
                    return r
                P.op("pe", truf, reads=["u2f", "ident_f"], writes=["bank7"])
                P.op("act", lambda e, g2=g2: e.copy(out=u2Tf[:, g2 * 4:(g2 + 1) * 4, :], in_=banks[7][:, :].rearrange("p (a b) -> p a b", b=128)), reads=["bank7", "u2Tf"], writes=["u2Tf"])

            def mml(e):
                for kt in range(8):
                    r = e.matmul(banks[6][:, 0:NE], lhsT=u2Tf[:, kt, :], rhs=rwt[:, kt, :], start=(kt == 0), stop=(kt == 7))
                return r
            P.op("pe", mml, reads=["u2Tf", "rwt"], writes=["bank6"])
            P.op("dve", lambda e: e.tensor_tensor(out=lg, in0=banks[6][:, 0:NE], in1=rbt, op=ALU.add), reads=["bank6", "rbt"], writes=["lg"])
            P.op("dve", lambda e: e.max(out=mx8, in_=lg), reads=["lg"], writes=["mx8"])
            P.op("dve", lambda e: e.tensor_scalar(out=msk, in0=lg, scalar1=mx8[:, 3:4], scalar2=None, op0=ALU.is_ge), reads=["lg", "mx8"], writes=["msk"])
            P.op("dve", lambda e: e.tensor_scalar(out=nmx, in0=mx8[:, 0:1], scalar1=-1.0, scalar2=None, op0=ALU.mult), reads=["mx8"], writes=["nmx"])
            P.op("act", lambda e: e.activation(out=em, in_=lg, func=AF.Exp, bias=nmx[:, 0:1], scale=1.0), reads=["lg", "nmx"], writes=["em"])
            P.op("dve", lambda e: e.tensor_tensor(out=em, in0=em, in1=msk, op=ALU.mult), reads=["em", "msk"], writes=["em"])
            P.op("dve", lambda e: e.reduce_sum(out=ssum, in_=em, axis=AX.X), reads=["em"], writes=["ssum"])
            P.op("dve", lambda e: e.reciprocal(out=rsum, in_=ssum), reads=["ssum"], writes=["rsum"])
            P.op("dve", lambda e, ti=ti: e.tensor_scalar(out=cmb[:, ti, :], in0=em, scalar1=rsum[:, 0:1], scalar2=None, op0=ALU.mult), reads=["em", "rsum"], writes=["cmb_%d" % ti])
            P.op("pe", lambda e, ti=ti: e.transpose(banks[7][0:NE, 0:128], cmb[:, ti, :], ident_f), reads=["cmb_%d" % ti, "ident_f"], writes=["bank7"])
            P.op("act", lambda e: e.copy(out=cT[0:NE, :], in_=banks[7][0:NE, 0:128]), reads=["bank7"], writes=["cT"])
            P.dma("sp", dm_combT[:, ti * 128:(ti + 1) * 128], cT[0:NE, :], "st_cT", reads=["cT"], writes=["dm_combT"])
    P.dma("sp", dm_comb, cmb, "st_cmb", reads=["cmb_%d" % t for t in range(16)], writes=["dm_comb"])
    P.barrier(keep=["dm_mod", "dm_x1", "dm_u2t", "dm_comb", "dm_combT"])
    A.release(m_pre_ut)
    if upto <= 6:
        return finish(P, nc)

    G2 = A.alloc((D,), F32)
    P.dma("sp", G2, dm_mod[:, 5 * D:6 * D], "ld_c", reads=["dm_mod"], writes=["G2"])
    bu = A.alloc((NE * 16,), F32)
    P.dma("sp", bu, bupT_d, "ld_c", writes=["bu"])
    bdn = A.alloc((D,), F32)
    P.dma("sp", bdn[0:NE, :], bdn_d, "ld_c", writes=["bdn"])
    cmb7 = A.alloc((16, NE), F32)
    P.dma("sp", cmb7, dm_comb, "ld_c", reads=["dm_comb"], writes=["cmb7"])
    cT2 = A.alloc((1024,), F32)
    u2T = A.alloc((8, 1024), BF16)
    acc = A.alloc((8, D), F32)
    wu = [A.alloc((8, 2 * D), BF16) for _ in range(2)]
    wd = [A.alloc((8, D), BF16) for _ in range(2)]
    aTraw = [A.alloc((4096,), BF16) for _ in range(2)]
    aT = [a.rearrange("p (a b) -> p a b", b=512) for a in aTraw]
    gc = [A.alloc((512,), F32) for _ in range(2)]
    sgm = [A.alloc((512,), F32) for _ in range(2)]
    lc = [A.alloc((512,), F32) for _ in range(2)]
    tg = [A.alloc((512,), F32) for _ in range(2)]
    xt7 = [aTraw[0][:, 0:2048].bitcast(F32)] * 2
    tmp7 = aTraw[0][:, 2048:4096].bitcast(F32)
    ot7 = [aTraw[1][:, 0:2048].bitcast(F32)] * 2
    junk7 = aTraw[1][:, 2048:3072]
    ss7 = A.alloc((16,), F32)
    ln7 = A.alloc((16,), F32)
    rs7 = A.alloc((16,), F32)
    P.op("pool", lambda e: e.memset(ss7, 0.0), writes=["ss7"])
    ecount = 0
    for half in range(2):
        hc = slice(half * 1024, (half + 1) * 1024)
        P.dma("sp", u2T, dm_u2t[:, :, hc], "ld_u2t", reads=["dm_u2t"], writes=["u2T"])
        P.dma("sp", cT2[0:NE, :], dm_combT[:, hc], "ld_cT2", reads=["dm_combT"], writes=["cT2"])
        for tt in range(8):
            for dh in range(2):
                bk = 4 + (tt * 2 + dh) % 4
                P.op("pe", lambda e, tt=tt, dh=dh, bk=bk: e.matmul(banks[bk][:, :], lhsT=cT2[0:NE, tt * 128:(tt + 1) * 128], rhs=bdn[0:NE, dh * 512:(dh + 1) * 512], start=True, stop=True),
                     reads=["cT2", "bdn"], writes=["bank%d" % bk])
                P.op("act", lambda e, tt=tt, dh=dh, bk=bk: e.copy(out=acc[:, tt, dh * 512:(dh + 1) * 512], in_=banks[bk][:, :]), reads=["bank%d" % bk], writes=["acc_%d_%d" % (tt, dh)])
        for ex in range(NE):
            ws = ecount % 2
            ecount += 1
            P.dma("pool", wu[ws], wup_d[ex].rearrange("(kt p) n -> p kt n", p=128), "ld_wu%d" % ws, writes=["wu%d" % ws])
            P.dma("pool", wd[ws], wdn_d[ex].rearrange("(kt p) n -> p kt n", p=128), "ld_wd%d" % ws, writes=["wd%d" % ws])
            for blk in range(2):
                bs = slice(blk * 512, (blk + 1) * 512)
                ab = aT[blk % 2]
                abk = "aT%d" % (blk % 2)
                for g in range(8):
                    s = g % 2
                    bg, bl = g % 2, 2 + g % 2

                    def mmu(e, g=g, bg=bg, bl=bl, ws=ws, bs=bs):
                        for kt in range(8):
                            e.matmul(banks[bg][:, :], lhsT=wu[ws][:, kt, g * 128:(g + 1) * 128], rhs=u2T[:, kt, bs], start=(kt == 0), stop=(kt == 7))
                        for kt in range(8):
                            r = e.matmul(banks[bl][:, :], lhsT=wu[ws][:, kt, 1024 + g * 128:1024 + (g + 1) * 128], rhs=u2T[:, kt, bs], start=(kt == 0), stop=(kt == 7))
                        return r
                    P.op("pe", mmu, reads=["wu%d" % ws, "u2T"], writes=["bank%d" % bg, "bank%d" % bl])
                    P.op("dve", lambda e, s=s, bg=bg, ex=ex, g=g: e.tensor_scalar(out=gc[s], in0=banks[bg][:, :], scalar1=bu[:, ex * 16 + g:ex * 16 + g + 1], scalar2=7.0, op0=ALU.add, op1=ALU.min),
                         reads=["bank%d" % bg, "bu"], writes=["gc%d" % s])
                    P.op("act", lambda e, s=s: e.activation(out=sgm[s], in_=gc[s], func=AF.Sigmoid, scale=1.702), reads=["gc%d" % s], writes=["sgm%d" % s])
                    P.op("dve", lambda e, s=s, bl=bl, ex=ex, g=g: e.tensor_scalar(out=lc[s], in0=banks[bl][:, :], scalar1=bu[:, ex * 16 + 8 + g:ex * 16 + 8 + g + 1], scalar2=7.0, op0=ALU.add, op1=ALU.min),
                         reads=["bank%d" % bl, "bu"], writes=["lc%d" % s])
                    P.op("pool", lambda e, s=s: e.tensor_scalar(out=lc[s], in0=lc[s], scalar1=-7.0, scalar2=1.0, op0=ALU.max, op1=ALU.add), reads=["lc%d" % s], writes=["lc%d" % s])
                    P.op("pool", lambda e, s=s: e.tensor_tensor(out=tg[s], in0=gc[s], in1=sgm[s], op=ALU.mult), reads=["gc%d" % s, "sgm%d" % s], writes=["tg%d" % s])
                    P.op("dve", lambda e, s=s, g=g, ab=ab: e.tensor_tensor(out=ab[:, g, :], in0=tg[s], in1=lc[s], op=ALU.mult), reads=["tg%d" % s, "lc%d" % s], writes=[abk])
                for tt in range(4):
                    til = blk * 4 + tt
                    for dh in range(2):
                        bk = 4 + (tt * 2 + dh) % 4

                        def mmd(e, tt=tt, dh=dh, bk=bk, ws=ws, ab=ab):
                            for fk in range(8):
                                r = e.matmul(banks[bk][:, :], lhsT=ab[:, fk, tt * 128:(tt + 1) * 128], rhs=wd[ws][:, fk, dh * 512:(dh + 1) * 512], start=(fk == 0), stop=(fk == 7))
                            return r
                        P.op("pe", mmd, reads=[abk, "wd%d" % ws], writes=["bank%d" % bk])
                        ak = "acc_%d_%d" % (til, dh)
                        P.op("dve", lambda e, til=til, dh=dh, bk=bk, ex=ex, half=half: e.scalar_tensor_tensor(
                            out=acc[:, til, dh * 512:(dh + 1) * 512], in0=banks[bk][:, :], scalar=cmb7[:, half * 8 + til, ex:ex + 1], in1=acc[:, til, dh * 512:(dh + 1) * 512], op0=ALU.mult, op1=ALU.add),
                            reads=["bank%d" % bk, "cmb7", ak], writes=[ak])
        P.barrier(keep=["dm_x1", "dm_u2t", "dm_combT"])
        for tt in range(8):
            ti = half * 8 + tt
            s = 0
            P.dma("sp", xt7[s], dm_x1[ti * 128:(ti + 1) * 128, :], "ld_x7%d" % s, reads=["dm_x1"], writes=["xt7%d" % s])
            P.op("act", lambda e, tt=tt, ti=ti: e.activation(out=junk7, in_=acc[:, tt, :], func=AF.Square, accum_out=ss7[:, ti:ti + 1]),
                 reads=["acc_%d_0" % tt, "acc_%d_1" % tt, "ss7"], writes=["junk7", "ss7_%d" % ti])
            rstd_from_ss(ss7[:, ti:ti + 1], D, rs7[:, ti:ti + 1], ln7[:, ti:ti + 1], ["ss7_%d" % ti], "rs7_%d" % ti)
            P.op("dve", lambda e, tt=tt, ti=ti: e.scalar_tensor_tensor(out=tmp7, in0=acc[:, tt, :], scalar=rs7[:, ti:ti + 1], in1=G2, op0=ALU.mult, op1=ALU.mult),
                 reads=["acc_%d_0" % tt, "acc_%d_1" % tt, "rs7_%d" % ti, "G2"], writes=["tmp7"])
            P.op("dve", lambda e, s=s: e.tensor_tensor(out=ot7[s], in0=tmp7, in1=xt7[s], op=ALU.add), reads=["tmp7", "xt7%d" % s], writes=["ot7%d" % s])
            P.dma("sp", out_d[ti * 128:(ti + 1) * 128, :], ot7[s], "st_out%d" % s, reads=["ot7%d" % s], writes=["out"])
        P.barrier(keep=["dm_x1", "dm_u2t", "dm_combT"])

    finish(P, nc)
    return nc


def finish(P, nc):
    P.wait_all("sp")
    P.build()
    P.close()
    return nc


def _rope_table(j):
    t = np.arange(NT) + j * NT
    rows = (t // 64).astype(np.float32)
    cols = (t % 64).astype(np.float32)
    inv = (10000.0 ** (-np.arange(0, 64, 2, dtype=np.float32) / 64)).astype(np.float32)
    ar = rows[:, None] * inv[None, :]
    ac = cols[:, None] * inv[None, :]
    cr, sr, cc, sc = np.cos(ar), np.sin(ar), np.cos(ac), np.sin(ac)
    return np.concatenate([cr, cr, cc, cc, -sr, sr, -sc, sc], axis=1).astype(np.float32)


def make_in_maps(inp, small=False):
    f = lambda a: np.ascontiguousarray(np.asarray(a, dtype=np.float32))
    x, c, ctx, c_ctx = f(inp["x"]), f(inp["c"]), f(inp["ctx"]), f(inp["c_ctx"])
    shared = {
        "w_mod": f(inp["w_mod"][0]), "b_mod": f(inp["b_mod"][0]).reshape(1, -1),
        "norm_g": f(inp["norm_g"][0]).reshape(1, -1), "w_in": f(inp["w_in"][0]),
        "lbv": f(np.asarray(inp["hgrn_lb"]).reshape(2, 2, 8, 128).transpose(3, 0, 1, 2).reshape(128, 32)),
        "hng": f(inp["hgrn_norm_g"][0]).reshape(128, 1), "qkg": f(inp["qk_norm_g"][0]).reshape(1, 256),
        "w_branch": f(inp["w_branch"][0]), "w_out": f(inp["w_out"][0]),
        "router_w": f(inp["router_w"][0]), "router_b": f(inp["router_b"][0]).reshape(1, -1),
        "w_up": f(inp["w_up"][0]), "b_upT": f(np.asarray(inp["b_up"][0]).reshape(32, 16, 128).transpose(2, 0, 1).reshape(128, 512)),
        "w_down": f(inp["w_down"][0]), "b_down": f(inp["b_down"][0]),
    }
    ropes = [_rope_table(j) for j in range(4)]
    maps = []
    for core in range(8):
        b, j = core // 4, core % 4
        cvec = np.concatenate([c[b].reshape(8, 128).T, c_ctx.reshape(8, 128).T], axis=1)
        sel = np.zeros((128, 8), np.float32)
        for r in range(4):
            sel[:, r] = 1.0 if r < j else 0.0
            sel[:, 4 + r] = 1.0 if r > j else 0.0
        m = dict(shared)
        if small:
            m["w_up"] = m["w_up"][0:1]
            m["w_down"] = m["w_down"][0:1]
        m.update({"x": f(x[b, j * NT:(j + 1) * NT]), "ctx": f(ctx[b]), "cvec": f(cvec), "rope": ropes[j], "sel": sel})
        maps.append(m)
    return maps


_NC_CACHE = {}


def kernel(**inputs):
    if "nc" not in _NC_CACHE:
        _NC_CACHE["nc"] = build()
    nc = _NC_CACHE["nc"]
    maps = make_in_maps(inputs)
    res = run_bass_kernel_spmd(nc, maps, core_ids=list(range(8)))
    out = np.empty((2, 8192, D), np.float32)
    for core in range(8):
        b, j = core // 4, core % 4
        out[b, j * NT:(j + 1) * NT] = res.results[core]["out"]
    return out
```

```python
from contextlib import ExitStack
import numpy as np
import concourse.bass as bass
import concourse.mybir as mybir
from concourse.bass_utils import run_bass_kernel_spmd

F32 = mybir.dt.float32
BF16 = mybir.dt.bfloat16
ALU = mybir.AluOpType
AF = mybir.ActivationFunctionType
AX = mybir.AxisListType

ENGS = ("pe", "act", "dve", "pool", "sp")
EPOCH = 16000
EPS = 1e-6


def _freeze(fn, memo=None):
    import types
    if memo is None:
        memo = {}
    if not isinstance(fn, types.FunctionType) or fn.__closure__ is None:
        return fn
    if id(fn) in memo:
        return memo[id(fn)]
    cells = []
    for c in fn.__closure__:
        try:
            v = c.cell_contents
        except ValueError:
            cells.append(c)
            continue
        if isinstance(v, types.FunctionType) and v.__closure__ is not None and v is not fn:
            v = _freeze(v, memo)
        cells.append(types.CellType(v))
    new = types.FunctionType(fn.__code__, fn.__globals__, fn.__name__, fn.__defaults__, tuple(cells))
    new.__kwdefaults__ = fn.__kwdefaults__
    memo[id(fn)] = new
    return new


class Prog:
    def __init__(self, nc, same_engine_sync=True):
        self.nc = nc
        self.es = ExitStack()
        self.q = {e: [] for e in ENGS}
        self.cnt = {e: 0 for e in ENGS}
        self.waited = {}
        self.buf = {}
        self.sems = {}
        self.dma_cnt = {}
        self.same_engine_sync = same_engine_sync
        self.n_sem = 0

    def sem(self, key):
        if key not in self.sems:
            self.n_sem += 1
            self.sems[key] = self.es.enter_context(self.nc.semaphore("s%d" % self.n_sem))
        return self.sems[key]

    def sbuf(self, name, shape, dtype):
        return self.es.enter_context(self.nc.sbuf_tensor(name, list(shape), dtype))

    def psum(self, name, shape, dtype=F32):
        return self.es.enter_context(self.nc.psum_tensor(name, list(shape), dtype))

    def _semkey_for(self, prod):
        kind, name, count = prod
        if kind == "e":
            ep = (count - 1) // EPOCH
            return ("e", name, ep), count - ep * EPOCH
        return ("d", name), count

    def _need(self, eng, prod, waits):
        if prod is None:
            return
        kind, name, count = prod
        if kind == "e" and name == eng and (eng in ("pe", "sp") or not self.same_engine_sync):
            return
        sk, val = self._semkey_for(prod)
        wk = (eng, kind, name)
        if self.waited.get(wk, 0) >= count:
            return
        self.waited[wk] = count
        waits.append((sk, val))

    def _deps(self, eng, reads, writes):
        waits = []
        for k in reads:
            b = self.buf.get(k)
            if b is not None:
                self._need(eng, b["w"], waits)
        for k in writes:
            b = self.buf.get(k)
            if b is not None:
                self._need(eng, b["w"], waits)
                for r in b["r"].values():
                    self._need(eng, r, waits)
        return waits

    def _record(self, prod, reads, writes):
        for k in reads:
            b = self.buf.setdefault(k, {"w": None, "r": {}})
            b["r"][(prod[0], prod[1])] = prod
        for k in writes:
            self.buf[k] = {"w": prod, "r": {}}

    def op(self, eng, fn, reads=(), writes=()):
        fn = _freeze(fn)
        waits = self._deps(eng, reads, writes)
        self.cnt[eng] += 1
        prod = ("e", eng, self.cnt[eng])
        sk, _ = self._semkey_for(prod)
        self.q[eng].append((fn, waits, (sk, 1)))
        self._record(prod, reads, writes)
        return prod

    def dma(self, eng, out, in_, semname, reads=(), writes=(), **kw):
        if writes:
            semname = semname + ":" + writes[0]
        waits = self._deps(eng, reads, writes)
        self.dma_cnt[semname] = self.dma_cnt.get(semname, 0) + 16
        prod = ("d", semname, self.dma_cnt[semname])
        self.q[eng].append((lambda e: e.dma_start(out=out, in_=in_, **kw), waits, (("d", semname), 16)))
        self._record(prod, reads, writes)
        return prod

    def custom(self, eng, fn, semname, inc, reads=(), writes=()):
        fn = _freeze(fn)
        waits = self._deps(eng, reads, writes)
        self.dma_cnt[semname] = self.dma_cnt.get(semname, 0) + inc
        prod = ("d", semname, self.dma_cnt[semname])
        self.q[eng].append((fn, waits, (("d", semname), inc)))
        self._record(prod, reads, writes)
        return prod

    def wait_all(self, eng):
        waits = []
        for e in ENGS:
            if self.cnt[e] > 0 and e != eng:
                self._need(eng, ("e", e, self.cnt[e]), waits)
        for name, c in self.dma_cnt.items():
            self._need(eng, ("d", name, c), waits)
        self.q[eng].append((None, waits, None))

    def barrier(self, keep=()):
        for e in ENGS:
            self.wait_all(e)
        self.buf = {k: v for k, v in self.buf.items() if k in keep}

    def build(self):
        nc = self.nc
        keys = []
        for e in ENGS:
            for (_, w, inc) in self.q[e]:
                for x in w:
                    keys.append(x[0])
                if inc:
                    keys.append(inc[0])
        for sk in dict.fromkeys(keys):
            self.sem(sk)
        engmap = {"pe": "tensor", "act": "scalar", "dve": "vector", "pool": "gpsimd", "sp": "sync"}
        with nc.Block() as block:
            for e in ENGS:
                items = self.q[e]

                def body(eng, items=items):
                    for fn, waits, inc in items:
                        for sk, val in waits:
                            eng.wait_ge(self.sems[sk], val)
                        if fn is not None:
                            ins = fn(eng)
                            if inc is not None:
                                ins.then_inc(self.sems[inc[0]], inc[1])

                getattr(block, engmap[e])(body)

    def close(self):
        self.es.close()


class Arena:
    def __init__(self, P, nbytes):
        self.t = P.sbuf("arena", [128, nbytes // 2], BF16)
        self.nbytes = nbytes
        self.off = 0

    def alloc(self, free_shape, dtype):
        n = int(np.prod(free_shape))
        size = n * (4 if dtype == F32 else 2)
        size = (size + 63) // 64 * 64
        assert self.off + size <= self.nbytes, ("SBUF arena overflow", self.off, size)
        v = self.t[:, self.off // 2:(self.off + size) // 2]
        if dtype == F32:
            v = v.bitcast(F32)
        v = v[:, 0:n]
        self.off += size
        if len(free_shape) == 2:
            v = v.rearrange("p (a b) -> p a b", b=free_shape[1])
        elif len(free_shape) == 3:
            v = v.rearrange("p (a b c) -> p a b c", b=free_shape[1], c=free_shape[2])
        elif len(free_shape) == 4:
            v = v.rearrange("p (a b c d) -> p a b c d", b=free_shape[1], c=free_shape[2], d=free_shape[3])
        return v

    def mark(self):
        return self.off

    def release(self, m):
        self.off = m


def bc(ap, axis, shape):
    return ap.unsqueeze(axis).to_broadcast(list(shape))


NT = 2048
NTT = 16
NCTX = 256
NALL = NT + NCTX
D = 1024
NE = 32
LAST_PHASE = 99


def build(upto=LAST_PHASE, debug=False):
    nc = bass.Bass("TRN2", target_bir_lowering=False)

    def din(name, shape, dt=F32):
        return nc.dram_tensor(name, list(shape), dt, kind="ExternalInput").ap()

    x_d = din("x", [NT, D])
    ctx_d = din("ctx", [NCTX, D])
    cvec_d = din("cvec", [128, 16])
    wmod_d = din("w_mod", [D, 6 * D])
    bmod_d = din("b_mod", [1, 6 * D])
    ng_d = din("norm_g", [1, 4 * D])
    win_d = din("w_in", [D, 8704])
    lbv_d = din("lbv", [128, 32])
    hng_d = din("hng", [128, 1])
    qkg_d = din("qkg", [1, 256])
    wbr_d = din("w_branch", [2, D, D])
    wout_d = din("w_out", [D, D])
    rw_d = din("router_w", [D, NE])
    rb_d = din("router_b", [1, NE])
    NEW = NE if upto >= 7 else 1
    wup_d = din("w_up", [NEW, D, 2 * D])
    bupT_d = din("b_upT", [128, NE * 16])
    wdn_d = din("w_down", [NEW, D, D])
    bdn_d = din("b_down", [NE, D])
    rope_d = din("rope", [NT, 256])
    sel_d = din("sel", [128, 8])
    out_d = nc.dram_tensor("out", [NT, D], F32, kind="ExternalOutput").ap()

    def dscr(name, shape, dt):
        if debug:
            return nc.dram_tensor(name, list(shape), dt, kind="ExternalOutput").ap()
        return nc.dram_tensor(name, list(shape), dt).ap()

    dm_mod = dscr("dm_mod", [128, 6 * D], F32)
    dm_ut = dscr("dm_ut", [128, 8, NALL], BF16)
    ag1_ins = [nc.dram_tensor("ag1_in%d" % q, [128, 2048], BF16).ap() for q in range(4)]
    ag1_outs = [nc.dram_tensor("ag1_out%d" % q, [4 * 128, 2048], BF16).ap() for q in range(4)]
    dm_kvctx = dscr("dm_kvctx", [512, NCTX], BF16)
    dm_oloc = dscr("dm_oloc", [8, 128, NT], F32)
    dm_qb = dscr("dm_qb", [2, 8, 128, NT], BF16)
    dm_gs = dscr("dm_gs", [8, 128, NT], BF16)
    ag2_ins = [nc.dram_tensor("ag2_in%d" % d, [128, 1032], F32).ap() for d in range(2)]
    ag2_outs = [nc.dram_tensor("ag2_out%d" % d, [4 * 128, 1032], F32).ap() for d in range(2)]
    dm_oth = dscr("dm_oth", [8, 128, NT], BF16)
    dm_ota = dscr("dm_ota", [8, 128, NT], BF16)
    dm_x1 = dscr("dm_x1", [NT, D], F32)
    dm_u2t = dscr("dm_u2t", [128, 8, NT], BF16)
    dm_comb = dscr("dm_comb", [128, NTT, NE], F32)
    dm_combT = dscr("dm_combT", [NE, NT], F32)

    P = Prog(nc)
    A = Arena(P, 207 * 1024)
    banks = [P.psum("bank%d" % i, [128, 512], F32) for i in range(8)]

    def bank_bf(i):
        return banks[i][:, :].bitcast(BF16)

    ident_f = A.alloc((128,), F32)
    ident_b = A.alloc((128,), BF16)
    ones_b = A.alloc((128,), BF16)
    ones_f = A.alloc((128,), F32)
    P.op("pool", lambda e: e.memset(ident_f, 0.0), writes=["ident_f"])
    P.op("pool", lambda e: e.affine_select(out=ident_f, in_=ident_f, pattern=[[-1, 128]], compare_op=ALU.not_equal,
                                           fill=1.0, base=0, channel_multiplier=1), reads=["ident_f"], writes=["ident_f"])
    P.op("dve", lambda e: e.tensor_copy(out=ident_b, in_=ident_f), reads=["ident_f"], writes=["ident_b"])
    P.op("pool", lambda e: e.memset(ones_f, 1.0), writes=["ones_f"])
    P.op("dve", lambda e: e.tensor_copy(out=ones_b, in_=ones_f), reads=["ones_f"], writes=["ones_b"])

    m_pre_ut = A.mark()
    uT = A.alloc((8, NALL), BF16)

    def rstd_from_ss(ss_ap, n, out_ap, tmp_ap, rk, wk):
        P.op("act", lambda e: e.activation(out=tmp_ap, in_=ss_ap, func=AF.Ln, scale=1.0 / n, bias=EPS), reads=rk, writes=[wk + "_ln"])
        P.op("act", lambda e: e.activation(out=out_ap, in_=tmp_ap, func=AF.Exp, scale=-0.5), reads=[wk + "_ln"], writes=[wk])

    m0 = A.mark()
    cv = A.alloc((16,), F32)
    scv = A.alloc((16,), F32)
    scb = A.alloc((16, 128), F32)
    bmod = A.alloc((6 * D,), F32)
    ng = A.alloc((4, D), F32)
    modl = A.alloc((6 * D,), F32)
    modc = A.alloc((2 * D,), F32)
    wm = [A.alloc((8, 512), F32) for _ in range(2)]
    P.dma("sp", cv, cvec_d, "ld_c", writes=["cv"])
    P.dma("sp", bmod, bmod_d[0].partition_broadcast(128), "ld_c", writes=["bmod"])
    P.dma("sp", ng, ng_d[0].partition_broadcast(128).rearrange("p (a b) -> p a b", b=D), "ld_c", writes=["ng"])
    P.op("act", lambda e: e.activation(out=scv, in_=cv, func=AF.Silu), reads=["cv"], writes=["scv"])
    for k in range(16):
        P.op("dve", lambda e, k=k: e.tensor_copy(out=scb[:, k, :], in_=scv[:, k:k + 1].to_broadcast([128, 128])),
             reads=["scv"], writes=["scb"])
    for s in range(12):
        w = wm[s % 2]
        wk = "wm%d" % (s % 2)
        P.dma("sp", w, wmod_d[:, s * 512:(s + 1) * 512].rearrange("(kt p) n -> p kt n", p=128), "ld_" + wk, writes=[wk])

        def mm(e, w=w, off=0, bk=0):
            for kt in range(8):
                r = e.matmul(banks[bk][:, :], lhsT=scb[:, off + kt, :], rhs=w[:, kt, :], start=(kt == 0), stop=(kt == 7))
            return r
        P.op("pe", lambda e, w=w: mm(e, w, 0, 0), reads=["scb", wk], writes=["bank0"])
        P.op("dve", lambda e, s=s: e.tensor_tensor(out=modl[:, s * 512:(s + 1) * 512], in0=banks[0][:, :], in1=bmod[:, s * 512:(s + 1) * 512], op=ALU.add),
             reads=["bank0", "bmod"], writes=["modl"])
        if s < 4:
            P.op("pe", lambda e, w=w: mm(e, w, 8, 1), reads=["scb", wk], writes=["bank1"])
            P.op("dve", lambda e, s=s: e.tensor_tensor(out=modc[:, s * 512:(s + 1) * 512], in0=banks[1][:, :], in1=bmod[:, s * 512:(s + 1) * 512], op=ALU.add),
                 reads=["bank1", "bmod"], writes=["modc"])
    P.op("dve", lambda e: e.scalar_tensor_tensor(out=modl[:, D:2 * D], in0=modl[:, D:2 * D], scalar=1.0, in1=ng[:, 0, :], op0=ALU.add, op1=ALU.mult),
         reads=["modl", "ng"], writes=["modl"])
    P.op("dve", lambda e: e.scalar_tensor_tensor(out=modc[:, D:2 * D], in0=modc[:, D:2 * D], scalar=1.0, in1=ng[:, 0, :], op0=ALU.add, op1=ALU.mult),
         reads=["modc", "ng"], writes=["modc"])
    P.op("dve", lambda e: e.tensor_tensor(out=modl[:, 2 * D:3 * D], in0=modl[:, 2 * D:3 * D], in1=ng[:, 1, :], op=ALU.mult), reads=["modl", "ng"], writes=["modl"])
    P.op("dve", lambda e: e.scalar_tensor_tensor(out=modl[:, 4 * D:5 * D], in0=modl[:, 4 * D:5 * D], scalar=1.0, in1=ng[:, 2, :], op0=ALU.add, op1=ALU.mult),
         reads=["modl", "ng"], writes=["modl"])
    P.op("dve", lambda e: e.tensor_tensor(out=modl[:, 5 * D:6 * D], in0=modl[:, 5 * D:6 * D], in1=ng[:, 3, :], op=ALU.mult), reads=["modl", "ng"], writes=["modl"])
    P.dma("sp", dm_mod, modl, "st_mod", reads=["modl"], writes=["dm_mod"])

    xt = [A.alloc((D,), F32) for _ in range(2)]
    junk = A.alloc((D,), BF16)
    tmpf = A.alloc((D,), F32)
    ub = [A.alloc((D,), BF16) for _ in range(2)]
    ss1 = A.alloc((18,), F32)
    ln1 = A.alloc((18,), F32)
    rs1 = A.alloc((18,), F32)
    P.op("pool", lambda e: e.memset(ss1, 0.0), writes=["ss1"])
    for ti in range(18):
        s = ti % 2
        src = x_d[ti * 128:(ti + 1) * 128, :] if ti < NTT else ctx_d[(ti - NTT) * 128:(ti - NTT + 1) * 128, :]
        Am = modl if ti < NTT else modc
        amk = "modl" if ti < NTT else "modc"
        P.dma("sp", xt[s], src, "ld_xt%d" % s, writes=["xt%d" % s])
        P.op("act", lambda e, s=s, ti=ti: e.activation(out=junk, in_=xt[s], func=AF.Square, accum_out=ss1[:, ti:ti + 1]),
             reads=["xt%d" % s, "ss1"], writes=["junk", "ss1_%d" % ti])
        rstd_from_ss(ss1[:, ti:ti + 1], D, rs1[:, ti:ti + 1], ln1[:, ti:ti + 1], ["ss1_%d" % ti], "rs1_%d" % ti)
        P.op("dve", lambda e, s=s, ti=ti, Am=Am: e.scalar_tensor_tensor(out=tmpf, in0=xt[s], scalar=rs1[:, ti:ti + 1], in1=Am[:, D:2 * D], op0=ALU.mult, op1=ALU.mult),
             reads=["xt%d" % s, "rs1_%d" % ti, amk], writes=["tmpf"])
        P.op("dve", lambda e, s=s, Am=Am: e.tensor_tensor(out=ub[s], in0=tmpf, in1=Am[:, 0:D], op=ALU.add), reads=["tmpf", amk], writes=["ub%d" % s])
        bk = 2 + s

        def tr(e, s=s, bk=bk):
            for kt in range(8):
                r = e.transpose(bank_bf(bk)[:, kt * 128:(kt + 1) * 128], ub[s][:, kt * 128:(kt + 1) * 128], ident_b)
            return r
        P.op("pe", tr, reads=["ub%d" % s, "ident_b"], writes=["bank%d" % bk])
        P.op("act", lambda e, ti=ti, bk=bk: e.copy(out=uT[:, :, ti * 128:(ti + 1) * 128], in_=bank_bf(bk).rearrange("p (a b) -> p a b", b=128)),
             reads=["bank%d" % bk], writes=["uT_%d" % ti])
    UT_KEYS = ["uT_%d" % ti for ti in range(18)]
    if debug:
        P.dma("sp", dm_ut, uT, "st_dbg", reads=UT_KEYS, writes=["dm_ut"])
    P.barrier()
    A.release(m0)
    if upto <= 1:
        return finish(P, nc)


    m2 = A.mark()
    wkv = A.alloc((8, 512), BF16)
    P.dma("pool", wkv, win_d[:, 6144:6656].rearrange("(kt p) n -> p kt n", p=128), "ld_wkv", writes=["wkv"])
    ropeT = A.alloc((NTT, 256), F32)
    P.dma("sp", ropeT, rope_d.rearrange("(t p) n -> p t n", p=128), "ld_rope", writes=["ropeT"])
    gqk = A.alloc((256,), F32)
    P.dma("sp", gqk, qkg_d[0].partition_broadcast(128), "ld_c", writes=["gqk"])
    KTl = A.alloc((2, NALL), BF16)
    Vl = A.alloc((18, 256), BF16)
    ssk = A.alloc((18, 2), F32)
    lnk = A.alloc((18, 2), F32)
    rsk = A.alloc((18, 2), F32)
    knb = [A.alloc((2, 128), F32) for _ in range(2)]
    t1b = A.alloc((2, 128), F32)
    t2b = A.alloc((2, 128), F32)
    kbf = [A.alloc((256,), BF16) for _ in range(2)]
    junk2 = A.alloc((128,), BF16)
    P.op("pool", lambda e: e.memset(ssk, 0.0), writes=["ssk"])

    def rope_apply(xn, nh, ti, outbf, pfx, eng2="dve"):
        cosv = ropeT[:, ti, 0:128]
        sinv = ropeT[:, ti, 128:256].rearrange("p (r x d) -> p r x d", r=2, x=2, d=32)
        t1 = t1b if nh == 2 else t1q
        t2 = t2b if nh == 2 else t2q
        P.op("dve", lambda e: e.tensor_tensor(out=t1, in0=xn, in1=bc(cosv, 1, [128, nh, 128]), op=ALU.mult),
             reads=[pfx + "xn", "ropeT"], writes=[pfx + "t1"])
        x6 = xn.rearrange("p h (r x d) -> p h r x d", r=2, x=2, d=32)
        t6 = t2.rearrange("p h (r x d) -> p h r x d", r=2, x=2, d=32)
        for xo in range(2):
            P.op(eng2, lambda e, xo=xo: e.tensor_tensor(out=t6[:, :, :, xo, :], in0=x6[:, :, :, 1 - xo, :],
                                                        in1=bc(sinv[:, :, xo, :], 1, [128, nh, 2, 32]), op=ALU.mult),
                 reads=[pfx + "xn", "ropeT"], writes=[pfx + "t2_%d" % xo])
        P.op("dve", lambda e: e.tensor_tensor(out=outbf.rearrange("p (h d) -> p h d", d=128), in0=t1, in1=t2, op=ALU.add),
             reads=[pfx + "t1", pfx + "t2_0", pfx + "t2_1"], writes=[pfx + "bf"])

    import os
    BIS = int(os.environ.get("BIS", "99"))
    for ti in range(18):
        s = ti % 2
        bk = s
        kn = knb[s]

        def mmkv(e, ti=ti, bk=bk):
            for kt in range(8):
                r = e.matmul(banks[bk][:, :], lhsT=uT[:, kt, ti * 128:(ti + 1) * 128], rhs=wkv[:, kt, :], start=(kt == 0), stop=(kt == 7))
            return r
        P.op("pe", mmkv, reads=["uT_%d" % ti, "wkv"], writes=["bank%d" % bk])
        kps = banks[bk][:, 0:256].rearrange("p (h d) -> p h d", d=128)
        P.op("act", lambda e, ti=ti, bk=bk: e.copy(out=Vl[:, ti, :], in_=banks[bk][:, 256:512]), reads=["bank%d" % bk], writes=["Vl_%d" % ti])
        if BIS < 2:
            continue
        for h in range(2):
            P.op("act", lambda e, h=h, ti=ti, kps=kps: e.activation(out=junk2, in_=kps[:, h, :], func=AF.Square, accum_out=ssk[:, ti, h:h + 1]),
                 reads=["bank%d" % bk, "ssk"], writes=["junk2", "ssk_%d_%d" % (ti, h)])
        rstd_from_ss(ssk[:, ti, :], 128, rsk[:, ti, :], lnk[:, ti, :], ["ssk_%d_0" % ti, "ssk_%d_1" % ti], "rsk_%d" % ti)
        P.op("dve", lambda e, kn=kn, kps=kps, ti=ti: e.tensor_tensor(out=kn, in0=kps, in1=bc(rsk[:, ti, :], 2, [128, 2, 128]), op=ALU.mult),
             reads=["bank%d" % bk, "rsk_%d" % ti], writes=["k%dxn" % s])
        P.op("dve", lambda e, kn=kn: e.tensor_tensor(out=kn, in0=kn, in1=bc(gqk[:, 128:256], 1, [128, 2, 128]), op=ALU.mult),
             reads=["k%dxn" % s, "gqk"], writes=["k%dxn" % s])
        if BIS < 3:
            continue
        if ti < NTT and BIS != 3:
            rope_apply(kn, 2, ti, kbf[s], "k%d" % s)
        else:
            P.op("dve", lambda e, kn=kn, s=s: e.tensor_copy(out=kbf[s].rearrange("p (h d) -> p h d", d=128), in_=kn), reads=["k%dxn" % s], writes=["k%dbf" % s])
        bk2 = 2 + s
        if BIS < 5:
            continue

        def trk(e, s=s, bk2=bk2):
            for h in range(2):
                r = e.transpose(bank_bf(bk2)[:, h * 128:(h + 1) * 128], kbf[s][:, h * 128:(h + 1) * 128], ident_b)
            return r
        P.op("pe", trk, reads=["k%dbf" % s, "ident_b"], writes=["bank%d" % bk2])
        P.op("act", lambda e, ti=ti, bk2=bk2: e.copy(out=KTl[:, :, ti * 128:(ti + 1) * 128], in_=bank_bf(bk2)[:, 0:256].rearrange("p (a b) -> p a b", b=128)),
             reads=["bank%d" % bk2], writes=["KTl_%d" % ti])
    for h in range(2 if BIS >= 6 else 0):
        P.dma("sp", ag1_ins[h], KTl[:, h, 0:NT], "st_ag1", reads=["KTl_%d" % t for t in range(16)], writes=["ag1_in"])
        P.dma("sp", ag1_ins[2 + h].rearrange("p (ti c) -> p ti c", c=256), Vl[:, 8 * h:8 * h + 8, :], "st_ag1", reads=["Vl_%d" % t for t in range(16)], writes=["ag1_in"])
    if BIS >= 6:
        P.dma("sp", dm_kvctx[0:256, :].rearrange("(h p) t -> p h t", p=128), KTl[:, :, NT:NALL], "st_kvc", reads=["KTl_16", "KTl_17"], writes=["dm_kvctx"])
        P.dma("sp", dm_kvctx[256:512, :].rearrange("(ti p) c -> p ti c", p=128), Vl[:, 16:18, :], "st_kvc", reads=["Vl_16", "Vl_17"], writes=["dm_kvctx"])
    for q in range(4 if BIS >= 7 else 0):
        P.custom("pool", lambda e, q=q: e.collective_compute("AllGather", ALU.bypass, replica_groups=[[0, 1, 2, 3], [4, 5, 6, 7]], ins=[ag1_ins[q]], outs=[ag1_outs[q]]),
                 "cc1", 1, reads=["ag1_in"] + (["ag1_out"] if q > 0 else []), writes=["ag1_out"])
    P.barrier(keep=["ag1_out", "dm_kvctx", "dm_mod"] + UT_KEYS)
    A.release(m2)
    if upto <= 2:
        return finish(P, nc)

    m3 = A.mark()
    rst = A.alloc((NALL,), F32)
    maskF = A.alloc((128,), F32)
    maskB = A.alloc((128,), F32)
    lbt = A.alloc((2, 2, 8), F32)
    lbd = A.alloc((2, 8), F32)
    lb = A.alloc((2, 8), F32)
    oml = A.alloc((2, 8), F32)
    hng = A.alloc((1,), F32)
    stage = A.alloc((2, 8, 128), F32)
    sctx = A.alloc((2, 8, 128), F32)
    Dv = A.alloc((2, 8), F32)
    m3h = A.mark()
    whb = [A.alloc((8, 5, 128), BF16)] * 2
    qT = A.alloc((NT,), F32)
    gsT = A.alloc((NT,), BF16)
    v_tm = A.alloc((18, 128), BF16)
    fa = A.alloc((NALL,), F32)
    lf = A.alloc((NALL,), F32)
    kk = A.alloc((NALL,), F32)
    bb = A.alloc((NALL,), F32)
    xx = A.alloc((NALL,), F32)
    ee = A.alloc((NALL,), F32)
    gtmp = ee[:, 0:NT]
    qe = [A.alloc((NT,), BF16) for _ in range(2)]
    ke = [A.alloc((NT,), BF16) for _ in range(2)]
    kd = [A.alloc((NALL,), BF16) for _ in range(2)]
    kd_tm = [A.alloc((18, 128), BF16) for _ in range(2)]
    qB = [A.alloc((NT,), BF16) for _ in range(2)]
    tot = [A.alloc((36,), F32) for _ in range(2)]
    etot = [A.alloc((36,), F32) for _ in range(2)]
    ipf = [A.alloc((32,), F32) for _ in range(2)]
    gg = [A.alloc((32,), F32) for _ in range(2)]
    eg = [A.alloc((32,), F32) for _ in range(2)]
    attm = [A.alloc((4, 128), BF16) for _ in range(2)]
    Sst = [A.alloc((128,), F32) for _ in range(2)]
    Sbf = [A.alloc((128,), BF16) for _ in range(2)]
    Scx = [A.alloc((128,), F32) for _ in range(2)]
    o_acc = A.alloc((NT,), F32)

    P.op("pool", lambda e: e.memset(rst, 1.0), writes=["rst"])
    P.op("pool", lambda e: e.memset(rst.rearrange("p (c t) -> p c t", t=64)[:, :, 0:1], 0.0), reads=["rst"], writes=["rst"])
    P.op("pool", lambda e: e.memset(maskF, 1.0), writes=["maskF"])
    P.op("pool", lambda e: e.affine_select(out=maskF, in_=maskF, pattern=[[1, 128]], compare_op=ALU.is_ge, fill=0.0, base=0, channel_multiplier=-1),
         reads=["maskF"], writes=["maskF"])
    P.op("pool", lambda e: e.memset(maskF[0:64, 64:128], 0.0), reads=["maskF"], writes=["maskF"])
    P.op("pool", lambda e: e.memset(maskB, 1.0), writes=["maskB"])
    P.op("pool", lambda e: e.affine_select(out=maskB, in_=maskB, pattern=[[-1, 128]], compare_op=ALU.is_ge, fill=0.0, base=0, channel_multiplier=1),
         reads=["maskB"], writes=["maskB"])
    P.op("pool", lambda e: e.memset(maskB[64:128, 0:64], 0.0), reads=["maskB"], writes=["maskB"])
    masks = [maskF, maskB]
    P.dma("sp", lbt, lbv_d.rearrange("p (a b c) -> p a b c", a=2, b=2), "ld_c", writes=["lbt"])
    P.dma("sp", hng, hng_d, "ld_c", writes=["hng"])
    P.op("dve", lambda e: e.tensor_tensor(out=lbd, in0=lbt[:, :, 0, :], in1=lbt[:, :, 1, :], op=ALU.subtract), reads=["lbt"], writes=["lbd"])
    P.op("act", lambda e: e.activation(out=lb, in_=lbd, func=AF.Sigmoid), reads=["lbd"], writes=["lb"])
    P.op("dve", lambda e: e.tensor_scalar(out=oml, in0=lb, scalar1=-1.0, scalar2=1.0, op0=ALU.mult, op1=ALU.add), reads=["lb"], writes=["oml"])

    win_v = win_d.rearrange("(kt p) (s n) -> p kt s n", p=128, n=128)
    BLK5 = [(0, 512), (512, 512), (1024, 512), (1536, 512), (2048, 256)]
    PB = 6
    pbc = [0]

    def proj_fm(wh, whk, sidx, c0, n, evac):
        bk = PB + (pbc[0] % 2)
        pbc[0] += 1

        def mm(e):
            for kt in range(8):
                r = e.matmul(banks[bk][:, 0:n], lhsT=wh[:, kt, sidx, :], rhs=uT[:, kt, c0:c0 + n], start=(kt == 0), stop=(kt == 7))
            return r
        P.op("pe", mm, reads=[whk] + ["uT_%d" % t for t in range(c0 // 128, (c0 + n) // 128)], writes=["bank%d" % bk])
        evac(banks[bk][:, 0:n], "bank%d" % bk)

    def hgrn_dir_prep(h, d):
        wh = whb[h % 2]
        whk = "wh0"
        dk = "d%d" % d
        for (c0, n) in BLK5:
            proj_fm(wh, whk, 2 + d, c0, n, lambda bap, bkey, c0=c0, n=n: P.op(
                "act", lambda e: e.activation(out=fa[:, c0:c0 + n], in_=bap, func=AF.Sigmoid), reads=[bkey], writes=["fa"]))
        fak = ["fa"]
        P.op("dve", lambda e: e.tensor_scalar(out=fa, in0=fa, scalar1=oml[:, d, h:h + 1], scalar2=lb[:, d, h:h + 1], op0=ALU.mult, op1=ALU.add),
             reads=fak + ["oml", "lb"], writes=["fa"])
        P.op("act", lambda e: e.activation(out=lf, in_=fa, func=AF.Ln), reads=["fa"], writes=["lf"])
        P.op("dve", lambda e: e.tensor_scalar(out=kk, in0=fa, scalar1=-1.0, scalar2=1.0, op0=ALU.mult, op1=ALU.add), reads=["fa"], writes=["kk"])
        P.op("dve", lambda e: e.tensor_tensor_scan(out=bb, data0=rst, data1=lf, initial=0.0, op0=ALU.mult, op1=ALU.add), reads=["rst", "lf"], writes=["bb"])
        b3 = bb.rearrange("p (c t) -> p c t", t=64)
        P.op("dve", lambda e: e.tensor_copy(out=tot[d], in_=b3[:, :, 63]), reads=["bb"], writes=["tot" + dk])
        P.op("act", lambda e: e.activation(out=etot[d], in_=tot[d], func=AF.Exp), reads=["tot" + dk], writes=["etot" + dk])
        x3 = xx.rearrange("p (c t) -> p c t", t=64)
        P.op("dve", lambda e: e.tensor_tensor(out=x3, in0=bc(tot[d], 2, [128, 36, 64]), in1=b3, op=ALU.subtract), reads=["tot" + dk, "bb"], writes=["xx"])
        if d == 0:
            bu, dd = bb, xx
            bk_, ddk = "bb", "xx"
        else:
            P.op("dve", lambda e: e.tensor_tensor(out=xx, in0=xx, in1=lf, op=ALU.add), reads=["xx", "lf"], writes=["xx"])
            P.op("dve", lambda e: e.tensor_tensor(out=bb, in0=bb, in1=lf, op=ALU.subtract), reads=["bb", "lf"], writes=["bb"])
            bu, dd = xx, bb
            bk_, ddk = "xx", "bb"
        P.op("act", lambda e: e.activation(out=ee[:, 0:NT], in_=bu[:, 0:NT], func=AF.Exp), reads=[bk_], writes=["ee"])
        P.op("dve", lambda e: e.tensor_tensor(out=qe[d], in0=qT, in1=ee[:, 0:NT], op=ALU.mult), reads=["ee", "qT"], writes=["qe" + dk])
        P.op("act", lambda e: e.activation(out=ee[:, 0:NT], in_=bu[:, 0:NT], func=AF.Exp, scale=-1.0), reads=[bk_, "ee"], writes=["ee"])
        P.op("dve", lambda e: e.tensor_tensor(out=ke[d], in0=kk[:, 0:NT], in1=ee[:, 0:NT], op=ALU.mult), reads=["ee", "kk"], writes=["ke" + dk])
        P.op("act", lambda e: e.activation(out=ee, in_=dd, func=AF.Exp), reads=[ddk, "ee"], writes=["ee"])
        P.op("dve", lambda e: e.tensor_tensor(out=kd[d], in0=kk, in1=ee, op=ALU.mult), reads=["ee", "kk"], writes=["kd" + dk])
        P.op("dve", lambda e: e.tensor_tensor_scan(out=ipf[d], data0=ones_f[:, 0:32], data1=tot[d][:, 0:32], initial=0.0, op0=ALU.mult, op1=ALU.add),
             reads=["tot" + dk, "ones_f"], writes=["ipf" + dk])
        if d == 0:
            P.op("dve", lambda e: e.tensor_tensor(out=gg[d], in0=ipf[d], in1=tot[d][:, 0:32], op=ALU.subtract), reads=["ipf" + dk, "tot" + dk], writes=["gg" + dk])
        else:
            P.op("dve", lambda e: e.tensor_tensor(out=gg[d], in0=ipf[d][:, 31:32].to_broadcast([128, 32]), in1=ipf[d], op=ALU.subtract),
                 reads=["ipf" + dk], writes=["gg" + dk])
        P.op("act", lambda e: e.activation(out=eg[d], in_=gg[d], func=AF.Exp), reads=["gg" + dk], writes=["eg" + dk])
        P.op("act", lambda e: e.activation(out=Dv[:, d, h:h + 1], in_=ipf[d][:, 31:32], func=AF.Exp), reads=["ipf" + dk], writes=["Dv_%d_%d" % (d, h)])
        P.op("dve", lambda e: e.tensor_tensor(out=qB[d].rearrange("p (c t) -> p c t", t=64), in0=qe[d].rearrange("p (c t) -> p c t", t=64),
                                               in1=bc(eg[d], 2, [128, 32, 64]), op=ALU.mult), reads=["qe" + dk, "eg" + dk], writes=["qB" + dk])
        P.dma("sp", dm_qb[d, h], qB[d], "st_qb", reads=["qB" + dk], writes=["dm_qb"])
        for g3 in range(3):
            bk = PB + (pbc[0] % 2)
            pbc[0] += 1

            def trd(e, g3=g3, bk=bk):
                for i in range(6):
                    ti = g3 * 6 + i
                    r = e.transpose(bank_bf(bk)[:, i * 128:(i + 1) * 128], kd[d][:, ti * 128:(ti + 1) * 128], ident_b)
                return r
            P.op("pe", trd, reads=["kd" + dk, "ident_b"], writes=["bank%d" % bk])
            P.op("act", lambda e, g3=g3, bk=bk: e.copy(out=kd_tm[d][:, g3 * 6:(g3 + 1) * 6, :], in_=bank_bf(bk)[:, 0:768].rearrange("p (a b) -> p a b", b=128)),
                 reads=["bank%d" % bk], writes=["kdtm%s_%d" % (dk, g3)])

    def hgrn_dir_scan(h, d):
        dk = "d%d" % d
        kdk = ["kdtm%s_%d" % (dk, g3) for g3 in range(3)]
        bA, bO, bS = 3 * d, 3 * d + 1, 3 * d + 2
        S, Sb, Sc = Sst[d], Sbf[d], Scx[d]

        def state_step(c, St, stk, first):
            ti, half = c // 2, c % 2
            ps = slice(half * 64, half * 64 + 64)
            P.op("pe", lambda e: e.matmul(banks[bS][:, 0:128], lhsT=kd_tm[d][ps, ti, :], rhs=v_tm[ps, ti, :], start=True, stop=True),
                 reads=kdk + ["v_tm"], writes=["bank%d" % bS])
            if first:
                P.op("dve", lambda e: e.tensor_copy(out=St, in_=banks[bS][:, 0:128]), reads=["bank%d" % bS], writes=[stk])
            else:
                P.op("dve", lambda e: e.scalar_tensor_tensor(out=St, in0=St, scalar=etot[d][:, c:c + 1], in1=banks[bS][:, 0:128], op0=ALU.mult, op1=ALU.add),
                     reads=["bank%d" % bS, stk, "etot" + dk], writes=[stk])
        corder = [32, 33, 34, 35] if d == 0 else [35, 34, 33, 32]
        for i, c in enumerate(corder):
            state_step(c, Sc, "Sc" + dk, i == 0)
            yield
        P.op("dve", lambda e: e.tensor_copy(out=sctx[:, d, h, :], in_=Sc), reads=["Sc" + dk], writes=["sctx_%d_%d" % (d, h)])
        P.op("pool", lambda e: e.memset(Sb, 0.0), reads=["Sb" + dk], writes=["Sb" + dk])
        groups = [0, 1, 2, 3] if d == 0 else [3, 2, 1, 0]
        first_state = True
        for g in groups:
            def att(e, g=g):
                for pi in range(4):
                    p = g * 4 + pi
                    r = e.matmul(banks[bA][:, pi * 128:(pi + 1) * 128], lhsT=ke[d][:, p * 128:(p + 1) * 128], rhs=qe[d][:, p * 128:(p + 1) * 128], start=True, stop=True)
                return r
            P.op("pe", att, reads=["ke" + dk, "qe" + dk], writes=["bank%d" % bA])
            P.op("dve", lambda e: e.tensor_tensor(out=attm[d], in0=banks[bA][:, :].rearrange("p (a b) -> p a b", b=128), in1=bc(masks[d], 1, [128, 4, 128]), op=ALU.mult),
                 reads=["bank%d" % bA, "mask"], writes=["attm" + dk])
            pis = [0, 1, 2, 3] if d == 0 else [3, 2, 1, 0]
            for pi in pis:
                p = g * 4 + pi
                P.op("pe", lambda e, pi=pi, p=p: e.matmul(banks[bO][:, pi * 128:(pi + 1) * 128], lhsT=v_tm[:, p, :], rhs=attm[d][:, pi, :], start=True, stop=False),
                     reads=["v_tm", "attm" + dk], writes=["bank%d" % bO])
                chunks = [2 * p, 2 * p + 1] if d == 0 else [2 * p + 1, 2 * p]
                for ci, c in enumerate(chunks):
                    col = pi * 128 + (c % 2) * 64
                    P.op("pe", lambda e, c=c, col=col, ci=ci: e.matmul(banks[bO][:, col:col + 64], lhsT=Sb, rhs=qe[d][:, c * 64:(c + 1) * 64], start=False, stop=(ci == 1)),
                         reads=["Sb" + dk, "qe" + dk], writes=["bank%d" % bO])
                    state_step(c, S, "S" + dk, first_state)
                    first_state = False
                    P.op("act", lambda e: e.copy(out=Sb, in_=S), reads=["S" + dk], writes=["Sb" + dk])
                    yield
            cs = slice(g * 512, (g + 1) * 512)
            if (d == 0 and g < 2) or (d == 1 and g >= 2):
                P.op("act", lambda e, cs=cs: e.copy(out=o_acc[:, cs], in_=banks[bO][:, :]), reads=["bank%d" % bO, "oacc_%d" % g], writes=["oacc_%d" % g])
            else:
                P.op("dve", lambda e, cs=cs: e.tensor_tensor(out=o_acc[:, cs], in0=o_acc[:, cs], in1=banks[bO][:, :], op=ALU.add),
                     reads=["bank%d" % bO, "oacc_%d" % g], writes=["oacc_%d" % g])
        P.op("dve", lambda e: e.tensor_copy(out=stage[:, d, h, :], in_=S), reads=["S" + dk], writes=["stage_%d_%d" % (d, h)])

    P.buf["mask"] = {"w": ("e", "pool", P.cnt["pool"]), "r": {}}
    for h in range(8):
        wh = whb[h % 2]
        whk = "wh0"
        for s5 in range(5):
            P.dma("pool", wh[:, :, s5, :], win_v[:, :, h + 8 * s5, :], "ld_" + whk, writes=[whk])
        for blk in range(4):
            proj_fm(wh, whk, 0, blk * 512, 512, lambda bap, bkey, blk=blk: P.op(
                "act", lambda e: e.copy(out=qT[:, blk * 512:(blk + 1) * 512], in_=bap), reads=[bkey, "qT"], writes=["qT"]))
        for blk in range(4):
            proj_fm(wh, whk, 4, blk * 512, 512, lambda bap, bkey, blk=blk: P.op(
                "act", lambda e: e.activation(out=gtmp[:, blk * 512:(blk + 1) * 512], in_=bap, func=AF.Silu), reads=[bkey, "ee"], writes=["ee"]))
        P.op("dve", lambda e: e.tensor_scalar(out=gsT, in0=gtmp, scalar1=hng[:, 0:1], scalar2=None, op0=ALU.mult), reads=["ee", "hng"], writes=["gsT"])
        P.dma("sp", dm_gs[h], gsT, "st_gs", reads=["gsT"], writes=["dm_gs"])
        for g4 in range(5):
            tiles = list(range(g4 * 4, min(18, g4 * 4 + 4)))
            bk = PB + (pbc[0] % 2)
            pbc[0] += 1

            def mmv(e, tiles=tiles, bk=bk, wh=wh):
                for i, ti in enumerate(tiles):
                    for kt in range(8):
                        r = e.matmul(banks[bk][:, i * 128:(i + 1) * 128], lhsT=uT[:, kt, ti * 128:(ti + 1) * 128], rhs=wh[:, kt, 1, :], start=(kt == 0), stop=(kt == 7))
                return r
            P.op("pe", mmv, reads=[whk] + ["uT_%d" % t for t in tiles], writes=["bank%d" % bk])
            nt_ = len(tiles)
            P.op("act", lambda e, tiles=tiles, bk=bk, nt_=nt_: e.copy(out=v_tm[:, tiles[0]:tiles[0] + nt_, :], in_=banks[bk][:, 0:nt_ * 128].rearrange("p (a b) -> p a b", b=128)),
                 reads=["bank%d" % bk, "v_tm"], writes=["v_tm"])
        hgrn_dir_prep(h, 0)
        hgrn_dir_prep(h, 1)
        gens = [hgrn_dir_scan(h, 0), hgrn_dir_scan(h, 1)]
        alive = [True, True]
        while any(alive):
            for i in range(2):
                if alive[i]:
                    try:
                        next(gens[i])
                    except StopIteration:
                        alive[i] = False
        P.dma("sp", dm_oloc[h], o_acc, "st_oloc", reads=["oacc_%d" % g for g in range(4)], writes=["dm_oloc"])
    for d in range(2):
        P.dma("sp", ag2_ins[d][:, 0:1024], stage[:, d, :, :].rearrange("p b c -> p (b c)"), "st_ag2", reads=["stage_%d_%d" % (d, h) for h in range(8)], writes=["ag2_in"])
        P.dma("sp", ag2_ins[d][:, 1024:1032], Dv[:, d, :], "st_ag2", reads=["Dv_%d_%d" % (d, h) for h in range(8)], writes=["ag2_in"])
    for d in range(2):
        P.custom("pool", lambda e, d=d: e.collective_compute("AllGather", ALU.bypass, replica_groups=[[0, 1, 2, 3], [4, 5, 6, 7]], ins=[ag2_ins[d]], outs=[ag2_outs[d]]),
                 "cc2", 1, reads=["ag2_in"] + (["ag2_out"] if d > 0 else []), writes=["ag2_out"])
    P.barrier(keep=["ag1_out", "dm_kvctx", "dm_mod", "ag2_out", "dm_oloc", "dm_qb", "dm_gs"] + UT_KEYS)
    A.release(m3h)
    gath = A.alloc((2, 4, 1032), F32)
    Rr = A.alloc((2, 8, 128), F32)
    Tt = A.alloc((8, 128), F32)
    selv = A.alloc((8,), F32)
    Sin = A.alloc((2, 8, 128), BF16)
    for d in range(2):
        P.dma("sp", gath[:, d, :, :], ag2_outs[d].rearrange("(r p) n -> p r n", p=128), "ld_gath", reads=["ag2_out"], writes=["gath"])
    P.dma("sp", selv, sel_d, "ld_c", writes=["selv"])
    SCK = ["sctx_%d_%d" % (d, h) for d in range(2) for h in range(8)]
    P.op("dve", lambda e: e.tensor_copy(out=Rr.rearrange("p a b c -> p (a b c)"), in_=sctx.rearrange("p a b c -> p (a b c)")), writes=["Rr"])
    for d in range(2):
        order = [0, 1, 2, 3] if d == 0 else [3, 2, 1, 0]
        for r in order:
            Sl = gath[:, d, r, 0:1024].rearrange("p (h v) -> p h v", v=128)
            Dr = gath[:, d, r, 1024:1032]
            P.op("dve", lambda e, d=d, Dr=Dr: e.tensor_tensor(out=Tt, in0=Rr[:, d, :, :], in1=bc(Dr, 2, [128, 8, 128]), op=ALU.mult), reads=["Rr", "gath"], writes=["Tt"])
            P.op("dve", lambda e, Sl=Sl: e.tensor_tensor(out=Tt, in0=Tt, in1=Sl, op=ALU.add), reads=["Tt", "gath"], writes=["Tt"])
            P.op("dve", lambda e, d=d: e.tensor_tensor(out=Tt, in0=Tt, in1=Rr[:, d, :, :], op=ALU.subtract), reads=["Tt", "Rr"], writes=["Tt"])
            P.op("dve", lambda e, d=d, r=r: e.scalar_tensor_tensor(out=Rr[:, d, :, :], in0=Tt, scalar=selv[:, d * 4 + r:d * 4 + r + 1], in1=Rr[:, d, :, :], op0=ALU.mult, op1=ALU.add),
                 reads=["Tt", "Rr", "selv"], writes=["Rr"])
    P.op("act", lambda e: e.copy(out=Sin.rearrange("p a b c -> p (a b c)"), in_=Rr.rearrange("p a b c -> p (a b c)")), reads=["Rr"], writes=["Sin"])
    ol = A.alloc((NT,), F32)
    qbf_ = A.alloc((NT,), BF16)
    qbb_ = A.alloc((NT,), BF16)
    gsl = A.alloc((NT,), BF16)
    sqb = A.alloc((NT,), BF16)
    lnr = A.alloc((NT,), F32)
    rsr = A.alloc((NT,), F32)
    for h in range(8):
        P.dma("sp", ol, dm_oloc[h], "ld_ol", reads=["dm_oloc"], writes=["ol"] + ["ol_%d" % b_ for b_ in range(4)])
        P.dma("sp", qbf_, dm_qb[0, h], "ld_qb0", reads=["dm_qb"], writes=["qbf_"])
        P.dma("sp", qbb_, dm_qb[1, h], "ld_qb1", reads=["dm_qb"], writes=["qbb_"])
        P.dma("sp", gsl, dm_gs[h], "ld_gs", reads=["dm_gs"], writes=["gsl"])
        for blk in range(4):
            cs = slice(blk * 512, (blk + 1) * 512)
            bk = blk % 2

            def corr(e, h=h, cs=cs, bk=bk):
                e.matmul(banks[bk][:, :], lhsT=Sin[:, 0, h, :], rhs=qbf_[:, cs], start=True, stop=False)
                return e.matmul(banks[bk][:, :], lhsT=Sin[:, 1, h, :], rhs=qbb_[:, cs], start=False, stop=True)
            P.op("pe", corr, reads=["Sin", "qbf_", "qbb_"], writes=["bank%d" % bk])
            P.op("dve", lambda e, cs=cs, bk=bk: e.tensor_tensor(out=ol[:, cs], in0=ol[:, cs], in1=banks[bk][:, :], op=ALU.add), reads=["bank%d" % bk, "ol", "ol_%d" % blk], writes=["ol_%d" % blk])
            P.op("act", lambda e, cs=cs: e.activation(out=sqb[:, cs], in_=ol[:, cs], func=AF.Square), reads=["ol_%d" % blk], writes=["sqb_%d" % blk])
            bk2 = 2 + blk % 2
            P.op("pe", lambda e, cs=cs, bk2=bk2: e.matmul(banks[bk2][:, :], lhsT=ones_b, rhs=sqb[:, cs], start=True, stop=True), reads=["sqb_%d" % blk, "ones_b"], writes=["bank%d" % bk2])
            P.op("act", lambda e, cs=cs, bk2=bk2: e.activation(out=lnr[:, cs], in_=banks[bk2][:, :], func=AF.Ln, scale=1.0 / 128, bias=EPS), reads=["bank%d" % bk2], writes=["lnr_%d" % blk])
            P.op("act", lambda e, cs=cs: e.activation(out=rsr[:, cs], in_=lnr[:, cs], func=AF.Exp, scale=-0.5), reads=["lnr_%d" % blk], writes=["rsr_%d" % blk])
            P.op("dve", lambda e, cs=cs: e.tensor_tensor(out=ol[:, cs], in0=ol[:, cs], in1=rsr[:, cs], op=ALU.mult), reads=["ol_%d" % blk, "rsr_%d" % blk], writes=["ol_%d" % blk])
            P.op("dve", lambda e, cs=cs: e.tensor_tensor(out=sqb[:, cs], in0=ol[:, cs], in1=gsl[:, cs], op=ALU.mult), reads=["ol_%d" % blk, "gsl"], writes=["sqb_%d" % blk])
        P.dma("sp", dm_oth[h], sqb, "st_oth", reads=["sqb_%d" % b_ for b_ in range(4)], writes=["dm_oth"])
        for b_ in range(4):
            for nm in ("ol_%d", "sqb_%d", "lnr_%d", "rsr_%d", "oth_%d"):
                pass
    P.barrier(keep=["ag1_out", "dm_kvctx", "dm_mod", "dm_oth"] + UT_KEYS)
    A.release(m3)
    if upto <= 3:
        return finish(P, nc)


    m4 = A.mark()
    QT = A.alloc((8, NT), BF16)
    KTa = A.alloc((2, 8448), BF16)
    Va = A.alloc((66, 256), BF16)
    for r in range(4):
        for h in range(2):
            P.dma("sp", KTa[:, h, r * NT:(r + 1) * NT], ag1_outs[h][r * 128:(r + 1) * 128, :], "ld_kta", reads=["ag1_out"], writes=["KTa"])
            P.dma("sp", Va[:, r * 16 + 8 * h:r * 16 + 8 * h + 8, :], ag1_outs[2 + h][r * 128:(r + 1) * 128, :].rearrange("p (ti c) -> p ti c", c=256), "ld_va", reads=["ag1_out"], writes=["Va"])
    P.dma("sp", KTa[:, :, 8192:8448], dm_kvctx[0:256, :].rearrange("(h p) t -> p h t", p=128), "ld_kta", reads=["dm_kvctx"], writes=["KTa"])
    P.dma("sp", Va[:, 64:66, :], dm_kvctx[256:512, :].rearrange("(ti p) c -> p ti c", p=128), "ld_va", reads=["dm_kvctx"], writes=["Va"])
    m4b = A.mark()
    wq = A.alloc((8, 1024), BF16)
    P.dma("pool", wq, win_d[:, 5120:6144].rearrange("(kt p) n -> p kt n", p=128), "ld_wq", writes=["wq"])
    ropeT = A.alloc((NTT, 256), F32)
    P.dma("sp", ropeT, rope_d.rearrange("(t p) n -> p t n", p=128), "ld_rope", writes=["ropeT"])
    gqk = A.alloc((256,), F32)
    P.dma("sp", gqk, qkg_d[0].partition_broadcast(128), "ld_c", writes=["gqk"])
    ssq = A.alloc((16, 8), F32)
    lnq = A.alloc((16, 8), F32)
    rsq = A.alloc((16, 8), F32)
    qn = A.alloc((8, 128), F32)
    t1q = A.alloc((8, 128), F32)
    t2q = A.alloc((8, 128), F32)
    qbf = A.alloc((1024,), BF16)
    junk2 = A.alloc((128,), BF16)
    P.op("pool", lambda e: e.memset(ssq, 0.0), writes=["ssq"])
    for ti in range(NTT):
        s = ti % 2
        for half in range(2):
            def mmq(e, ti=ti, half=half):
                for kt in range(8):
                    r = e.matmul(banks[half][:, :], lhsT=uT[:, kt, ti * 128:(ti + 1) * 128], rhs=wq[:, kt, half * 512:(half + 1) * 512], start=(kt == 0), stop=(kt == 7))
                return r
            P.op("pe", mmq, reads=["uT_%d" % ti, "wq"], writes=["bank%d" % half])
        for h in range(8):
            P.op("act", lambda e, h=h, ti=ti: e.activation(out=junk2, in_=banks[h // 4][:, (h % 4) * 128:(h % 4 + 1) * 128], func=AF.Square, accum_out=ssq[:, ti, h:h + 1]),
                 reads=["bank%d" % (h // 4), "ssq"], writes=["junk2", "ssq_%d" % ti])
        rstd_from_ss(ssq[:, ti, :], 128, rsq[:, ti, :], lnq[:, ti, :], ["ssq_%d" % ti], "rsq_%d" % ti)
        for half in range(2):
            P.op("dve", lambda e, half=half, ti=ti: e.tensor_tensor(out=qn[:, half * 4:(half + 1) * 4, :], in0=banks[half][:, :].rearrange("p (h d) -> p h d", d=128),
                                                                in1=bc(rsq[:, ti, half * 4:(half + 1) * 4], 2, [128, 4, 128]), op=ALU.mult),
                 reads=["bank%d" % half, "rsq_%d" % ti, "qxn"], writes=["qxn"])
        P.op("dve", lambda e: e.tensor_tensor(out=qn, in0=qn, in1=bc(gqk[:, 0:128], 1, [128, 8, 128]), op=ALU.mult), reads=["qxn", "gqk"], writes=["qxn"])
        rope_apply(qn, 8, ti, qbf, "q")
        bk2 = 2 + s

        def trq(e, bk2=bk2):
            for h in range(8):
                r = e.transpose(bank_bf(bk2)[:, h * 128:(h + 1) * 128], qbf[:, h * 128:(h + 1) * 128], ident_b)
            return r
        P.op("pe", trq, reads=["qbf", "ident_b"], writes=["bank%d" % bk2])
        P.op("act", lambda e, ti=ti, bk2=bk2: e.copy(out=QT[:, :, ti * 128:(ti + 1) * 128], in_=bank_bf(bk2).rearrange("p (a b) -> p a b", b=128)),
             reads=["bank%d" % bk2], writes=["QT"])
    P.barrier(keep=["dm_mod", "dm_oth", "KTa", "Va"] + UT_KEYS)
    A.release(m4b)
    if upto <= 4:
        return finish(P, nc)

    pT = [A.alloc((512,), BF16) for _ in range(3)]
    rden = A.alloc((512,), F32)
    ob = [A.alloc((512,), BF16) for _ in range(2)]
    dacc = [A.alloc((512,), F32) for _ in range(2)]
    SCALE = float(128 ** -0.5)
    NKT = 66
    it = 0
    for kvh in range(2):
        for qb in range(4):
            for g in range(4):
                head = kvh * 4 + g
                bo, bd = 4 + it % 2, 6 + it % 2
                qs = slice(qb * 512, (qb + 1) * 512)

                def s_mm(kt, kvh=kvh, head=head, qs=qs):
                    P.op("pe", lambda e: e.matmul(banks[kt % 3][:, :], lhsT=KTa[:, kvh, kt * 128:(kt + 1) * 128], rhs=QT[:, head, qs], start=True, stop=True),
                         reads=["KTa", "QT"], writes=["bank%d" % (kt % 3)])
                s_mm(0)
                for kt in range(NKT):
                    if kt + 1 < NKT:
                        s_mm(kt + 1)
                    P.op("act", lambda e, kt=kt: e.activation(out=pT[kt % 3], in_=banks[kt % 3][:, :], func=AF.Exp, scale=SCALE),
                         reads=["bank%d" % (kt % 3)], writes=["pT%d" % (kt % 3)])

                    P.op("pe", lambda e, kt=kt, kvh=kvh, bo=bo: e.matmul(banks[bo][:, :], lhsT=Va[:, kt, kvh * 128:(kvh + 1) * 128], rhs=pT[kt % 3], start=(kt == 0), stop=(kt == NKT - 1)),
                         reads=["pT%d" % (kt % 3), "Va"], writes=["bank%d" % bo])
                    da = dacc[0]
                    if kt == 0:
                        P.op("dve", lambda e, kt=kt, da=da: e.tensor_copy(out=da, in_=pT[kt % 3]), reads=["pT%d" % (kt % 3)], writes=["dacc0"])
                    else:
                        P.op("dve", lambda e, kt=kt, da=da: e.tensor_tensor(out=da, in0=da, in1=pT[kt % 3], op=ALU.add), reads=["pT%d" % (kt % 3), "dacc0"], writes=["dacc0"])

                def dsum(e, bd=bd):
                    return e.matmul(banks[bd][:, :], lhsT=ones_f, rhs=dacc[0], start=True, stop=True)
                P.op("pe", dsum, reads=["dacc0", "ones_f"], writes=["bank%d" % bd])
                P.op("dve", lambda e, bd=bd: e.reciprocal(out=rden, in_=banks[bd][:, :]), reads=["bank%d" % bd], writes=["rden"])
                P.op("dve", lambda e, bo=bo, it=it: e.tensor_tensor(out=ob[it % 2], in0=banks[bo][:, :], in1=rden, op=ALU.mult), reads=["bank%d" % bo, "rden"], writes=["ob%d" % (it % 2)])
                P.dma("sp", dm_ota[head][:, qs], ob[it % 2], "st_ota%d" % (it % 2), reads=["ob%d" % (it % 2)], writes=["dm_ota"])
                it += 1
    P.barrier(keep=["dm_mod", "dm_oth", "dm_ota"] + UT_KEYS)
    A.release(m4)
    if upto <= 5:
        return finish(P, nc)

    m6 = A.mark()
    wg = A.alloc((8, 2048), BF16)
    wb0 = A.alloc((8, 1024), BF16)
    wb1 = A.alloc((8, 1024), BF16)
    wo = A.alloc((8, 1024), BF16)
    P.dma("pool", wg, win_d[:, 6656:8704].rearrange("(kt p) n -> p kt n", p=128), "ld_w6", writes=["wg"])
    P.dma("pool", wb0, wbr_d[0].rearrange("(kt p) n -> p kt n", p=128), "ld_w6", writes=["wb0"])
    P.dma("pool", wb1, wbr_d[1].rearrange("(kt p) n -> p kt n", p=128), "ld_w6", writes=["wb1"])
    P.dma("pool", wo, wout_d.rearrange("(kt p) n -> p kt n", p=128), "ld_w6", writes=["wo"])
    G1 = A.alloc((D,), F32)
    A2 = A.alloc((D,), F32)
    B2 = A.alloc((D,), F32)
    P.dma("sp", G1, dm_mod[:, 2 * D:3 * D], "ld_c", reads=["dm_mod"], writes=["G1"])
    P.dma("sp", A2, dm_mod[:, 4 * D:5 * D], "ld_c", reads=["dm_mod"], writes=["A2"])
    P.dma("sp", B2, dm_mod[:, 3 * D:4 * D], "ld_c", reads=["dm_mod"], writes=["B2"])
    rwt = A.alloc((8, NE), F32)
    rbt = A.alloc((NE,), F32)
    P.dma("sp", rwt, rw_d.rearrange("(kt p) e -> p kt e", p=128), "ld_c", writes=["rwt"])
    P.dma("sp", rbt, rb_d[0].partition_broadcast(128), "ld_c", writes=["rbt"])
    othb = A.alloc((8, 512), BF16)
    otab = A.alloc((8, 512), BF16)
    y1T = A.alloc((8, 512), BF16)
    sgh = [A.alloc((512,), F32)] * 2
    sga = [A.alloc((512,), F32)] * 2
    tA = [A.alloc((512,), F32)] * 2
    tB = [A.alloc((512,), F32)] * 2
    xt6 = [A.alloc((D,), F32) for _ in range(2)]
    tmp6 = A.alloc((D,), F32)
    x1t = [A.alloc((D,), F32)] * 2
    u2f = A.alloc((D,), F32)
    u2b = A.alloc((D,), BF16)
    junk6 = A.alloc((D,), BF16)
    ssy = A.alloc((16, 2), F32)
    ssy1 = A.alloc((16,), F32)
    lny = A.alloc((16,), F32)
    rsy = A.alloc((16,), F32)
    ssx = A.alloc((16,), F32)
    lnx = A.alloc((16,), F32)
    rsx = A.alloc((16,), F32)
    u2Tf = A.alloc((8, 128), F32)
    u2Tb = [A.alloc((8, 128), BF16) for _ in range(2)]
    lg = A.alloc((NE,), F32)
    mx8 = A.alloc((8,), F32)
    msk = A.alloc((NE,), F32)
    em = A.alloc((NE,), F32)
    nmx = A.alloc((1,), F32)
    ssum = A.alloc((1,), F32)
    rsum = A.alloc((1,), F32)
    cmb = A.alloc((16, NE), F32)
    cT = A.alloc((128,), F32)
    P.op("pool", lambda e: e.memset(ssy, 0.0), writes=["ssy"])
    P.op("pool", lambda e: e.memset(ssx, 0.0), writes=["ssx"])
    oth_v = dm_oth.rearrange("h p t -> p h t")
    ota_v = dm_ota.rearrange("h p t -> p h t")
    for blk in range(4):
        cs = slice(blk * 512, (blk + 1) * 512)
        P.dma("sp", othb, oth_v[:, :, cs], "ld_oth", reads=["dm_oth"], writes=["othb"])
        P.dma("sp", otab, ota_v[:, :, cs], "ld_ota", reads=["dm_ota"], writes=["otab"])
        utk = ["uT_%d" % t for t in range(blk * 4, blk * 4 + 4)]
        for dt in range(8):
            s = 0
            ds = slice(dt * 128, (dt + 1) * 128)

            def mm4(e, ds=ds, dt=dt, cs=cs):
                for kt in range(8):
                    e.matmul(banks[0][:, :], lhsT=wg[:, kt, dt * 128:(dt + 1) * 128], rhs=uT[:, kt, cs], start=(kt == 0), stop=(kt == 7))
                for kt in range(8):
                    e.matmul(banks[1][:, :], lhsT=wg[:, kt, 1024 + dt * 128:1024 + (dt + 1) * 128], rhs=uT[:, kt, cs], start=(kt == 0), stop=(kt == 7))
                for kt in range(8):
                    e.matmul(banks[2][:, :], lhsT=wb0[:, kt, ds], rhs=othb[:, kt, :], start=(kt == 0), stop=(kt == 7))
                for kt in range(8):
                    r = e.matmul(banks[3][:, :], lhsT=wb1[:, kt, ds], rhs=otab[:, kt, :], start=(kt == 0), stop=(kt == 7))
                return r
            P.op("pe", mm4, reads=["wg", "wb0", "wb1", "othb", "otab"] + utk, writes=["bank0", "bank1", "bank2", "bank3"])
            P.op("act", lambda e, s=s: e.activation(out=sgh[s], in_=banks[0][:, :], func=AF.Sigmoid), reads=["bank0"], writes=["sgh%d" % s])
            P.op("act", lambda e, s=s: e.activation(out=sga[s], in_=banks[1][:, :], func=AF.Sigmoid), reads=["bank1"], writes=["sga%d" % s])
            P.op("dve", lambda e, s=s: e.tensor_tensor(out=tA[s], in0=sgh[s], in1=banks[2][:, :], op=ALU.mult), reads=["sgh%d" % s, "bank2"], writes=["tA%d" % s])
            P.op("dve", lambda e, s=s: e.tensor_tensor(out=tB[s], in0=sga[s], in1=banks[3][:, :], op=ALU.mult), reads=["sga%d" % s, "bank3"], writes=["tB%d" % s])
            P.op("dve", lambda e, s=s, dt=dt: e.tensor_tensor(out=y1T[:, dt, :], in0=tA[s], in1=tB[s], op=ALU.add), reads=["tA%d" % s, "tB%d" % s], writes=["y1T"])
        for tt in range(4):
            ti = blk * 4 + tt
            s = ti % 2
            ts_ = slice(tt * 128, (tt + 1) * 128)
            P.dma("sp", xt6[s], x_d[ti * 128:(ti + 1) * 128, :], "ld_x6%d" % s, writes=["xt6%d" % s])
            for half in range(2):
                def mmy(e, half=half, ts_=ts_):
                    for kt in range(8):
                        r = e.matmul(banks[4 + half][:, :], lhsT=y1T[:, kt, ts_], rhs=wo[:, kt, half * 512:(half + 1) * 512], start=(kt == 0), stop=(kt == 7))
                    return r
                P.op("pe", mmy, reads=["y1T", "wo"], writes=["bank%d" % (4 + half)])
                P.op("act", lambda e, half=half, ti=ti: e.activation(out=junk6[:, 0:512], in_=banks[4 + half][:, :], func=AF.Square, accum_out=ssy[:, ti, half:half + 1]),
                     reads=["bank%d" % (4 + half), "ssy"], writes=["junk6", "ssy_%d_%d" % (ti, half)])
            P.op("dve", lambda e, ti=ti: e.tensor_tensor(out=ssy1[:, ti:ti + 1], in0=ssy[:, ti, 0:1], in1=ssy[:, ti, 1:2], op=ALU.add),
                 reads=["ssy_%d_0" % ti, "ssy_%d_1" % ti], writes=["ssy1_%d" % ti])
            rstd_from_ss(ssy1[:, ti:ti + 1], D, rsy[:, ti:ti + 1], lny[:, ti:ti + 1], ["ssy1_%d" % ti], "rsy_%d" % ti)
            for half in range(2):
                hs = slice(half * 512, (half + 1) * 512)
                P.op("dve", lambda e, half=half, hs=hs, ti=ti: e.scalar_tensor_tensor(out=tmp6[:, hs], in0=banks[4 + half][:, :], scalar=rsy[:, ti:ti + 1], in1=G1[:, hs], op0=ALU.mult, op1=ALU.mult),
                     reads=["bank%d" % (4 + half), "rsy_%d" % ti, "G1", "tmp6"], writes=["tmp6"])
            P.op("dve", lambda e, s=s: e.tensor_tensor(out=x1t[s], in0=tmp6, in1=xt6[s], op=ALU.add), reads=["tmp6", "xt6%d" % s], writes=["x1t"])
            P.dma("sp", dm_x1[ti * 128:(ti + 1) * 128, :], x1t[s], "st_x1%d" % s, reads=["x1t"], writes=["dm_x1"])
            P.op("act", lambda e, s=s, ti=ti: e.activation(out=junk6, in_=x1t[s], func=AF.Square, accum_out=ssx[:, ti:ti + 1]), reads=["x1t", "ssx"], writes=["junk6", "ssx_%d" % ti])
            rstd_from_ss(ssx[:, ti:ti + 1], D, rsx[:, ti:ti + 1], lnx[:, ti:ti + 1], ["ssx_%d" % ti], "rsx_%d" % ti)
            P.op("dve", lambda e, s=s, ti=ti: e.scalar_tensor_tensor(out=tmp6, in0=x1t[s], scalar=rsx[:, ti:ti + 1], in1=A2, op0=ALU.mult, op1=ALU.mult),
                 reads=["x1t", "rsx_%d" % ti, "A2", "tmp6"], writes=["tmp6"])
            P.op("dve", lambda e: e.tensor_tensor(out=u2f, in0=tmp6, in1=B2, op=ALU.add), reads=["tmp6", "B2"], writes=["u2f"])
            P.op("act", lambda e: e.copy(out=u2b, in_=u2f), reads=["u2f"], writes=["u2b"])

            def tru(e):
                for kt in range(8):
                    r = e.transpose(bank_bf(6)[:, kt * 128:(kt + 1) * 128], u2b[:, kt * 128:(kt + 1) * 128], ident_b)
                return r
            P.op("pe", tru, reads=["u2b", "ident_b"], writes=["bank6"])
            P.op("act", lambda e, s=s: e.copy(out=u2Tb[s], in_=bank_bf(6).rearrange("p (a b) -> p a b", b=128)), reads=["bank6"], writes=["u2Tb%d" % s])
            P.dma("sp", dm_u2t[:, :, ti * 128:(ti + 1) * 128], u2Tb[s], "st_u2t%d" % s, reads=["u2Tb%d" % s], writes=["dm_u2t"])
            for g2 in range(2):
                def truf(e, g2=g2):
                    for i in range(4):
                        kt = g2 * 4 + i
                        r = e.transpose(banks[7][:, i * 128:(i + 1) * 128], u2f[:, kt * 128:(kt + 1) * 128], ident_f)
                    return r
                P.op("pe", truf, reads=["u2f", "ident_f"], writes=["bank7"])
                P.op("act", lambda e, g2=g2: e.copy(out=u2Tf[:, g2 * 4:(g2 + 1) * 4, :], in_=banks[7][:, :].rearrange("p (a b) -> p a b", b=128)), reads=["bank7", "u2Tf"], writes=["u2Tf"])

            def mml(e):
                for kt in range(8):
                    r = e.matmul(banks[6][:, 0:NE], lhsT=u2Tf[:, kt, :], rhs=rwt[:, kt, :], start=(kt == 0), stop=(kt == 7))
                return r
            P.op("pe", mml, reads=["u2Tf", "rwt"], writes=["bank6"])
            P.op("dve", lambda e: e.tensor_tensor(out=lg, in0=banks[6][:, 0:NE], in1=rbt, op=ALU.add), reads=["bank6", "rbt"], writes=["lg"])
            P.op("dve", lambda e: e.max(out=mx8, in_=lg), reads=["lg"], writes=["mx8"])
            P.op("dve", lambda e: e.tensor_scalar(out=msk, in0=lg, scalar1=mx8[:, 3:4], scalar2=None, op0=ALU.is_ge), reads=["lg", "mx8"], writes=["msk"])
            P.op("dve", lambda e: e.tensor_scalar(out=nmx, in0=mx8[:, 0:1], scalar1=-1.0, scalar2=None, op0=ALU.mult), reads=["mx8"], writes=["nmx"])
            P.op("act", lambda e: e.activation(out=em, in_=lg, func=AF.Exp, bias=nmx[:, 0:1], scale=1.0), reads=["lg", "nmx"], writes=["em"])
            P.op("dve", lambda e: e.tensor_tensor(out=em, in0=em, in1=msk, op=ALU.mult), reads=["em", "msk"], writes=["em"])
            P.op("dve", lambda e: e.reduce_sum(out=ssum, in_=em, axis=AX.X), reads=["em"], writes=["ssum"])
            P.op("dve", lambda e: e.reciprocal(out=rsum, in_=ssum), reads=["ssum"], writes=["rsum"])
            P.op("dve", lambda e, ti=ti: e.tensor_scalar(out=cmb[:, ti, :], in0=em, scalar1=rsum[:, 0:1], scalar2=None, op0=ALU.mult), reads=["em", "rsum"], writes=["cmb_%d" % ti])
            P.op("pe", lambda e, ti=ti: e.transpose(banks[7][0:NE, 0:128], cmb[:, ti, :], ident_f), reads=["cmb_%d" % ti, "ident_f"], writes=["bank7"])
            P.op("act", lambda e: e.copy(out=cT[0:NE, :], in_=banks[7][0:NE, 0:128]), reads=["bank7"], writes=["cT"])
            P.dma("sp", dm_combT[:, ti * 128:(ti + 1) * 128], cT[0:NE, :], "st_cT", reads=["cT"], writes=["dm_combT"])
    P.dma("sp", dm_comb, cmb, "st_cmb", reads=["cmb_%d" % t for t in range(16)], writes=["dm_comb"])
    P.barrier(keep=["dm_mod", "dm_x1", "dm_u2t", "dm_comb", "dm_combT"])
    A.release(m_pre_ut)
    if upto <= 6:
        return finish(P, nc)

    G2 = A.alloc((D,), F32)
    P.dma("sp", G2, dm_mod[:, 5 * D:6 * D], "ld_c", reads=["dm_mod"], writes=["G2"])
    bu = A.alloc((NE * 16,), F32)
    P.dma("sp", bu, bupT_d, "ld_c", writes=["bu"])
    bdn = A.alloc((D,), F32)
    P.dma("sp", bdn[0:NE, :], bdn_d, "ld_c", writes=["bdn"])
    cmb7 = A.alloc((16, NE), F32)
    P.dma("sp", cmb7, dm_comb, "ld_c", reads=["dm_comb"], writes=["cmb7"])
    P.op("dve", lambda e: e.tensor_scalar(out=cmb7, in0=cmb7, scalar1=1.0 / 1.702, scalar2=None, op0=ALU.mult), reads=["cmb7"], writes=["cmb7"])
    bu1 = A.alloc((NE * 16,), F32)
    P.op("dve", lambda e: e.tensor_scalar(out=bu1, in0=bu, scalar1=1.0, scalar2=None, op0=ALU.add), reads=["bu"], writes=["bu1"])
    cT2 = A.alloc((1024,), F32)
    u2T = A.alloc((8, 1024), BF16)
    acc = A.alloc((8, D), F32)
    wu = [A.alloc((8, 2 * D), BF16) for _ in range(2)]
    wd = [A.alloc((8, D), BF16) for _ in range(2)]
    aTraw = [A.alloc((4096,), BF16) for _ in range(2)]
    aT = [a.rearrange("p (a b) -> p a b", b=512) for a in aTraw]
    gc = [A.alloc((512,), F32) for _ in range(2)]
    sgm = [A.alloc((512,), F32) for _ in range(2)]
    lc = [A.alloc((512,), F32) for _ in range(2)]
    xt7 = [aTraw[0][:, 0:2048].bitcast(F32)] * 2
    tmp7 = aTraw[0][:, 2048:4096].bitcast(F32)
    ot7 = [aTraw[1][:, 0:2048].bitcast(F32)] * 2
    junk7 = aTraw[1][:, 2048:3072]
    ss7 = A.alloc((16,), F32)
    ln7 = A.alloc((16,), F32)
    rs7 = A.alloc((16,), F32)
    P.op("pool", lambda e: e.memset(ss7, 0.0), writes=["ss7"])
    ecount = 0
    for half in range(2):
        hc = slice(half * 1024, (half + 1) * 1024)
        P.dma("sp", u2T, dm_u2t[:, :, hc], "ld_u2t", reads=["dm_u2t"], writes=["u2T"])
        P.dma("sp", cT2[0:NE, :], dm_combT[:, hc], "ld_cT2", reads=["dm_combT"], writes=["cT2"])
        for tt in range(8):
            for dh in range(2):
                bk = 4 + (tt * 2 + dh) % 4
                P.op("pe", lambda e, tt=tt, dh=dh, bk=bk: e.matmul(banks[bk][:, :], lhsT=cT2[0:NE, tt * 128:(tt + 1) * 128], rhs=bdn[0:NE, dh * 512:(dh + 1) * 512], start=True, stop=True),
                     reads=["cT2", "bdn"], writes=["bank%d" % bk])
                P.op("act", lambda e, tt=tt, dh=dh, bk=bk: e.copy(out=acc[:, tt, dh * 512:(dh + 1) * 512], in_=banks[bk][:, :]), reads=["bank%d" % bk], writes=["acc_%d_%d" % (tt, dh)])
        for ex in range(NE):
            ws = ecount % 2
            ecount += 1
            P.dma("pool", wu[ws], wup_d[ex].rearrange("(kt p) n -> p kt n", p=128), "ld_wu%d" % ws, writes=["wu%d" % ws])
            P.dma("pool", wd[ws], wdn_d[ex].rearrange("(kt p) n -> p kt n", p=128), "ld_wd%d" % ws, writes=["wd%d" % ws])
            for blk in range(2):
                bs = slice(blk * 512, (blk + 1) * 512)
                ab = aT[blk % 2]
                abk = "aT%d" % (blk % 2)
                for g in range(8):
                    s = g % 2
                    bg, bl = g % 2, 2 + g % 2

                    def mmu(e, g=g, bg=bg, bl=bl, ws=ws, bs=bs):
                        for kt in range(8):
                            e.matmul(banks[bg][:, :], lhsT=wu[ws][:, kt, g * 128:(g + 1) * 128], rhs=u2T[:, kt, bs], start=(kt == 0), stop=(kt == 7))
                        for kt in range(8):
                            r = e.matmul(banks[bl][:, :], lhsT=wu[ws][:, kt, 1024 + g * 128:1024 + (g + 1) * 128], rhs=u2T[:, kt, bs], start=(kt == 0), stop=(kt == 7))
                        return r
                    P.op("pe", mmu, reads=["wu%d" % ws, "u2T"], writes=["bank%d" % bg, "bank%d" % bl])
                    P.op("dve", lambda e, s=s, bg=bg, ex=ex, g=g: e.tensor_scalar(out=gc[s], in0=banks[bg][:, :], scalar1=bu[:, ex * 16 + g:ex * 16 + g + 1], scalar2=7.0, op0=ALU.add, op1=ALU.min),
                         reads=["bank%d" % bg, "bu"], writes=["gc%d" % s])
                    P.op("act", lambda e, s=s: e.activation(out=sgm[s], in_=gc[s], func=AF.Silu, scale=1.702), reads=["gc%d" % s], writes=["sgm%d" % s])
                    P.op("dve", lambda e, s=s, bl=bl, ex=ex, g=g: e.tensor_scalar(out=lc[s], in0=banks[bl][:, :], scalar1=bu1[:, ex * 16 + 8 + g:ex * 16 + 8 + g + 1], scalar2=8.0, op0=ALU.add, op1=ALU.min),
                         reads=["bank%d" % bl, "bu1"], writes=["lc%d" % s])
                    P.op("dve", lambda e, s=s, g=g, ab=ab: e.scalar_tensor_tensor(out=ab[:, g, :], in0=lc[s], scalar=-6.0, in1=sgm[s], op0=ALU.max, op1=ALU.mult),
                         reads=["sgm%d" % s, "lc%d" % s], writes=[abk])
                for tt in range(4):
                    til = blk * 4 + tt
                    for dh in range(2):
                        bk = 4 + (tt * 2 + dh) % 4

                        def mmd(e, tt=tt, dh=dh, bk=bk, ws=ws, ab=ab):
                            for fk in range(8):
                                r = e.matmul(banks[bk][:, :], lhsT=ab[:, fk, tt * 128:(tt + 1) * 128], rhs=wd[ws][:, fk, dh * 512:(dh + 1) * 512], start=(fk == 0), stop=(fk == 7))
                            return r
                        P.op("pe", mmd, reads=[abk, "wd%d" % ws], writes=["bank%d" % bk])
                        ak = "acc_%d_%d" % (til, dh)
                        P.op("dve", lambda e, til=til, dh=dh, bk=bk, ex=ex, half=half: e.scalar_tensor_tensor(
                            out=acc[:, til, dh * 512:(dh + 1) * 512], in0=banks[bk][:, :], scalar=cmb7[:, half * 8 + til, ex:ex + 1], in1=acc[:, til, dh * 512:(dh + 1) * 512], op0=ALU.mult, op1=ALU.add),
                            reads=["bank%d" % bk, "cmb7", ak], writes=[ak])
        P.barrier(keep=["dm_x1", "dm_u2t", "dm_combT"])
        for tt in range(8):
            ti = half * 8 + tt
            s = 0
            P.dma("sp", xt7[s], dm_x1[ti * 128:(ti + 1) * 128, :], "ld_x7%d" % s, reads=["dm_x1"], writes=["xt7%d" % s])
            P.op("act", lambda e, tt=tt, ti=ti: e.activation(out=junk7, in_=acc[:, tt, :], func=AF.Square, accum_out=ss7[:, ti:ti + 1]),
                 reads=["acc_%d_0" % tt, "acc_%d_1" % tt, "ss7"], writes=["junk7", "ss7_%d" % ti])
            rstd_from_ss(ss7[:, ti:ti + 1], D, rs7[:, ti:ti + 1], ln7[:, ti:ti + 1], ["ss7_%d" % ti], "rs7_%d" % ti)
            P.op("dve", lambda e, tt=tt, ti=ti: e.scalar_tensor_tensor(out=tmp7, in0=acc[:, tt, :], scalar=rs7[:, ti:ti + 1], in1=G2, op0=ALU.mult, op1=ALU.mult),
                 reads=["acc_%d_0" % tt, "acc_%d_1" % tt, "rs7_%d" % ti, "G2"], writes=["tmp7"])
            P.op("dve", lambda e, s=s: e.tensor_tensor(out=ot7[s], in0=tmp7, in1=xt7[s], op=ALU.add), reads=["tmp7", "xt7%d" % s], writes=["ot7%d" % s])
            P.dma("sp", out_d[ti * 128:(ti + 1) * 128, :], ot7[s], "st_out%d" % s, reads=["ot7%d" % s], writes=["out"])
        P.barrier(keep=["dm_x1", "dm_u2t", "dm_combT"])

    finish(P, nc)
    return nc


def finish(P, nc):
    P.wait_all("sp")
    P.build()
    P.close()
    return nc


def _rope_table(j):
    t = np.arange(NT) + j * NT
    rows = (t // 64).astype(np.float32)
    cols = (t % 64).astype(np.float32)
    inv = (10000.0 ** (-np.arange(0, 64, 2, dtype=np.float32) / 64)).astype(np.float32)
    ar = rows[:, None] * inv[None, :]
    ac = cols[:, None] * inv[None, :]
    cr, sr, cc, sc = np.cos(ar), np.sin(ar), np.cos(ac), np.sin(ac)
    return np.concatenate([cr, cr, cc, cc, -sr, sr, -sc, sc], axis=1).astype(np.float32)


def make_in_maps(inp, small=False):
    f = lambda a: np.ascontiguousarray(np.asarray(a, dtype=np.float32))
    x, c, ctx, c_ctx = f(inp["x"]), f(inp["c"]), f(inp["ctx"]), f(inp["c_ctx"])
    shared = {
        "w_mod": f(inp["w_mod"][0]), "b_mod": f(inp["b_mod"][0]).reshape(1, -1),
        "norm_g": f(inp["norm_g"][0]).reshape(1, -1), "w_in": f(inp["w_in"][0]),
        "lbv": f(np.asarray(inp["hgrn_lb"]).reshape(2, 2, 8, 128).transpose(3, 0, 1, 2).reshape(128, 32)),
        "hng": f(inp["hgrn_norm_g"][0]).reshape(128, 1), "qkg": f(inp["qk_norm_g"][0]).reshape(1, 256),
        "w_branch": f(inp["w_branch"][0]), "w_out": f(inp["w_out"][0]),
        "router_w": f(inp["router_w"][0]), "router_b": f(inp["router_b"][0]).reshape(1, -1),
        "w_up": f(inp["w_up"][0]), "b_upT": f(np.asarray(inp["b_up"][0]).reshape(32, 16, 128).transpose(2, 0, 1).reshape(128, 512)),
        "w_down": f(inp["w_down"][0]), "b_down": f(inp["b_down"][0]),
    }
    ropes = [_rope_table(j) for j in range(4)]
    maps = []
    for core in range(8):
        b, j = core // 4, core % 4
        cvec = np.concatenate([c[b].reshape(8, 128).T, c_ctx.reshape(8, 128).T], axis=1)
        sel = np.zeros((128, 8), np.float32)
        for r in range(4):
            sel[:, r] = 1.0 if r < j else 0.0
            sel[:, 4 + r] = 1.0 if r > j else 0.0
        m = dict(shared)
        if small:
            m["w_up"] = m["w_up"][0:1]
            m["w_down"] = m["w_down"][0:1]
        m.update({"x": f(x[b, j * NT:(j + 1) * NT]), "ctx": f(ctx[b]), "cvec": f(cvec), "rope": ropes[j], "sel": sel})
        maps.append(m)
    return maps


_NC_CACHE = {}


def kernel(**inputs):
    if "nc" not in _NC_CACHE:
        _NC_CACHE["nc"] = build()
    nc = _NC_CACHE["nc"]
    maps = make_in_maps(inputs)
    res = run_bass_kernel_spmd(nc, maps, core_ids=list(range(8)))
    out = np.empty((2, 8192, D), np.float32)
    for core in range(8):
        b, j = core // 4, core % 4
        out[b, j * NT:(j + 1) * NT] = res.results[core]["out"]
    return out
```

```python
from contextlib import ExitStack
import numpy as np
import concourse.bass as bass
import concourse.mybir as mybir
from concourse.bass_utils import run_bass_kernel_spmd

F32 = mybir.dt.float32
BF16 = mybir.dt.bfloat16
ALU = mybir.AluOpType
AF = mybir.ActivationFunctionType
AX = mybir.AxisListType

ENGS = ("pe", "act", "dve", "pool", "sp")
EPOCH = 16000
EPS = 1e-6


def _freeze(fn, memo=None):
    import types
    if memo is None:
        memo = {}
    if not isinstance(fn, types.FunctionType) or fn.__closure__ is None:
        return fn
    if id(fn) in memo:
        return memo[id(fn)]
    cells = []
    for c in fn.__closure__:
        try:
            v = c.cell_contents
        except ValueError:
            cells.append(c)
            continue
        if isinstance(v, types.FunctionType) and v.__closure__ is not None and v is not fn:
            v = _freeze(v, memo)
        cells.append(types.CellType(v))
    new = types.FunctionType(fn.__code__, fn.__globals__, fn.__name__, fn.__defaults__, tuple(cells))
    new.__kwdefaults__ = fn.__kwdefaults__
    memo[id(fn)] = new
    return new


class Prog:
    def __init__(self, nc, same_engine_sync=True):
        self.nc = nc
        self.es = ExitStack()
        self.q = {e: [] for e in ENGS}
        self.cnt = {e: 0 for e in ENGS}
        self.waited = {}
        self.buf = {}
        self.sems = {}
        self.dma_cnt = {}
        self.same_engine_sync = same_engine_sync
        self.n_sem = 0

    def sem(self, key):
        if key not in self.sems:
            self.n_sem += 1
            self.sems[key] = self.es.enter_context(self.nc.semaphore("s%d" % self.n_sem))
        return self.sems[key]

    def sbuf(self, name, shape, dtype):
        return self.es.enter_context(self.nc.sbuf_tensor(name, list(shape), dtype))

    def psum(self, name, shape, dtype=F32):
        return self.es.enter_context(self.nc.psum_tensor(name, list(shape), dtype))

    def _semkey_for(self, prod):
        kind, name, count = prod
        if kind == "e":
            ep = (count - 1) // EPOCH
            return ("e", name, ep), count - ep * EPOCH
        return ("d", name), count

    def _need(self, eng, prod, waits):
        if prod is None:
            return
        kind, name, count = prod
        if kind == "e" and name == eng and (eng in ("pe", "sp") or not self.same_engine_sync):
            return
        sk, val = self._semkey_for(prod)
        wk = (eng, kind, name)
        if self.waited.get(wk, 0) >= count:
            return
        self.waited[wk] = count
        waits.append((sk, val))

    def _deps(self, eng, reads, writes):
        waits = []
        for k in reads:
            b = self.buf.get(k)
            if b is not None:
                self._need(eng, b["w"], waits)
        for k in writes:
            b = self.buf.get(k)
            if b is not None:
                self._need(eng, b["w"], waits)
                for r in b["r"].values():
                    self._need(eng, r, waits)
        return waits

    def _record(self, prod, reads, writes):
        for k in reads:
            b = self.buf.setdefault(k, {"w": None, "r": {}})
            b["r"][(prod[0], prod[1])] = prod
        for k in writes:
            self.buf[k] = {"w": prod, "r": {}}

    def op(self, eng, fn, reads=(), writes=()):
        fn = _freeze(fn)
        waits = self._deps(eng, reads, writes)
        self.cnt[eng] += 1
        prod = ("e", eng, self.cnt[eng])
        sk, _ = self._semkey_for(prod)
        self.q[eng].append((fn, waits, (sk, 1)))
        self._record(prod, reads, writes)
        return prod

    def dma(self, eng, out, in_, semname, reads=(), writes=(), **kw):
        if writes:
            semname = semname + ":" + writes[0]
        waits = self._deps(eng, reads, writes)
        self.dma_cnt[semname] = self.dma_cnt.get(semname, 0) + 16
        prod = ("d", semname, self.dma_cnt[semname])
        self.q[eng].append((lambda e: e.dma_start(out=out, in_=in_, **kw), waits, (("d", semname), 16)))
        self._record(prod, reads, writes)
        return prod

    def custom(self, eng, fn, semname, inc, reads=(), writes=()):
        fn = _freeze(fn)
        waits = self._deps(eng, reads, writes)
        self.dma_cnt[semname] = self.dma_cnt.get(semname, 0) + inc
        prod = ("d", semname, self.dma_cnt[semname])
        self.q[eng].append((fn, waits, (("d", semname), inc)))
        self._record(prod, reads, writes)
        return prod

    def wait_all(self, eng):
        waits = []
        for e in ENGS:
            if self.cnt[e] > 0 and e != eng:
                self._need(eng, ("e", e, self.cnt[e]), waits)
        for name, c in self.dma_cnt.items():
            self._need(eng, ("d", name, c), waits)
        self.q[eng].append((None, waits, None))

    def barrier(self, keep=()):
        for e in ENGS:
            self.wait_all(e)
        self.buf = {k: v for k, v in self.buf.items() if k in keep}

    def build(self):
        nc = self.nc
        keys = []
        for e in ENGS:
            for (_, w, inc) in self.q[e]:
                for x in w:
                    keys.append(x[0])
                if inc:
                    keys.append(inc[0])
        for sk in dict.fromkeys(keys):
            self.sem(sk)
        engmap = {"pe": "tensor", "act": "scalar", "dve": "vector", "pool": "gpsimd", "sp": "sync"}
        with nc.Block() as block:
            for e in ENGS:
                items = self.q[e]

                def body(eng, items=items):
                    for fn, waits, inc in items:
                        for sk, val in waits:
                            eng.wait_ge(self.sems[sk], val)
                        if fn is not None:
                            ins = fn(eng)
                            if inc is not None:
                                ins.then_inc(self.sems[inc[0]], inc[1])

                getattr(block, engmap[e])(body)

    def close(self):
        self.es.close()


class Arena:
    def __init__(self, P, nbytes):
        self.t = P.sbuf("arena", [128, nbytes // 2], BF16)
        self.nbytes = nbytes
        self.off = 0

    def alloc(self, free_shape, dtype):
        n = int(np.prod(free_shape))
        size = n * (4 if dtype == F32 else 2)
        size = (size + 63) // 64 * 64
        assert self.off + size <= self.nbytes, ("SBUF arena overflow", self.off, size)
        v = self.t[:, self.off // 2:(self.off + size) // 2]
        if dtype == F32:
            v = v.bitcast(F32)
        v = v[:, 0:n]
        self.off += size
        if len(free_shape) == 2:
            v = v.rearrange("p (a b) -> p a b", b=free_shape[1])
        elif len(free_shape) == 3:
            v = v.rearrange("p (a b c) -> p a b c", b=free_shape[1], c=free_shape[2])
        elif len(free_shape) == 4:
            v = v.rearrange("p (a b c d) -> p a b c d", b=free_shape[1], c=free_shape[2], d=free_shape[3])
        return v

    def mark(self):
        return self.off

    def release(self, m):
        self.off = m


def bc(ap, axis, shape):
    return ap.unsqueeze(axis).to_broadcast(list(shape))


NT = 2048
NTT = 16
NCTX = 256
NALL = NT + NCTX
D = 1024
NE = 32
LAST_PHASE = 99


def build(upto=LAST_PHASE, debug=False):
    nc = bass.Bass("TRN2", target_bir_lowering=False)

    def din(name, shape, dt=F32):
        return nc.dram_tensor(name, list(shape), dt, kind="ExternalInput").ap()

    x_d = din("x", [NT, D])
    ctx_d = din("ctx", [NCTX, D])
    cvec_d = din("cvec", [128, 16])
    wmod_d = din("w_mod", [D, 6 * D])
    bmod_d = din("b_mod", [1, 6 * D])
    ng_d = din("norm_g", [1, 4 * D])
    win_d = din("w_in", [D, 8704])
    lbv_d = din("lbv", [128, 32])
    hng_d = din("hng", [128, 1])
    qkg_d = din("qkg", [1, 256])
    wbr_d = din("w_branch", [2, D, D])
    wout_d = din("w_out", [D, D])
    rw_d = din("router_w", [D, NE])
    rb_d = din("router_b", [1, NE])
    NEW = NE if upto >= 7 else 1
    wup_d = din("w_up", [NEW, D, 2 * D])
    bupT_d = din("b_upT", [128, NE * 16])
    wdn_d = din("w_down", [NEW, D, D])
    bdn_d = din("b_down", [NE, D])
    rope_d = din("rope", [NT, 256])
    sel_d = din("sel", [128, 8])
    out_d = nc.dram_tensor("out", [NT, D], F32, kind="ExternalOutput").ap()

    def dscr(name, shape, dt):
        if debug:
            return nc.dram_tensor(name, list(shape), dt, kind="ExternalOutput").ap()
        return nc.dram_tensor(name, list(shape), dt).ap()

    dm_mod = dscr("dm_mod", [128, 6 * D], F32)
    dm_ut = dscr("dm_ut", [128, 8, NALL], BF16)
    ag1_ins = [nc.dram_tensor("ag1_in%d" % q, [128, 2048], BF16).ap() for q in range(4)]
    ag1_outs = [nc.dram_tensor("ag1_out%d" % q, [4 * 128, 2048], BF16).ap() for q in range(4)]
    dm_kvctx = dscr("dm_kvctx", [512, NCTX], BF16)
    dm_oloc = dscr("dm_oloc", [8, 128, NT], F32)
    dm_qb = dscr("dm_qb", [2, 8, 128, NT], BF16)
    dm_gs = dscr("dm_gs", [8, 128, NT], BF16)
    ag2_ins = [nc.dram_tensor("ag2_in%d" % d, [128, 1032], F32).ap() for d in range(2)]
    ag2_outs = [nc.dram_tensor("ag2_out%d" % d, [4 * 128, 1032], F32).ap() for d in range(2)]
    dm_oth = dscr("dm_oth", [8, 128, NT], BF16)
    dm_ota = dscr("dm_ota", [8, 128, NT], BF16)
    dm_x1 = dscr("dm_x1", [NT, D], F32)
    dm_u2t = dscr("dm_u2t", [128, 8, NT], BF16)
    dm_comb = dscr("dm_comb", [128, NTT, NE], F32)
    dm_combT = dscr("dm_combT", [NE, NT], F32)

    P = Prog(nc)
    A = Arena(P, 207 * 1024)
    banks = [P.psum("bank%d" % i, [128, 512], F32) for i in range(8)]

    def bank_bf(i):
        return banks[i][:, :].bitcast(BF16)

    ident_f = A.alloc((128,), F32)
    ident_b = A.alloc((128,), BF16)
    ones_b = A.alloc((128,), BF16)
    ones_f = A.alloc((128,), F32)
    P.op("pool", lambda e: e.memset(ident_f, 0.0), writes=["ident_f"])
    P.op("pool", lambda e: e.affine_select(out=ident_f, in_=ident_f, pattern=[[-1, 128]], compare_op=ALU.not_equal,
                                           fill=1.0, base=0, channel_multiplier=1), reads=["ident_f"], writes=["ident_f"])
    P.op("dve", lambda e: e.tensor_copy(out=ident_b, in_=ident_f), reads=["ident_f"], writes=["ident_b"])
    P.op("pool", lambda e: e.memset(ones_f, 1.0), writes=["ones_f"])
    P.op("dve", lambda e: e.tensor_copy(out=ones_b, in_=ones_f), reads=["ones_f"], writes=["ones_b"])

    m_pre_ut = A.mark()
    uT = A.alloc((8, NALL), BF16)

    def rstd_from_ss(ss_ap, n, out_ap, tmp_ap, rk, wk):
        P.op("act", lambda e: e.activation(out=tmp_ap, in_=ss_ap, func=AF.Ln, scale=1.0 / n, bias=EPS), reads=rk, writes=[wk + "_ln"])
        P.op("act", lambda e: e.activation(out=out_ap, in_=tmp_ap, func=AF.Exp, scale=-0.5), reads=[wk + "_ln"], writes=[wk])

    m0 = A.mark()
    cv = A.alloc((16,), F32)
    scv = A.alloc((16,), F32)
    scb = A.alloc((16, 128), F32)
    bmod = A.alloc((6 * D,), F32)
    ng = A.alloc((4, D), F32)
    modl = A.alloc((6 * D,), F32)
    modc = A.alloc((2 * D,), F32)
    wm = [A.alloc((8, 512), F32) for _ in range(2)]
    P.dma("sp", cv, cvec_d, "ld_c", writes=["cv"])
    P.dma("sp", bmod, bmod_d[0].partition_broadcast(128), "ld_c", writes=["bmod"])
    P.dma("sp", ng, ng_d[0].partition_broadcast(128).rearrange("p (a b) -> p a b", b=D), "ld_c", writes=["ng"])
    P.op("act", lambda e: e.activation(out=scv, in_=cv, func=AF.Silu), reads=["cv"], writes=["scv"])
    for k in range(16):
        P.op("dve", lambda e, k=k: e.tensor_copy(out=scb[:, k, :], in_=scv[:, k:k + 1].to_broadcast([128, 128])),
             reads=["scv"], writes=["scb"])
    for s in range(12):
        w = wm[s % 2]
        wk = "wm%d" % (s % 2)
        P.dma("sp", w, wmod_d[:, s * 512:(s + 1) * 512].rearrange("(kt p) n -> p kt n", p=128), "ld_" + wk, writes=[wk])

        def mm(e, w=w, off=0, bk=0):
            for kt in range(8):
                r = e.matmul(banks[bk][:, :], lhsT=scb[:, off + kt, :], rhs=w[:, kt, :], start=(kt == 0), stop=(kt == 7))
            return r
        P.op("pe", lambda e, w=w: mm(e, w, 0, 0), reads=["scb", wk], writes=["bank0"])
        P.op("dve", lambda e, s=s: e.tensor_tensor(out=modl[:, s * 512:(s + 1) * 512], in0=banks[0][:, :], in1=bmod[:, s * 512:(s + 1) * 512], op=ALU.add),
             reads=["bank0", "bmod"], writes=["modl"])
        if s < 4:
            P.op("pe", lambda e, w=w: mm(e, w, 8, 1), reads=["scb", wk], writes=["bank1"])
            P.op("dve", lambda e, s=s: e.tensor_tensor(out=modc[:, s * 512:(s + 1) * 512], in0=banks[1][:, :], in1=bmod[:, s * 512:(s + 1) * 512], op=ALU.add),
                 reads=["bank1", "bmod"], writes=["modc"])
    P.op("dve", lambda e: e.scalar_tensor_tensor(out=modl[:, D:2 * D], in0=modl[:, D:2 * D], scalar=1.0, in1=ng[:, 0, :], op0=ALU.add, op1=ALU.mult),
         reads=["modl", "ng"], writes=["modl"])
    P.op("dve", lambda e: e.scalar_tensor_tensor(out=modc[:, D:2 * D], in0=modc[:, D:2 * D], scalar=1.0, in1=ng[:, 0, :], op0=ALU.add, op1=ALU.mult),
         reads=["modc", "ng"], writes=["modc"])
    P.op("dve", lambda e: e.tensor_tensor(out=modl[:, 2 * D:3 * D], in0=modl[:, 2 * D:3 * D], in1=ng[:, 1, :], op=ALU.mult), reads=["modl", "ng"], writes=["modl"])
    P.op("dve", lambda e: e.scalar_tensor_tensor(out=modl[:, 4 * D:5 * D], in0=modl[:, 4 * D:5 * D], scalar=1.0, in1=ng[:, 2, :], op0=ALU.add, op1=ALU.mult),
         reads=["modl", "ng"], writes=["modl"])
    P.op("dve", lambda e: e.tensor_tensor(out=modl[:, 5 * D:6 * D], in0=modl[:, 5 * D:6 * D], in1=ng[:, 3, :], op=ALU.mult), reads=["modl", "ng"], writes=["modl"])
    P.dma("sp", dm_mod, modl, "st_mod", reads=["modl"], writes=["dm_mod"])

    xt = [A.alloc((D,), F32) for _ in range(2)]
    junk = A.alloc((D,), BF16)
    tmpf = A.alloc((D,), F32)
    ub = [A.alloc((D,), BF16) for _ in range(2)]
    ss1 = A.alloc((18,), F32)
    ln1 = A.alloc((18,), F32)
    rs1 = A.alloc((18,), F32)
    P.op("pool", lambda e: e.memset(ss1, 0.0), writes=["ss1"])
    for ti in range(18):
        s = ti % 2
        src = x_d[ti * 128:(ti + 1) * 128, :] if ti < NTT else ctx_d[(ti - NTT) * 128:(ti - NTT + 1) * 128, :]
        Am = modl if ti < NTT else modc
        amk = "modl" if ti < NTT else "modc"
        P.dma("sp", xt[s], src, "ld_xt%d" % s, writes=["xt%d" % s])
        P.op("act", lambda e, s=s, ti=ti: e.activation(out=junk, in_=xt[s], func=AF.Square, accum_out=ss1[:, ti:ti + 1]),
             reads=["xt%d" % s, "ss1"], writes=["junk", "ss1_%d" % ti])
        rstd_from_ss(ss1[:, ti:ti + 1], D, rs1[:, ti:ti + 1], ln1[:, ti:ti + 1], ["ss1_%d" % ti], "rs1_%d" % ti)
        P.op("dve", lambda e, s=s, ti=ti, Am=Am: e.scalar_tensor_tensor(out=tmpf, in0=xt[s], scalar=rs1[:, ti:ti + 1], in1=Am[:, D:2 * D], op0=ALU.mult, op1=ALU.mult),
             reads=["xt%d" % s, "rs1_%d" % ti, amk], writes=["tmpf"])
        P.op("dve", lambda e, s=s, Am=Am: e.tensor_tensor(out=ub[s], in0=tmpf, in1=Am[:, 0:D], op=ALU.add), reads=["tmpf", amk], writes=["ub%d" % s])
        bk = 2 + s

        def tr(e, s=s, bk=bk):
            for kt in range(8):
                r = e.transpose(bank_bf(bk)[:, kt * 128:(kt + 1) * 128], ub[s][:, kt * 128:(kt + 1) * 128], ident_b)
            return r
        P.op("pe", tr, reads=["ub%d" % s, "ident_b"], writes=["bank%d" % bk])
        P.op("act", lambda e, ti=ti, bk=bk: e.copy(out=uT[:, :, ti * 128:(ti + 1) * 128], in_=bank_bf(bk).rearrange("p (a b) -> p a b", b=128)),
             reads=["bank%d" % bk], writes=["uT_%d" % ti])
    UT_KEYS = ["uT_%d" % ti for ti in range(18)]
    if debug:
        P.dma("sp", dm_ut, uT, "st_dbg", reads=UT_KEYS, writes=["dm_ut"])
    P.barrier()
    A.release(m0)
    if upto <= 1:
        return finish(P, nc)


    m2 = A.mark()
    wkv = A.alloc((8, 512), BF16)
    P.dma("pool", wkv, win_d[:, 6144:6656].rearrange("(kt p) n -> p kt n", p=128), "ld_wkv", writes=["wkv"])
    ropeT = A.alloc((NTT, 256), F32)
    P.dma("sp", ropeT, rope_d.rearrange("(t p) n -> p t n", p=128), "ld_rope", writes=["ropeT"])
    gqk = A.alloc((256,), F32)
    P.dma("sp", gqk, qkg_d[0].partition_broadcast(128), "ld_c", writes=["gqk"])
    KTl = A.alloc((2, NALL), BF16)
    Vl = A.alloc((18, 256), BF16)
    ssk = A.alloc((18, 2), F32)
    lnk = A.alloc((18, 2), F32)
    rsk = A.alloc((18, 2), F32)
    knb = [A.alloc((2, 128), F32) for _ in range(2)]
    t1b = A.alloc((2, 128), F32)
    t2b = A.alloc((2, 128), F32)
    kbf = [A.alloc((256,), BF16) for _ in range(2)]
    junk2 = A.alloc((128,), BF16)
    P.op("pool", lambda e: e.memset(ssk, 0.0), writes=["ssk"])

    def rope_apply(xn, nh, ti, outbf, pfx, eng2="dve"):
        cosv = ropeT[:, ti, 0:128]
        sinv = ropeT[:, ti, 128:256].rearrange("p (r x d) -> p r x d", r=2, x=2, d=32)
        t1 = t1b if nh == 2 else t1q
        t2 = t2b if nh == 2 else t2q
        P.op("dve", lambda e: e.tensor_tensor(out=t1, in0=xn, in1=bc(cosv, 1, [128, nh, 128]), op=ALU.mult),
             reads=[pfx + "xn", "ropeT"], writes=[pfx + "t1"])
        x6 = xn.rearrange("p h (r x d) -> p h r x d", r=2, x=2, d=32)
        t6 = t2.rearrange("p h (r x d) -> p h r x d", r=2, x=2, d=32)
        for xo in range(2):
            P.op(eng2, lambda e, xo=xo: e.tensor_tensor(out=t6[:, :, :, xo, :], in0=x6[:, :, :, 1 - xo, :],
                                                        in1=bc(sinv[:, :, xo, :], 1, [128, nh, 2, 32]), op=ALU.mult),
                 reads=[pfx + "xn", "ropeT"], writes=[pfx + "t2_%d" % xo])
        P.op("dve", lambda e: e.tensor_tensor(out=outbf.rearrange("p (h d) -> p h d", d=128), in0=t1, in1=t2, op=ALU.add),
             reads=[pfx + "t1", pfx + "t2_0", pfx + "t2_1"], writes=[pfx + "bf"])

    import os
    BIS = int(os.environ.get("BIS", "99"))
    for ti in range(18):
        s = ti % 2
        bk = s
        kn = knb[s]

        def mmkv(e, ti=ti, bk=bk):
            for kt in range(8):
                r = e.matmul(banks[bk][:, :], lhsT=uT[:, kt, ti * 128:(ti + 1) * 128], rhs=wkv[:, kt, :], start=(kt == 0), stop=(kt == 7))
            return r
        P.op("pe", mmkv, reads=["uT_%d" % ti, "wkv"], writes=["bank%d" % bk])
        kps = banks[bk][:, 0:256].rearrange("p (h d) -> p h d", d=128)
        P.op("act", lambda e, ti=ti, bk=bk: e.copy(out=Vl[:, ti, :], in_=banks[bk][:, 256:512]), reads=["bank%d" % bk], writes=["Vl_%d" % ti])
        if BIS < 2:
            continue
        for h in range(2):
            P.op("act", lambda e, h=h, ti=ti, kps=kps: e.activation(out=junk2, in_=kps[:, h, :], func=AF.Square, accum_out=ssk[:, ti, h:h + 1]),
                 reads=["bank%d" % bk, "ssk"], writes=["junk2", "ssk_%d_%d" % (ti, h)])
        rstd_from_ss(ssk[:, ti, :], 128, rsk[:, ti, :], lnk[:, ti, :], ["ssk_%d_0" % ti, "ssk_%d_1" % ti], "rsk_%d" % ti)
        P.op("dve", lambda e, kn=kn, kps=kps, ti=ti: e.tensor_tensor(out=kn, in0=kps, in1=bc(rsk[:, ti, :], 2, [128, 2, 128]), op=ALU.mult),
             reads=["bank%d" % bk, "rsk_%d" % ti], writes=["k%dxn" % s])
        P.op("dve", lambda e, kn=kn: e.tensor_tensor(out=kn, in0=kn, in1=bc(gqk[:, 128:256], 1, [128, 2, 128]), op=ALU.mult),
             reads=["k%dxn" % s, "gqk"], writes=["k%dxn" % s])
        if BIS < 3:
            continue
        if ti < NTT and BIS != 3:
            rope_apply(kn, 2, ti, kbf[s], "k%d" % s)
        else:
            P.op("dve", lambda e, kn=kn, s=s: e.tensor_copy(out=kbf[s].rearrange("p (h d) -> p h d", d=128), in_=kn), reads=["k%dxn" % s], writes=["k%dbf" % s])
        bk2 = 2 + s
        if BIS < 5:
            continue

        def trk(e, s=s, bk2=bk2):
            for h in range(2):
                r = e.transpose(bank_bf(bk2)[:, h * 128:(h + 1) * 128], kbf[s][:, h * 128:(h + 1) * 128], ident_b)
            return r
        P.op("pe", trk, reads=["k%dbf" % s, "ident_b"], writes=["bank%d" % bk2])
        P.op("act", lambda e, ti=ti, bk2=bk2: e.copy(out=KTl[:, :, ti * 128:(ti + 1) * 128], in_=bank_bf(bk2)[:, 0:256].rearrange("p (a b) -> p a b", b=128)),
             reads=["bank%d" % bk2], writes=["KTl_%d" % ti])
    for h in range(2 if BIS >= 6 else 0):
        P.dma("sp", ag1_ins[h], KTl[:, h, 0:NT], "st_ag1", reads=["KTl_%d" % t for t in range(16)], writes=["ag1_in"])
        P.dma("sp", ag1_ins[2 + h].rearrange("p (ti c) -> p ti c", c=256), Vl[:, 8 * h:8 * h + 8, :], "st_ag1", reads=["Vl_%d" % t for t in range(16)], writes=["ag1_in"])
    if BIS >= 6:
        P.dma("sp", dm_kvctx[0:256, :].rearrange("(h p) t -> p h t", p=128), KTl[:, :, NT:NALL], "st_kvc", reads=["KTl_16", "KTl_17"], writes=["dm_kvctx"])
        P.dma("sp", dm_kvctx[256:512, :].rearrange("(ti p) c -> p ti c", p=128), Vl[:, 16:18, :], "st_kvc", reads=["Vl_16", "Vl_17"], writes=["dm_kvctx"])
    for q in range(4 if BIS >= 7 else 0):
        P.custom("pool", lambda e, q=q: e.collective_compute("AllGather", ALU.bypass, replica_groups=[[0, 1, 2, 3], [4, 5, 6, 7]], ins=[ag1_ins[q]], outs=[ag1_outs[q]]),
                 "cc1", 1, reads=["ag1_in"] + (["ag1_out"] if q > 0 else []), writes=["ag1_out"])
    P.barrier(keep=["ag1_out", "dm_kvctx", "dm_mod"] + UT_KEYS)
    A.release(m2)
    if upto <= 2:
        return finish(P, nc)

    m3 = A.mark()
    rst = A.alloc((NALL,), F32)
    maskF = A.alloc((128,), F32)
    maskB = A.alloc((128,), F32)
    lbt = A.alloc((2, 2, 8), F32)
    lbd = A.alloc((2, 8), F32)
    lb = A.alloc((2, 8), F32)
    oml = A.alloc((2, 8), F32)
    hng = A.alloc((1,), F32)
    stage = A.alloc((2, 8, 128), F32)
    sctx = A.alloc((2, 8, 128), F32)
    Dv = A.alloc((2, 8), F32)
    m3h = A.mark()
    whb = [A.alloc((8, 5, 128), BF16)] * 2
    qT = A.alloc((NT,), F32)
    gsT = A.alloc((NT,), BF16)
    v_tm = A.alloc((18, 128), BF16)
    fa = A.alloc((NALL,), F32)
    lf = A.alloc((NALL,), F32)
    kk = A.alloc((NALL,), F32)
    bb = A.alloc((NALL,), F32)
    xx = A.alloc((NALL,), F32)
    ee = A.alloc((NALL,), F32)
    gtmp = ee[:, 0:NT]
    qe = [A.alloc((NT,), BF16) for _ in range(2)]
    ke = [A.alloc((NT,), BF16) for _ in range(2)]
    kd = [A.alloc((NALL,), BF16) for _ in range(2)]
    kd_tm = [A.alloc((18, 128), BF16) for _ in range(2)]
    qB = [A.alloc((NT,), BF16) for _ in range(2)]
    tot = [A.alloc((36,), F32) for _ in range(2)]
    etot = [A.alloc((36,), F32) for _ in range(2)]
    ipf = [A.alloc((32,), F32) for _ in range(2)]
    gg = [A.alloc((32,), F32) for _ in range(2)]
    eg = [A.alloc((32,), F32) for _ in range(2)]
    attm = [A.alloc((4, 128), BF16) for _ in range(2)]
    Sst = [[A.alloc((128,), F32) for _ in range(2)] for _ in range(2)]
    Sbf = [[A.alloc((128,), BF16) for _ in range(3)] for _ in range(2)]
    Scx = [A.alloc((128,), F32) for _ in range(2)]
    o_acc = A.alloc((NT,), F32)

    P.op("pool", lambda e: e.memset(rst, 1.0), writes=["rst"])
    P.op("pool", lambda e: e.memset(rst.rearrange("p (c t) -> p c t", t=64)[:, :, 0:1], 0.0), reads=["rst"], writes=["rst"])
    P.op("pool", lambda e: e.memset(maskF, 1.0), writes=["maskF"])
    P.op("pool", lambda e: e.affine_select(out=maskF, in_=maskF, pattern=[[1, 128]], compare_op=ALU.is_ge, fill=0.0, base=0, channel_multiplier=-1),
         reads=["maskF"], writes=["maskF"])
    P.op("pool", lambda e: e.memset(maskF[0:64, 64:128], 0.0), reads=["maskF"], writes=["maskF"])
    P.op("pool", lambda e: e.memset(maskB, 1.0), writes=["maskB"])
    P.op("pool", lambda e: e.affine_select(out=maskB, in_=maskB, pattern=[[-1, 128]], compare_op=ALU.is_ge, fill=0.0, base=0, channel_multiplier=1),
         reads=["maskB"], writes=["maskB"])
    P.op("pool", lambda e: e.memset(maskB[64:128, 0:64], 0.0), reads=["maskB"], writes=["maskB"])
    masks = [maskF, maskB]
    P.dma("sp", lbt, lbv_d.rearrange("p (a b c) -> p a b c", a=2, b=2), "ld_c", writes=["lbt"])
    P.dma("sp", hng, hng_d, "ld_c", writes=["hng"])
    P.op("dve", lambda e: e.tensor_tensor(out=lbd, in0=lbt[:, :, 0, :], in1=lbt[:, :, 1, :], op=ALU.subtract), reads=["lbt"], writes=["lbd"])
    P.op("act", lambda e: e.activation(out=lb, in_=lbd, func=AF.Sigmoid), reads=["lbd"], writes=["lb"])
    P.op("dve", lambda e: e.tensor_scalar(out=oml, in0=lb, scalar1=-1.0, scalar2=1.0, op0=ALU.mult, op1=ALU.add), reads=["lb"], writes=["oml"])

    win_v = win_d.rearrange("(kt p) (s n) -> p kt s n", p=128, n=128)
    BLK5 = [(0, 512), (512, 512), (1024, 512), (1536, 512), (2048, 256)]
    PB = 6
    pbc = [0]

    def proj_fm(wh, whk, sidx, c0, n, evac):
        bk = PB + (pbc[0] % 2)
        pbc[0] += 1

        def mm(e):
            for kt in range(8):
                r = e.matmul(banks[bk][:, 0:n], lhsT=wh[:, kt, sidx, :], rhs=uT[:, kt, c0:c0 + n], start=(kt == 0), stop=(kt == 7))
            return r
        P.op("pe", mm, reads=[whk] + ["uT_%d" % t for t in range(c0 // 128, (c0 + n) // 128)], writes=["bank%d" % bk])
        evac(banks[bk][:, 0:n], "bank%d" % bk)

    def hgrn_dir_prep(h, d):
        wh = whb[h % 2]
        whk = "wh0"
        dk = "d%d" % d
        for (c0, n) in BLK5:
            proj_fm(wh, whk, 2 + d, c0, n, lambda bap, bkey, c0=c0, n=n: P.op(
                "act", lambda e: e.activation(out=fa[:, c0:c0 + n], in_=bap, func=AF.Sigmoid), reads=[bkey], writes=["fa"]))
        fak = ["fa"]
        P.op("dve", lambda e: e.tensor_scalar(out=fa, in0=fa, scalar1=oml[:, d, h:h + 1], scalar2=lb[:, d, h:h + 1], op0=ALU.mult, op1=ALU.add),
             reads=fak + ["oml", "lb"], writes=["fa"])
        P.op("act", lambda e: e.activation(out=lf, in_=fa, func=AF.Ln), reads=["fa"], writes=["lf"])
        P.op("dve", lambda e: e.tensor_scalar(out=kk, in0=fa, scalar1=-1.0, scalar2=1.0, op0=ALU.mult, op1=ALU.add), reads=["fa"], writes=["kk"])
        P.op("dve", lambda e: e.tensor_tensor_scan(out=bb, data0=rst, data1=lf, initial=0.0, op0=ALU.mult, op1=ALU.add), reads=["rst", "lf"], writes=["bb"])
        b3 = bb.rearrange("p (c t) -> p c t", t=64)
        P.op("dve", lambda e: e.tensor_copy(out=tot[d], in_=b3[:, :, 63]), reads=["bb"], writes=["tot" + dk])
        P.op("act", lambda e: e.activation(out=etot[d], in_=tot[d], func=AF.Exp), reads=["tot" + dk], writes=["etot" + dk])
        x3 = xx.rearrange("p (c t) -> p c t", t=64)
        P.op("dve", lambda e: e.tensor_tensor(out=x3, in0=bc(tot[d], 2, [128, 36, 64]), in1=b3, op=ALU.subtract), reads=["tot" + dk, "bb"], writes=["xx"])
        if d == 0:
            bu, dd = bb, xx
            bk_, ddk = "bb", "xx"
        else:
            P.op("dve", lambda e: e.tensor_tensor(out=xx, in0=xx, in1=lf, op=ALU.add), reads=["xx", "lf"], writes=["xx"])
            P.op("dve", lambda e: e.tensor_tensor(out=bb, in0=bb, in1=lf, op=ALU.subtract), reads=["bb", "lf"], writes=["bb"])
            bu, dd = xx, bb
            bk_, ddk = "xx", "bb"
        P.op("act", lambda e: e.activation(out=ee[:, 0:NT], in_=bu[:, 0:NT], func=AF.Exp), reads=[bk_], writes=["ee"])
        P.op("dve", lambda e: e.tensor_tensor(out=qe[d], in0=qT, in1=ee[:, 0:NT], op=ALU.mult), reads=["ee", "qT"], writes=["qe" + dk])
        P.op("act", lambda e: e.activation(out=ee[:, 0:NT], in_=bu[:, 0:NT], func=AF.Exp, scale=-1.0), reads=[bk_, "ee"], writes=["ee"])
        P.op("dve", lambda e: e.tensor_tensor(out=ke[d], in0=kk[:, 0:NT], in1=ee[:, 0:NT], op=ALU.mult), reads=["ee", "kk"], writes=["ke" + dk])
        P.op("act", lambda e: e.activation(out=ee, in_=dd, func=AF.Exp), reads=[ddk, "ee"], writes=["ee"])
        P.op("dve", lambda e: e.tensor_tensor(out=kd[d], in0=kk, in1=ee, op=ALU.mult), reads=["ee", "kk"], writes=["kd" + dk])
        P.op("dve", lambda e: e.tensor_tensor_scan(out=ipf[d], data0=ones_f[:, 0:32], data1=tot[d][:, 0:32], initial=0.0, op0=ALU.mult, op1=ALU.add),
             reads=["tot" + dk, "ones_f"], writes=["ipf" + dk])
        if d == 0:
            P.op("dve", lambda e: e.tensor_tensor(out=gg[d], in0=ipf[d], in1=tot[d][:, 0:32], op=ALU.subtract), reads=["ipf" + dk, "tot" + dk], writes=["gg" + dk])
        else:
            P.op("dve", lambda e: e.tensor_tensor(out=gg[d], in0=ipf[d][:, 31:32].to_broadcast([128, 32]), in1=ipf[d], op=ALU.subtract),
                 reads=["ipf" + dk], writes=["gg" + dk])
        P.op("act", lambda e: e.activation(out=eg[d], in_=gg[d], func=AF.Exp), reads=["gg" + dk], writes=["eg" + dk])
        P.op("act", lambda e: e.activation(out=Dv[:, d, h:h + 1], in_=ipf[d][:, 31:32], func=AF.Exp), reads=["ipf" + dk], writes=["Dv_%d_%d" % (d, h)])
        P.op("dve", lambda e: e.tensor_tensor(out=qB[d].rearrange("p (c t) -> p c t", t=64), in0=qe[d].rearrange("p (c t) -> p c t", t=64),
                                               in1=bc(eg[d], 2, [128, 32, 64]), op=ALU.mult), reads=["qe" + dk, "eg" + dk], writes=["qB" + dk])
        P.dma("sp", dm_qb[d, h], qB[d], "st_qb", reads=["qB" + dk], writes=["dm_qb"])
        for g3 in range(3):
            bk = PB + (pbc[0] % 2)
            pbc[0] += 1

            def trd(e, g3=g3, bk=bk):
                for i in range(6):
                    ti = g3 * 6 + i
                    r = e.transpose(bank_bf(bk)[:, i * 128:(i + 1) * 128], kd[d][:, ti * 128:(ti + 1) * 128], ident_b)
                return r
            P.op("pe", trd, reads=["kd" + dk, "ident_b"], writes=["bank%d" % bk])
            P.op("act", lambda e, g3=g3, bk=bk: e.copy(out=kd_tm[d][:, g3 * 6:(g3 + 1) * 6, :], in_=bank_bf(bk)[:, 0:768].rearrange("p (a b) -> p a b", b=128)),
                 reads=["bank%d" % bk], writes=["kdtm%s_%d" % (dk, g3)])

    def hgrn_dir_scan(h, d):
        dk = "d%d" % d
        kdk = ["kdtm%s_%d" % (dk, g3) for g3 in range(3)]
        bA, bO, bS = 3 * d, 3 * d + 1, 3 * d + 2
        Sc = Scx[d]
        Sbufs, Sbb = Sst[d], Sbf[d]
        slot = [0]

        def delta_mm(c):
            sl = slot[0] % 2
            slot[0] += 1
            ti, half = c // 2, c % 2
            ps = slice(half * 64, half * 64 + 64)
            bidx = (bS, PB + d)[sl]
            bap = banks[bidx][:, 0:128]
            bkey = "bank%d" % bidx
            P.op("pe", lambda e: e.matmul(bap, lhsT=kd_tm[d][ps, ti, :], rhs=v_tm[ps, ti, :], start=True, stop=True),
                 reads=kdk + ["v_tm"], writes=[bkey])
            return bap, bkey
        corder = [32, 33, 34, 35] if d == 0 else [35, 34, 33, 32]
        for i, c in enumerate(corder):
            bap, bkey = delta_mm(c)
            if i == 0:
                P.op("dve", lambda e: e.tensor_copy(out=Sc, in_=bap), reads=[bkey], writes=["Sc" + dk])
            else:
                P.op("dve", lambda e: e.scalar_tensor_tensor(out=Sc, in0=Sc, scalar=etot[d][:, c:c + 1], in1=bap, op0=ALU.mult, op1=ALU.add),
                     reads=[bkey, "Sc" + dk, "etot" + dk], writes=["Sc" + dk])
            yield
        P.op("dve", lambda e: e.tensor_copy(out=sctx[:, d, h, :], in_=Sc), reads=["Sc" + dk], writes=["sctx_%d_%d" % (d, h)])
        P.op("pool", lambda e: e.memset(Sbb[0], 0.0), reads=["Sb%s_0" % dk], writes=["Sb%s_0" % dk])
        si, bi, nstate = 0, 0, 0
        groups = [0, 1, 2, 3] if d == 0 else [3, 2, 1, 0]
        for g in groups:
            def att(e, g=g):
                for pi in range(4):
                    p = g * 4 + pi
                    r = e.matmul(banks[bA][:, pi * 128:(pi + 1) * 128], lhsT=ke[d][:, p * 128:(p + 1) * 128], rhs=qe[d][:, p * 128:(p + 1) * 128], start=True, stop=True)
                return r
            P.op("pe", att, reads=["ke" + dk, "qe" + dk], writes=["bank%d" % bA])
            P.op("dve", lambda e: e.tensor_tensor(out=attm[d], in0=banks[bA][:, :].rearrange("p (a b) -> p a b", b=128), in1=bc(masks[d], 1, [128, 4, 128]), op=ALU.mult),
                 reads=["bank%d" % bA, "mask"], writes=["attm" + dk])
            pis = [0, 1, 2, 3] if d == 0 else [3, 2, 1, 0]
            for pi in pis:
                p = g * 4 + pi
                P.op("pe", lambda e, pi=pi, p=p: e.matmul(banks[bO][:, pi * 128:(pi + 1) * 128], lhsT=v_tm[:, p, :], rhs=attm[d][:, pi, :], start=True, stop=False),
                     reads=["v_tm", "attm" + dk], writes=["bank%d" % bO])
                chunks = [2 * p, 2 * p + 1] if d == 0 else [2 * p + 1, 2 * p]
                for ci, c in enumerate(chunks):
                    col = pi * 128 + (c % 2) * 64
                    sbc = Sbb[bi]
                    P.op("pe", lambda e, c=c, col=col, ci=ci, sbc=sbc: e.matmul(banks[bO][:, col:col + 64], lhsT=sbc, rhs=qe[d][:, c * 64:(c + 1) * 64], start=False, stop=(ci == 1)),
                         reads=["Sb%s_%d" % (dk, bi), "qe" + dk], writes=["bank%d" % bO])
                    bap, bkey = delta_mm(c)
                    nsi = (si + 1) % 2
                    So, Sn = Sbufs[si], Sbufs[nsi]
                    if nstate == 0:
                        P.op("dve", lambda e, Sn=Sn, bap=bap: e.tensor_copy(out=Sn, in_=bap), reads=[bkey], writes=["S%s_%d" % (dk, nsi)])
                    else:
                        P.op("dve", lambda e, So=So, Sn=Sn, bap=bap, c=c: e.scalar_tensor_tensor(out=Sn, in0=So, scalar=etot[d][:, c:c + 1], in1=bap, op0=ALU.mult, op1=ALU.add),
                             reads=[bkey, "S%s_%d" % (dk, si), "etot" + dk], writes=["S%s_%d" % (dk, nsi)])
                    si = nsi
                    nstate += 1
                    nbi = (bi + 1) % 3
                    sbn = Sbb[nbi]
                    P.op("act", lambda e, Sn=Sn, sbn=sbn: e.copy(out=sbn, in_=Sn), reads=["S%s_%d" % (dk, si)], writes=["Sb%s_%d" % (dk, nbi)])
                    bi = nbi
                    yield
            cs = slice(g * 512, (g + 1) * 512)
            if (d == 0 and g < 2) or (d == 1 and g >= 2):
                P.op("act", lambda e, cs=cs: e.copy(out=o_acc[:, cs], in_=banks[bO][:, :]), reads=["bank%d" % bO, "oacc_%d" % g], writes=["oacc_%d" % g])
            else:
                P.op("dve", lambda e, cs=cs: e.tensor_tensor(out=o_acc[:, cs], in0=o_acc[:, cs], in1=banks[bO][:, :], op=ALU.add),
                     reads=["bank%d" % bO, "oacc_%d" % g], writes=["oacc_%d" % g])
        Sl = Sbufs[si]
        P.op("dve", lambda e, Sl=Sl: e.tensor_copy(out=stage[:, d, h, :], in_=Sl), reads=["S%s_%d" % (dk, si)], writes=["stage_%d_%d" % (d, h)])

    P.buf["mask"] = {"w": ("e", "pool", P.cnt["pool"]), "r": {}}
    for h in range(8):
        wh = whb[h % 2]
        whk = "wh0"
        for s5 in range(5):
            P.dma("pool", wh[:, :, s5, :], win_v[:, :, h + 8 * s5, :], "ld_" + whk, writes=[whk])
        for blk in range(4):
            proj_fm(wh, whk, 0, blk * 512, 512, lambda bap, bkey, blk=blk: P.op(
                "act", lambda e: e.copy(out=qT[:, blk * 512:(blk + 1) * 512], in_=bap), reads=[bkey, "qT"], writes=["qT"]))
        for blk in range(4):
            proj_fm(wh, whk, 4, blk * 512, 512, lambda bap, bkey, blk=blk: P.op(
                "act", lambda e: e.activation(out=gtmp[:, blk * 512:(blk + 1) * 512], in_=bap, func=AF.Silu), reads=[bkey, "ee"], writes=["ee"]))
        P.op("dve", lambda e: e.tensor_scalar(out=gsT, in0=gtmp, scalar1=hng[:, 0:1], scalar2=None, op0=ALU.mult), reads=["ee", "hng"], writes=["gsT"])
        P.dma("sp", dm_gs[h], gsT, "st_gs", reads=["gsT"], writes=["dm_gs"])
        for g4 in range(5):
            tiles = list(range(g4 * 4, min(18, g4 * 4 + 4)))
            bk = PB + (pbc[0] % 2)
            pbc[0] += 1

            def mmv(e, tiles=tiles, bk=bk, wh=wh):
                for i, ti in enumerate(tiles):
                    for kt in range(8):
                        r = e.matmul(banks[bk][:, i * 128:(i + 1) * 128], lhsT=uT[:, kt, ti * 128:(ti + 1) * 128], rhs=wh[:, kt, 1, :], start=(kt == 0), stop=(kt == 7))
                return r
            P.op("pe", mmv, reads=[whk] + ["uT_%d" % t for t in tiles], writes=["bank%d" % bk])
            nt_ = len(tiles)
            P.op("act", lambda e, tiles=tiles, bk=bk, nt_=nt_: e.copy(out=v_tm[:, tiles[0]:tiles[0] + nt_, :], in_=banks[bk][:, 0:nt_ * 128].rearrange("p (a b) -> p a b", b=128)),
                 reads=["bank%d" % bk, "v_tm"], writes=["v_tm"])
        hgrn_dir_prep(h, 0)
        hgrn_dir_prep(h, 1)
        gens = [hgrn_dir_scan(h, 0), hgrn_dir_scan(h, 1)]
        alive = [True, True]
        while any(alive):
            for i in range(2):
                if alive[i]:
                    try:
                        next(gens[i])
                    except StopIteration:
                        alive[i] = False
        P.dma("sp", dm_oloc[h], o_acc, "st_oloc", reads=["oacc_%d" % g for g in range(4)], writes=["dm_oloc"])
    for d in range(2):
        P.dma("sp", ag2_ins[d][:, 0:1024], stage[:, d, :, :].rearrange("p b c -> p (b c)"), "st_ag2", reads=["stage_%d_%d" % (d, h) for h in range(8)], writes=["ag2_in"])
        P.dma("sp", ag2_ins[d][:, 1024:1032], Dv[:, d, :], "st_ag2", reads=["Dv_%d_%d" % (d, h) for h in range(8)], writes=["ag2_in"])
    for d in range(2):
        P.custom("pool", lambda e, d=d: e.collective_compute("AllGather", ALU.bypass, replica_groups=[[0, 1, 2, 3], [4, 5, 6, 7]], ins=[ag2_ins[d]], outs=[ag2_outs[d]]),
                 "cc2", 1, reads=["ag2_in"] + (["ag2_out"] if d > 0 else []), writes=["ag2_out"])
    P.barrier(keep=["ag1_out", "dm_kvctx", "dm_mod", "ag2_out", "dm_oloc", "dm_qb", "dm_gs"] + UT_KEYS)
    A.release(m3h)
    gath = A.alloc((2, 4, 1032), F32)
    Rr = A.alloc((2, 8, 128), F32)
    Tt = A.alloc((8, 128), F32)
    selv = A.alloc((8,), F32)
    Sin = A.alloc((2, 8, 128), BF16)
    for d in range(2):
        P.dma("sp", gath[:, d, :, :], ag2_outs[d].rearrange("(r p) n -> p r n", p=128), "ld_gath", reads=["ag2_out"], writes=["gath"])
    P.dma("sp", selv, sel_d, "ld_c", writes=["selv"])
    SCK = ["sctx_%d_%d" % (d, h) for d in range(2) for h in range(8)]
    P.op("dve", lambda e: e.tensor_copy(out=Rr.rearrange("p a b c -> p (a b c)"), in_=sctx.rearrange("p a b c -> p (a b c)")), writes=["Rr"])
    for d in range(2):
        order = [0, 1, 2, 3] if d == 0 else [3, 2, 1, 0]
        for r in order:
            Sl = gath[:, d, r, 0:1024].rearrange("p (h v) -> p h v", v=128)
            Dr = gath[:, d, r, 1024:1032]
            P.op("dve", lambda e, d=d, Dr=Dr: e.tensor_tensor(out=Tt, in0=Rr[:, d, :, :], in1=bc(Dr, 2, [128, 8, 128]), op=ALU.mult), reads=["Rr", "gath"], writes=["Tt"])
            P.op("dve", lambda e, Sl=Sl: e.tensor_tensor(out=Tt, in0=Tt, in1=Sl, op=ALU.add), reads=["Tt", "gath"], writes=["Tt"])
            P.op("dve", lambda e, d=d: e.tensor_tensor(out=Tt, in0=Tt, in1=Rr[:, d, :, :], op=ALU.subtract), reads=["Tt", "Rr"], writes=["Tt"])
            P.op("dve", lambda e, d=d, r=r: e.scalar_tensor_tensor(out=Rr[:, d, :, :], in0=Tt, scalar=selv[:, d * 4 + r:d * 4 + r + 1], in1=Rr[:, d, :, :], op0=ALU.mult, op1=ALU.add),
                 reads=["Tt", "Rr", "selv"], writes=["Rr"])
    P.op("act", lambda e: e.copy(out=Sin.rearrange("p a b c -> p (a b c)"), in_=Rr.rearrange("p a b c -> p (a b c)")), reads=["Rr"], writes=["Sin"])
    ol = A.alloc((NT,), F32)
    qbf_ = A.alloc((NT,), BF16)
    qbb_ = A.alloc((NT,), BF16)
    gsl = A.alloc((NT,), BF16)
    sqb = A.alloc((NT,), BF16)
    lnr = A.alloc((NT,), F32)
    rsr = A.alloc((NT,), F32)
    for h in range(8):
        P.dma("sp", ol, dm_oloc[h], "ld_ol", reads=["dm_oloc"], writes=["ol"] + ["ol_%d" % b_ for b_ in range(4)])
        P.dma("sp", qbf_, dm_qb[0, h], "ld_qb0", reads=["dm_qb"], writes=["qbf_"])
        P.dma("sp", qbb_, dm_qb[1, h], "ld_qb1", reads=["dm_qb"], writes=["qbb_"])
        P.dma("sp", gsl, dm_gs[h], "ld_gs", reads=["dm_gs"], writes=["gsl"])
        for blk in range(4):
            cs = slice(blk * 512, (blk + 1) * 512)
            bk = blk % 2

            def corr(e, h=h, cs=cs, bk=bk):
                e.matmul(banks[bk][:, :], lhsT=Sin[:, 0, h, :], rhs=qbf_[:, cs], start=True, stop=False)
                return e.matmul(banks[bk][:, :], lhsT=Sin[:, 1, h, :], rhs=qbb_[:, cs], start=False, stop=True)
            P.op("pe", corr, reads=["Sin", "qbf_", "qbb_"], writes=["bank%d" % bk])
            P.op("dve", lambda e, cs=cs, bk=bk: e.tensor_tensor(out=ol[:, cs], in0=ol[:, cs], in1=banks[bk][:, :], op=ALU.add), reads=["bank%d" % bk, "ol", "ol_%d" % blk], writes=["ol_%d" % blk])
            P.op("act", lambda e, cs=cs: e.activation(out=sqb[:, cs], in_=ol[:, cs], func=AF.Square), reads=["ol_%d" % blk], writes=["sqb_%d" % blk])
            bk2 = 2 + blk % 2
            P.op("pe", lambda e, cs=cs, bk2=bk2: e.matmul(banks[bk2][:, :], lhsT=ones_b, rhs=sqb[:, cs], start=True, stop=True), reads=["sqb_%d" % blk, "ones_b"], writes=["bank%d" % bk2])
            P.op("act", lambda e, cs=cs, bk2=bk2: e.activation(out=lnr[:, cs], in_=banks[bk2][:, :], func=AF.Ln, scale=1.0 / 128, bias=EPS), reads=["bank%d" % bk2], writes=["lnr_%d" % blk])
            P.op("act", lambda e, cs=cs: e.activation(out=rsr[:, cs], in_=lnr[:, cs], func=AF.Exp, scale=-0.5), reads=["lnr_%d" % blk], writes=["rsr_%d" % blk])
            P.op("dve", lambda e, cs=cs: e.tensor_tensor(out=ol[:, cs], in0=ol[:, cs], in1=rsr[:, cs], op=ALU.mult), reads=["ol_%d" % blk, "rsr_%d" % blk], writes=["ol_%d" % blk])
            P.op("dve", lambda e, cs=cs: e.tensor_tensor(out=sqb[:, cs], in0=ol[:, cs], in1=gsl[:, cs], op=ALU.mult), reads=["ol_%d" % blk, "gsl"], writes=["sqb_%d" % blk])
        P.dma("sp", dm_oth[h], sqb, "st_oth", reads=["sqb_%d" % b_ for b_ in range(4)], writes=["dm_oth"])
        for b_ in range(4):
            for nm in ("ol_%d", "sqb_%d", "lnr_%d", "rsr_%d", "oth_%d"):
                pass
    P.barrier(keep=["ag1_out", "dm_kvctx", "dm_mod", "dm_oth"] + UT_KEYS)
    A.release(m3)
    if upto <= 3:
        return finish(P, nc)


    m4 = A.mark()
    QT = A.alloc((8, NT), BF16)
    KTa = A.alloc((2, 8448), BF16)
    Va = A.alloc((66, 256), BF16)
    for r in range(4):
        for h in range(2):
            P.dma("sp", KTa[:, h, r * NT:(r + 1) * NT], ag1_outs[h][r * 128:(r + 1) * 128, :], "ld_kta", reads=["ag1_out"], writes=["KTa"])
            P.dma("sp", Va[:, r * 16 + 8 * h:r * 16 + 8 * h + 8, :], ag1_outs[2 + h][r * 128:(r + 1) * 128, :].rearrange("p (ti c) -> p ti c", c=256), "ld_va", reads=["ag1_out"], writes=["Va"])
    P.dma("sp", KTa[:, :, 8192:8448], dm_kvctx[0:256, :].rearrange("(h p) t -> p h t", p=128), "ld_kta", reads=["dm_kvctx"], writes=["KTa"])
    P.dma("sp", Va[:, 64:66, :], dm_kvctx[256:512, :].rearrange("(ti p) c -> p ti c", p=128), "ld_va", reads=["dm_kvctx"], writes=["Va"])
    m4b = A.mark()
    wq = A.alloc((8, 1024), BF16)
    P.dma("pool", wq, win_d[:, 5120:6144].rearrange("(kt p) n -> p kt n", p=128), "ld_wq", writes=["wq"])
    ropeT = A.alloc((NTT, 256), F32)
    P.dma("sp", ropeT, rope_d.rearrange("(t p) n -> p t n", p=128), "ld_rope", writes=["ropeT"])
    gqk = A.alloc((256,), F32)
    P.dma("sp", gqk, qkg_d[0].partition_broadcast(128), "ld_c", writes=["gqk"])
    ssq = A.alloc((16, 8), F32)
    lnq = A.alloc((16, 8), F32)
    rsq = A.alloc((16, 8), F32)
    qn = A.alloc((8, 128), F32)
    t1q = A.alloc((8, 128), F32)
    t2q = A.alloc((8, 128), F32)
    qbf = A.alloc((1024,), BF16)
    junk2 = A.alloc((128,), BF16)
    P.op("pool", lambda e: e.memset(ssq, 0.0), writes=["ssq"])
    for ti in range(NTT):
        s = ti % 2
        for half in range(2):
            def mmq(e, ti=ti, half=half):
                for kt in range(8):
                    r = e.matmul(banks[half][:, :], lhsT=uT[:, kt, ti * 128:(ti + 1) * 128], rhs=wq[:, kt, half * 512:(half + 1) * 512], start=(kt == 0), stop=(kt == 7))
                return r
            P.op("pe", mmq, reads=["uT_%d" % ti, "wq"], writes=["bank%d" % half])
        for h in range(8):
            P.op("act", lambda e, h=h, ti=ti: e.activation(out=junk2, in_=banks[h // 4][:, (h % 4) * 128:(h % 4 + 1) * 128], func=AF.Square, accum_out=ssq[:, ti, h:h + 1]),
                 reads=["bank%d" % (h // 4), "ssq"], writes=["junk2", "ssq_%d" % ti])
        rstd_from_ss(ssq[:, ti, :], 128, rsq[:, ti, :], lnq[:, ti, :], ["ssq_%d" % ti], "rsq_%d" % ti)
        for half in range(2):
            P.op("dve", lambda e, half=half, ti=ti: e.tensor_tensor(out=qn[:, half * 4:(half + 1) * 4, :], in0=banks[half][:, :].rearrange("p (h d) -> p h d", d=128),
                                                                in1=bc(rsq[:, ti, half * 4:(half + 1) * 4], 2, [128, 4, 128]), op=ALU.mult),
                 reads=["bank%d" % half, "rsq_%d" % ti, "qxn"], writes=["qxn"])
        P.op("dve", lambda e: e.tensor_tensor(out=qn, in0=qn, in1=bc(gqk[:, 0:128], 1, [128, 8, 128]), op=ALU.mult), reads=["qxn", "gqk"], writes=["qxn"])
        rope_apply(qn, 8, ti, qbf, "q")
        bk2 = 2 + s

        def trq(e, bk2=bk2):
            for h in range(8):
                r = e.transpose(bank_bf(bk2)[:, h * 128:(h + 1) * 128], qbf[:, h * 128:(h + 1) * 128], ident_b)
            return r
        P.op("pe", trq, reads=["qbf", "ident_b"], writes=["bank%d" % bk2])
        P.op("act", lambda e, ti=ti, bk2=bk2: e.copy(out=QT[:, :, ti * 128:(ti + 1) * 128], in_=bank_bf(bk2).rearrange("p (a b) -> p a b", b=128)),
             reads=["bank%d" % bk2], writes=["QT"])
    P.barrier(keep=["dm_mod", "dm_oth", "KTa", "Va"] + UT_KEYS)
    A.release(m4b)
    if upto <= 4:
        return finish(P, nc)

    pT = [A.alloc((512,), BF16) for _ in range(3)]
    rden = A.alloc((512,), F32)
    ob = [A.alloc((512,), BF16) for _ in range(2)]
    dacc = [A.alloc((512,), F32) for _ in range(2)]
    SCALE = float(128 ** -0.5)
    NKT = 66
    it = 0
    for kvh in range(2):
        for qb in range(4):
            for g in range(4):
                head = kvh * 4 + g
                bo, bd = 4 + it % 2, 6 + it % 2
                qs = slice(qb * 512, (qb + 1) * 512)

                def s_mm(kt, kvh=kvh, head=head, qs=qs):
                    P.op("pe", lambda e: e.matmul(banks[kt % 3][:, :], lhsT=KTa[:, kvh, kt * 128:(kt + 1) * 128], rhs=QT[:, head, qs], start=True, stop=True),
                         reads=["KTa", "QT"], writes=["bank%d" % (kt % 3)])
                s_mm(0)
                for kt in range(NKT):
                    if kt + 1 < NKT:
                        s_mm(kt + 1)
                    P.op("act", lambda e, kt=kt: e.activation(out=pT[kt % 3], in_=banks[kt % 3][:, :], func=AF.Exp, scale=SCALE),
                         reads=["bank%d" % (kt % 3)], writes=["pT%d" % (kt % 3)])

                    P.op("pe", lambda e, kt=kt, kvh=kvh, bo=bo: e.matmul(banks[bo][:, :], lhsT=Va[:, kt, kvh * 128:(kvh + 1) * 128], rhs=pT[kt % 3], start=(kt == 0), stop=(kt == NKT - 1)),
                         reads=["pT%d" % (kt % 3), "Va"], writes=["bank%d" % bo])
                    da = dacc[0]
                    if kt % 3 == 2:
                        P.op("pe", lambda e, kt=kt, bd=bd: e.matmul(banks[bd][:, :], lhsT=ones_b, rhs=pT[kt % 3], start=(kt == 2), stop=False),
                             reads=["pT%d" % (kt % 3), "ones_b"], writes=["bank%d" % bd])
                    elif kt == 0:
                        P.op("dve", lambda e, kt=kt, da=da: e.tensor_copy(out=da, in_=pT[kt % 3]), reads=["pT%d" % (kt % 3)], writes=["dacc0"])
                    else:
                        P.op("dve", lambda e, kt=kt, da=da: e.tensor_tensor(out=da, in0=da, in1=pT[kt % 3], op=ALU.add), reads=["pT%d" % (kt % 3), "dacc0"], writes=["dacc0"])

                def dsum(e, bd=bd):
                    return e.matmul(banks[bd][:, :], lhsT=ones_f, rhs=dacc[0], start=False, stop=True)
                P.op("pe", dsum, reads=["dacc0", "ones_f"], writes=["bank%d" % bd])
                P.op("dve", lambda e, bd=bd: e.reciprocal(out=rden, in_=banks[bd][:, :]), reads=["bank%d" % bd], writes=["rden"])
                P.op("dve", lambda e, bo=bo, it=it: e.tensor_tensor(out=ob[it % 2], in0=banks[bo][:, :], in1=rden, op=ALU.mult), reads=["bank%d" % bo, "rden"], writes=["ob%d" % (it % 2)])
                P.dma("sp", dm_ota[head][:, qs], ob[it % 2], "st_ota%d" % (it % 2), reads=["ob%d" % (it % 2)], writes=["dm_ota"])
                it += 1
    P.barrier(keep=["dm_mod", "dm_oth", "dm_ota"] + UT_KEYS)
    A.release(m4)
    if upto <= 5:
        return finish(P, nc)

    m6 = A.mark()
    wg = A.alloc((8, 2048), BF16)
    wb0 = A.alloc((8, 1024), BF16)
    wb1 = A.alloc((8, 1024), BF16)
    wo = A.alloc((8, 1024), BF16)
    P.dma("pool", wg, win_d[:, 6656:8704].rearrange("(kt p) n -> p kt n", p=128), "ld_w6", writes=["wg"])
    P.dma("pool", wb0, wbr_d[0].rearrange("(kt p) n -> p kt n", p=128), "ld_w6", writes=["wb0"])
    P.dma("pool", wb1, wbr_d[1].rearrange("(kt p) n -> p kt n", p=128), "ld_w6", writes=["wb1"])
    P.dma("pool", wo, wout_d.rearrange("(kt p) n -> p kt n", p=128), "ld_w6", writes=["wo"])
    G1 = A.alloc((D,), F32)
    A2 = A.alloc((D,), F32)
    B2 = A.alloc((D,), F32)
    P.dma("sp", G1, dm_mod[:, 2 * D:3 * D], "ld_c", reads=["dm_mod"], writes=["G1"])
    P.dma("sp", A2, dm_mod[:, 4 * D:5 * D], "ld_c", reads=["dm_mod"], writes=["A2"])
    P.dma("sp", B2, dm_mod[:, 3 * D:4 * D], "ld_c", reads=["dm_mod"], writes=["B2"])
    rwt = A.alloc((8, NE), F32)
    rbt = A.alloc((NE,), F32)
    P.dma("sp", rwt, rw_d.rearrange("(kt p) e -> p kt e", p=128), "ld_c", writes=["rwt"])
    P.dma("sp", rbt, rb_d[0].partition_broadcast(128), "ld_c", writes=["rbt"])
    othb = A.alloc((8, 512), BF16)
    otab = A.alloc((8, 512), BF16)
    y1T = A.alloc((8, 512), BF16)
    sgh = [A.alloc((512,), F32)] * 2
    sga = [A.alloc((512,), F32)] * 2
    tA = [A.alloc((512,), F32)] * 2
    tB = [A.alloc((512,), F32)] * 2
    xt6 = [A.alloc((D,), F32) for _ in range(2)]
    tmp6 = A.alloc((D,), F32)
    x1t = [A.alloc((D,), F32)] * 2
    u2f = A.alloc((D,), F32)
    u2b = A.alloc((D,), BF16)
    junk6 = A.alloc((D,), BF16)
    ssy = A.alloc((16, 2), F32)
    ssy1 = A.alloc((16,), F32)
    lny = A.alloc((16,), F32)
    rsy = A.alloc((16,), F32)
    ssx = A.alloc((16,), F32)
    lnx = A.alloc((16,), F32)
    rsx = A.alloc((16,), F32)
    u2Tf = A.alloc((8, 128), F32)
    u2Tb = [A.alloc((8, 128), BF16) for _ in range(2)]
    lg = A.alloc((NE,), F32)
    mx8 = A.alloc((8,), F32)
    msk = A.alloc((NE,), F32)
    em = A.alloc((NE,), F32)
    nmx = A.alloc((1,), F32)
    ssum = A.alloc((1,), F32)
    rsum = A.alloc((1,), F32)
    cmb = A.alloc((16, NE), F32)
    cT = A.alloc((128,), F32)
    P.op("pool", lambda e: e.memset(ssy, 0.0), writes=["ssy"])
    P.op("pool", lambda e: e.memset(ssx, 0.0), writes=["ssx"])
    oth_v = dm_oth.rearrange("h p t -> p h t")
    ota_v = dm_ota.rearrange("h p t -> p h t")
    for blk in range(4):
        cs = slice(blk * 512, (blk + 1) * 512)
        P.dma("sp", othb, oth_v[:, :, cs], "ld_oth", reads=["dm_oth"], writes=["othb"])
        P.dma("sp", otab, ota_v[:, :, cs], "ld_ota", reads=["dm_ota"], writes=["otab"])
        utk = ["uT_%d" % t for t in range(blk * 4, blk * 4 + 4)]
        for dt in range(8):
            s = 0
            ds = slice(dt * 128, (dt + 1) * 128)

            def mm4(e, ds=ds, dt=dt, cs=cs):
                for kt in range(8):
                    e.matmul(banks[0][:, :], lhsT=wg[:, kt, dt * 128:(dt + 1) * 128], rhs=uT[:, kt, cs], start=(kt == 0), stop=(kt == 7))
                for kt in range(8):
                    e.matmul(banks[1][:, :], lhsT=wg[:, kt, 1024 + dt * 128:1024 + (dt + 1) * 128], rhs=uT[:, kt, cs], start=(kt == 0), stop=(kt == 7))
                for kt in range(8):
                    e.matmul(banks[2][:, :], lhsT=wb0[:, kt, ds], rhs=othb[:, kt, :], start=(kt == 0), stop=(kt == 7))
                for kt in range(8):
                    r = e.matmul(banks[3][:, :], lhsT=wb1[:, kt, ds], rhs=otab[:, kt, :], start=(kt == 0), stop=(kt == 7))
                return r
            P.op("pe", mm4, reads=["wg", "wb0", "wb1", "othb", "otab"] + utk, writes=["bank0", "bank1", "bank2", "bank3"])
            P.op("act", lambda e, s=s: e.activation(out=sgh[s], in_=banks[0][:, :], func=AF.Sigmoid), reads=["bank0"], writes=["sgh%d" % s])
            P.op("act", lambda e, s=s: e.activation(out=sga[s], in_=banks[1][:, :], func=AF.Sigmoid), reads=["bank1"], writes=["sga%d" % s])
            P.op("dve", lambda e, s=s: e.tensor_tensor(out=tA[s], in0=sgh[s], in1=banks[2][:, :], op=ALU.mult), reads=["sgh%d" % s, "bank2"], writes=["tA%d" % s])
            P.op("dve", lambda e, s=s: e.tensor_tensor(out=tB[s], in0=sga[s], in1=banks[3][:, :], op=ALU.mult), reads=["sga%d" % s, "bank3"], writes=["tB%d" % s])
            P.op("dve", lambda e, s=s, dt=dt: e.tensor_tensor(out=y1T[:, dt, :], in0=tA[s], in1=tB[s], op=ALU.add), reads=["tA%d" % s, "tB%d" % s], writes=["y1T"])
        for tt in range(4):
            ti = blk * 4 + tt
            s = ti % 2
            ts_ = slice(tt * 128, (tt + 1) * 128)
            P.dma("sp", xt6[s], x_d[ti * 128:(ti + 1) * 128, :], "ld_x6%d" % s, writes=["xt6%d" % s])
            for half in range(2):
                def mmy(e, half=half, ts_=ts_):
                    for kt in range(8):
                        r = e.matmul(banks[4 + half][:, :], lhsT=y1T[:, kt, ts_], rhs=wo[:, kt, half * 512:(half + 1) * 512], start=(kt == 0), stop=(kt == 7))
                    return r
                P.op("pe", mmy, reads=["y1T", "wo"], writes=["bank%d" % (4 + half)])
                P.op("act", lambda e, half=half, ti=ti: e.activation(out=junk6[:, 0:512], in_=banks[4 + half][:, :], func=AF.Square, accum_out=ssy[:, ti, half:half + 1]),
                     reads=["bank%d" % (4 + half), "ssy"], writes=["junk6", "ssy_%d_%d" % (ti, half)])
            P.op("dve", lambda e, ti=ti: e.tensor_tensor(out=ssy1[:, ti:ti + 1], in0=ssy[:, ti, 0:1], in1=ssy[:, ti, 1:2], op=ALU.add),
                 reads=["ssy_%d_0" % ti, "ssy_%d_1" % ti], writes=["ssy1_%d" % ti])
            rstd_from_ss(ssy1[:, ti:ti + 1], D, rsy[:, ti:ti + 1], lny[:, ti:ti + 1], ["ssy1_%d" % ti], "rsy_%d" % ti)
            for half in range(2):
                hs = slice(half * 512, (half + 1) * 512)
                P.op("dve", lambda e, half=half, hs=hs, ti=ti: e.scalar_tensor_tensor(out=tmp6[:, hs], in0=banks[4 + half][:, :], scalar=rsy[:, ti:ti + 1], in1=G1[:, hs], op0=ALU.mult, op1=ALU.mult),
                     reads=["bank%d" % (4 + half), "rsy_%d" % ti, "G1", "tmp6"], writes=["tmp6"])
            P.op("dve", lambda e, s=s: e.tensor_tensor(out=x1t[s], in0=tmp6, in1=xt6[s], op=ALU.add), reads=["tmp6", "xt6%d" % s], writes=["x1t"])
            P.dma("sp", dm_x1[ti * 128:(ti + 1) * 128, :], x1t[s], "st_x1%d" % s, reads=["x1t"], writes=["dm_x1"])
            P.op("act", lambda e, s=s, ti=ti: e.activation(out=junk6, in_=x1t[s], func=AF.Square, accum_out=ssx[:, ti:ti + 1]), reads=["x1t", "ssx"], writes=["junk6", "ssx_%d" % ti])
            rstd_from_ss(ssx[:, ti:ti + 1], D, rsx[:, ti:ti + 1], lnx[:, ti:ti + 1], ["ssx_%d" % ti], "rsx_%d" % ti)
            P.op("dve", lambda e, s=s, ti=ti: e.scalar_tensor_tensor(out=tmp6, in0=x1t[s], scalar=rsx[:, ti:ti + 1], in1=A2, op0=ALU.mult, op1=ALU.mult),
                 reads=["x1t", "rsx_%d" % ti, "A2", "tmp6"], writes=["tmp6"])
            P.op("dve", lambda e: e.tensor_tensor(out=u2f, in0=tmp6, in1=B2, op=ALU.add), reads=["tmp6", "B2"], writes=["u2f"])
            P.op("act", lambda e: e.copy(out=u2b, in_=u2f), reads=["u2f"], writes=["u2b"])

            def tru(e):
                for kt in range(8):
                    r = e.transpose(bank_bf(6)[:, kt * 128:(kt + 1) * 128], u2b[:, kt * 128:(kt + 1) * 128], ident_b)
                return r
            P.op("pe", tru, reads=["u2b", "ident_b"], writes=["bank6"])
            P.op("act", lambda e, s=s: e.copy(out=u2Tb[s], in_=bank_bf(6).rearrange("p (a b) -> p a b", b=128)), reads=["bank6"], writes=["u2Tb%d" % s])
            P.dma("sp", dm_u2t[:, :, ti * 128:(ti + 1) * 128], u2Tb[s], "st_u2t%d" % s, reads=["u2Tb%d" % s], writes=["dm_u2t"])
            for g2 in range(2):
                def truf(e, g2=g2):
                    for i in range(4):
                        kt = g2 * 4 + i
                        r = e.transpose(banks[7][:, i * 128:(i + 1) * 128], u2f[:, kt * 128:(kt + 1) * 128], ident_f)
                    return r
                P.op("pe", truf, reads=["u2f", "ident_f"], writes=["bank7"])
                P.op("act", lambda e, g2=g2: e.copy(out=u2Tf[:, g2 * 4:(g2 + 1) * 4, :], in_=banks[7][:, :].rearrange("p (a b) -> p a b", b=128)), reads=["bank7", "u2Tf"], writes=["u2Tf"])

            def mml(e):
                for kt in range(8):
                    r = e.matmul(banks[6][:, 0:NE], lhsT=u2Tf[:, kt, :], rhs=rwt[:, kt, :], start=(kt == 0), stop=(kt == 7))
                return r
            P.op("pe", mml, reads=["u2Tf", "rwt"], writes=["bank6"])
            P.op("dve", lambda e: e.tensor_tensor(out=lg, in0=banks[6][:, 0:NE], in1=rbt, op=ALU.add), reads=["bank6", "rbt"], writes=["lg"])
            P.op("dve", lambda e: e.max(out=mx8, in_=lg), reads=["lg"], writes=["mx8"])
            P.op("dve", lambda e: e.tensor_scalar(out=msk, in0=lg, scalar1=mx8[:, 3:4], scalar2=None, op0=ALU.is_ge), reads=["lg", "mx8"], writes=["msk"])
            P.op("dve", lambda e: e.tensor_scalar(out=nmx, in0=mx8[:, 0:1], scalar1=-1.0, scalar2=None, op0=ALU.mult), reads=["mx8"], writes=["nmx"])
            P.op("act", lambda e: e.activation(out=em, in_=lg, func=AF.Exp, bias=nmx[:, 0:1], scale=1.0), reads=["lg", "nmx"], writes=["em"])
            P.op("dve", lambda e: e.tensor_tensor(out=em, in0=em, in1=msk, op=ALU.mult), reads=["em", "msk"], writes=["em"])
            P.op("dve", lambda e: e.reduce_sum(out=ssum, in_=em, axis=AX.X), reads=["em"], writes=["ssum"])
            P.op("dve", lambda e: e.reciprocal(out=rsum, in_=ssum), reads=["ssum"], writes=["rsum"])
            P.op("dve", lambda e, ti=ti: e.tensor_scalar(out=cmb[:, ti, :], in0=em, scalar1=rsum[:, 0:1], scalar2=None, op0=ALU.mult), reads=["em", "rsum"], writes=["cmb_%d" % ti])
            P.op("pe", lambda e, ti=ti: e.transpose(banks[7][0:NE, 0:128], cmb[:, ti, :], ident_f), reads=["cmb_%d" % ti, "ident_f"], writes=["bank7"])
            P.op("act", lambda e: e.copy(out=cT[0:NE, :], in_=banks[7][0:NE, 0:128]), reads=["bank7"], writes=["cT"])
            P.dma("sp", dm_combT[:, ti * 128:(ti + 1) * 128], cT[0:NE, :], "st_cT", reads=["cT"], writes=["dm_combT"])
    P.dma("sp", dm_comb, cmb, "st_cmb", reads=["cmb_%d" % t for t in range(16)], writes=["dm_comb"])
    P.barrier(keep=["dm_mod", "dm_x1", "dm_u2t", "dm_comb", "dm_combT"])
    A.release(m_pre_ut)
    if upto <= 6:
        return finish(P, nc)

    G2 = A.alloc((D,), F32)
    P.dma("sp", G2, dm_mod[:, 5 * D:6 * D], "ld_c", reads=["dm_mod"], writes=["G2"])
    bu = A.alloc((NE * 16,), F32)
    P.dma("sp", bu, bupT_d, "ld_c", writes=["bu"])
    bdn = A.alloc((D,), F32)
    P.dma("sp", bdn[0:NE, :], bdn_d, "ld_c", writes=["bdn"])
    cmb7 = A.alloc((16, NE), F32)
    P.dma("sp", cmb7, dm_comb, "ld_c", reads=["dm_comb"], writes=["cmb7"])
    P.op("dve", lambda e: e.tensor_scalar(out=cmb7, in0=cmb7, scalar1=1.0 / 1.702, scalar2=None, op0=ALU.mult), reads=["cmb7"], writes=["cmb7"])
    bu1 = A.alloc((NE * 16,), F32)
    P.op("dve", lambda e: e.tensor_scalar(out=bu1, in0=bu, scalar1=1.0, scalar2=None, op0=ALU.add), reads=["bu"], writes=["bu1"])
    cT2 = A.alloc((1024,), F32)
    u2T = A.alloc((8, 1024), BF16)
    acc = A.alloc((8, D), F32)
    wu = [A.alloc((8, 2 * D), BF16) for _ in range(2)]
    wd = [A.alloc((8, D), BF16) for _ in range(2)]
    aTraw = [A.alloc((4096,), BF16) for _ in range(2)]
    aT = [a.rearrange("p (a b) -> p a b", b=512) for a in aTraw]
    gc = [A.alloc((512,), F32) for _ in range(2)]
    sgm = [A.alloc((512,), F32) for _ in range(2)]
    lc = [A.alloc((512,), F32) for _ in range(2)]
    xt7 = [aTraw[0][:, 0:2048].bitcast(F32)] * 2
    tmp7 = aTraw[0][:, 2048:4096].bitcast(F32)
    ot7 = [aTraw[1][:, 0:2048].bitcast(F32)] * 2
    junk7 = aTraw[1][:, 2048:3072]
    ss7 = A.alloc((16,), F32)
    ln7 = A.alloc((16,), F32)
    rs7 = A.alloc((16,), F32)
    P.op("pool", lambda e: e.memset(ss7, 0.0), writes=["ss7"])
    ecount = 0
    for half in range(2):
        hc = slice(half * 1024, (half + 1) * 1024)
        P.dma("sp", u2T, dm_u2t[:, :, hc], "ld_u2t", reads=["dm_u2t"], writes=["u2T"])
        P.dma("sp", cT2[0:NE, :], dm_combT[:, hc], "ld_cT2", reads=["dm_combT"], writes=["cT2"])
        for tt in range(8):
            for dh in range(2):
                bk = 4 + (tt * 2 + dh) % 4
                P.op("pe", lambda e, tt=tt, dh=dh, bk=bk: e.matmul(banks[bk][:, :], lhsT=cT2[0:NE, tt * 128:(tt + 1) * 128], rhs=bdn[0:NE, dh * 512:(dh + 1) * 512], start=True, stop=True),
                     reads=["cT2", "bdn"], writes=["bank%d" % bk])
                P.op("act", lambda e, tt=tt, dh=dh, bk=bk: e.copy(out=acc[:, tt, dh * 512:(dh + 1) * 512], in_=banks[bk][:, :]), reads=["bank%d" % bk], writes=["acc_%d_%d" % (tt, dh)])
        for ex in range(NE):
            ws = ecount % 2
            ecount += 1
            P.dma("pool", wu[ws], wup_d[ex].rearrange("(kt p) n -> p kt n", p=128), "ld_wu%d" % ws, writes=["wu%d" % ws])
            P.dma("pool", wd[ws], wdn_d[ex].rearrange("(kt p) n -> p kt n", p=128), "ld_wd%d" % ws, writes=["wd%d" % ws])
            for blk in range(2):
                bs = slice(blk * 512, (blk + 1) * 512)
                ab = aT[blk % 2]
                abk = "aT%d" % (blk % 2)
                for g in range(8):
                    s = g % 2
                    bg, bl = g % 2, 2 + g % 2

                    def mmu(e, g=g, bg=bg, bl=bl, ws=ws, bs=bs):
                        for kt in range(8):
                            e.matmul(banks[bg][:, :], lhsT=wu[ws][:, kt, g * 128:(g + 1) * 128], rhs=u2T[:, kt, bs], start=(kt == 0), stop=(kt == 7))
                        for kt in range(8):
                            r = e.matmul(banks[bl][:, :], lhsT=wu[ws][:, kt, 1024 + g * 128:1024 + (g + 1) * 128], rhs=u2T[:, kt, bs], start=(kt == 0), stop=(kt == 7))
                        return r
                    P.op("pe", mmu, reads=["wu%d" % ws, "u2T"], writes=["bank%d" % bg, "bank%d" % bl])
                    P.op("dve", lambda e, s=s, bg=bg, ex=ex, g=g: e.tensor_scalar(out=gc[s], in0=banks[bg][:, :], scalar1=bu[:, ex * 16 + g:ex * 16 + g + 1], scalar2=7.0, op0=ALU.add, op1=ALU.min),
                         reads=["bank%d" % bg, "bu"], writes=["gc%d" % s])
                    P.op("act", lambda e, s=s: e.activation(out=sgm[s], in_=gc[s], func=AF.Silu, scale=1.702), reads=["gc%d" % s], writes=["sgm%d" % s])
                    P.op("dve", lambda e, s=s, bl=bl, ex=ex, g=g: e.tensor_scalar(out=lc[s], in0=banks[bl][:, :], scalar1=bu1[:, ex * 16 + 8 + g:ex * 16 + 8 + g + 1], scalar2=8.0, op0=ALU.add, op1=ALU.min),
                         reads=["bank%d" % bl, "bu1"], writes=["lc%d" % s])
                    P.op("dve", lambda e, s=s, g=g, ab=ab: e.scalar_tensor_tensor(out=ab[:, g, :], in0=lc[s], scalar=-6.0, in1=sgm[s], op0=ALU.max, op1=ALU.mult),
                         reads=["sgm%d" % s, "lc%d" % s], writes=[abk])
                for tt in range(4):
                    til = blk * 4 + tt
                    for dh in range(2):
                        bk = 4 + (tt * 2 + dh) % 4

                        def mmd(e, tt=tt, dh=dh, bk=bk, ws=ws, ab=ab):
                            for fk in range(8):
                                r = e.matmul(banks[bk][:, :], lhsT=ab[:, fk, tt * 128:(tt + 1) * 128], rhs=wd[ws][:, fk, dh * 512:(dh + 1) * 512], start=(fk == 0), stop=(fk == 7))
                            return r
                        P.op("pe", mmd, reads=[abk, "wd%d" % ws], writes=["bank%d" % bk])
                        ak = "acc_%d_%d" % (til, dh)
                        P.op("dve", lambda e, til=til, dh=dh, bk=bk, ex=ex, half=half: e.scalar_tensor_tensor(
                            out=acc[:, til, dh * 512:(dh + 1) * 512], in0=banks[bk][:, :], scalar=cmb7[:, half * 8 + til, ex:ex + 1], in1=acc[:, til, dh * 512:(dh + 1) * 512], op0=ALU.mult, op1=ALU.add),
                            reads=["bank%d" % bk, "cmb7", ak], writes=[ak])
        P.barrier(keep=["dm_x1", "dm_u2t", "dm_combT"])
        for tt in range(8):
            ti = half * 8 + tt
            s = 0
            P.dma("sp", xt7[s], dm_x1[ti * 128:(ti + 1) * 128, :], "ld_x7%d" % s, reads=["dm_x1"], writes=["xt7%d" % s])
            P.op("act", lambda e, tt=tt, ti=ti: e.activation(out=junk7, in_=acc[:, tt, :], func=AF.Square, accum_out=ss7[:, ti:ti + 1]),
                 reads=["acc_%d_0" % tt, "acc_%d_1" % tt, "ss7"], writes=["junk7", "ss7_%d" % ti])
            rstd_from_ss(ss7[:, ti:ti + 1], D, rs7[:, ti:ti + 1], ln7[:, ti:ti + 1], ["ss7_%d" % ti], "rs7_%d" % ti)
            P.op("dve", lambda e, tt=tt, ti=ti: e.scalar_tensor_tensor(out=tmp7, in0=acc[:, tt, :], scalar=rs7[:, ti:ti + 1], in1=G2, op0=ALU.mult, op1=ALU.mult),
                 reads=["acc_%d_0" % tt, "acc_%d_1" % tt, "rs7_%d" % ti, "G2"], writes=["tmp7"])
            P.op("dve", lambda e, s=s: e.tensor_tensor(out=ot7[s], in0=tmp7, in1=xt7[s], op=ALU.add), reads=["tmp7", "xt7%d" % s], writes=["ot7%d" % s])
            P.dma("sp", out_d[ti * 128:(ti + 1) * 128, :], ot7[s], "st_out%d" % s, reads=["ot7%d" % s], writes=["out"])
        P.barrier(keep=["dm_x1", "dm_u2t", "dm_combT"])

    finish(P, nc)
    return nc


def finish(P, nc):
    P.wait_all("sp")
    P.build()
    P.close()
    return nc


def _rope_table(j):
    t = np.arange(NT) + j * NT
    rows = (t // 64).astype(np.float32)
    cols = (t % 64).astype(np.float32)
    inv = (10000.0 ** (-np.arange(0, 64, 2, dtype=np.float32) / 64)).astype(np.float32)
    ar = rows[:, None] * inv[None, :]
    ac = cols[:, None] * inv[None, :]
    cr, sr, cc, sc = np.cos(ar), np.sin(ar), np.cos(ac), np.sin(ac)
    return np.concatenate([cr, cr, cc, cc, -sr, sr, -sc, sc], axis=1).astype(np.float32)


def make_in_maps(inp, small=False):
    f = lambda a: np.ascontiguousarray(np.asarray(a, dtype=np.float32))
    x, c, ctx, c_ctx = f(inp["x"]), f(inp["c"]), f(inp["ctx"]), f(inp["c_ctx"])
    shared = {
        "w_mod": f(inp["w_mod"][0]), "b_mod": f(inp["b_mod"][0]).reshape(1, -1),
        "norm_g": f(inp["norm_g"][0]).reshape(1, -1), "w_in": f(inp["w_in"][0]),
        "lbv": f(np.asarray(inp["hgrn_lb"]).reshape(2, 2, 8, 128).transpose(3, 0, 1, 2).reshape(128, 32)),
        "hng": f(inp["hgrn_norm_g"][0]).reshape(128, 1), "qkg": f(inp["qk_norm_g"][0]).reshape(1, 256),
        "w_branch": f(inp["w_branch"][0]), "w_out": f(inp["w_out"][0]),
        "router_w": f(inp["router_w"][0]), "router_b": f(inp["router_b"][0]).reshape(1, -1),
        "w_up": f(inp["w_up"][0]), "b_upT": f(np.asarray(inp["b_up"][0]).reshape(32, 16, 128).transpose(2, 0, 1).reshape(128, 512)),
        "w_down": f(inp["w_down"][0]), "b_down": f(inp["b_down"][0]),
    }
    ropes = [_rope_table(j) for j in range(4)]
    maps = []
    for core in range(8):
        b, j = core // 4, core % 4
        cvec = np.concatenate([c[b].reshape(8, 128).T, c_ctx.reshape(8, 128).T], axis=1)
        sel = np.zeros((128, 8), np.float32)
        for r in range(4):
            sel[:, r] = 1.0 if r < j else 0.0
            sel[:, 4 + r] = 1.0 if r > j else 0.0
        m = dict(shared)
        if small:
            m["w_up"] = m["w_up"][0:1]
            m["w_down"] = m["w_down"][0:1]
        m.update({"x": f(x[b, j * NT:(j + 1) * NT]), "ctx": f(ctx[b]), "cvec": f(cvec), "rope": ropes[j], "sel": sel})
        maps.append(m)
    return maps


_NC_CACHE = {}


def kernel(**inputs):
    if "nc" not in _NC_CACHE:
        _NC_CACHE["nc"] = build()
    nc = _NC_CACHE["nc"]
    maps = make_in_maps(inputs)
    res = run_bass_kernel_spmd(nc, maps, core_ids=list(range(8)))
    out = np.empty((2, 8192, D), np.float32)
    for core in range(8):
        b, j = core // 4, core % 4
        out[b, j * NT:(j + 1) * NT] = res.results[core]["out"]
    return out
```

```python
from contextlib import ExitStack
import numpy as np
import concourse.bass as bass
import concourse.mybir as mybir
from concourse.bass_utils import run_bass_kernel_spmd

F32 = mybir.dt.float32
BF16 = mybir.dt.bfloat16
ALU = mybir.AluOpType
AF = mybir.ActivationFunctionType
AX = mybir.AxisListType

ENGS = ("pe", "act", "dve", "pool", "sp")
EPOCH = 16000
EPS = 1e-6


def _freeze(fn, memo=None):
    import types
    if memo is None:
        memo = {}
    if not isinstance(fn, types.FunctionType) or fn.__closure__ is None:
        return fn
    if id(fn) in memo:
        return memo[id(fn)]
    cells = []
    for c in fn.__closure__:
        try:
            v = c.cell_contents
        except ValueError:
            cells.append(c)
            continue
        if isinstance(v, types.FunctionType) and v.__closure__ is not None and v is not fn:
            v = _freeze(v, memo)
        cells.append(types.CellType(v))
    new = types.FunctionType(fn.__code__, fn.__globals__, fn.__name__, fn.__defaults__, tuple(cells))
    new.__kwdefaults__ = fn.__kwdefaults__
    memo[id(fn)] = new
    return new


class Prog:
    def __init__(self, nc, same_engine_sync=True):
        self.nc = nc
        self.es = ExitStack()
        self.q = {e: [] for e in ENGS}
        self.cnt = {e: 0 for e in ENGS}
        self.waited = {}
        self.buf = {}
        self.sems = {}
        self.dma_cnt = {}
        self.same_engine_sync = same_engine_sync
        self.n_sem = 0

    def sem(self, key):
        if key not in self.sems:
            self.n_sem += 1
            self.sems[key] = self.es.enter_context(self.nc.semaphore("s%d" % self.n_sem))
        return self.sems[key]

    def sbuf(self, name, shape, dtype):
        return self.es.enter_context(self.nc.sbuf_tensor(name, list(shape), dtype))

    def psum(self, name, shape, dtype=F32):
        return self.es.enter_context(self.nc.psum_tensor(name, list(shape), dtype))

    def _semkey_for(self, prod):
        kind, name, count = prod
        if kind == "e":
            ep = (count - 1) // EPOCH
            return ("e", name, ep), count - ep * EPOCH
        return ("d", name), count

    def _need(self, eng, prod, waits):
        if prod is None:
            return
        kind, name, count = prod
        if kind == "e" and name == eng and (eng in ("pe", "sp") or not self.same_engine_sync):
            return
        sk, val = self._semkey_for(prod)
        wk = (eng, kind, name)
        if self.waited.get(wk, 0) >= count:
            return
        self.waited[wk] = count
        waits.append((sk, val))

    def _deps(self, eng, reads, writes):
        waits = []
        for k in reads:
            b = self.buf.get(k)
            if b is not None:
                self._need(eng, b["w"], waits)
        for k in writes:
            b = self.buf.get(k)
            if b is not None:
                self._need(eng, b["w"], waits)
                for r in b["r"].values():
                    self._need(eng, r, waits)
        return waits

    def _record(self, prod, reads, writes):
        for k in reads:
            b = self.buf.setdefault(k, {"w": None, "r": {}})
            b["r"][(prod[0], prod[1])] = prod
        for k in writes:
            self.buf[k] = {"w": prod, "r": {}}

    def op(self, eng, fn, reads=(), writes=()):
        fn = _freeze(fn)
        waits = self._deps(eng, reads, writes)
        self.cnt[eng] += 1
        prod = ("e", eng, self.cnt[eng])
        sk, _ = self._semkey_for(prod)
        self.q[eng].append((fn, waits, (sk, 1)))
        self._record(prod, reads, writes)
        return prod

    def dma(self, eng, out, in_, semname, reads=(), writes=(), **kw):
        if writes:
            semname = semname + ":" + writes[0]
        waits = self._deps(eng, reads, writes)
        self.dma_cnt[semname] = self.dma_cnt.get(semname, 0) + 16
        prod = ("d", semname, self.dma_cnt[semname])
        self.q[eng].append((lambda e: e.dma_start(out=out, in_=in_, **kw), waits, (("d", semname), 16)))
        self._record(prod, reads, writes)
        return prod

    def custom(self, eng, fn, semname, inc, reads=(), writes=()):
        fn = _freeze(fn)
        waits = self._deps(eng, reads, writes)
        self.dma_cnt[semname] = self.dma_cnt.get(semname, 0) + inc
        prod = ("d", semname, self.dma_cnt[semname])
        self.q[eng].append((fn, waits, (("d", semname), inc)))
        self._record(prod, reads, writes)
        return prod

    def wait_all(self, eng):
        waits = []
        for e in ENGS:
            if self.cnt[e] > 0 and e != eng:
                self._need(eng, ("e", e, self.cnt[e]), waits)
        for name, c in self.dma_cnt.items():
            self._need(eng, ("d", name, c), waits)
        self.q[eng].append((None, waits, None))

    def barrier(self, keep=()):
        for e in ENGS:
            self.wait_all(e)
        self.buf = {k: v for k, v in self.buf.items() if k in keep}

    def build(self):
        nc = self.nc
        keys = []
        for e in ENGS:
            for (_, w, inc) in self.q[e]:
                for x in w:
                    keys.append(x[0])
                if inc:
                    keys.append(inc[0])
        for sk in dict.fromkeys(keys):
            self.sem(sk)
        engmap = {"pe": "tensor", "act": "scalar", "dve": "vector", "pool": "gpsimd", "sp": "sync"}
        with nc.Block() as block:
            for e in ENGS:
                items = self.q[e]

                def body(eng, items=items):
                    for fn, waits, inc in items:
                        for sk, val in waits:
                            eng.wait_ge(self.sems[sk], val)
                        if fn is not None:
                            ins = fn(eng)
                            if inc is not None:
                                ins.then_inc(self.sems[inc[0]], inc[1])

                getattr(block, engmap[e])(body)

    def close(self):
        self.es.close()


class Arena:
    def __init__(self, P, nbytes):
        self.t = P.sbuf("arena", [128, nbytes // 2], BF16)
        self.nbytes = nbytes
        self.off = 0

    def alloc(self, free_shape, dtype):
        n = int(np.prod(free_shape))
        size = n * (4 if dtype == F32 else 2)
        size = (size + 63) // 64 * 64
        assert self.off + size <= self.nbytes, ("SBUF arena overflow", self.off, size)
        v = self.t[:, self.off // 2:(self.off + size) // 2]
        if dtype == F32:
            v = v.bitcast(F32)
        v = v[:, 0:n]
        self.off += size
        if len(free_shape) == 2:
            v = v.rearrange("p (a b) -> p a b", b=free_shape[1])
        elif len(free_shape) == 3:
            v = v.rearrange("p (a b c) -> p a b c", b=free_shape[1], c=free_shape[2])
        elif len(free_shape) == 4:
            v = v.rearrange("p (a b c d) -> p a b c d", b=free_shape[1], c=free_shape[2], d=free_shape[3])
        return v

    def mark(self):
        return self.off

    def release(self, m):
        self.off = m


def bc(ap, axis, shape):
    return ap.unsqueeze(axis).to_broadcast(list(shape))


NT = 2048
NTT = 16
NCTX = 256
NALL = NT + NCTX
D = 1024
NE = 32
LAST_PHASE = 99


def build(upto=LAST_PHASE, debug=False):
    nc = bass.Bass("TRN2", target_bir_lowering=False)

    def din(name, shape, dt=F32):
        return nc.dram_tensor(name, list(shape), dt, kind="ExternalInput").ap()

    x_d = din("x", [NT, D])
    ctx_d = din("ctx", [NCTX, D])
    cvec_d = din("cvec", [128, 16])
    wmod_d = din("w_mod", [D, 6 * D])
    bmod_d = din("b_mod", [1, 6 * D])
    ng_d = din("norm_g", [1, 4 * D])
    win_d = din("w_in", [D, 8704])
    lbv_d = din("lbv", [128, 32])
    hng_d = din("hng", [128, 1])
    qkg_d = din("qkg", [1, 256])
    wbr_d = din("w_branch", [2, D, D])
    wout_d = din("w_out", [D, D])
    rw_d = din("router_w", [D, NE])
    rb_d = din("router_b", [1, NE])
    NEW = NE if upto >= 7 else 1
    wup_d = din("w_up", [NEW, D, 2 * D])
    bupT_d = din("b_upT", [128, NE * 16])
    wdn_d = din("w_down", [NEW, D, D])
    bdn_d = din("b_down", [NE, D])
    rope_d = din("rope", [NT, 256])
    sel_d = din("sel", [128, 8])
    out_d = nc.dram_tensor("out", [NT, D], F32, kind="ExternalOutput").ap()

    def dscr(name, shape, dt):
        if debug:
            return nc.dram_tensor(name, list(shape), dt, kind="ExternalOutput").ap()
        return nc.dram_tensor(name, list(shape), dt).ap()

    dm_mod = dscr("dm_mod", [128, 6 * D], F32)
    dm_ut = dscr("dm_ut", [128, 8, NALL], BF16)
    ag1_ins = [nc.dram_tensor("ag1_in%d" % q, [128, 2048], BF16).ap() for q in range(4)]
    ag1_outs = [nc.dram_tensor("ag1_out%d" % q, [4 * 128, 2048], BF16).ap() for q in range(4)]
    dm_kvctx = dscr("dm_kvctx", [512, NCTX], BF16)
    dm_oloc = dscr("dm_oloc", [8, 128, NT], F32)
    dm_qb = dscr("dm_qb", [2, 8, 128, NT], BF16)
    dm_gs = dscr("dm_gs", [8, 128, NT], BF16)
    ag2_ins = [nc.dram_tensor("ag2_in%d" % d, [128, 1032], F32).ap() for d in range(2)]
    ag2_outs = [nc.dram_tensor("ag2_out%d" % d, [4 * 128, 1032], F32).ap() for d in range(2)]
    dm_oth = dscr("dm_oth", [8, 128, NT], BF16)
    dm_ota = dscr("dm_ota", [8, 128, NT], BF16)
    dm_x1 = dscr("dm_x1", [NT, D], F32)
    dm_u2t = dscr("dm_u2t", [128, 8, NT], BF16)
    dm_comb = dscr("dm_comb", [128, NTT, NE], F32)
    dm_combT = dscr("dm_combT", [NE, NT], F32)

    P = Prog(nc)
    A = Arena(P, 207 * 1024)
    banks = [P.psum("bank%d" % i, [128, 512], F32) for i in range(8)]

    def bank_bf(i):
        return banks[i][:, :].bitcast(BF16)

    ident_f = A.alloc((128,), F32)
    ident_b = A.alloc((128,), BF16)
    ones_b = A.alloc((128,), BF16)
    ones_f = A.alloc((128,), F32)
    P.op("pool", lambda e: e.memset(ident_f, 0.0), writes=["ident_f"])
    P.op("pool", lambda e: e.affine_select(out=ident_f, in_=ident_f, pattern=[[-1, 128]], compare_op=ALU.not_equal,
                                           fill=1.0, base=0, channel_multiplier=1), reads=["ident_f"], writes=["ident_f"])
    P.op("dve", lambda e: e.tensor_copy(out=ident_b, in_=ident_f), reads=["ident_f"], writes=["ident_b"])
    P.op("pool", lambda e: e.memset(ones_f, 1.0), writes=["ones_f"])
    P.op("dve", lambda e: e.tensor_copy(out=ones_b, in_=ones_f), reads=["ones_f"], writes=["ones_b"])

    m_pre_ut = A.mark()
    uT = A.alloc((8, NALL), BF16)

    def rstd_from_ss(ss_ap, n, out_ap, tmp_ap, rk, wk):
        P.op("act", lambda e: e.activation(out=tmp_ap, in_=ss_ap, func=AF.Ln, scale=1.0 / n, bias=EPS), reads=rk, writes=[wk + "_ln"])
        P.op("act", lambda e: e.activation(out=out_ap, in_=tmp_ap, func=AF.Exp, scale=-0.5), reads=[wk + "_ln"], writes=[wk])

    m0 = A.mark()
    cv = A.alloc((16,), F32)
    scv = A.alloc((16,), F32)
    scb = A.alloc((16, 128), F32)
    bmod = A.alloc((6 * D,), F32)
    ng = A.alloc((4, D), F32)
    modl = A.alloc((6 * D,), F32)
    modc = A.alloc((2 * D,), F32)
    wm = [A.alloc((8, 512), F32) for _ in range(2)]
    P.dma("sp", cv, cvec_d, "ld_c", writes=["cv"])
    P.dma("sp", bmod, bmod_d[0].partition_broadcast(128), "ld_c", writes=["bmod"])
    P.dma("sp", ng, ng_d[0].partition_broadcast(128).rearrange("p (a b) -> p a b", b=D), "ld_c", writes=["ng"])
    P.op("act", lambda e: e.activation(out=scv, in_=cv, func=AF.Silu), reads=["cv"], writes=["scv"])
    for k in range(16):
        P.op("dve", lambda e, k=k: e.tensor_copy(out=scb[:, k, :], in_=scv[:, k:k + 1].to_broadcast([128, 128])),
             reads=["scv"], writes=["scb"])
    for s in range(12):
        w = wm[s % 2]
        wk = "wm%d" % (s % 2)
        P.dma("sp", w, wmod_d[:, s * 512:(s + 1) * 512].rearrange("(kt p) n -> p kt n", p=128), "ld_" + wk, writes=[wk])

        def mm(e, w=w, off=0, bk=0):
            for kt in range(8):
                r = e.matmul(banks[bk][:, :], lhsT=scb[:, off + kt, :], rhs=w[:, kt, :], start=(kt == 0), stop=(kt == 7))
            return r
        P.op("pe", lambda e, w=w: mm(e, w, 0, 0), reads=["scb", wk], writes=["bank0"])
        P.op("dve", lambda e, s=s: e.tensor_tensor(out=modl[:, s * 512:(s + 1) * 512], in0=banks[0][:, :], in1=bmod[:, s * 512:(s + 1) * 512], op=ALU.add),
             reads=["bank0", "bmod"], writes=["modl"])
        if s < 4:
            P.op("pe", lambda e, w=w: mm(e, w, 8, 1), reads=["scb", wk], writes=["bank1"])
            P.op("dve", lambda e, s=s: e.tensor_tensor(out=modc[:, s * 512:(s + 1) * 512], in0=banks[1][:, :], in1=bmod[:, s * 512:(s + 1) * 512], op=ALU.add),
                 reads=["bank1", "bmod"], writes=["modc"])
    P.op("dve", lambda e: e.scalar_tensor_tensor(out=modl[:, D:2 * D], in0=modl[:, D:2 * D], scalar=1.0, in1=ng[:, 0, :], op0=ALU.add, op1=ALU.mult),
         reads=["modl", "ng"], writes=["modl"])
    P.op("dve", lambda e: e.scalar_tensor_tensor(out=modc[:, D:2 * D], in0=modc[:, D:2 * D], scalar=1.0, in1=ng[:, 0, :], op0=ALU.add, op1=ALU.mult),
         reads=["modc", "ng"], writes=["modc"])
    P.op("dve", lambda e: e.tensor_tensor(out=modl[:, 2 * D:3 * D], in0=modl[:, 2 * D:3 * D], in1=ng[:, 1, :], op=ALU.mult), reads=["modl", "ng"], writes=["modl"])
    P.op("dve", lambda e: e.scalar_tensor_tensor(out=modl[:, 4 * D:5 * D], in0=modl[:, 4 * D:5 * D], scalar=1.0, in1=ng[:, 2, :], op0=ALU.add, op1=ALU.mult),
         reads=["modl", "ng"], writes=["modl"])
    P.op("dve", lambda e: e.tensor_tensor(out=modl[:, 5 * D:6 * D], in0=modl[:, 5 * D:6 * D], in1=ng[:, 3, :], op=ALU.mult), reads=["modl", "ng"], writes=["modl"])
    P.dma("sp", dm_mod, modl, "st_mod", reads=["modl"], writes=["dm_mod"])

    xt = [A.alloc((D,), F32) for _ in range(2)]
    junk = A.alloc((D,), BF16)
    tmpf = A.alloc((D,), F32)
    ub = [A.alloc((D,), BF16) for _ in range(2)]
    ss1 = A.alloc((18,), F32)
    ln1 = A.alloc((18,), F32)
    rs1 = A.alloc((18,), F32)
    P.op("pool", lambda e: e.memset(ss1, 0.0), writes=["ss1"])
    for ti in range(18):
        s = ti % 2
        src = x_d[ti * 128:(ti + 1) * 128, :] if ti < NTT else ctx_d[(ti - NTT) * 128:(ti - NTT + 1) * 128, :]
        Am = modl if ti < NTT else modc
        amk = "modl" if ti < NTT else "modc"
        P.dma("sp", xt[s], src, "ld_xt%d" % s, writes=["xt%d" % s])
        P.op("act", lambda e, s=s, ti=ti: e.activation(out=junk, in_=xt[s], func=AF.Square, accum_out=ss1[:, ti:ti + 1]),
             reads=["xt%d" % s, "ss1"], writes=["junk", "ss1_%d" % ti])
        rstd_from_ss(ss1[:, ti:ti + 1], D, rs1[:, ti:ti + 1], ln1[:, ti:ti + 1], ["ss1_%d" % ti], "rs1_%d" % ti)
        P.op("dve", lambda e, s=s, ti=ti, Am=Am: e.scalar_tensor_tensor(out=tmpf, in0=xt[s], scalar=rs1[:, ti:ti + 1], in1=Am[:, D:2 * D], op0=ALU.mult, op1=ALU.mult),
             reads=["xt%d" % s, "rs1_%d" % ti, amk], writes=["tmpf"])
        P.op("dve", lambda e, s=s, Am=Am: e.tensor_tensor(out=ub[s], in0=tmpf, in1=Am[:, 0:D], op=ALU.add), reads=["tmpf", amk], writes=["ub%d" % s])
        bk = 2 + s

        def tr(e, s=s, bk=bk):
            for kt in range(8):
                r = e.transpose(bank_bf(bk)[:, kt * 128:(kt + 1) * 128], ub[s][:, kt * 128:(kt + 1) * 128], ident_b)
            return r
        P.op("pe", tr, reads=["ub%d" % s, "ident_b"], writes=["bank%d" % bk])
        P.op("act", lambda e, ti=ti, bk=bk: e.copy(out=uT[:, :, ti * 128:(ti + 1) * 128], in_=bank_bf(bk).rearrange("p (a b) -> p a b", b=128)),
             reads=["bank%d" % bk], writes=["uT_%d" % ti])
    UT_KEYS = ["uT_%d" % ti for ti in range(18)]
    if debug:
        P.dma("sp", dm_ut, uT, "st_dbg", reads=UT_KEYS, writes=["dm_ut"])
    P.barrier()
    A.release(m0)
    if upto <= 1:
        return finish(P, nc)


    m2 = A.mark()
    wkv = A.alloc((8, 512), BF16)
    P.dma("pool", wkv, win_d[:, 6144:6656].rearrange("(kt p) n -> p kt n", p=128), "ld_wkv", writes=["wkv"])
    ropeT = A.alloc((NTT, 256), F32)
    P.dma("sp", ropeT, rope_d.rearrange("(t p) n -> p t n", p=128), "ld_rope", writes=["ropeT"])
    gqk = A.alloc((256,), F32)
    P.dma("sp", gqk, qkg_d[0].partition_broadcast(128), "ld_c", writes=["gqk"])
    KTl = A.alloc((2, NALL), BF16)
    Vl = A.alloc((18, 256), BF16)
    ssk = A.alloc((18, 2), F32)
    lnk = A.alloc((18, 2), F32)
    rsk = A.alloc((18, 2), F32)
    knb = [A.alloc((2, 128), F32) for _ in range(2)]
    t1b = A.alloc((2, 128), F32)
    t2b = A.alloc((2, 128), F32)
    kbf = [A.alloc((256,), BF16) for _ in range(2)]
    junk2 = A.alloc((128,), BF16)
    P.op("pool", lambda e: e.memset(ssk, 0.0), writes=["ssk"])

    def rope_apply(xn, nh, ti, outbf, pfx, eng2="dve"):
        cosv = ropeT[:, ti, 0:128]
        sinv = ropeT[:, ti, 128:256].rearrange("p (r x d) -> p r x d", r=2, x=2, d=32)
        t1 = t1b if nh == 2 else t1q
        t2 = t2b if nh == 2 else t2q
        P.op("dve", lambda e: e.tensor_tensor(out=t1, in0=xn, in1=bc(cosv, 1, [128, nh, 128]), op=ALU.mult),
             reads=[pfx + "xn", "ropeT"], writes=[pfx + "t1"])
        x6 = xn.rearrange("p h (r x d) -> p h r x d", r=2, x=2, d=32)
        t6 = t2.rearrange("p h (r x d) -> p h r x d", r=2, x=2, d=32)
        for xo in range(2):
            P.op(eng2, lambda e, xo=xo: e.tensor_tensor(out=t6[:, :, :, xo, :], in0=x6[:, :, :, 1 - xo, :],
                                                        in1=bc(sinv[:, :, xo, :], 1, [128, nh, 2, 32]), op=ALU.mult),
                 reads=[pfx + "xn", "ropeT"], writes=[pfx + "t2_%d" % xo])
        P.op("dve", lambda e: e.tensor_tensor(out=outbf.rearrange("p (h d) -> p h d", d=128), in0=t1, in1=t2, op=ALU.add),
             reads=[pfx + "t1", pfx + "t2_0", pfx + "t2_1"], writes=[pfx + "bf"])

    import os
    BIS = int(os.environ.get("BIS", "99"))
    for ti in range(18):
        s = ti % 2
        bk = s
        kn = knb[s]

        def mmkv(e, ti=ti, bk=bk):
            for kt in range(8):
                r = e.matmul(banks[bk][:, :], lhsT=uT[:, kt, ti * 128:(ti + 1) * 128], rhs=wkv[:, kt, :], start=(kt == 0), stop=(kt == 7))
            return r
        P.op("pe", mmkv, reads=["uT_%d" % ti, "wkv"], writes=["bank%d" % bk])
        kps = banks[bk][:, 0:256].rearrange("p (h d) -> p h d", d=128)
        P.op("act", lambda e, ti=ti, bk=bk: e.copy(out=Vl[:, ti, :], in_=banks[bk][:, 256:512]), reads=["bank%d" % bk], writes=["Vl_%d" % ti])
        if BIS < 2:
            continue
        for h in range(2):
            P.op("act", lambda e, h=h, ti=ti, kps=kps: e.activation(out=junk2, in_=kps[:, h, :], func=AF.Square, accum_out=ssk[:, ti, h:h + 1]),
                 reads=["bank%d" % bk, "ssk"], writes=["junk2", "ssk_%d_%d" % (ti, h)])
        rstd_from_ss(ssk[:, ti, :], 128, rsk[:, ti, :], lnk[:, ti, :], ["ssk_%d_0" % ti, "ssk_%d_1" % ti], "rsk_%d" % ti)
        P.op("dve", lambda e, kn=kn, kps=kps, ti=ti: e.tensor_tensor(out=kn, in0=kps, in1=bc(rsk[:, ti, :], 2, [128, 2, 128]), op=ALU.mult),
             reads=["bank%d" % bk, "rsk_%d" % ti], writes=["k%dxn" % s])
        P.op("dve", lambda e, kn=kn: e.tensor_tensor(out=kn, in0=kn, in1=bc(gqk[:, 128:256], 1, [128, 2, 128]), op=ALU.mult),
             reads=["k%dxn" % s, "gqk"], writes=["k%dxn" % s])
        if BIS < 3:
            continue
        if ti < NTT and BIS != 3:
            rope_apply(kn, 2, ti, kbf[s], "k%d" % s)
        else:
            P.op("dve", lambda e, kn=kn, s=s: e.tensor_copy(out=kbf[s].rearrange("p (h d) -> p h d", d=128), in_=kn), reads=["k%dxn" % s], writes=["k%dbf" % s])
        bk2 = 2 + s
        if BIS < 5:
            continue

        def trk(e, s=s, bk2=bk2):
            for h in range(2):
                r = e.transpose(bank_bf(bk2)[:, h * 128:(h + 1) * 128], kbf[s][:, h * 128:(h + 1) * 128], ident_b)
            return r
        P.op("pe", trk, reads=["k%dbf" % s, "ident_b"], writes=["bank%d" % bk2])
        P.op("act", lambda e, ti=ti, bk2=bk2: e.copy(out=KTl[:, :, ti * 128:(ti + 1) * 128], in_=bank_bf(bk2)[:, 0:256].rearrange("p (a b) -> p a b", b=128)),
             reads=["bank%d" % bk2], writes=["KTl_%d" % ti])
    for h in range(2 if BIS >= 6 else 0):
        P.dma("sp", ag1_ins[h], KTl[:, h, 0:NT], "st_ag1", reads=["KTl_%d" % t for t in range(16)], writes=["ag1_in"])
        P.dma("sp", ag1_ins[2 + h].rearrange("p (ti c) -> p ti c", c=256), Vl[:, 8 * h:8 * h + 8, :], "st_ag1", reads=["Vl_%d" % t for t in range(16)], writes=["ag1_in"])
    if BIS >= 6:
        P.dma("sp", dm_kvctx[0:256, :].rearrange("(h p) t -> p h t", p=128), KTl[:, :, NT:NALL], "st_kvc", reads=["KTl_16", "KTl_17"], writes=["dm_kvctx"])
        P.dma("sp", dm_kvctx[256:512, :].rearrange("(ti p) c -> p ti c", p=128), Vl[:, 16:18, :], "st_kvc", reads=["Vl_16", "Vl_17"], writes=["dm_kvctx"])
    for q in range(4 if BIS >= 7 else 0):
        P.custom("pool", lambda e, q=q: e.collective_compute("AllGather", ALU.bypass, replica_groups=[[0, 1, 2, 3], [4, 5, 6, 7]], ins=[ag1_ins[q]], outs=[ag1_outs[q]]),
                 "cc1", 1, reads=["ag1_in"] + (["ag1_out"] if q > 0 else []), writes=["ag1_out"])
    P.barrier(keep=["ag1_out", "dm_kvctx", "dm_mod"] + UT_KEYS)
    A.release(m2)
    if upto <= 2:
        return finish(P, nc)

    m3 = A.mark()
    rst = A.alloc((NALL,), F32)
    maskF = A.alloc((128,), F32)
    maskB = A.alloc((128,), F32)
    lbt = A.alloc((2, 2, 8), F32)
    lbd = A.alloc((2, 8), F32)
    lb = A.alloc((2, 8), F32)
    oml = A.alloc((2, 8), F32)
    hng = A.alloc((1,), F32)
    stage = A.alloc((2, 8, 128), F32)
    sctx = A.alloc((2, 8, 128), F32)
    Dv = A.alloc((2, 8), F32)
    m3h = A.mark()
    whb = [A.alloc((8, 5, 128), BF16)] * 2
    qT = A.alloc((NT,), F32)
    gsT = A.alloc((NT,), BF16)
    v_tm = A.alloc((18, 128), BF16)
    fa = A.alloc((NALL,), F32)
    lf = A.alloc((NALL,), F32)
    kk = A.alloc((NALL,), F32)
    bb = A.alloc((NALL,), F32)
    xx = A.alloc((NALL,), F32)
    ee = A.alloc((NALL,), F32)
    gtmp = ee[:, 0:NT]
    qe = [A.alloc((NT,), BF16) for _ in range(2)]
    ke = [A.alloc((NT,), BF16) for _ in range(2)]
    kd = [A.alloc((NALL,), BF16) for _ in range(2)]
    kd_tm = [A.alloc((18, 128), BF16) for _ in range(2)]
    qB = [A.alloc((NT,), BF16) for _ in range(2)]
    tot = [A.alloc((36,), F32) for _ in range(2)]
    etot = [A.alloc((36,), F32) for _ in range(2)]
    ipf = [A.alloc((32,), F32) for _ in range(2)]
    gg = [A.alloc((32,), F32) for _ in range(2)]
    eg = [A.alloc((32,), F32) for _ in range(2)]
    attm = [A.alloc((4, 128), BF16) for _ in range(2)]
    Sst = [[A.alloc((128,), F32) for _ in range(2)] for _ in range(2)]
    Sbf = [[A.alloc((128,), BF16) for _ in range(3)] for _ in range(2)]
    Scx = [A.alloc((128,), F32) for _ in range(2)]
    o_acc = A.alloc((NT,), F32)

    P.op("pool", lambda e: e.memset(rst, 1.0), writes=["rst"])
    P.op("pool", lambda e: e.memset(rst.rearrange("p (c t) -> p c t", t=64)[:, :, 0:1], 0.0), reads=["rst"], writes=["rst"])
    P.op("pool", lambda e: e.memset(maskF, 1.0), writes=["maskF"])
    P.op("pool", lambda e: e.affine_select(out=maskF, in_=maskF, pattern=[[1, 128]], compare_op=ALU.is_ge, fill=0.0, base=0, channel_multiplier=-1),
         reads=["maskF"], writes=["maskF"])
    P.op("pool", lambda e: e.memset(maskF[0:64, 64:128], 0.0), reads=["maskF"], writes=["maskF"])
    P.op("pool", lambda e: e.memset(maskB, 1.0), writes=["maskB"])
    P.op("pool", lambda e: e.affine_select(out=maskB, in_=maskB, pattern=[[-1, 128]], compare_op=ALU.is_ge, fill=0.0, base=0, channel_multiplier=1),
         reads=["maskB"], writes=["maskB"])
    P.op("pool", lambda e: e.memset(maskB[64:128, 0:64], 0.0), reads=["maskB"], writes=["maskB"])
    masks = [maskF, maskB]
    P.dma("sp", lbt, lbv_d.rearrange("p (a b c) -> p a b c", a=2, b=2), "ld_c", writes=["lbt"])
    P.dma("sp", hng, hng_d, "ld_c", writes=["hng"])
    P.op("dve", lambda e: e.tensor_tensor(out=lbd, in0=lbt[:, :, 0, :], in1=lbt[:, :, 1, :], op=ALU.subtract), reads=["lbt"], writes=["lbd"])
    P.op("act", lambda e: e.activation(out=lb, in_=lbd, func=AF.Sigmoid), reads=["lbd"], writes=["lb"])
    P.op("dve", lambda e: e.tensor_scalar(out=oml, in0=lb, scalar1=-1.0, scalar2=1.0, op0=ALU.mult, op1=ALU.add), reads=["lb"], writes=["oml"])

    win_v = win_d.rearrange("(kt p) (s n) -> p kt s n", p=128, n=128)
    BLK5 = [(0, 512), (512, 512), (1024, 512), (1536, 512), (2048, 256)]
    PB = 6
    pbc = [0]

    def proj_fm(wh, whk, sidx, c0, n, evac):
        bk = PB + (pbc[0] % 2)
        pbc[0] += 1

        def mm(e):
            for kt in range(8):
                r = e.matmul(banks[bk][:, 0:n], lhsT=wh[:, kt, sidx, :], rhs=uT[:, kt, c0:c0 + n], start=(kt == 0), stop=(kt == 7))
            return r
        P.op("pe", mm, reads=[whk] + ["uT_%d" % t for t in range(c0 // 128, (c0 + n) // 128)], writes=["bank%d" % bk])
        evac(banks[bk][:, 0:n], "bank%d" % bk)

    def hgrn_dir_prep(h, d):
        wh = whb[h % 2]
        whk = "wh0"
        dk = "d%d" % d
        for (c0, n) in BLK5:
            proj_fm(wh, whk, 2 + d, c0, n, lambda bap, bkey, c0=c0, n=n: P.op(
                "act", lambda e: e.activation(out=fa[:, c0:c0 + n], in_=bap, func=AF.Sigmoid), reads=[bkey], writes=["fa"]))
        fak = ["fa"]
        P.op("dve", lambda e: e.tensor_scalar(out=fa, in0=fa, scalar1=oml[:, d, h:h + 1], scalar2=lb[:, d, h:h + 1], op0=ALU.mult, op1=ALU.add),
             reads=fak + ["oml", "lb"], writes=["fa"])
        P.op("act", lambda e: e.activation(out=lf, in_=fa, func=AF.Ln), reads=["fa"], writes=["lf"])
        P.op("dve", lambda e: e.tensor_scalar(out=kk, in0=fa, scalar1=-1.0, scalar2=1.0, op0=ALU.mult, op1=ALU.add), reads=["fa"], writes=["kk"])
        P.op("dve", lambda e: e.tensor_tensor_scan(out=bb, data0=rst, data1=lf, initial=0.0, op0=ALU.mult, op1=ALU.add), reads=["rst", "lf"], writes=["bb"])
        b3 = bb.rearrange("p (c t) -> p c t", t=64)
        P.op("dve", lambda e: e.tensor_copy(out=tot[d], in_=b3[:, :, 63]), reads=["bb"], writes=["tot" + dk])
        P.op("act", lambda e: e.activation(out=etot[d], in_=tot[d], func=AF.Exp), reads=["tot" + dk], writes=["etot" + dk])
        x3 = xx.rearrange("p (c t) -> p c t", t=64)
        P.op("dve", lambda e: e.tensor_tensor(out=x3, in0=bc(tot[d], 2, [128, 36, 64]), in1=b3, op=ALU.subtract), reads=["tot" + dk, "bb"], writes=["xx"])
        if d == 0:
            bu, dd = bb, xx
            bk_, ddk = "bb", "xx"
        else:
            P.op("dve", lambda e: e.tensor_tensor(out=xx, in0=xx, in1=lf, op=ALU.add), reads=["xx", "lf"], writes=["xx"])
            P.op("dve", lambda e: e.tensor_tensor(out=bb, in0=bb, in1=lf, op=ALU.subtract), reads=["bb", "lf"], writes=["bb"])
            bu, dd = xx, bb
            bk_, ddk = "xx", "bb"
        P.op("act", lambda e: e.activation(out=ee[:, 0:NT], in_=bu[:, 0:NT], func=AF.Exp), reads=[bk_], writes=["ee"])
        P.op("dve", lambda e: e.tensor_tensor(out=qe[d], in0=qT, in1=ee[:, 0:NT], op=ALU.mult), reads=["ee", "qT"], writes=["qe" + dk])
        P.op("act", lambda e: e.activation(out=ee[:, 0:NT], in_=bu[:, 0:NT], func=AF.Exp, scale=-1.0), reads=[bk_, "ee"], writes=["ee"])
        P.op("dve", lambda e: e.tensor_tensor(out=ke[d], in0=kk[:, 0:NT], in1=ee[:, 0:NT], op=ALU.mult), reads=["ee", "kk"], writes=["ke" + dk])
        P.op("act", lambda e: e.activation(out=ee, in_=dd, func=AF.Exp), reads=[ddk, "ee"], writes=["ee"])
        P.op("dve", lambda e: e.tensor_tensor(out=kd[d], in0=kk, in1=ee, op=ALU.mult), reads=["ee", "kk"], writes=["kd" + dk])
        P.op("dve", lambda e: e.tensor_tensor_scan(out=ipf[d], data0=ones_f[:, 0:32], data1=tot[d][:, 0:32], initial=0.0, op0=ALU.mult, op1=ALU.add),
             reads=["tot" + dk, "ones_f"], writes=["ipf" + dk])
        if d == 0:
            P.op("dve", lambda e: e.tensor_tensor(out=gg[d], in0=ipf[d], in1=tot[d][:, 0:32], op=ALU.subtract), reads=["ipf" + dk, "tot" + dk], writes=["gg" + dk])
        else:
            P.op("dve", lambda e: e.tensor_tensor(out=gg[d], in0=ipf[d][:, 31:32].to_broadcast([128, 32]), in1=ipf[d], op=ALU.subtract),
                 reads=["ipf" + dk], writes=["gg" + dk])
        P.op("act", lambda e: e.activation(out=eg[d], in_=gg[d], func=AF.Exp), reads=["gg" + dk], writes=["eg" + dk])
        P.op("act", lambda e: e.activation(out=Dv[:, d, h:h + 1], in_=ipf[d][:, 31:32], func=AF.Exp), reads=["ipf" + dk], writes=["Dv_%d_%d" % (d, h)])
        P.op("dve", lambda e: e.tensor_tensor(out=qB[d].rearrange("p (c t) -> p c t", t=64), in0=qe[d].rearrange("p (c t) -> p c t", t=64),
                                               in1=bc(eg[d], 2, [128, 32, 64]), op=ALU.mult), reads=["qe" + dk, "eg" + dk], writes=["qB" + dk])
        P.dma("sp", dm_qb[d, h], qB[d], "st_qb", reads=["qB" + dk], writes=["dm_qb"])
        for g3 in range(3):
            bk = PB + (pbc[0] % 2)
            pbc[0] += 1

            def trd(e, g3=g3, bk=bk):
                for i in range(6):
                    ti = g3 * 6 + i
                    r = e.transpose(bank_bf(bk)[:, i * 128:(i + 1) * 128], kd[d][:, ti * 128:(ti + 1) * 128], ident_b)
                return r
            P.op("pe", trd, reads=["kd" + dk, "ident_b"], writes=["bank%d" % bk])
            P.op("act", lambda e, g3=g3, bk=bk: e.copy(out=kd_tm[d][:, g3 * 6:(g3 + 1) * 6, :], in_=bank_bf(bk)[:, 0:768].rearrange("p (a b) -> p a b", b=128)),
                 reads=["bank%d" % bk], writes=["kdtm%s_%d" % (dk, g3)])

    def hgrn_dir_scan(h, d):
        dk = "d%d" % d
        kdk = ["kdtm%s_%d" % (dk, g3) for g3 in range(3)]
        bA, bO, bS = 3 * d, 3 * d + 1, 3 * d + 2
        Sc = Scx[d]
        Sbufs, Sbb = Sst[d], Sbf[d]
        slot = [0]

        def delta_mm(c):
            sl = slot[0] % 2
            slot[0] += 1
            ti, half = c // 2, c % 2
            ps = slice(half * 64, half * 64 + 64)
            bidx = (bS, PB + d)[sl]
            bap = banks[bidx][:, 0:128]
            bkey = "bank%d" % bidx
            P.op("pe", lambda e: e.matmul(bap, lhsT=kd_tm[d][ps, ti, :], rhs=v_tm[ps, ti, :], start=True, stop=True),
                 reads=kdk + ["v_tm"], writes=[bkey])
            return bap, bkey
        corder = [32, 33, 34, 35] if d == 0 else [35, 34, 33, 32]
        for i, c in enumerate(corder):
            bap, bkey = delta_mm(c)
            if i == 0:
                P.op("dve", lambda e: e.tensor_copy(out=Sc, in_=bap), reads=[bkey], writes=["Sc" + dk])
            else:
                P.op("dve", lambda e: e.scalar_tensor_tensor(out=Sc, in0=Sc, scalar=etot[d][:, c:c + 1], in1=bap, op0=ALU.mult, op1=ALU.add),
                     reads=[bkey, "Sc" + dk, "etot" + dk], writes=["Sc" + dk])
            yield
        P.op("dve", lambda e: e.tensor_copy(out=sctx[:, d, h, :], in_=Sc), reads=["Sc" + dk], writes=["sctx_%d_%d" % (d, h)])
        P.op("pool", lambda e: e.memset(Sbb[0], 0.0), reads=["Sb%s_0" % dk], writes=["Sb%s_0" % dk])
        si, bi, nstate = 0, 0, 0
        groups = [0, 1, 2, 3] if d == 0 else [3, 2, 1, 0]
        pis = [0, 1, 2, 3] if d == 0 else [3, 2, 1, 0]
        seq = []
        for g in groups:
            for pi in pis:
                p = g * 4 + pi
                for c in ([2 * p, 2 * p + 1] if d == 0 else [2 * p + 1, 2 * p]):
                    seq.append(c)
        dl = {}
        for i in range(min(2, len(seq))):
            dl[i] = delta_mm(seq[i])
        step = 0
        for g in groups:
            def att(e, g=g):
                for pi in range(4):
                    p = g * 4 + pi
                    r = e.matmul(banks[bA][:, pi * 128:(pi + 1) * 128], lhsT=ke[d][:, p * 128:(p + 1) * 128], rhs=qe[d][:, p * 128:(p + 1) * 128], start=True, stop=True)
                return r
            P.op("pe", att, reads=["ke" + dk, "qe" + dk], writes=["bank%d" % bA])
            P.op("dve", lambda e: e.tensor_tensor(out=attm[d], in0=banks[bA][:, :].rearrange("p (a b) -> p a b", b=128), in1=bc(masks[d], 1, [128, 4, 128]), op=ALU.mult),
                 reads=["bank%d" % bA, "mask"], writes=["attm" + dk])
            for pi in pis:
                p = g * 4 + pi
                P.op("pe", lambda e, pi=pi, p=p: e.matmul(banks[bO][:, pi * 128:(pi + 1) * 128], lhsT=v_tm[:, p, :], rhs=attm[d][:, pi, :], start=True, stop=False),
                     reads=["v_tm", "attm" + dk], writes=["bank%d" % bO])
                chunks = [2 * p, 2 * p + 1] if d == 0 else [2 * p + 1, 2 * p]
                for ci, c in enumerate(chunks):
                    col = pi * 128 + (c % 2) * 64
                    sbc = Sbb[bi]
                    P.op("pe", lambda e, c=c, col=col, ci=ci, sbc=sbc: e.matmul(banks[bO][:, col:col + 64], lhsT=sbc, rhs=qe[d][:, c * 64:(c + 1) * 64], start=False, stop=(ci == 1)),
                         reads=["Sb%s_%d" % (dk, bi), "qe" + dk], writes=["bank%d" % bO])
                    assert seq[step] == c
                    bap, bkey = dl.pop(step)
                    nsi = (si + 1) % 2
                    So, Sn = Sbufs[si], Sbufs[nsi]
                    if nstate == 0:
                        P.op("dve", lambda e, Sn=Sn, bap=bap: e.tensor_copy(out=Sn, in_=bap), reads=[bkey], writes=["S%s_%d" % (dk, nsi)])
                    else:
                        P.op("dve", lambda e, So=So, Sn=Sn, bap=bap, c=c: e.scalar_tensor_tensor(out=Sn, in0=So, scalar=etot[d][:, c:c + 1], in1=bap, op0=ALU.mult, op1=ALU.add),
                             reads=[bkey, "S%s_%d" % (dk, si), "etot" + dk], writes=["S%s_%d" % (dk, nsi)])
                    si = nsi
                    nstate += 1
                    nbi = (bi + 1) % 3
                    sbn = Sbb[nbi]
                    P.op("act", lambda e, Sn=Sn, sbn=sbn: e.copy(out=sbn, in_=Sn), reads=["S%s_%d" % (dk, si)], writes=["Sb%s_%d" % (dk, nbi)])
                    bi = nbi
                    if step + 2 < len(seq):
                        dl[step + 2] = delta_mm(seq[step + 2])
                    step += 1
                    yield
            cs = slice(g * 512, (g + 1) * 512)
            if (d == 0 and g < 2) or (d == 1 and g >= 2):
                P.op("act", lambda e, cs=cs: e.copy(out=o_acc[:, cs], in_=banks[bO][:, :]), reads=["bank%d" % bO, "oacc_%d" % g], writes=["oacc_%d" % g])
            else:
                P.op("dve", lambda e, cs=cs: e.tensor_tensor(out=o_acc[:, cs], in0=o_acc[:, cs], in1=banks[bO][:, :], op=ALU.add),
                     reads=["bank%d" % bO, "oacc_%d" % g], writes=["oacc_%d" % g])
        Sl = Sbufs[si]
        P.op("dve", lambda e, Sl=Sl: e.tensor_copy(out=stage[:, d, h, :], in_=Sl), reads=["S%s_%d" % (dk, si)], writes=["stage_%d_%d" % (d, h)])

    P.buf["mask"] = {"w": ("e", "pool", P.cnt["pool"]), "r": {}}
    for h in range(8):
        wh = whb[h % 2]
        whk = "wh0"
        for s5 in range(5):
            P.dma("pool", wh[:, :, s5, :], win_v[:, :, h + 8 * s5, :], "ld_" + whk, writes=[whk])
        for blk in range(4):
            proj_fm(wh, whk, 0, blk * 512, 512, lambda bap, bkey, blk=blk: P.op(
                "act", lambda e: e.copy(out=qT[:, blk * 512:(blk + 1) * 512], in_=bap), reads=[bkey, "qT"], writes=["qT"]))
        for blk in range(4):
            proj_fm(wh, whk, 4, blk * 512, 512, lambda bap, bkey, blk=blk: P.op(
                "act", lambda e: e.activation(out=gtmp[:, blk * 512:(blk + 1) * 512], in_=bap, func=AF.Silu), reads=[bkey, "ee"], writes=["ee"]))
        P.op("dve", lambda e: e.tensor_scalar(out=gsT, in0=gtmp, scalar1=hng[:, 0:1], scalar2=None, op0=ALU.mult), reads=["ee", "hng"], writes=["gsT"])
        P.dma("sp", dm_gs[h], gsT, "st_gs", reads=["gsT"], writes=["dm_gs"])
        for g4 in range(5):
            tiles = list(range(g4 * 4, min(18, g4 * 4 + 4)))
            bk = PB + (pbc[0] % 2)
            pbc[0] += 1

            def mmv(e, tiles=tiles, bk=bk, wh=wh):
                for i, ti in enumerate(tiles):
                    for kt in range(8):
                        r = e.matmul(banks[bk][:, i * 128:(i + 1) * 128], lhsT=uT[:, kt, ti * 128:(ti + 1) * 128], rhs=wh[:, kt, 1, :], start=(kt == 0), stop=(kt == 7))
                return r
            P.op("pe", mmv, reads=[whk] + ["uT_%d" % t for t in tiles], writes=["bank%d" % bk])
            nt_ = len(tiles)
            P.op("act", lambda e, tiles=tiles, bk=bk, nt_=nt_: e.copy(out=v_tm[:, tiles[0]:tiles[0] + nt_, :], in_=banks[bk][:, 0:nt_ * 128].rearrange("p (a b) -> p a b", b=128)),
                 reads=["bank%d" % bk, "v_tm"], writes=["v_tm"])
        hgrn_dir_prep(h, 0)
        hgrn_dir_prep(h, 1)
        gens = [hgrn_dir_scan(h, 0), hgrn_dir_scan(h, 1)]
        alive = [True, True]
        while any(alive):
            for i in range(2):
                if alive[i]:
                    try:
                        next(gens[i])
                    except StopIteration:
                        alive[i] = False
        P.dma("sp", dm_oloc[h], o_acc, "st_oloc", reads=["oacc_%d" % g for g in range(4)], writes=["dm_oloc"])
    for d in range(2):
        P.dma("sp", ag2_ins[d][:, 0:1024], stage[:, d, :, :].rearrange("p b c -> p (b c)"), "st_ag2", reads=["stage_%d_%d" % (d, h) for h in range(8)], writes=["ag2_in"])
        P.dma("sp", ag2_ins[d][:, 1024:1032], Dv[:, d, :], "st_ag2", reads=["Dv_%d_%d" % (d, h) for h in range(8)], writes=["ag2_in"])
    for d in range(2):
        P.custom("pool", lambda e, d=d: e.collective_compute("AllGather", ALU.bypass, replica_groups=[[0, 1, 2, 3], [4, 5, 6, 7]], ins=[ag2_ins[d]], outs=[ag2_outs[d]]),
                 "cc2", 1, reads=["ag2_in"] + (["ag2_out"] if d > 0 else []), writes=["ag2_out"])
    P.barrier(keep=["ag1_out", "dm_kvctx", "dm_mod", "ag2_out", "dm_oloc", "dm_qb", "dm_gs"] + UT_KEYS)
    A.release(m3h)
    gath = A.alloc((2, 4, 1032), F32)
    Rr = A.alloc((2, 8, 128), F32)
    Tt = A.alloc((8, 128), F32)
    selv = A.alloc((8,), F32)
    Sin = A.alloc((2, 8, 128), BF16)
    for d in range(2):
        P.dma("sp", gath[:, d, :, :], ag2_outs[d].rearrange("(r p) n -> p r n", p=128), "ld_gath", reads=["ag2_out"], writes=["gath"])
    P.dma("sp", selv, sel_d, "ld_c", writes=["selv"])
    SCK = ["sctx_%d_%d" % (d, h) for d in range(2) for h in range(8)]
    P.op("dve", lambda e: e.tensor_copy(out=Rr.rearrange("p a b c -> p (a b c)"), in_=sctx.rearrange("p a b c -> p (a b c)")), writes=["Rr"])
    for d in range(2):
        order = [0, 1, 2, 3] if d == 0 else [3, 2, 1, 0]
        for r in order:
            Sl = gath[:, d, r, 0:1024].rearrange("p (h v) -> p h v", v=128)
            Dr = gath[:, d, r, 1024:1032]
            P.op("dve", lambda e, d=d, Dr=Dr: e.tensor_tensor(out=Tt, in0=Rr[:, d, :, :], in1=bc(Dr, 2, [128, 8, 128]), op=ALU.mult), reads=["Rr", "gath"], writes=["Tt"])
            P.op("dve", lambda e, Sl=Sl: e.tensor_tensor(out=Tt, in0=Tt, in1=Sl, op=ALU.add), reads=["Tt", "gath"], writes=["Tt"])
            P.op("dve", lambda e, d=d: e.tensor_tensor(out=Tt, in0=Tt, in1=Rr[:, d, :, :], op=ALU.subtract), reads=["Tt", "Rr"], writes=["Tt"])
            P.op("dve", lambda e, d=d, r=r: e.scalar_tensor_tensor(out=Rr[:, d, :, :], in0=Tt, scalar=selv[:, d * 4 + r:d * 4 + r + 1], in1=Rr[:, d, :, :], op0=ALU.mult, op1=ALU.add),
                 reads=["Tt", "Rr", "selv"], writes=["Rr"])
    P.op("act", lambda e: e.copy(out=Sin.rearrange("p a b c -> p (a b c)"), in_=Rr.rearrange("p a b c -> p (a b c)")), reads=["Rr"], writes=["Sin"])
    ol = A.alloc((NT,), F32)
    qbf_ = A.alloc((NT,), BF16)
    qbb_ = A.alloc((NT,), BF16)
    gsl = A.alloc((NT,), BF16)
    sqb = A.alloc((NT,), BF16)
    lnr = A.alloc((NT,), F32)
    rsr = A.alloc((NT,), F32)
    for h in range(8):
        P.dma("sp", ol, dm_oloc[h], "ld_ol", reads=["dm_oloc"], writes=["ol"] + ["ol_%d" % b_ for b_ in range(4)])
        P.dma("sp", qbf_, dm_qb[0, h], "ld_qb0", reads=["dm_qb"], writes=["qbf_"])
        P.dma("sp", qbb_, dm_qb[1, h], "ld_qb1", reads=["dm_qb"], writes=["qbb_"])
        P.dma("sp", gsl, dm_gs[h], "ld_gs", reads=["dm_gs"], writes=["gsl"])
        for blk in range(4):
            cs = slice(blk * 512, (blk + 1) * 512)
            bk = blk % 2

            def corr(e, h=h, cs=cs, bk=bk):
                e.matmul(banks[bk][:, :], lhsT=Sin[:, 0, h, :], rhs=qbf_[:, cs], start=True, stop=False)
                return e.matmul(banks[bk][:, :], lhsT=Sin[:, 1, h, :], rhs=qbb_[:, cs], start=False, stop=True)
            P.op("pe", corr, reads=["Sin", "qbf_", "qbb_"], writes=["bank%d" % bk])
            P.op("dve", lambda e, cs=cs, bk=bk: e.tensor_tensor(out=ol[:, cs], in0=ol[:, cs], in1=banks[bk][:, :], op=ALU.add), reads=["bank%d" % bk, "ol", "ol_%d" % blk], writes=["ol_%d" % blk])
            P.op("act", lambda e, cs=cs: e.activation(out=sqb[:, cs], in_=ol[:, cs], func=AF.Square), reads=["ol_%d" % blk], writes=["sqb_%d" % blk])
            bk2 = 2 + blk % 2
            P.op("pe", lambda e, cs=cs, bk2=bk2: e.matmul(banks[bk2][:, :], lhsT=ones_b, rhs=sqb[:, cs], start=True, stop=True), reads=["sqb_%d" % blk, "ones_b"], writes=["bank%d" % bk2])
            P.op("act", lambda e, cs=cs, bk2=bk2: e.activation(out=lnr[:, cs], in_=banks[bk2][:, :], func=AF.Ln, scale=1.0 / 128, bias=EPS), reads=["bank%d" % bk2], writes=["lnr_%d" % blk])
            P.op("act", lambda e, cs=cs: e.activation(out=rsr[:, cs], in_=lnr[:, cs], func=AF.Exp, scale=-0.5), reads=["lnr_%d" % blk], writes=["rsr_%d" % blk])
            P.op("dve", lambda e, cs=cs: e.tensor_tensor(out=ol[:, cs], in0=ol[:, cs], in1=rsr[:, cs], op=ALU.mult), reads=["ol_%d" % blk, "rsr_%d" % blk], writes=["ol_%d" % blk])
            P.op("dve", lambda e, cs=cs: e.tensor_tensor(out=sqb[:, cs], in0=ol[:, cs], in1=gsl[:, cs], op=ALU.mult), reads=["ol_%d" % blk, "gsl"], writes=["sqb_%d" % blk])
        P.dma("sp", dm_oth[h], sqb, "st_oth", reads=["sqb_%d" % b_ for b_ in range(4)], writes=["dm_oth"])
        for b_ in range(4):
            for nm in ("ol_%d", "sqb_%d", "lnr_%d", "rsr_%d", "oth_%d"):
                pass
    P.barrier(keep=["ag1_out", "dm_kvctx", "dm_mod", "dm_oth"] + UT_KEYS)
    A.release(m3)
    if upto <= 3:
        return finish(P, nc)


    m4 = A.mark()
    QT = A.alloc((8, NT), BF16)
    KTa = A.alloc((2, 8448), BF16)
    Va = A.alloc((66, 256), BF16)
    for r in range(4):
        for h in range(2):
            P.dma("sp", KTa[:, h, r * NT:(r + 1) * NT], ag1_outs[h][r * 128:(r + 1) * 128, :], "ld_kta", reads=["ag1_out"], writes=["KTa"])
            P.dma("sp", Va[:, r * 16 + 8 * h:r * 16 + 8 * h + 8, :], ag1_outs[2 + h][r * 128:(r + 1) * 128, :].rearrange("p (ti c) -> p ti c", c=256), "ld_va", reads=["ag1_out"], writes=["Va"])
    P.dma("sp", KTa[:, :, 8192:8448], dm_kvctx[0:256, :].rearrange("(h p) t -> p h t", p=128), "ld_kta", reads=["dm_kvctx"], writes=["KTa"])
    P.dma("sp", Va[:, 64:66, :], dm_kvctx[256:512, :].rearrange("(ti p) c -> p ti c", p=128), "ld_va", reads=["dm_kvctx"], writes=["Va"])
    m4b = A.mark()
    wq = A.alloc((8, 1024), BF16)
    P.dma("pool", wq, win_d[:, 5120:6144].rearrange("(kt p) n -> p kt n", p=128), "ld_wq", writes=["wq"])
    ropeT = A.alloc((NTT, 256), F32)
    P.dma("sp", ropeT, rope_d.rearrange("(t p) n -> p t n", p=128), "ld_rope", writes=["ropeT"])
    gqk = A.alloc((256,), F32)
    P.dma("sp", gqk, qkg_d[0].partition_broadcast(128), "ld_c", writes=["gqk"])
    ssq = A.alloc((16, 8), F32)
    lnq = A.alloc((16, 8), F32)
    rsq = A.alloc((16, 8), F32)
    qn = A.alloc((8, 128), F32)
    t1q = A.alloc((8, 128), F32)
    t2q = A.alloc((8, 128), F32)
    qbf = A.alloc((1024,), BF16)
    junk2 = A.alloc((128,), BF16)
    P.op("pool", lambda e: e.memset(ssq, 0.0), writes=["ssq"])
    for ti in range(NTT):
        s = ti % 2
        for half in range(2):
            def mmq(e, ti=ti, half=half):
                for kt in range(8):
                    r = e.matmul(banks[half][:, :], lhsT=uT[:, kt, ti * 128:(ti + 1) * 128], rhs=wq[:, kt, half * 512:(half + 1) * 512], start=(kt == 0), stop=(kt == 7))
                return r
            P.op("pe", mmq, reads=["uT_%d" % ti, "wq"], writes=["bank%d" % half])
        for h in range(8):
            P.op("act", lambda e, h=h, ti=ti: e.activation(out=junk2, in_=banks[h // 4][:, (h % 4) * 128:(h % 4 + 1) * 128], func=AF.Square, accum_out=ssq[:, ti, h:h + 1]),
                 reads=["bank%d" % (h // 4), "ssq"], writes=["junk2", "ssq_%d" % ti])
        rstd_from_ss(ssq[:, ti, :], 128, rsq[:, ti, :], lnq[:, ti, :], ["ssq_%d" % ti], "rsq_%d" % ti)
        for half in range(2):
            P.op("dve", lambda e, half=half, ti=ti: e.tensor_tensor(out=qn[:, half * 4:(half + 1) * 4, :], in0=banks[half][:, :].rearrange("p (h d) -> p h d", d=128),
                                                                in1=bc(rsq[:, ti, half * 4:(half + 1) * 4], 2, [128, 4, 128]), op=ALU.mult),
                 reads=["bank%d" % half, "rsq_%d" % ti, "qxn"], writes=["qxn"])
        P.op("dve", lambda e: e.tensor_tensor(out=qn, in0=qn, in1=bc(gqk[:, 0:128], 1, [128, 8, 128]), op=ALU.mult), reads=["qxn", "gqk"], writes=["qxn"])
        rope_apply(qn, 8, ti, qbf, "q")
        bk2 = 2 + s

        def trq(e, bk2=bk2):
            for h in range(8):
                r = e.transpose(bank_bf(bk2)[:, h * 128:(h + 1) * 128], qbf[:, h * 128:(h + 1) * 128], ident_b)
            return r
        P.op("pe", trq, reads=["qbf", "ident_b"], writes=["bank%d" % bk2])
        P.op("act", lambda e, ti=ti, bk2=bk2: e.copy(out=QT[:, :, ti * 128:(ti + 1) * 128], in_=bank_bf(bk2).rearrange("p (a b) -> p a b", b=128)),
             reads=["bank%d" % bk2], writes=["QT"])
    P.barrier(keep=["dm_mod", "dm_oth", "KTa", "Va"] + UT_KEYS)
    A.release(m4b)
    if upto <= 4:
        return finish(P, nc)

    pT = [A.alloc((512,), BF16) for _ in range(3)]
    rden = A.alloc((512,), F32)
    ob = [A.alloc((512,), BF16) for _ in range(2)]
    dacc = [A.alloc((512,), F32) for _ in range(2)]
    SCALE = float(128 ** -0.5)
    NKT = 66
    it = 0
    for kvh in range(2):
        for qb in range(4):
            for g in range(4):
                head = kvh * 4 + g
                bo, bd = 4 + it % 2, 6 + it % 2
                qs = slice(qb * 512, (qb + 1) * 512)

                def s_mm(kt, kvh=kvh, head=head, qs=qs):
                    P.op("pe", lambda e: e.matmul(banks[kt % 3][:, :], lhsT=KTa[:, kvh, kt * 128:(kt + 1) * 128], rhs=QT[:, head, qs], start=True, stop=True),
                         reads=["KTa", "QT"], writes=["bank%d" % (kt % 3)])
                s_mm(0)
                for kt in range(NKT):
                    if kt + 1 < NKT:
                        s_mm(kt + 1)
                    P.op("act", lambda e, kt=kt: e.activation(out=pT[kt % 3], in_=banks[kt % 3][:, :], func=AF.Exp, scale=SCALE),
                         reads=["bank%d" % (kt % 3)], writes=["pT%d" % (kt % 3)])

                    P.op("pe", lambda e, kt=kt, kvh=kvh, bo=bo: e.matmul(banks[bo][:, :], lhsT=Va[:, kt, kvh * 128:(kvh + 1) * 128], rhs=pT[kt % 3], start=(kt == 0), stop=(kt == NKT - 1)),
                         reads=["pT%d" % (kt % 3), "Va"], writes=["bank%d" % bo])
                    da = dacc[0]
                    if kt % 3 == 2:
                        P.op("pe", lambda e, kt=kt, bd=bd: e.matmul(banks[bd][:, :], lhsT=ones_b, rhs=pT[kt % 3], start=(kt == 2), stop=False),
                             reads=["pT%d" % (kt % 3), "ones_b"], writes=["bank%d" % bd])
                    elif kt == 0:
                        P.op("dve", lambda e, kt=kt, da=da: e.tensor_copy(out=da, in_=pT[kt % 3]), reads=["pT%d" % (kt % 3)], writes=["dacc0"])
                    else:
                        P.op("dve", lambda e, kt=kt, da=da: e.tensor_tensor(out=da, in0=da, in1=pT[kt % 3], op=ALU.add), reads=["pT%d" % (kt % 3), "dacc0"], writes=["dacc0"])

                def dsum(e, bd=bd):
                    return e.matmul(banks[bd][:, :], lhsT=ones_f, rhs=dacc[0], start=False, stop=True)
                P.op("pe", dsum, reads=["dacc0", "ones_f"], writes=["bank%d" % bd])
                P.op("dve", lambda e, bd=bd: e.reciprocal(out=rden, in_=banks[bd][:, :]), reads=["bank%d" % bd], writes=["rden"])
                P.op("dve", lambda e, bo=bo, it=it: e.tensor_tensor(out=ob[it % 2], in0=banks[bo][:, :], in1=rden, op=ALU.mult), reads=["bank%d" % bo, "rden"], writes=["ob%d" % (it % 2)])
                P.dma("sp", dm_ota[head][:, qs], ob[it % 2], "st_ota%d" % (it % 2), reads=["ob%d" % (it % 2)], writes=["dm_ota"])
                it += 1
    P.barrier(keep=["dm_mod", "dm_oth", "dm_ota"] + UT_KEYS)
    A.release(m4)
    if upto <= 5:
        return finish(P, nc)

    m6 = A.mark()
    wg = A.alloc((8, 2048), BF16)
    wb0 = A.alloc((8, 1024), BF16)
    wb1 = A.alloc((8, 1024), BF16)
    wo = A.alloc((8, 1024), BF16)
    P.dma("pool", wg, win_d[:, 6656:8704].rearrange("(kt p) n -> p kt n", p=128), "ld_w6", writes=["wg"])
    P.dma("pool", wb0, wbr_d[0].rearrange("(kt p) n -> p kt n", p=128), "ld_w6", writes=["wb0"])
    P.dma("pool", wb1, wbr_d[1].rearrange("(kt p) n -> p kt n", p=128), "ld_w6", writes=["wb1"])
    P.dma("pool", wo, wout_d.rearrange("(kt p) n -> p kt n", p=128), "ld_w6", writes=["wo"])
    G1 = A.alloc((D,), F32)
    A2 = A.alloc((D,), F32)
    B2 = A.alloc((D,), F32)
    P.dma("sp", G1, dm_mod[:, 2 * D:3 * D], "ld_c", reads=["dm_mod"], writes=["G1"])
    P.dma("sp", A2, dm_mod[:, 4 * D:5 * D], "ld_c", reads=["dm_mod"], writes=["A2"])
    P.dma("sp", B2, dm_mod[:, 3 * D:4 * D], "ld_c", reads=["dm_mod"], writes=["B2"])
    rwt = A.alloc((8, NE), F32)
    rbt = A.alloc((NE,), F32)
    P.dma("sp", rwt, rw_d.rearrange("(kt p) e -> p kt e", p=128), "ld_c", writes=["rwt"])
    P.dma("sp", rbt, rb_d[0].partition_broadcast(128), "ld_c", writes=["rbt"])
    othb = A.alloc((8, 512), BF16)
    otab = A.alloc((8, 512), BF16)
    y1T = A.alloc((8, 512), BF16)
    sgh = [A.alloc((512,), F32)] * 2
    sga = [A.alloc((512,), F32)] * 2
    tA = [A.alloc((512,), F32)] * 2
    tB = [A.alloc((512,), F32)] * 2
    xt6 = [A.alloc((D,), F32) for _ in range(2)]
    tmp6 = A.alloc((D,), F32)
    x1t = [A.alloc((D,), F32)] * 2
    u2f = A.alloc((D,), F32)
    u2b = A.alloc((D,), BF16)
    junk6 = A.alloc((D,), BF16)
    ssy = A.alloc((16, 2), F32)
    ssy1 = A.alloc((16,), F32)
    lny = A.alloc((16,), F32)
    rsy = A.alloc((16,), F32)
    ssx = A.alloc((16,), F32)
    lnx = A.alloc((16,), F32)
    rsx = A.alloc((16,), F32)
    u2Tf = A.alloc((8, 128), F32)
    u2Tb = [A.alloc((8, 128), BF16) for _ in range(2)]
    lg = A.alloc((NE,), F32)
    mx8 = A.alloc((8,), F32)
    msk = A.alloc((NE,), F32)
    em = A.alloc((NE,), F32)
    nmx = A.alloc((1,), F32)
    ssum = A.alloc((1,), F32)
    rsum = A.alloc((1,), F32)
    cmb = A.alloc((16, NE), F32)
    cT = A.alloc((128,), F32)
    P.op("pool", lambda e: e.memset(ssy, 0.0), writes=["ssy"])
    P.op("pool", lambda e: e.memset(ssx, 0.0), writes=["ssx"])
    oth_v = dm_oth.rearrange("h p t -> p h t")
    ota_v = dm_ota.rearrange("h p t -> p h t")
    for blk in range(4):
        cs = slice(blk * 512, (blk + 1) * 512)
        P.dma("sp", othb, oth_v[:, :, cs], "ld_oth", reads=["dm_oth"], writes=["othb"])
        P.dma("sp", otab, ota_v[:, :, cs], "ld_ota", reads=["dm_ota"], writes=["otab"])
        utk = ["uT_%d" % t for t in range(blk * 4, blk * 4 + 4)]
        for dt in range(8):
            s = 0
            ds = slice(dt * 128, (dt + 1) * 128)

            def mm4(e, ds=ds, dt=dt, cs=cs):
                for kt in range(8):
                    e.matmul(banks[0][:, :], lhsT=wg[:, kt, dt * 128:(dt + 1) * 128], rhs=uT[:, kt, cs], start=(kt == 0), stop=(kt == 7))
                for kt in range(8):
                    e.matmul(banks[1][:, :], lhsT=wg[:, kt, 1024 + dt * 128:1024 + (dt + 1) * 128], rhs=uT[:, kt, cs], start=(kt == 0), stop=(kt == 7))
                for kt in range(8):
                    e.matmul(banks[2][:, :], lhsT=wb0[:, kt, ds], rhs=othb[:, kt, :], start=(kt == 0), stop=(kt == 7))
                for kt in range(8):
                    r = e.matmul(banks[3][:, :], lhsT=wb1[:, kt, ds], rhs=otab[:, kt, :], start=(kt == 0), stop=(kt == 7))
                return r
            P.op("pe", mm4, reads=["wg", "wb0", "wb1", "othb", "otab"] + utk, writes=["bank0", "bank1", "bank2", "bank3"])
            P.op("act", lambda e, s=s: e.activation(out=sgh[s], in_=banks[0][:, :], func=AF.Sigmoid), reads=["bank0"], writes=["sgh%d" % s])
            P.op("act", lambda e, s=s: e.activation(out=sga[s], in_=banks[1][:, :], func=AF.Sigmoid), reads=["bank1"], writes=["sga%d" % s])
            P.op("dve", lambda e, s=s: e.tensor_tensor(out=tA[s], in0=sgh[s], in1=banks[2][:, :], op=ALU.mult), reads=["sgh%d" % s, "bank2"], writes=["tA%d" % s])
            P.op("dve", lambda e, s=s: e.tensor_tensor(out=tB[s], in0=sga[s], in1=banks[3][:, :], op=ALU.mult), reads=["sga%d" % s, "bank3"], writes=["tB%d" % s])
            P.op("dve", lambda e, s=s, dt=dt: e.tensor_tensor(out=y1T[:, dt, :], in0=tA[s], in1=tB[s], op=ALU.add), reads=["tA%d" % s, "tB%d" % s], writes=["y1T"])
        for tt in range(4):
            ti = blk * 4 + tt
            s = ti % 2
            ts_ = slice(tt * 128, (tt + 1) * 128)
            P.dma("sp", xt6[s], x_d[ti * 128:(ti + 1) * 128, :], "ld_x6%d" % s, writes=["xt6%d" % s])
            for half in range(2):
                def mmy(e, half=half, ts_=ts_):
                    for kt in range(8):
                        r = e.matmul(banks[4 + half][:, :], lhsT=y1T[:, kt, ts_], rhs=wo[:, kt, half * 512:(half + 1) * 512], start=(kt == 0), stop=(kt == 7))
                    return r
                P.op("pe", mmy, reads=["y1T", "wo"], writes=["bank%d" % (4 + half)])
                P.op("act", lambda e, half=half, ti=ti: e.activation(out=junk6[:, 0:512], in_=banks[4 + half][:, :], func=AF.Square, accum_out=ssy[:, ti, half:half + 1]),
                     reads=["bank%d" % (4 + half), "ssy"], writes=["junk6", "ssy_%d_%d" % (ti, half)])
            P.op("dve", lambda e, ti=ti: e.tensor_tensor(out=ssy1[:, ti:ti + 1], in0=ssy[:, ti, 0:1], in1=ssy[:, ti, 1:2], op=ALU.add),
                 reads=["ssy_%d_0" % ti, "ssy_%d_1" % ti], writes=["ssy1_%d" % ti])
            rstd_from_ss(ssy1[:, ti:ti + 1], D, rsy[:, ti:ti + 1], lny[:, ti:ti + 1], ["ssy1_%d" % ti], "rsy_%d" % ti)
            for half in range(2):
                hs = slice(half * 512, (half + 1) * 512)
                P.op("dve", lambda e, half=half, hs=hs, ti=ti: e.scalar_tensor_tensor(out=tmp6[:, hs], in0=banks[4 + half][:, :], scalar=rsy[:, ti:ti + 1], in1=G1[:, hs], op0=ALU.mult, op1=ALU.mult),
                     reads=["bank%d" % (4 + half), "rsy_%d" % ti, "G1", "tmp6"], writes=["tmp6"])
            P.op("dve", lambda e, s=s: e.tensor_tensor(out=x1t[s], in0=tmp6, in1=xt6[s], op=ALU.add), reads=["tmp6", "xt6%d" % s], writes=["x1t"])
            P.dma("sp", dm_x1[ti * 128:(ti + 1) * 128, :], x1t[s], "st_x1%d" % s, reads=["x1t"], writes=["dm_x1"])
            P.op("act", lambda e, s=s, ti=ti: e.activation(out=junk6, in_=x1t[s], func=AF.Square, accum_out=ssx[:, ti:ti + 1]), reads=["x1t", "ssx"], writes=["junk6", "ssx_%d" % ti])
            rstd_from_ss(ssx[:, ti:ti + 1], D, rsx[:, ti:ti + 1], lnx[:, ti:ti + 1], ["ssx_%d" % ti], "rsx_%d" % ti)
            P.op("dve", lambda e, s=s, ti=ti: e.scalar_tensor_tensor(out=tmp6, in0=x1t[s], scalar=rsx[:, ti:ti + 1], in1=A2, op0=ALU.mult, op1=ALU.mult),
                 reads=["x1t", "rsx_%d" % ti, "A2", "tmp6"], writes=["tmp6"])
            P.op("dve", lambda e: e.tensor_tensor(out=u2f, in0=tmp6, in1=B2, op=ALU.add), reads=["tmp6", "B2"], writes=["u2f"])
            P.op("act", lambda e: e.copy(out=u2b, in_=u2f), reads=["u2f"], writes=["u2b"])

            def tru(e):
                for kt in range(8):
                    r = e.transpose(bank_bf(6)[:, kt * 128:(kt + 1) * 128], u2b[:, kt * 128:(kt + 1) * 128], ident_b)
                return r
            P.op("pe", tru, reads=["u2b", "ident_b"], writes=["bank6"])
            P.op("act", lambda e, s=s: e.copy(out=u2Tb[s], in_=bank_bf(6).rearrange("p (a b) -> p a b", b=128)), reads=["bank6"], writes=["u2Tb%d" % s])
            P.dma("sp", dm_u2t[:, :, ti * 128:(ti + 1) * 128], u2Tb[s], "st_u2t%d" % s, reads=["u2Tb%d" % s], writes=["dm_u2t"])
            for g2 in range(2):
                def truf(e, g2=g2):
                    for i in range(4):
                        kt = g2 * 4 + i
                        r = e.transpose(banks[7][:, i * 128:(i + 1) * 128], u2f[:, kt * 128:(kt + 1) * 128], ident_f)
                    return r
                P.op("pe", truf, reads=["u2f", "ident_f"], writes=["bank7"])
                P.op("act", lambda e, g2=g2: e.copy(out=u2Tf[:, g2 * 4:(g2 + 1) * 4, :], in_=banks[7][:, :].rearrange("p (a b) -> p a b", b=128)), reads=["bank7", "u2Tf"], writes=["u2Tf"])

            def mml(e):
                for kt in range(8):
                    r = e.matmul(banks[6][:, 0:NE], lhsT=u2Tf[:, kt, :], rhs=rwt[:, kt, :], start=(kt == 0), stop=(kt == 7))
                return r
            P.op("pe", mml, reads=["u2Tf", "rwt"], writes=["bank6"])
            P.op("dve", lambda e: e.tensor_tensor(out=lg, in0=banks[6][:, 0:NE], in1=rbt, op=ALU.add), reads=["bank6", "rbt"], writes=["lg"])
            P.op("dve", lambda e: e.max(out=mx8, in_=lg), reads=["lg"], writes=["mx8"])
            P.op("dve", lambda e: e.tensor_scalar(out=msk, in0=lg, scalar1=mx8[:, 3:4], scalar2=None, op0=ALU.is_ge), reads=["lg", "mx8"], writes=["msk"])
            P.op("dve", lambda e: e.tensor_scalar(out=nmx, in0=mx8[:, 0:1], scalar1=-1.0, scalar2=None, op0=ALU.mult), reads=["mx8"], writes=["nmx"])
            P.op("act", lambda e: e.activation(out=em, in_=lg, func=AF.Exp, bias=nmx[:, 0:1], scale=1.0), reads=["lg", "nmx"], writes=["em"])
            P.op("dve", lambda e: e.tensor_tensor(out=em, in0=em, in1=msk, op=ALU.mult), reads=["em", "msk"], writes=["em"])
            P.op("dve", lambda e: e.reduce_sum(out=ssum, in_=em, axis=AX.X), reads=["em"], writes=["ssum"])
            P.op("dve", lambda e: e.reciprocal(out=rsum, in_=ssum), reads=["ssum"], writes=["rsum"])
            P.op("dve", lambda e, ti=ti: e.tensor_scalar(out=cmb[:, ti, :], in0=em, scalar1=rsum[:, 0:1], scalar2=None, op0=ALU.mult), reads=["em", "rsum"], writes=["cmb_%d" % ti])
            P.op("pe", lambda e, ti=ti: e.transpose(banks[7][0:NE, 0:128], cmb[:, ti, :], ident_f), reads=["cmb_%d" % ti, "ident_f"], writes=["bank7"])
            P.op("act", lambda e: e.copy(out=cT[0:NE, :], in_=banks[7][0:NE, 0:128]), reads=["bank7"], writes=["cT"])
            P.dma("sp", dm_combT[:, ti * 128:(ti + 1) * 128], cT[0:NE, :], "st_cT", reads=["cT"], writes=["dm_combT"])
    P.dma("sp", dm_comb, cmb, "st_cmb", reads=["cmb_%d" % t for t in range(16)], writes=["dm_comb"])
    P.barrier(keep=["dm_mod", "dm_x1", "dm_u2t", "dm_comb", "dm_combT"])
    A.release(m_pre_ut)
    if upto <= 6:
        return finish(P, nc)

    G2 = A.alloc((D,), F32)
    P.dma("sp", G2, dm_mod[:, 5 * D:6 * D], "ld_c", reads=["dm_mod"], writes=["G2"])
    bu = A.alloc((NE * 16,), F32)
    P.dma("sp", bu, bupT_d, "ld_c", writes=["bu"])
    bdn = A.alloc((D,), F32)
    P.dma("sp", bdn[0:NE, :], bdn_d, "ld_c", writes=["bdn"])
    cmb7 = A.alloc((16, NE), F32)
    P.dma("sp", cmb7, dm_comb, "ld_c", reads=["dm_comb"], writes=["cmb7"])
    P.op("dve", lambda e: e.tensor_scalar(out=cmb7, in0=cmb7, scalar1=1.0 / 1.702, scalar2=None, op0=ALU.mult), reads=["cmb7"], writes=["cmb7"])
    bu1 = A.alloc((NE * 16,), F32)
    P.op("dve", lambda e: e.tensor_scalar(out=bu1, in0=bu, scalar1=1.0, scalar2=None, op0=ALU.add), reads=["bu"], writes=["bu1"])
    cT2 = A.alloc((1024,), F32)
    u2T = A.alloc((8, 1024), BF16)
    acc = A.alloc((8, D), F32)
    wu = [A.alloc((8, 2 * D), BF16) for _ in range(2)]
    wd = [A.alloc((8, D), BF16) for _ in range(2)]
    aTraw = [A.alloc((4096,), BF16) for _ in range(2)]
    aT = [a.rearrange("p (a b) -> p a b", b=512) for a in aTraw]
    gc = [A.alloc((512,), F32) for _ in range(2)]
    sgm = [A.alloc((512,), F32) for _ in range(2)]
    lc = [A.alloc((512,), F32) for _ in range(2)]
    xt7 = [aTraw[0][:, 0:2048].bitcast(F32)] * 2
    tmp7 = aTraw[0][:, 2048:4096].bitcast(F32)
    ot7 = [aTraw[1][:, 0:2048].bitcast(F32)] * 2
    junk7 = aTraw[1][:, 2048:3072]
    ss7 = A.alloc((16,), F32)
    ln7 = A.alloc((16,), F32)
    rs7 = A.alloc((16,), F32)
    P.op("pool", lambda e: e.memset(ss7, 0.0), writes=["ss7"])
    ecount = 0
    for half in range(2):
        hc = slice(half * 1024, (half + 1) * 1024)
        P.dma("sp", u2T, dm_u2t[:, :, hc], "ld_u2t", reads=["dm_u2t"], writes=["u2T"])
        P.dma("sp", cT2[0:NE, :], dm_combT[:, hc], "ld_cT2", reads=["dm_combT"], writes=["cT2"])
        for tt in range(8):
            for dh in range(2):
                bk = 4 + (tt * 2 + dh) % 4
                P.op("pe", lambda e, tt=tt, dh=dh, bk=bk: e.matmul(banks[bk][:, :], lhsT=cT2[0:NE, tt * 128:(tt + 1) * 128], rhs=bdn[0:NE, dh * 512:(dh + 1) * 512], start=True, stop=True),
                     reads=["cT2", "bdn"], writes=["bank%d" % bk])
                P.op("act", lambda e, tt=tt, dh=dh, bk=bk: e.copy(out=acc[:, tt, dh * 512:(dh + 1) * 512], in_=banks[bk][:, :]), reads=["bank%d" % bk], writes=["acc_%d_%d" % (tt, dh)])
        for ex in range(NE):
            ws = ecount % 2
            ecount += 1
            P.dma("pool", wu[ws], wup_d[ex].rearrange("(kt p) n -> p kt n", p=128), "ld_wu%d" % ws, writes=["wu%d" % ws])
            P.dma("pool", wd[ws], wdn_d[ex].rearrange("(kt p) n -> p kt n", p=128), "ld_wd%d" % ws, writes=["wd%d" % ws])
            for blk in range(2):
                bs = slice(blk * 512, (blk + 1) * 512)
                ab = aT[blk % 2]
                abk = "aT%d" % (blk % 2)
                for g in range(8):
                    s = g % 2
                    bg, bl = g % 2, 2 + g % 2

                    def mmu(e, g=g, bg=bg, bl=bl, ws=ws, bs=bs):
                        for kt in range(8):
                            e.matmul(banks[bg][:, :], lhsT=wu[ws][:, kt, g * 128:(g + 1) * 128], rhs=u2T[:, kt, bs], start=(kt == 0), stop=(kt == 7))
                        for kt in range(8):
                            r = e.matmul(banks[bl][:, :], lhsT=wu[ws][:, kt, 1024 + g * 128:1024 + (g + 1) * 128], rhs=u2T[:, kt, bs], start=(kt == 0), stop=(kt == 7))
                        return r
                    P.op("pe", mmu, reads=["wu%d" % ws, "u2T"], writes=["bank%d" % bg, "bank%d" % bl])
                    P.op("dve", lambda e, s=s, bg=bg, ex=ex, g=g: e.tensor_scalar(out=gc[s], in0=banks[bg][:, :], scalar1=bu[:, ex * 16 + g:ex * 16 + g + 1], scalar2=7.0, op0=ALU.add, op1=ALU.min),
                         reads=["bank%d" % bg, "bu"], writes=["gc%d" % s])
                    P.op("act", lambda e, s=s: e.activation(out=sgm[s], in_=gc[s], func=AF.Silu, scale=1.702), reads=["gc%d" % s], writes=["sgm%d" % s])
                    P.op("dve", lambda e, s=s, bl=bl, ex=ex, g=g: e.tensor_scalar(out=lc[s], in0=banks[bl][:, :], scalar1=bu1[:, ex * 16 + 8 + g:ex * 16 + 8 + g + 1], scalar2=8.0, op0=ALU.add, op1=ALU.min),
                         reads=["bank%d" % bl, "bu1"], writes=["lc%d" % s])
                    P.op("dve", lambda e, s=s, g=g, ab=ab: e.scalar_tensor_tensor(out=ab[:, g, :], in0=lc[s], scalar=-6.0, in1=sgm[s], op0=ALU.max, op1=ALU.mult),
                         reads=["sgm%d" % s, "lc%d" % s], writes=[abk])
                for tt in range(4):
                    til = blk * 4 + tt
                    for dh in range(2):
                        bk = 4 + (tt * 2 + dh) % 4

                        def mmd(e, tt=tt, dh=dh, bk=bk, ws=ws, ab=ab):
                            for fk in range(8):
                                r = e.matmul(banks[bk][:, :], lhsT=ab[:, fk, tt * 128:(tt + 1) * 128], rhs=wd[ws][:, fk, dh * 512:(dh + 1) * 512], start=(fk == 0), stop=(fk == 7))
                            return r
                        P.op("pe", mmd, reads=[abk, "wd%d" % ws], writes=["bank%d" % bk])
                        ak = "acc_%d_%d" % (til, dh)
                        P.op("dve", lambda e, til=til, dh=dh, bk=bk, ex=ex, half=half: e.scalar_tensor_tensor(
                            out=acc[:, til, dh * 512:(dh + 1) * 512], in0=banks[bk][:, :], scalar=cmb7[:, half * 8 + til, ex:ex + 1], in1=acc[:, til, dh * 512:(dh + 1) * 512], op0=ALU.mult, op1=ALU.add),
                            reads=["bank%d" % bk, "cmb7", ak], writes=[ak])
        P.barrier(keep=["dm_x1", "dm_u2t", "dm_combT"])
        for tt in range(8):
            ti = half * 8 + tt
            s = 0
            P.dma("sp", xt7[s], dm_x1[ti * 128:(ti + 1) * 128, :], "ld_x7%d" % s, reads=["dm_x1"], writes=["xt7%d" % s])
            P.op("act", lambda e, tt=tt, ti=ti: e.activation(out=junk7, in_=acc[:, tt, :], func=AF.Square, accum_out=ss7[:, ti:ti + 1]),
                 reads=["acc_%d_0" % tt, "acc_%d_1" % tt, "ss7"], writes=["junk7", "ss7_%d" % ti])
            rstd_from_ss(ss7[:, ti:ti + 1], D, rs7[:, ti:ti + 1], ln7[:, ti:ti + 1], ["ss7_%d" % ti], "rs7_%d" % ti)
            P.op("dve", lambda e, tt=tt, ti=ti: e.scalar_tensor_tensor(out=tmp7, in0=acc[:, tt, :], scalar=rs7[:, ti:ti + 1], in1=G2, op0=ALU.mult, op1=ALU.mult),
                 reads=["acc_%d_0" % tt, "acc_%d_1" % tt, "rs7_%d" % ti, "G2"], writes=["tmp7"])
            P.op("dve", lambda e, s=s: e.tensor_tensor(out=ot7[s], in0=tmp7, in1=xt7[s], op=ALU.add), reads=["tmp7", "xt7%d" % s], writes=["ot7%d" % s])
            P.dma("sp", out_d[ti * 128:(ti + 1) * 128, :], ot7[s], "st_out%d" % s, reads=["ot7%d" % s], writes=["out"])
        P.barrier(keep=["dm_x1", "dm_u2t", "dm_combT"])

    finish(P, nc)
    return nc


def finish(P, nc):
    P.wait_all("sp")
    P.build()
    P.close()
    return nc


def _rope_table(j):
    t = np.arange(NT) + j * NT
    rows = (t // 64).astype(np.float32)
    cols = (t % 64).astype(np.float32)
    inv = (10000.0 ** (-np.arange(0, 64, 2, dtype=np.float32) / 64)).astype(np.float32)
    ar = rows[:, None] * inv[None, :]
    ac = cols[:, None] * inv[None, :]
    cr, sr, cc, sc = np.cos(ar), np.sin(ar), np.cos(ac), np.sin(ac)
    return np.concatenate([cr, cr, cc, cc, -sr, sr, -sc, sc], axis=1).astype(np.float32)


def make_in_maps(inp, small=False):
    f = lambda a: np.ascontiguousarray(np.asarray(a, dtype=np.float32))
    x, c, ctx, c_ctx = f(inp["x"]), f(inp["c"]), f(inp["ctx"]), f(inp["c_ctx"])
    shared = {
        "w_mod": f(inp["w_mod"][0]), "b_mod": f(inp["b_mod"][0]).reshape(1, -1),
        "norm_g": f(inp["norm_g"][0]).reshape(1, -1), "w_in": f(inp["w_in"][0]),
        "lbv": f(np.asarray(inp["hgrn_lb"]).reshape(2, 2, 8, 128).transpose(3, 0, 1, 2).reshape(128, 32)),
        "hng": f(inp["hgrn_norm_g"][0]).reshape(128, 1), "qkg": f(inp["qk_norm_g"][0]).reshape(1, 256),
        "w_branch": f(inp["w_branch"][0]), "w_out": f(inp["w_out"][0]),
        "router_w": f(inp["router_w"][0]), "router_b": f(inp["router_b"][0]).reshape(1, -1),
        "w_up": f(inp["w_up"][0]), "b_upT": f(np.asarray(inp["b_up"][0]).reshape(32, 16, 128).transpose(2, 0, 1).reshape(128, 512)),
        "w_down": f(inp["w_down"][0]), "b_down": f(inp["b_down"][0]),
    }
    ropes = [_rope_table(j) for j in range(4)]
    maps = []
    for core in range(8):
        b, j = core // 4, core % 4
        cvec = np.concatenate([c[b].reshape(8, 128).T, c_ctx.reshape(8, 128).T], axis=1)
        sel = np.zeros((128, 8), np.float32)
        for r in range(4):
            sel[:, r] = 1.0 if r < j else 0.0
            sel[:, 4 + r] = 1.0 if r > j else 0.0
        m = dict(shared)
        if small:
            m["w_up"] = m["w_up"][0:1]
            m["w_down"] = m["w_down"][0:1]
        m.update({"x": f(x[b, j * NT:(j + 1) * NT]), "ctx": f(ctx[b]), "cvec": f(cvec), "rope": ropes[j], "sel": sel})
        maps.append(m)
    return maps


_NC_CACHE = {}


def kernel(**inputs):
    if "nc" not in _NC_CACHE:
        _NC_CACHE["nc"] = build()
    nc = _NC_CACHE["nc"]
    maps = make_in_maps(inputs)
    res = run_bass_kernel_spmd(nc, maps, core_ids=list(range(8)))
    out = np.empty((2, 8192, D), np.float32)
    for core in range(8):
        b, j = core // 4, core % 4
        out[b, j * NT:(j + 1) * NT] = res.results[core]["out"]
    return out
```

```python
from contextlib import ExitStack
import numpy as np
import concourse.bass as bass
import concourse.mybir as mybir
from concourse.bass_utils import run_bass_kernel_spmd

F32 = mybir.dt.float32
BF16 = mybir.dt.bfloat16
ALU = mybir.AluOpType
AF = mybir.ActivationFunctionType
AX = mybir.AxisListType

ENGS = ("pe", "act", "dve", "pool", "sp")
EPOCH = 16000
EPS = 1e-6


def _freeze(fn, memo=None):
    import types
    if memo is None:
        memo = {}
    if not isinstance(fn, types.FunctionType) or fn.__closure__ is None:
        return fn
    if id(fn) in memo:
        return memo[id(fn)]
    cells = []
    for c in fn.__closure__:
        try:
            v = c.cell_contents
        except ValueError:
            cells.append(c)
            continue
        if isinstance(v, types.FunctionType) and v.__closure__ is not None and v is not fn:
            v = _freeze(v, memo)
        cells.append(types.CellType(v))
    new = types.FunctionType(fn.__code__, fn.__globals__, fn.__name__, fn.__defaults__, tuple(cells))
    new.__kwdefaults__ = fn.__kwdefaults__
    memo[id(fn)] = new
    return new


class Prog:
    def __init__(self, nc, same_engine_sync=True):
        self.nc = nc
        self.es = ExitStack()
        self.q = {e: [] for e in ENGS}
        self.cnt = {e: 0 for e in ENGS}
        self.waited = {}
        self.buf = {}
        self.sems = {}
        self.dma_cnt = {}
        self.same_engine_sync = same_engine_sync
        self.n_sem = 0

    def sem(self, key):
        if key not in self.sems:
            self.n_sem += 1
            self.sems[key] = self.es.enter_context(self.nc.semaphore("s%d" % self.n_sem))
        return self.sems[key]

    def sbuf(self, name, shape, dtype):
        return self.es.enter_context(self.nc.sbuf_tensor(name, list(shape), dtype))

    def psum(self, name, shape, dtype=F32):
        return self.es.enter_context(self.nc.psum_tensor(name, list(shape), dtype))

    def _semkey_for(self, prod):
        kind, name, count = prod
        if kind == "e":
            ep = (count - 1) // EPOCH
            return ("e", name, ep), count - ep * EPOCH
        return ("d", name), count

    def _need(self, eng, prod, waits):
        if prod is None:
            return
        kind, name, count = prod
        if kind == "e" and name == eng and (eng in ("pe", "sp") or not self.same_engine_sync):
            return
        sk, val = self._semkey_for(prod)
        wk = (eng, kind, name)
        if self.waited.get(wk, 0) >= count:
            return
        self.waited[wk] = count
        waits.append((sk, val))

    def _deps(self, eng, reads, writes):
        waits = []
        for k in reads:
            b = self.buf.get(k)
            if b is not None:
                self._need(eng, b["w"], waits)
        for k in writes:
            b = self.buf.get(k)
            if b is not None:
                self._need(eng, b["w"], waits)
                for r in b["r"].values():
                    self._need(eng, r, waits)
        return waits

    def _record(self, prod, reads, writes):
        for k in reads:
            b = self.buf.setdefault(k, {"w": None, "r": {}})
            b["r"][(prod[0], prod[1])] = prod
        for k in writes:
            self.buf[k] = {"w": prod, "r": {}}

    def op(self, eng, fn, reads=(), writes=()):
        fn = _freeze(fn)
        waits = self._deps(eng, reads, writes)
        self.cnt[eng] += 1
        prod = ("e", eng, self.cnt[eng])
        sk, _ = self._semkey_for(prod)
        self.q[eng].append((fn, waits, (sk, 1)))
        self._record(prod, reads, writes)
        return prod

    def dma(self, eng, out, in_, semname, reads=(), writes=(), **kw):
        if writes:
            semname = semname + ":" + writes[0]
        waits = self._deps(eng, reads, writes)
        self.dma_cnt[semname] = self.dma_cnt.get(semname, 0) + 16
        prod = ("d", semname, self.dma_cnt[semname])
        self.q[eng].append((lambda e: e.dma_start(out=out, in_=in_, **kw), waits, (("d", semname), 16)))
        self._record(prod, reads, writes)
        return prod

    def custom(self, eng, fn, semname, inc, reads=(), writes=()):
        fn = _freeze(fn)
        waits = self._deps(eng, reads, writes)
        self.dma_cnt[semname] = self.dma_cnt.get(semname, 0) + inc
        prod = ("d", semname, self.dma_cnt[semname])
        self.q[eng].append((fn, waits, (("d", semname), inc)))
        self._record(prod, reads, writes)
        return prod

    def wait_all(self, eng):
        waits = []
        for e in ENGS:
            if self.cnt[e] > 0 and e != eng:
                self._need(eng, ("e", e, self.cnt[e]), waits)
        for name, c in self.dma_cnt.items():
            self._need(eng, ("d", name, c), waits)
        self.q[eng].append((None, waits, None))

    def barrier(self, keep=()):
        for e in ENGS:
            self.wait_all(e)
        self.buf = {k: v for k, v in self.buf.items() if k in keep}

    def build(self):
        nc = self.nc
        keys = []
        for e in ENGS:
            for (_, w, inc) in self.q[e]:
                for x in w:
                    keys.append(x[0])
                if inc:
                    keys.append(inc[0])
        for sk in dict.fromkeys(keys):
            self.sem(sk)
        engmap = {"pe": "tensor", "act": "scalar", "dve": "vector", "pool": "gpsimd", "sp": "sync"}
        with nc.Block() as block:
            for e in ENGS:
                items = self.q[e]

                def body(eng, items=items):
                    for fn, waits, inc in items:
                        for sk, val in waits:
                            eng.wait_ge(self.sems[sk], val)
                        if fn is not None:
                            ins = fn(eng)
                            if inc is not None:
                                ins.then_inc(self.sems[inc[0]], inc[1])

                getattr(block, engmap[e])(body)

    def close(self):
        self.es.close()


class Arena:
    def __init__(self, P, nbytes):
        self.t = P.sbuf("arena", [128, nbytes // 2], BF16)
        self.nbytes = nbytes
        self.off = 0

    def alloc(self, free_shape, dtype):
        n = int(np.prod(free_shape))
        size = n * (4 if dtype == F32 else 2)
        size = (size + 63) // 64 * 64
        assert self.off + size <= self.nbytes, ("SBUF arena overflow", self.off, size)
        v = self.t[:, self.off // 2:(self.off + size) // 2]
        if dtype == F32:
            v = v.bitcast(F32)
        v = v[:, 0:n]
        self.off += size
        if len(free_shape) == 2:
            v = v.rearrange("p (a b) -> p a b", b=free_shape[1])
        elif len(free_shape) == 3:
            v = v.rearrange("p (a b c) -> p a b c", b=free_shape[1], c=free_shape[2])
        elif len(free_shape) == 4:
            v = v.rearrange("p (a b c d) -> p a b c d", b=free_shape[1], c=free_shape[2], d=free_shape[3])
        return v

    def mark(self):
        return self.off

    def release(self, m):
        self.off = m


def bc(ap, axis, shape):
    return ap.unsqueeze(axis).to_broadcast(list(shape))


NT = 2048
NTT = 16
NCTX = 256
NALL = NT + NCTX
D = 1024
NE = 32
LAST_PHASE = 99


def build(upto=LAST_PHASE, debug=False):
    nc = bass.Bass("TRN2", target_bir_lowering=False)

    def din(name, shape, dt=F32):
        return nc.dram_tensor(name, list(shape), dt, kind="ExternalInput").ap()

    x_d = din("x", [NT, D])
    ctx_d = din("ctx", [NCTX, D])
    cvec_d = din("cvec", [128, 16])
    wmod_d = din("w_mod", [D, 6 * D])
    bmod_d = din("b_mod", [1, 6 * D])
    ng_d = din("norm_g", [1, 4 * D])
    win_d = din("w_in", [D, 8704])
    lbv_d = din("lbv", [128, 32])
    hng_d = din("hng", [128, 1])
    qkg_d = din("qkg", [1, 256])
    wbr_d = din("w_branch", [2, D, D])
    wout_d = din("w_out", [D, D])
    rw_d = din("router_w", [D, NE])
    rb_d = din("router_b", [1, NE])
    NEW = NE if upto >= 7 else 1
    wup_d = din("w_up", [NEW, D, 2 * D])
    bupT_d = din("b_upT", [128, NE * 16])
    wdn_d = din("w_down", [NEW, D, D])
    bdn_d = din("b_down", [NE, D])
    rope_d = din("rope", [NT, 256])
    sel_d = din("sel", [128, 8])
    out_d = nc.dram_tensor("out", [NT, D], F32, kind="ExternalOutput").ap()

    def dscr(name, shape, dt):
        if debug:
            return nc.dram_tensor(name, list(shape), dt, kind="ExternalOutput").ap()
        return nc.dram_tensor(name, list(shape), dt).ap()

    dm_mod = dscr("dm_mod", [128, 6 * D], F32)
    dm_ut = dscr("dm_ut", [128, 8, NALL], BF16)
    ag1_ins = [nc.dram_tensor("ag1_in%d" % q, [128, 2048], BF16).ap() for q in range(4)]
    ag1_outs = [nc.dram_tensor("ag1_out%d" % q, [4 * 128, 2048], BF16).ap() for q in range(4)]
    dm_kvctx = dscr("dm_kvctx", [512, NCTX], BF16)
    dm_oloc = dscr("dm_oloc", [8, 128, NT], F32)
    dm_qb = dscr("dm_qb", [2, 8, 128, NT], BF16)
    dm_gs = dscr("dm_gs", [8, 128, NT], BF16)
    ag2_ins = [nc.dram_tensor("ag2_in%d" % d, [128, 1032], F32).ap() for d in range(2)]
    ag2_outs = [nc.dram_tensor("ag2_out%d" % d, [4 * 128, 1032], F32).ap() for d in range(2)]
    dm_oth = dscr("dm_oth", [8, 128, NT], BF16)
    dm_ota = dscr("dm_ota", [8, 128, NT], BF16)
    dm_x1 = dscr("dm_x1", [NT, D], F32)
    dm_u2t = dscr("dm_u2t", [128, 8, NT], BF16)
    dm_comb = dscr("dm_comb", [128, NTT, NE], F32)
    dm_combT = dscr("dm_combT", [NE, NT], F32)

    P = Prog(nc)
    A = Arena(P, 207 * 1024)
    banks = [P.psum("bank%d" % i, [128, 512], F32) for i in range(8)]

    def bank_bf(i):
        return banks[i][:, :].bitcast(BF16)

    ident_f = A.alloc((128,), F32)
    ident_b = A.alloc((128,), BF16)
    ones_b = A.alloc((128,), BF16)
    ones_f = A.alloc((128,), F32)
    P.op("pool", lambda e: e.memset(ident_f, 0.0), writes=["ident_f"])
    P.op("pool", lambda e: e.affine_select(out=ident_f, in_=ident_f, pattern=[[-1, 128]], compare_op=ALU.not_equal,
                                           fill=1.0, base=0, channel_multiplier=1), reads=["ident_f"], writes=["ident_f"])
    P.op("dve", lambda e: e.tensor_copy(out=ident_b, in_=ident_f), reads=["ident_f"], writes=["ident_b"])
    P.op("pool", lambda e: e.memset(ones_f, 1.0), writes=["ones_f"])
    P.op("dve", lambda e: e.tensor_copy(out=ones_b, in_=ones_f), reads=["ones_f"], writes=["ones_b"])

    m_pre_ut = A.mark()
    uT = A.alloc((8, NALL), BF16)

    def rstd_from_ss(ss_ap, n, out_ap, tmp_ap, rk, wk):
        P.op("act", lambda e: e.activation(out=tmp_ap, in_=ss_ap, func=AF.Ln, scale=1.0 / n, bias=EPS), reads=rk, writes=[wk + "_ln"])
        P.op("act", lambda e: e.activation(out=out_ap, in_=tmp_ap, func=AF.Exp, scale=-0.5), reads=[wk + "_ln"], writes=[wk])

    m0 = A.mark()
    cv = A.alloc((16,), F32)
    scv = A.alloc((16,), F32)
    scb = A.alloc((16, 128), F32)
    bmod = A.alloc((6 * D,), F32)
    ng = A.alloc((4, D), F32)
    modl = A.alloc((6 * D,), F32)
    modc = A.alloc((2 * D,), F32)
    wm = [A.alloc((8, 512), F32) for _ in range(2)]
    P.dma("sp", cv, cvec_d, "ld_c", writes=["cv"])
    P.dma("sp", bmod, bmod_d[0].partition_broadcast(128), "ld_c", writes=["bmod"])
    P.dma("sp", ng, ng_d[0].partition_broadcast(128).rearrange("p (a b) -> p a b", b=D), "ld_c", writes=["ng"])
    P.op("act", lambda e: e.activation(out=scv, in_=cv, func=AF.Silu), reads=["cv"], writes=["scv"])
    for k in range(16):
        P.op("dve", lambda e, k=k: e.tensor_copy(out=scb[:, k, :], in_=scv[:, k:k + 1].to_broadcast([128, 128])),
             reads=["scv"], writes=["scb"])
    for s in range(12):
        w = wm[s % 2]
        wk = "wm%d" % (s % 2)
        P.dma("sp", w, wmod_d[:, s * 512:(s + 1) * 512].rearrange("(kt p) n -> p kt n", p=128), "ld_" + wk, writes=[wk])

        def mm(e, w=w, off=0, bk=0):
            for kt in range(8):
                r = e.matmul(banks[bk][:, :], lhsT=scb[:, off + kt, :], rhs=w[:, kt, :], start=(kt == 0), stop=(kt == 7))
            return r
        P.op("pe", lambda e, w=w: mm(e, w, 0, 0), reads=["scb", wk], writes=["bank0"])
        P.op("dve", lambda e, s=s: e.tensor_tensor(out=modl[:, s * 512:(s + 1) * 512], in0=banks[0][:, :], in1=bmod[:, s * 512:(s + 1) * 512], op=ALU.add),
             reads=["bank0", "bmod"], writes=["modl"])
        if s < 4:
            P.op("pe", lambda e, w=w: mm(e, w, 8, 1), reads=["scb", wk], writes=["bank1"])
            P.op("dve", lambda e, s=s: e.tensor_tensor(out=modc[:, s * 512:(s + 1) * 512], in0=banks[1][:, :], in1=bmod[:, s * 512:(s + 1) * 512], op=ALU.add),
                 reads=["bank1", "bmod"], writes=["modc"])
    P.op("dve", lambda e: e.scalar_tensor_tensor(out=modl[:, D:2 * D], in0=modl[:, D:2 * D], scalar=1.0, in1=ng[:, 0, :], op0=ALU.add, op1=ALU.mult),
         reads=["modl", "ng"], writes=["modl"])
    P.op("dve", lambda e: e.scalar_tensor_tensor(out=modc[:, D:2 * D], in0=modc[:, D:2 * D], scalar=1.0, in1=ng[:, 0, :], op0=ALU.add, op1=ALU.mult),
         reads=["modc", "ng"], writes=["modc"])
    P.op("dve", lambda e: e.tensor_tensor(out=modl[:, 2 * D:3 * D], in0=modl[:, 2 * D:3 * D], in1=ng[:, 1, :], op=ALU.mult), reads=["modl", "ng"], writes=["modl"])
    P.op("dve", lambda e: e.scalar_tensor_tensor(out=modl[:, 4 * D:5 * D], in0=modl[:, 4 * D:5 * D], scalar=1.0, in1=ng[:, 2, :], op0=ALU.add, op1=ALU.mult),
         reads=["modl", "ng"], writes=["modl"])
    P.op("dve", lambda e: e.tensor_tensor(out=modl[:, 5 * D:6 * D], in0=modl[:, 5 * D:6 * D], in1=ng[:, 3, :], op=ALU.mult), reads=["modl", "ng"], writes=["modl"])
    P.dma("sp", dm_mod, modl, "st_mod", reads=["modl"], writes=["dm_mod"])

    xt = [A.alloc((D,), F32) for _ in range(2)]
    junk = A.alloc((D,), BF16)
    tmpf = A.alloc((D,), F32)
    ub = [A.alloc((D,), BF16) for _ in range(2)]
    ss1 = A.alloc((18,), F32)
    ln1 = A.alloc((18,), F32)
    rs1 = A.alloc((18,), F32)
    P.op("pool", lambda e: e.memset(ss1, 0.0), writes=["ss1"])
    for ti in range(18):
        s = ti % 2
        src = x_d[ti * 128:(ti + 1) * 128, :] if ti < NTT else ctx_d[(ti - NTT) * 128:(ti - NTT + 1) * 128, :]
        Am = modl if ti < NTT else modc
        amk = "modl" if ti < NTT else "modc"
        P.dma("sp", xt[s], src, "ld_xt%d" % s, writes=["xt%d" % s])
        P.op("act", lambda e, s=s, ti=ti: e.activation(out=junk, in_=xt[s], func=AF.Square, accum_out=ss1[:, ti:ti + 1]),
             reads=["xt%d" % s, "ss1"], writes=["junk", "ss1_%d" % ti])
        rstd_from_ss(ss1[:, ti:ti + 1], D, rs1[:, ti:ti + 1], ln1[:, ti:ti + 1], ["ss1_%d" % ti], "rs1_%d" % ti)
        P.op("dve", lambda e, s=s, ti=ti, Am=Am: e.scalar_tensor_tensor(out=tmpf, in0=xt[s], scalar=rs1[:, ti:ti + 1], in1=Am[:, D:2 * D], op0=ALU.mult, op1=ALU.mult),
             reads=["xt%d" % s, "rs1_%d" % ti, amk], writes=["tmpf"])
        P.op("dve", lambda e, s=s, Am=Am: e.tensor_tensor(out=ub[s], in0=tmpf, in1=Am[:, 0:D], op=ALU.add), reads=["tmpf", amk], writes=["ub%d" % s])
        bk = 2 + s

        def tr(e, s=s, bk=bk):
            for kt in range(8):
                r = e.transpose(bank_bf(bk)[:, kt * 128:(kt + 1) * 128], ub[s][:, kt * 128:(kt + 1) * 128], ident_b)
            return r
        P.op("pe", tr, reads=["ub%d" % s, "ident_b"], writes=["bank%d" % bk])
        P.op("act", lambda e, ti=ti, bk=bk: e.copy(out=uT[:, :, ti * 128:(ti + 1) * 128], in_=bank_bf(bk).rearrange("p (a b) -> p a b", b=128)),
             reads=["bank%d" % bk], writes=["uT_%d" % ti])
    UT_KEYS = ["uT_%d" % ti for ti in range(18)]
    if debug:
        P.dma("sp", dm_ut, uT, "st_dbg", reads=UT_KEYS, writes=["dm_ut"])
    P.barrier()
    A.release(m0)
    if upto <= 1:
        return finish(P, nc)


    m2 = A.mark()
    wkv = A.alloc((8, 512), BF16)
    P.dma("pool", wkv, win_d[:, 6144:6656].rearrange("(kt p) n -> p kt n", p=128), "ld_wkv", writes=["wkv"])
    ropeT = A.alloc((NTT, 256), F32)
    P.dma("sp", ropeT, rope_d.rearrange("(t p) n -> p t n", p=128), "ld_rope", writes=["ropeT"])
    gqk = A.alloc((256,), F32)
    P.dma("sp", gqk, qkg_d[0].partition_broadcast(128), "ld_c", writes=["gqk"])
    KTl = A.alloc((2, NALL), BF16)
    Vl = A.alloc((18, 256), BF16)
    ssk = A.alloc((18, 2), F32)
    lnk = A.alloc((18, 2), F32)
    rsk = A.alloc((18, 2), F32)
    knb = [A.alloc((2, 128), F32) for _ in range(2)]
    t1b = A.alloc((2, 128), F32)
    t2b = A.alloc((2, 128), F32)
    kbf = [A.alloc((256,), BF16) for _ in range(2)]
    junk2 = A.alloc((128,), BF16)
    P.op("pool", lambda e: e.memset(ssk, 0.0), writes=["ssk"])

    def rope_apply(xn, nh, ti, outbf, pfx, eng2="dve"):
        cosv = ropeT[:, ti, 0:128]
        sinv = ropeT[:, ti, 128:256].rearrange("p (r x d) -> p r x d", r=2, x=2, d=32)
        t1 = t1b if nh == 2 else t1q
        t2 = t2b if nh == 2 else t2q
        P.op("dve", lambda e: e.tensor_tensor(out=t1, in0=xn, in1=bc(cosv, 1, [128, nh, 128]), op=ALU.mult),
             reads=[pfx + "xn", "ropeT"], writes=[pfx + "t1"])
        x6 = xn.rearrange("p h (r x d) -> p h r x d", r=2, x=2, d=32)
        t6 = t2.rearrange("p h (r x d) -> p h r x d", r=2, x=2, d=32)
        for xo in range(2):
            P.op(eng2, lambda e, xo=xo: e.tensor_tensor(out=t6[:, :, :, xo, :], in0=x6[:, :, :, 1 - xo, :],
                                                        in1=bc(sinv[:, :, xo, :], 1, [128, nh, 2, 32]), op=ALU.mult),
                 reads=[pfx + "xn", "ropeT"], writes=[pfx + "t2_%d" % xo])
        P.op("dve", lambda e: e.tensor_tensor(out=outbf.rearrange("p (h d) -> p h d", d=128), in0=t1, in1=t2, op=ALU.add),
             reads=[pfx + "t1", pfx + "t2_0", pfx + "t2_1"], writes=[pfx + "bf"])

    import os
    BIS = int(os.environ.get("BIS", "99"))
    for ti in range(18):
        s = ti % 2
        bk = s
        kn = knb[s]

        def mmkv(e, ti=ti, bk=bk):
            for kt in range(8):
                r = e.matmul(banks[bk][:, :], lhsT=uT[:, kt, ti * 128:(ti + 1) * 128], rhs=wkv[:, kt, :], start=(kt == 0), stop=(kt == 7))
            return r
        P.op("pe", mmkv, reads=["uT_%d" % ti, "wkv"], writes=["bank%d" % bk])
        kps = banks[bk][:, 0:256].rearrange("p (h d) -> p h d", d=128)
        P.op("act", lambda e, ti=ti, bk=bk: e.copy(out=Vl[:, ti, :], in_=banks[bk][:, 256:512]), reads=["bank%d" % bk], writes=["Vl_%d" % ti])
        if BIS < 2:
            continue
        for h in range(2):
            P.op("act", lambda e, h=h, ti=ti, kps=kps: e.activation(out=junk2, in_=kps[:, h, :], func=AF.Square, accum_out=ssk[:, ti, h:h + 1]),
                 reads=["bank%d" % bk, "ssk"], writes=["junk2", "ssk_%d_%d" % (ti, h)])
        rstd_from_ss(ssk[:, ti, :], 128, rsk[:, ti, :], lnk[:, ti, :], ["ssk_%d_0" % ti, "ssk_%d_1" % ti], "rsk_%d" % ti)
        P.op("dve", lambda e, kn=kn, kps=kps, ti=ti: e.tensor_tensor(out=kn, in0=kps, in1=bc(rsk[:, ti, :], 2, [128, 2, 128]), op=ALU.mult),
             reads=["bank%d" % bk, "rsk_%d" % ti], writes=["k%dxn" % s])
        P.op("dve", lambda e, kn=kn: e.tensor_tensor(out=kn, in0=kn, in1=bc(gqk[:, 128:256], 1, [128, 2, 128]), op=ALU.mult),
             reads=["k%dxn" % s, "gqk"], writes=["k%dxn" % s])
        if BIS < 3:
            continue
        if ti < NTT and BIS != 3:
            rope_apply(kn, 2, ti, kbf[s], "k%d" % s)
        else:
            P.op("dve", lambda e, kn=kn, s=s: e.tensor_copy(out=kbf[s].rearrange("p (h d) -> p h d", d=128), in_=kn), reads=["k%dxn" % s], writes=["k%dbf" % s])
        bk2 = 2 + s
        if BIS < 5:
            continue

        def trk(e, s=s, bk2=bk2):
            for h in range(2):
                r = e.transpose(bank_bf(bk2)[:, h * 128:(h + 1) * 128], kbf[s][:, h * 128:(h + 1) * 128], ident_b)
            return r
        P.op("pe", trk, reads=["k%dbf" % s, "ident_b"], writes=["bank%d" % bk2])
        P.op("act", lambda e, ti=ti, bk2=bk2: e.copy(out=KTl[:, :, ti * 128:(ti + 1) * 128], in_=bank_bf(bk2)[:, 0:256].rearrange("p (a b) -> p a b", b=128)),
             reads=["bank%d" % bk2], writes=["KTl_%d" % ti])
    for h in range(2 if BIS >= 6 else 0):
        P.dma("sp", ag1_ins[h], KTl[:, h, 0:NT], "st_ag1", reads=["KTl_%d" % t for t in range(16)], writes=["ag1_in"])
        P.dma("sp", ag1_ins[2 + h].rearrange("p (ti c) -> p ti c", c=256), Vl[:, 8 * h:8 * h + 8, :], "st_ag1", reads=["Vl_%d" % t for t in range(16)], writes=["ag1_in"])
    if BIS >= 6:
        P.dma("sp", dm_kvctx[0:256, :].rearrange("(h p) t -> p h t", p=128), KTl[:, :, NT:NALL], "st_kvc", reads=["KTl_16", "KTl_17"], writes=["dm_kvctx"])
        P.dma("sp", dm_kvctx[256:512, :].rearrange("(ti p) c -> p ti c", p=128), Vl[:, 16:18, :], "st_kvc", reads=["Vl_16", "Vl_17"], writes=["dm_kvctx"])
    for q in range(4 if BIS >= 7 else 0):
        P.custom("pool", lambda e, q=q: e.collective_compute("AllGather", ALU.bypass, replica_groups=[[0, 1, 2, 3], [4, 5, 6, 7]], ins=[ag1_ins[q]], outs=[ag1_outs[q]]),
                 "cc1", 1, reads=["ag1_in"] + (["ag1_out"] if q > 0 else []), writes=["ag1_out"])
    P.barrier(keep=["ag1_out", "dm_kvctx", "dm_mod"] + UT_KEYS)
    A.release(m2)
    if upto <= 2:
        return finish(P, nc)

    m3 = A.mark()
    rst = A.alloc((NALL,), F32)
    maskF = A.alloc((128,), F32)
    maskB = A.alloc((128,), F32)
    lbt = A.alloc((2, 2, 8), F32)
    lbd = A.alloc((2, 8), F32)
    lb = A.alloc((2, 8), F32)
    oml = A.alloc((2, 8), F32)
    hng = A.alloc((1,), F32)
    stage = A.alloc((2, 8, 128), F32)
    sctx = A.alloc((2, 8, 128), F32)
    Dv = A.alloc((2, 8), F32)
    m3h = A.mark()
    whb = [A.alloc((8, 5, 128), BF16)] * 2
    qT = A.alloc((NT,), F32)
    gsT = A.alloc((NT,), BF16)
    v_tm = A.alloc((18, 128), BF16)
    fa = A.alloc((NALL,), F32)
    lf = A.alloc((NALL,), F32)
    kk = A.alloc((NALL,), F32)
    bb = A.alloc((NALL,), F32)
    xx = A.alloc((NALL,), F32)
    ee = A.alloc((NALL,), F32)
    gtmp = ee[:, 0:NT]
    qe = [A.alloc((NT,), BF16) for _ in range(2)]
    ke = [A.alloc((NT,), BF16) for _ in range(2)]
    kd = [A.alloc((NALL,), BF16) for _ in range(2)]
    kd_tm = [A.alloc((18, 128), BF16) for _ in range(2)]
    qB = [A.alloc((NT,), BF16) for _ in range(2)]
    tot = [A.alloc((36,), F32) for _ in range(2)]
    etot = [A.alloc((36,), F32) for _ in range(2)]
    ipf = [A.alloc((32,), F32) for _ in range(2)]
    gg = [A.alloc((32,), F32) for _ in range(2)]
    eg = [A.alloc((32,), F32) for _ in range(2)]
    attm = [A.alloc((4, 128), BF16) for _ in range(2)]
    Sst = [[A.alloc((128,), F32) for _ in range(2)] for _ in range(2)]
    Sbf = [[A.alloc((128,), BF16) for _ in range(3)] for _ in range(2)]
    Scx = [A.alloc((128,), F32) for _ in range(2)]
    o_acc = A.alloc((NT,), F32)

    P.op("pool", lambda e: e.memset(rst, 1.0), writes=["rst"])
    P.op("pool", lambda e: e.memset(rst.rearrange("p (c t) -> p c t", t=64)[:, :, 0:1], 0.0), reads=["rst"], writes=["rst"])
    P.op("pool", lambda e: e.memset(maskF, 1.0), writes=["maskF"])
    P.op("pool", lambda e: e.affine_select(out=maskF, in_=maskF, pattern=[[1, 128]], compare_op=ALU.is_ge, fill=0.0, base=0, channel_multiplier=-1),
         reads=["maskF"], writes=["maskF"])
    P.op("pool", lambda e: e.memset(maskF[0:64, 64:128], 0.0), reads=["maskF"], writes=["maskF"])
    P.op("pool", lambda e: e.memset(maskB, 1.0), writes=["maskB"])
    P.op("pool", lambda e: e.affine_select(out=maskB, in_=maskB, pattern=[[-1, 128]], compare_op=ALU.is_ge, fill=0.0, base=0, channel_multiplier=1),
         reads=["maskB"], writes=["maskB"])
    P.op("pool", lambda e: e.memset(maskB[64:128, 0:64], 0.0), reads=["maskB"], writes=["maskB"])
    masks = [maskF, maskB]
    P.dma("sp", lbt, lbv_d.rearrange("p (a b c) -> p a b c", a=2, b=2), "ld_c", writes=["lbt"])
    P.dma("sp", hng, hng_d, "ld_c", writes=["hng"])
    P.op("dve", lambda e: e.tensor_tensor(out=lbd, in0=lbt[:, :, 0, :], in1=lbt[:, :, 1, :], op=ALU.subtract), reads=["lbt"], writes=["lbd"])
    P.op("act", lambda e: e.activation(out=lb, in_=lbd, func=AF.Sigmoid), reads=["lbd"], writes=["lb"])
    P.op("dve", lambda e: e.tensor_scalar(out=oml, in0=lb, scalar1=-1.0, scalar2=1.0, op0=ALU.mult, op1=ALU.add), reads=["lb"], writes=["oml"])

    win_v = win_d.rearrange("(kt p) (s n) -> p kt s n", p=128, n=128)
    BLK5 = [(0, 512), (512, 512), (1024, 512), (1536, 512), (2048, 256)]
    PB = 6
    pbc = [0]

    def proj_fm(wh, whk, sidx, c0, n, evac):
        bk = PB + (pbc[0] % 2)
        pbc[0] += 1

        def mm(e):
            for kt in range(8):
                r = e.matmul(banks[bk][:, 0:n], lhsT=wh[:, kt, sidx, :], rhs=uT[:, kt, c0:c0 + n], start=(kt == 0), stop=(kt == 7))
            return r
        P.op("pe", mm, reads=[whk] + ["uT_%d" % t for t in range(c0 // 128, (c0 + n) // 128)], writes=["bank%d" % bk])
        evac(banks[bk][:, 0:n], "bank%d" % bk)

    def hgrn_dir_prep(h, d):
        wh = whb[h % 2]
        whk = "wh0"
        dk = "d%d" % d
        for (c0, n) in BLK5:
            proj_fm(wh, whk, 2 + d, c0, n, lambda bap, bkey, c0=c0, n=n: P.op(
                "act", lambda e: e.activation(out=fa[:, c0:c0 + n], in_=bap, func=AF.Sigmoid), reads=[bkey], writes=["fa"]))
        fak = ["fa"]
        P.op("dve", lambda e: e.tensor_scalar(out=fa, in0=fa, scalar1=oml[:, d, h:h + 1], scalar2=lb[:, d, h:h + 1], op0=ALU.mult, op1=ALU.add),
             reads=fak + ["oml", "lb"], writes=["fa"])
        P.op("act", lambda e: e.activation(out=lf, in_=fa, func=AF.Ln), reads=["fa"], writes=["lf"])
        P.op("dve", lambda e: e.tensor_scalar(out=kk, in0=fa, scalar1=-1.0, scalar2=1.0, op0=ALU.mult, op1=ALU.add), reads=["fa"], writes=["kk"])
        P.op("dve", lambda e: e.tensor_tensor_scan(out=bb, data0=rst, data1=lf, initial=0.0, op0=ALU.mult, op1=ALU.add), reads=["rst", "lf"], writes=["bb"])
        b3 = bb.rearrange("p (c t) -> p c t", t=64)
        P.op("dve", lambda e: e.tensor_copy(out=tot[d], in_=b3[:, :, 63]), reads=["bb"], writes=["tot" + dk])
        P.op("act", lambda e: e.activation(out=etot[d], in_=tot[d], func=AF.Exp), reads=["tot" + dk], writes=["etot" + dk])
        x3 = xx.rearrange("p (c t) -> p c t", t=64)
        P.op("dve", lambda e: e.tensor_tensor(out=x3, in0=bc(tot[d], 2, [128, 36, 64]), in1=b3, op=ALU.subtract), reads=["tot" + dk, "bb"], writes=["xx"])
        if d == 0:
            bu, dd = bb, xx
            bk_, ddk = "bb", "xx"
        else:
            P.op("dve", lambda e: e.tensor_tensor(out=xx, in0=xx, in1=lf, op=ALU.add), reads=["xx", "lf"], writes=["xx"])
            P.op("dve", lambda e: e.tensor_tensor(out=bb, in0=bb, in1=lf, op=ALU.subtract), reads=["bb", "lf"], writes=["bb"])
            bu, dd = xx, bb
            bk_, ddk = "xx", "bb"
        P.op("act", lambda e: e.activation(out=ee[:, 0:NT], in_=bu[:, 0:NT], func=AF.Exp), reads=[bk_], writes=["ee"])
        P.op("dve", lambda e: e.tensor_tensor(out=qe[d], in0=qT, in1=ee[:, 0:NT], op=ALU.mult), reads=["ee", "qT"], writes=["qe" + dk])
        P.op("act", lambda e: e.activation(out=ee[:, 0:NT], in_=bu[:, 0:NT], func=AF.Exp, scale=-1.0), reads=[bk_, "ee"], writes=["ee"])
        P.op("dve", lambda e: e.tensor_tensor(out=ke[d], in0=kk[:, 0:NT], in1=ee[:, 0:NT], op=ALU.mult), reads=["ee", "kk"], writes=["ke" + dk])
        P.op("act", lambda e: e.activation(out=ee, in_=dd, func=AF.Exp), reads=[ddk, "ee"], writes=["ee"])
        P.op("dve", lambda e: e.tensor_tensor(out=kd[d], in0=kk, in1=ee, op=ALU.mult), reads=["ee", "kk"], writes=["kd" + dk])
        P.op("dve", lambda e: e.tensor_tensor_scan(out=ipf[d], data0=ones_f[:, 0:32], data1=tot[d][:, 0:32], initial=0.0, op0=ALU.mult, op1=ALU.add),
             reads=["tot" + dk, "ones_f"], writes=["ipf" + dk])
        if d == 0:
            P.op("dve", lambda e: e.tensor_tensor(out=gg[d], in0=ipf[d], in1=tot[d][:, 0:32], op=ALU.subtract), reads=["ipf" + dk, "tot" + dk], writes=["gg" + dk])
        else:
            P.op("dve", lambda e: e.tensor_tensor(out=gg[d], in0=ipf[d][:, 31:32].to_broadcast([128, 32]), in1=ipf[d], op=ALU.subtract),
                 reads=["ipf" + dk], writes=["gg" + dk])
        P.op("act", lambda e: e.activation(out=eg[d], in_=gg[d], func=AF.Exp), reads=["gg" + dk], writes=["eg" + dk])
        P.op("act", lambda e: e.activation(out=Dv[:, d, h:h + 1], in_=ipf[d][:, 31:32], func=AF.Exp), reads=["ipf" + dk], writes=["Dv_%d_%d" % (d, h)])
        P.op("dve", lambda e: e.tensor_tensor(out=qB[d].rearrange("p (c t) -> p c t", t=64), in0=qe[d].rearrange("p (c t) -> p c t", t=64),
                                               in1=bc(eg[d], 2, [128, 32, 64]), op=ALU.mult), reads=["qe" + dk, "eg" + dk], writes=["qB" + dk])
        P.dma("sp", dm_qb[d, h], qB[d], "st_qb", reads=["qB" + dk], writes=["dm_qb"])
        for g3 in range(3):
            bk = PB + (pbc[0] % 2)
            pbc[0] += 1

            def trd(e, g3=g3, bk=bk):
                for i in range(6):
                    ti = g3 * 6 + i
                    r = e.transpose(bank_bf(bk)[:, i * 128:(i + 1) * 128], kd[d][:, ti * 128:(ti + 1) * 128], ident_b)
                return r
            P.op("pe", trd, reads=["kd" + dk, "ident_b"], writes=["bank%d" % bk])
            P.op("act", lambda e, g3=g3, bk=bk: e.copy(out=kd_tm[d][:, g3 * 6:(g3 + 1) * 6, :], in_=bank_bf(bk)[:, 0:768].rearrange("p (a b) -> p a b", b=128)),
                 reads=["bank%d" % bk], writes=["kdtm%s_%d" % (dk, g3)])

    def hgrn_dir_scan(h, d):
        dk = "d%d" % d
        kdk = ["kdtm%s_%d" % (dk, g3) for g3 in range(3)]
        bA, bO, bS = 3 * d, 3 * d + 1, 3 * d + 2
        Sc = Scx[d]
        Sbufs, Sbb = Sst[d], Sbf[d]
        slot = [0]

        def delta_mm(c):
            sl = slot[0] % 2
            slot[0] += 1
            ti, half = c // 2, c % 2
            ps = slice(half * 64, half * 64 + 64)
            bidx = (bS, PB + d)[sl]
            bap = banks[bidx][:, 0:128]
            bkey = "bank%d" % bidx
            P.op("pe", lambda e: e.matmul(bap, lhsT=kd_tm[d][ps, ti, :], rhs=v_tm[ps, ti, :], start=True, stop=True),
                 reads=kdk + ["v_tm"], writes=[bkey])
            return bap, bkey
        corder = [32, 33, 34, 35] if d == 0 else [35, 34, 33, 32]
        for i, c in enumerate(corder):
            bap, bkey = delta_mm(c)
            if i == 0:
                P.op("dve", lambda e: e.tensor_copy(out=Sc, in_=bap), reads=[bkey], writes=["Sc" + dk])
            else:
                P.op("dve", lambda e: e.scalar_tensor_tensor(out=Sc, in0=Sc, scalar=etot[d][:, c:c + 1], in1=bap, op0=ALU.mult, op1=ALU.add),
                     reads=[bkey, "Sc" + dk, "etot" + dk], writes=["Sc" + dk])
            yield
        P.op("dve", lambda e: e.tensor_copy(out=sctx[:, d, h, :], in_=Sc), reads=["Sc" + dk], writes=["sctx_%d_%d" % (d, h)])
        P.op("pool", lambda e: e.memset(Sbb[0], 0.0), reads=["Sb%s_0" % dk], writes=["Sb%s_0" % dk])
        si, bi, nstate = 0, 0, 0
        groups = [0, 1, 2, 3] if d == 0 else [3, 2, 1, 0]
        pis = [0, 1, 2, 3] if d == 0 else [3, 2, 1, 0]
        seq = []
        for g in groups:
            for pi in pis:
                p = g * 4 + pi
                for c in ([2 * p, 2 * p + 1] if d == 0 else [2 * p + 1, 2 * p]):
                    seq.append(c)
        dl = {}
        for i in range(min(2, len(seq))):
            dl[i] = delta_mm(seq[i])
        step = 0
        for g in groups:
            def att(e, g=g):
                for pi in range(4):
                    p = g * 4 + pi
                    r = e.matmul(banks[bA][:, pi * 128:(pi + 1) * 128], lhsT=ke[d][:, p * 128:(p + 1) * 128], rhs=qe[d][:, p * 128:(p + 1) * 128], start=True, stop=True)
                return r
            P.op("pe", att, reads=["ke" + dk, "qe" + dk], writes=["bank%d" % bA])
            P.op("dve", lambda e: e.tensor_tensor(out=attm[d], in0=banks[bA][:, :].rearrange("p (a b) -> p a b", b=128), in1=bc(masks[d], 1, [128, 4, 128]), op=ALU.mult),
                 reads=["bank%d" % bA, "mask"], writes=["attm" + dk])
            for pi in pis:
                p = g * 4 + pi
                P.op("pe", lambda e, pi=pi, p=p: e.matmul(banks[bO][:, pi * 128:(pi + 1) * 128], lhsT=v_tm[:, p, :], rhs=attm[d][:, pi, :], start=True, stop=False),
                     reads=["v_tm", "attm" + dk], writes=["bank%d" % bO])
                chunks = [2 * p, 2 * p + 1] if d == 0 else [2 * p + 1, 2 * p]
                for ci, c in enumerate(chunks):
                    col = pi * 128 + (c % 2) * 64
                    sbc = Sbb[bi]
                    P.op("pe", lambda e, c=c, col=col, ci=ci, sbc=sbc: e.matmul(banks[bO][:, col:col + 64], lhsT=sbc, rhs=qe[d][:, c * 64:(c + 1) * 64], start=False, stop=(ci == 1)),
                         reads=["Sb%s_%d" % (dk, bi), "qe" + dk], writes=["bank%d" % bO])
                    assert seq[step] == c
                    bap, bkey = dl.pop(step)
                    nsi = (si + 1) % 2
                    So, Sn = Sbufs[si], Sbufs[nsi]
                    if nstate == 0:
                        P.op("dve", lambda e, Sn=Sn, bap=bap: e.tensor_copy(out=Sn, in_=bap), reads=[bkey], writes=["S%s_%d" % (dk, nsi)])
                    else:
                        P.op("dve", lambda e, So=So, Sn=Sn, bap=bap, c=c: e.scalar_tensor_tensor(out=Sn, in0=So, scalar=etot[d][:, c:c + 1], in1=bap, op0=ALU.mult, op1=ALU.add),
                             reads=[bkey, "S%s_%d" % (dk, si), "etot" + dk], writes=["S%s_%d" % (dk, nsi)])
                    si = nsi
                    nstate += 1
                    nbi = (bi + 1) % 3
                    sbn = Sbb[nbi]
                    P.op("act", lambda e, Sn=Sn, sbn=sbn: e.copy(out=sbn, in_=Sn), reads=["S%s_%d" % (dk, si)], writes=["Sb%s_%d" % (dk, nbi)])
                    bi = nbi
                    if step + 2 < len(seq):
                        dl[step + 2] = delta_mm(seq[step + 2])
                    step += 1
                    yield
            cs = slice(g * 512, (g + 1) * 512)
            if (d == 0 and g < 2) or (d == 1 and g >= 2):
                P.op("act", lambda e, cs=cs: e.copy(out=o_acc[:, cs], in_=banks[bO][:, :]), reads=["bank%d" % bO, "oacc_%d" % g], writes=["oacc_%d" % g])
            else:
                P.op("dve", lambda e, cs=cs: e.tensor_tensor(out=o_acc[:, cs], in0=o_acc[:, cs], in1=banks[bO][:, :], op=ALU.add),
                     reads=["bank%d" % bO, "oacc_%d" % g], writes=["oacc_%d" % g])
        Sl = Sbufs[si]
        P.op("dve", lambda e, Sl=Sl: e.tensor_copy(out=stage[:, d, h, :], in_=Sl), reads=["S%s_%d" % (dk, si)], writes=["stage_%d_%d" % (d, h)])

    P.buf["mask"] = {"w": ("e", "pool", P.cnt["pool"]), "r": {}}
    for h in range(8):
        wh = whb[h % 2]
        whk = "wh0"
        for s5 in range(5):
            P.dma("pool", wh[:, :, s5, :], win_v[:, :, h + 8 * s5, :], "ld_" + whk, writes=[whk])
        for blk in range(4):
            proj_fm(wh, whk, 0, blk * 512, 512, lambda bap, bkey, blk=blk: P.op(
                "act", lambda e: e.copy(out=qT[:, blk * 512:(blk + 1) * 512], in_=bap), reads=[bkey, "qT"], writes=["qT"]))
        for blk in range(4):
            proj_fm(wh, whk, 4, blk * 512, 512, lambda bap, bkey, blk=blk: P.op(
                "act", lambda e: e.activation(out=gtmp[:, blk * 512:(blk + 1) * 512], in_=bap, func=AF.Silu), reads=[bkey, "ee"], writes=["ee"]))
        P.op("dve", lambda e: e.tensor_scalar(out=gsT, in0=gtmp, scalar1=hng[:, 0:1], scalar2=None, op0=ALU.mult), reads=["ee", "hng"], writes=["gsT"])
        P.dma("sp", dm_gs[h], gsT, "st_gs", reads=["gsT"], writes=["dm_gs"])
        for g4 in range(5):
            tiles = list(range(g4 * 4, min(18, g4 * 4 + 4)))
            bk = PB + (pbc[0] % 2)
            pbc[0] += 1

            def mmv(e, tiles=tiles, bk=bk, wh=wh):
                for i, ti in enumerate(tiles):
                    for kt in range(8):
                        r = e.matmul(banks[bk][:, i * 128:(i + 1) * 128], lhsT=uT[:, kt, ti * 128:(ti + 1) * 128], rhs=wh[:, kt, 1, :], start=(kt == 0), stop=(kt == 7))
                return r
            P.op("pe", mmv, reads=[whk] + ["uT_%d" % t for t in tiles], writes=["bank%d" % bk])
            nt_ = len(tiles)
            P.op("act", lambda e, tiles=tiles, bk=bk, nt_=nt_: e.copy(out=v_tm[:, tiles[0]:tiles[0] + nt_, :], in_=banks[bk][:, 0:nt_ * 128].rearrange("p (a b) -> p a b", b=128)),
                 reads=["bank%d" % bk, "v_tm"], writes=["v_tm"])
        hgrn_dir_prep(h, 0)
        hgrn_dir_prep(h, 1)
        gens = [hgrn_dir_scan(h, 0), hgrn_dir_scan(h, 1)]
        alive = [True, True]
        while any(alive):
            for i in range(2):
                if alive[i]:
                    try:
                        next(gens[i])
                    except StopIteration:
                        alive[i] = False
        P.dma("sp", dm_oloc[h], o_acc, "st_oloc", reads=["oacc_%d" % g for g in range(4)], writes=["dm_oloc"])
    for d in range(2):
        P.dma("sp", ag2_ins[d][:, 0:1024], stage[:, d, :, :].rearrange("p b c -> p (b c)"), "st_ag2", reads=["stage_%d_%d" % (d, h) for h in range(8)], writes=["ag2_in"])
        P.dma("sp", ag2_ins[d][:, 1024:1032], Dv[:, d, :], "st_ag2", reads=["Dv_%d_%d" % (d, h) for h in range(8)], writes=["ag2_in"])
    for d in range(2):
        P.custom("pool", lambda e, d=d: e.collective_compute("AllGather", ALU.bypass, replica_groups=[[0, 1, 2, 3], [4, 5, 6, 7]], ins=[ag2_ins[d]], outs=[ag2_outs[d]]),
                 "cc2", 1, reads=["ag2_in"] + (["ag2_out"] if d > 0 else []), writes=["ag2_out"])
    P.barrier(keep=["ag1_out", "dm_kvctx", "dm_mod", "ag2_out", "dm_oloc", "dm_qb", "dm_gs"] + UT_KEYS)
    A.release(m3h)
    gath = A.alloc((2, 4, 1032), F32)
    Rr = A.alloc((2, 8, 128), F32)
    Tt = A.alloc((8, 128), F32)
    selv = A.alloc((8,), F32)
    Sin = A.alloc((2, 8, 128), BF16)
    for d in range(2):
        P.dma("sp", gath[:, d, :, :], ag2_outs[d].rearrange("(r p) n -> p r n", p=128), "ld_gath", reads=["ag2_out"], writes=["gath"])
    P.dma("sp", selv, sel_d, "ld_c", writes=["selv"])
    SCK = ["sctx_%d_%d" % (d, h) for d in range(2) for h in range(8)]
    P.op("dve", lambda e: e.tensor_copy(out=Rr.rearrange("p a b c -> p (a b c)"), in_=sctx.rearrange("p a b c -> p (a b c)")), writes=["Rr"])
    for d in range(2):
        order = [0, 1, 2, 3] if d == 0 else [3, 2, 1, 0]
        for r in order:
            Sl = gath[:, d, r, 0:1024].rearrange("p (h v) -> p h v", v=128)
            Dr = gath[:, d, r, 1024:1032]
            P.op("dve", lambda e, d=d, Dr=Dr: e.tensor_tensor(out=Tt, in0=Rr[:, d, :, :], in1=bc(Dr, 2, [128, 8, 128]), op=ALU.mult), reads=["Rr", "gath"], writes=["Tt"])
            P.op("dve", lambda e, Sl=Sl: e.tensor_tensor(out=Tt, in0=Tt, in1=Sl, op=ALU.add), reads=["Tt", "gath"], writes=["Tt"])
            P.op("dve", lambda e, d=d: e.tensor_tensor(out=Tt, in0=Tt, in1=Rr[:, d, :, :], op=ALU.subtract), reads=["Tt", "Rr"], writes=["Tt"])
            P.op("dve", lambda e, d=d, r=r: e.scalar_tensor_tensor(out=Rr[:, d, :, :], in0=Tt, scalar=selv[:, d * 4 + r:d * 4 + r + 1], in1=Rr[:, d, :, :], op0=ALU.mult, op1=ALU.add),
                 reads=["Tt", "Rr", "selv"], writes=["Rr"])
    P.op("act", lambda e: e.copy(out=Sin.rearrange("p a b c -> p (a b c)"), in_=Rr.rearrange("p a b c -> p (a b c)")), reads=["Rr"], writes=["Sin"])
    ol = A.alloc((NT,), F32)
    qbf_ = A.alloc((NT,), BF16)
    qbb_ = A.alloc((NT,), BF16)
    gsl = A.alloc((NT,), BF16)
    sqb = A.alloc((NT,), BF16)
    lnr = A.alloc((NT,), F32)
    rsr = A.alloc((NT,), F32)
    for h in range(8):
        P.dma("sp", ol, dm_oloc[h], "ld_ol", reads=["dm_oloc"], writes=["ol"] + ["ol_%d" % b_ for b_ in range(4)])
        P.dma("sp", qbf_, dm_qb[0, h], "ld_qb0", reads=["dm_qb"], writes=["qbf_"])
        P.dma("sp", qbb_, dm_qb[1, h], "ld_qb1", reads=["dm_qb"], writes=["qbb_"])
        P.dma("sp", gsl, dm_gs[h], "ld_gs", reads=["dm_gs"], writes=["gsl"])
        for blk in range(4):
            cs = slice(blk * 512, (blk + 1) * 512)
            bk = blk % 2

            def corr(e, h=h, cs=cs, bk=bk):
                e.matmul(banks[bk][:, :], lhsT=Sin[:, 0, h, :], rhs=qbf_[:, cs], start=True, stop=False)
                return e.matmul(banks[bk][:, :], lhsT=Sin[:, 1, h, :], rhs=qbb_[:, cs], start=False, stop=True)
            P.op("pe", corr, reads=["Sin", "qbf_", "qbb_"], writes=["bank%d" % bk])
            P.op("dve", lambda e, cs=cs, bk=bk: e.tensor_tensor(out=ol[:, cs], in0=ol[:, cs], in1=banks[bk][:, :], op=ALU.add), reads=["bank%d" % bk, "ol", "ol_%d" % blk], writes=["ol_%d" % blk])
            P.op("act", lambda e, cs=cs: e.activation(out=sqb[:, cs], in_=ol[:, cs], func=AF.Square), reads=["ol_%d" % blk], writes=["sqb_%d" % blk])
            bk2 = 2 + blk % 2
            P.op("pe", lambda e, cs=cs, bk2=bk2: e.matmul(banks[bk2][:, :], lhsT=ones_b, rhs=sqb[:, cs], start=True, stop=True), reads=["sqb_%d" % blk, "ones_b"], writes=["bank%d" % bk2])
            P.op("act", lambda e, cs=cs, bk2=bk2: e.activation(out=lnr[:, cs], in_=banks[bk2][:, :], func=AF.Ln, scale=1.0 / 128, bias=EPS), reads=["bank%d" % bk2], writes=["lnr_%d" % blk])
            P.op("act", lambda e, cs=cs: e.activation(out=rsr[:, cs], in_=lnr[:, cs], func=AF.Exp, scale=-0.5), reads=["lnr_%d" % blk], writes=["rsr_%d" % blk])
            P.op("dve", lambda e, cs=cs: e.tensor_tensor(out=ol[:, cs], in0=ol[:, cs], in1=rsr[:, cs], op=ALU.mult), reads=["ol_%d" % blk, "rsr_%d" % blk], writes=["ol_%d" % blk])
            P.op("dve", lambda e, cs=cs: e.tensor_tensor(out=sqb[:, cs], in0=ol[:, cs], in1=gsl[:, cs], op=ALU.mult), reads=["ol_%d" % blk, "gsl"], writes=["sqb_%d" % blk])
        P.dma("sp", dm_oth[h], sqb, "st_oth", reads=["sqb_%d" % b_ for b_ in range(4)], writes=["dm_oth"])
        for b_ in range(4):
            for nm in ("ol_%d", "sqb_%d", "lnr_%d", "rsr_%d", "oth_%d"):
                pass
    P.barrier(keep=["ag1_out", "dm_kvctx", "dm_mod", "dm_oth"] + UT_KEYS)
    A.release(m3)
    if upto <= 3:
        return finish(P, nc)


    m4 = A.mark()
    QT = A.alloc((8, NT), BF16)
    KTa = A.alloc((2, 8448), BF16)
    Va = A.alloc((66, 256), BF16)
    for r in range(4):
        for h in range(2):
            P.dma("sp", KTa[:, h, r * NT:(r + 1) * NT], ag1_outs[h][r * 128:(r + 1) * 128, :], "ld_kta", reads=["ag1_out"], writes=["KTa"])
            P.dma("sp", Va[:, r * 16 + 8 * h:r * 16 + 8 * h + 8, :], ag1_outs[2 + h][r * 128:(r + 1) * 128, :].rearrange("p (ti c) -> p ti c", c=256), "ld_va", reads=["ag1_out"], writes=["Va"])
    P.dma("sp", KTa[:, :, 8192:8448], dm_kvctx[0:256, :].rearrange("(h p) t -> p h t", p=128), "ld_kta", reads=["dm_kvctx"], writes=["KTa"])
    P.dma("sp", Va[:, 64:66, :], dm_kvctx[256:512, :].rearrange("(ti p) c -> p ti c", p=128), "ld_va", reads=["dm_kvctx"], writes=["Va"])
    m4b = A.mark()
    wq = A.alloc((8, 1024), BF16)
    P.dma("pool", wq, win_d[:, 5120:6144].rearrange("(kt p) n -> p kt n", p=128), "ld_wq", writes=["wq"])
    ropeT = A.alloc((NTT, 256), F32)
    P.dma("sp", ropeT, rope_d.rearrange("(t p) n -> p t n", p=128), "ld_rope", writes=["ropeT"])
    gqk = A.alloc((256,), F32)
    P.dma("sp", gqk, qkg_d[0].partition_broadcast(128), "ld_c", writes=["gqk"])
    ssq = A.alloc((16, 8), F32)
    lnq = A.alloc((16, 8), F32)
    rsq = A.alloc((16, 8), F32)
    qn = A.alloc((8, 128), F32)
    t1q = A.alloc((8, 128), F32)
    t2q = A.alloc((8, 128), F32)
    qbf = A.alloc((1024,), BF16)
    junk2 = A.alloc((128,), BF16)
    P.op("pool", lambda e: e.memset(ssq, 0.0), writes=["ssq"])
    for ti in range(NTT):
        s = ti % 2
        for half in range(2):
            def mmq(e, ti=ti, half=half):
                for kt in range(8):
                    r = e.matmul(banks[half][:, :], lhsT=uT[:, kt, ti * 128:(ti + 1) * 128], rhs=wq[:, kt, half * 512:(half + 1) * 512], start=(kt == 0), stop=(kt == 7))
                return r
            P.op("pe", mmq, reads=["uT_%d" % ti, "wq"], writes=["bank%d" % half])
        for h in range(8):
            P.op("act", lambda e, h=h, ti=ti: e.activation(out=junk2, in_=banks[h // 4][:, (h % 4) * 128:(h % 4 + 1) * 128], func=AF.Square, accum_out=ssq[:, ti, h:h + 1]),
                 reads=["bank%d" % (h // 4), "ssq"], writes=["junk2", "ssq_%d" % ti])
        rstd_from_ss(ssq[:, ti, :], 128, rsq[:, ti, :], lnq[:, ti, :], ["ssq_%d" % ti], "rsq_%d" % ti)
        for half in range(2):
            P.op("dve", lambda e, half=half, ti=ti: e.tensor_tensor(out=qn[:, half * 4:(half + 1) * 4, :], in0=banks[half][:, :].rearrange("p (h d) -> p h d", d=128),
                                                                in1=bc(rsq[:, ti, half * 4:(half + 1) * 4], 2, [128, 4, 128]), op=ALU.mult),
                 reads=["bank%d" % half, "rsq_%d" % ti, "qxn"], writes=["qxn"])
        P.op("dve", lambda e: e.tensor_tensor(out=qn, in0=qn, in1=bc(gqk[:, 0:128], 1, [128, 8, 128]), op=ALU.mult), reads=["qxn", "gqk"], writes=["qxn"])
        rope_apply(qn, 8, ti, qbf, "q")
        bk2 = 2 + s

        def trq(e, bk2=bk2):
            for h in range(8):
                r = e.transpose(bank_bf(bk2)[:, h * 128:(h + 1) * 128], qbf[:, h * 128:(h + 1) * 128], ident_b)
            return r
        P.op("pe", trq, reads=["qbf", "ident_b"], writes=["bank%d" % bk2])
        P.op("act", lambda e, ti=ti, bk2=bk2: e.copy(out=QT[:, :, ti * 128:(ti + 1) * 128], in_=bank_bf(bk2).rearrange("p (a b) -> p a b", b=128)),
             reads=["bank%d" % bk2], writes=["QT"])
    P.barrier(keep=["dm_mod", "dm_oth", "KTa", "Va"] + UT_KEYS)
    A.release(m4b)
    if upto <= 4:
        return finish(P, nc)

    pT = [A.alloc((512,), BF16) for _ in range(3)]
    rden = A.alloc((512,), F32)
    ob = [A.alloc((512,), BF16) for _ in range(2)]
    dacc = [A.alloc((512,), F32) for _ in range(2)]
    SCALE = float(128 ** -0.5)
    NKT = 66
    it = 0
    for kvh in range(2):
        for qb in range(4):
            for g in range(4):
                head = kvh * 4 + g
                bo, bd = 4 + it % 2, 6 + it % 2
                qs = slice(qb * 512, (qb + 1) * 512)

                def s_mm(kt, kvh=kvh, head=head, qs=qs):
                    P.op("pe", lambda e: e.matmul(banks[kt % 3][:, :], lhsT=KTa[:, kvh, kt * 128:(kt + 1) * 128], rhs=QT[:, head, qs], start=True, stop=True),
                         reads=["KTa", "QT"], writes=["bank%d" % (kt % 3)])
                s_mm(0)
                s_mm(1)
                for kt in range(NKT):
                    if kt + 2 < NKT:
                        s_mm(kt + 2)
                    P.op("act", lambda e, kt=kt: e.activation(out=pT[kt % 3], in_=banks[kt % 3][:, :], func=AF.Exp, scale=SCALE),
                         reads=["bank%d" % (kt % 3)], writes=["pT%d" % (kt % 3)])

                    P.op("pe", lambda e, kt=kt, kvh=kvh, bo=bo: e.matmul(banks[bo][:, :], lhsT=Va[:, kt, kvh * 128:(kvh + 1) * 128], rhs=pT[kt % 3], start=(kt == 0), stop=(kt == NKT - 1)),
                         reads=["pT%d" % (kt % 3), "Va"], writes=["bank%d" % bo])
                    da = dacc[0]
                    if kt % 3 == 2:
                        P.op("pe", lambda e, kt=kt, bd=bd: e.matmul(banks[bd][:, :], lhsT=ones_b, rhs=pT[kt % 3], start=(kt == 2), stop=False),
                             reads=["pT%d" % (kt % 3), "ones_b"], writes=["bank%d" % bd])
                    elif kt == 0:
                        P.op("dve", lambda e, kt=kt, da=da: e.tensor_copy(out=da, in_=pT[kt % 3]), reads=["pT%d" % (kt % 3)], writes=["dacc0"])
                    else:
                        P.op("dve", lambda e, kt=kt, da=da: e.tensor_tensor(out=da, in0=da, in1=pT[kt % 3], op=ALU.add), reads=["pT%d" % (kt % 3), "dacc0"], writes=["dacc0"])

                def dsum(e, bd=bd):
                    return e.matmul(banks[bd][:, :], lhsT=ones_f, rhs=dacc[0], start=False, stop=True)
                P.op("pe", dsum, reads=["dacc0", "ones_f"], writes=["bank%d" % bd])
                P.op("dve", lambda e, bd=bd: e.reciprocal(out=rden, in_=banks[bd][:, :]), reads=["bank%d" % bd], writes=["rden"])
                P.op("dve", lambda e, bo=bo, it=it: e.tensor_tensor(out=ob[it % 2], in0=banks[bo][:, :], in1=rden, op=ALU.mult), reads=["bank%d" % bo, "rden"], writes=["ob%d" % (it % 2)])
                P.dma("sp", dm_ota[head][:, qs], ob[it % 2], "st_ota%d" % (it % 2), reads=["ob%d" % (it % 2)], writes=["dm_ota"])
                it += 1
    P.barrier(keep=["dm_mod", "dm_oth", "dm_ota"] + UT_KEYS)
    A.release(m4)
    if upto <= 5:
        return finish(P, nc)

    m6 = A.mark()
    wg = A.alloc((8, 2048), BF16)
    wb0 = A.alloc((8, 1024), BF16)
    wb1 = A.alloc((8, 1024), BF16)
    wo = A.alloc((8, 1024), BF16)
    P.dma("pool", wg, win_d[:, 6656:8704].rearrange("(kt p) n -> p kt n", p=128), "ld_w6", writes=["wg"])
    P.dma("pool", wb0, wbr_d[0].rearrange("(kt p) n -> p kt n", p=128), "ld_w6", writes=["wb0"])
    P.dma("pool", wb1, wbr_d[1].rearrange("(kt p) n -> p kt n", p=128), "ld_w6", writes=["wb1"])
    P.dma("pool", wo, wout_d.rearrange("(kt p) n -> p kt n", p=128), "ld_w6", writes=["wo"])
    G1 = A.alloc((D,), F32)
    A2 = A.alloc((D,), F32)
    B2 = A.alloc((D,), F32)
    P.dma("sp", G1, dm_mod[:, 2 * D:3 * D], "ld_c", reads=["dm_mod"], writes=["G1"])
    P.dma("sp", A2, dm_mod[:, 4 * D:5 * D], "ld_c", reads=["dm_mod"], writes=["A2"])
    P.dma("sp", B2, dm_mod[:, 3 * D:4 * D], "ld_c", reads=["dm_mod"], writes=["B2"])
    rwt = A.alloc((8, NE), F32)
    rbt = A.alloc((NE,), F32)
    P.dma("sp", rwt, rw_d.rearrange("(kt p) e -> p kt e", p=128), "ld_c", writes=["rwt"])
    P.dma("sp", rbt, rb_d[0].partition_broadcast(128), "ld_c", writes=["rbt"])
    othb = A.alloc((8, 512), BF16)
    otab = A.alloc((8, 512), BF16)
    y1T = A.alloc((8, 512), BF16)
    sgh = [A.alloc((512,), F32)] * 2
    sga = [A.alloc((512,), F32)] * 2
    tA = [A.alloc((512,), F32)] * 2
    tB = [A.alloc((512,), F32)] * 2
    xt6 = [A.alloc((D,), F32) for _ in range(2)]
    tmp6 = A.alloc((D,), F32)
    x1t = [A.alloc((D,), F32)] * 2
    u2f = A.alloc((D,), F32)
    u2b = A.alloc((D,), BF16)
    junk6 = A.alloc((D,), BF16)
    ssy = A.alloc((16, 2), F32)
    ssy1 = A.alloc((16,), F32)
    lny = A.alloc((16,), F32)
    rsy = A.alloc((16,), F32)
    ssx = A.alloc((16,), F32)
    lnx = A.alloc((16,), F32)
    rsx = A.alloc((16,), F32)
    u2Tf = A.alloc((8, 128), F32)
    u2Tb = [A.alloc((8, 128), BF16) for _ in range(2)]
    lg = A.alloc((NE,), F32)
    mx8 = A.alloc((8,), F32)
    msk = A.alloc((NE,), F32)
    em = A.alloc((NE,), F32)
    nmx = A.alloc((1,), F32)
    ssum = A.alloc((1,), F32)
    rsum = A.alloc((1,), F32)
    cmb = A.alloc((16, NE), F32)
    cT = A.alloc((128,), F32)
    P.op("pool", lambda e: e.memset(ssy, 0.0), writes=["ssy"])
    P.op("pool", lambda e: e.memset(ssx, 0.0), writes=["ssx"])
    oth_v = dm_oth.rearrange("h p t -> p h t")
    ota_v = dm_ota.rearrange("h p t -> p h t")
    for blk in range(4):
        cs = slice(blk * 512, (blk + 1) * 512)
        P.dma("sp", othb, oth_v[:, :, cs], "ld_oth", reads=["dm_oth"], writes=["othb"])
        P.dma("sp", otab, ota_v[:, :, cs], "ld_ota", reads=["dm_ota"], writes=["otab"])
        utk = ["uT_%d" % t for t in range(blk * 4, blk * 4 + 4)]
        for dt in range(8):
            s = 0
            ds = slice(dt * 128, (dt + 1) * 128)

            def mm4(e, ds=ds, dt=dt, cs=cs):
                for kt in range(8):
                    e.matmul(banks[0][:, :], lhsT=wg[:, kt, dt * 128:(dt + 1) * 128], rhs=uT[:, kt, cs], start=(kt == 0), stop=(kt == 7))
                for kt in range(8):
                    e.matmul(banks[1][:, :], lhsT=wg[:, kt, 1024 + dt * 128:1024 + (dt + 1) * 128], rhs=uT[:, kt, cs], start=(kt == 0), stop=(kt == 7))
                for kt in range(8):
                    e.matmul(banks[2][:, :], lhsT=wb0[:, kt, ds], rhs=othb[:, kt, :], start=(kt == 0), stop=(kt == 7))
                for kt in range(8):
                    r = e.matmul(banks[3][:, :], lhsT=wb1[:, kt, ds], rhs=otab[:, kt, :], start=(kt == 0), stop=(kt == 7))
                return r
            P.op("pe", mm4, reads=["wg", "wb0", "wb1", "othb", "otab"] + utk, writes=["bank0", "bank1", "bank2", "bank3"])
            P.op("act", lambda e, s=s: e.activation(out=sgh[s], in_=banks[0][:, :], func=AF.Sigmoid), reads=["bank0"], writes=["sgh%d" % s])
            P.op("act", lambda e, s=s: e.activation(out=sga[s], in_=banks[1][:, :], func=AF.Sigmoid), reads=["bank1"], writes=["sga%d" % s])
            P.op("dve", lambda e, s=s: e.tensor_tensor(out=tA[s], in0=sgh[s], in1=banks[2][:, :], op=ALU.mult), reads=["sgh%d" % s, "bank2"], writes=["tA%d" % s])
            P.op("dve", lambda e, s=s: e.tensor_tensor(out=tB[s], in0=sga[s], in1=banks[3][:, :], op=ALU.mult), reads=["sga%d" % s, "bank3"], writes=["tB%d" % s])
            P.op("dve", lambda e, s=s, dt=dt: e.tensor_tensor(out=y1T[:, dt, :], in0=tA[s], in1=tB[s], op=ALU.add), reads=["tA%d" % s, "tB%d" % s], writes=["y1T"])
        for tt in range(4):
            ti = blk * 4 + tt
            s = ti % 2
            ts_ = slice(tt * 128, (tt + 1) * 128)
            P.dma("sp", xt6[s], x_d[ti * 128:(ti + 1) * 128, :], "ld_x6%d" % s, writes=["xt6%d" % s])
            for half in range(2):
                def mmy(e, half=half, ts_=ts_):
                    for kt in range(8):
                        r = e.matmul(banks[4 + half][:, :], lhsT=y1T[:, kt, ts_], rhs=wo[:, kt, half * 512:(half + 1) * 512], start=(kt == 0), stop=(kt == 7))
                    return r
                P.op("pe", mmy, reads=["y1T", "wo"], writes=["bank%d" % (4 + half)])
                P.op("act", lambda e, half=half, ti=ti: e.activation(out=junk6[:, 0:512], in_=banks[4 + half][:, :], func=AF.Square, accum_out=ssy[:, ti, half:half + 1]),
                     reads=["bank%d" % (4 + half), "ssy"], writes=["junk6", "ssy_%d_%d" % (ti, half)])
            P.op("dve", lambda e, ti=ti: e.tensor_tensor(out=ssy1[:, ti:ti + 1], in0=ssy[:, ti, 0:1], in1=ssy[:, ti, 1:2], op=ALU.add),
                 reads=["ssy_%d_0" % ti, "ssy_%d_1" % ti], writes=["ssy1_%d" % ti])
            rstd_from_ss(ssy1[:, ti:ti + 1], D, rsy[:, ti:ti + 1], lny[:, ti:ti + 1], ["ssy1_%d" % ti], "rsy_%d" % ti)
            for half in range(2):
                hs = slice(half * 512, (half + 1) * 512)
                P.op("dve", lambda e, half=half, hs=hs, ti=ti: e.scalar_tensor_tensor(out=tmp6[:, hs], in0=banks[4 + half][:, :], scalar=rsy[:, ti:ti + 1], in1=G1[:, hs], op0=ALU.mult, op1=ALU.mult),
                     reads=["bank%d" % (4 + half), "rsy_%d" % ti, "G1", "tmp6"], writes=["tmp6"])
            P.op("dve", lambda e, s=s: e.tensor_tensor(out=x1t[s], in0=tmp6, in1=xt6[s], op=ALU.add), reads=["tmp6", "xt6%d" % s], writes=["x1t"])
            P.dma("sp", dm_x1[ti * 128:(ti + 1) * 128, :], x1t[s], "st_x1%d" % s, reads=["x1t"], writes=["dm_x1"])
            P.op("act", lambda e, s=s, ti=ti: e.activation(out=junk6, in_=x1t[s], func=AF.Square, accum_out=ssx[:, ti:ti + 1]), reads=["x1t", "ssx"], writes=["junk6", "ssx_%d" % ti])
            rstd_from_ss(ssx[:, ti:ti + 1], D, rsx[:, ti:ti + 1], lnx[:, ti:ti + 1], ["ssx_%d" % ti], "rsx_%d" % ti)
            P.op("dve", lambda e, s=s, ti=ti: e.scalar_tensor_tensor(out=tmp6, in0=x1t[s], scalar=rsx[:, ti:ti + 1], in1=A2, op0=ALU.mult, op1=ALU.mult),
                 reads=["x1t", "rsx_%d" % ti, "A2", "tmp6"], writes=["tmp6"])
            P.op("dve", lambda e: e.tensor_tensor(out=u2f, in0=tmp6, in1=B2, op=ALU.add), reads=["tmp6", "B2"], writes=["u2f"])
            P.op("act", lambda e: e.copy(out=u2b, in_=u2f), reads=["u2f"], writes=["u2b"])

            def tru(e):
                for kt in range(8):
                    r = e.transpose(bank_bf(6)[:, kt * 128:(kt + 1) * 128], u2b[:, kt * 128:(kt + 1) * 128], ident_b)
                return r
            P.op("pe", tru, reads=["u2b", "ident_b"], writes=["bank6"])
            P.op("act", lambda e, s=s: e.copy(out=u2Tb[s], in_=bank_bf(6).rearrange("p (a b) -> p a b", b=128)), reads=["bank6"], writes=["u2Tb%d" % s])
            P.dma("sp", dm_u2t[:, :, ti * 128:(ti + 1) * 128], u2Tb[s], "st_u2t%d" % s, reads=["u2Tb%d" % s], writes=["dm_u2t"])
            for g2 in range(2):
                def truf(e, g2=g2):
                    for i in range(4):
                        kt = g2 * 4 + i
                        r = e.transpose(banks[7][:, i * 128:(i + 1) * 128], u2f[:, kt * 128:(kt + 1) * 128], ident_f)
                    return r
                P.op("pe", truf, reads=["u2f", "ident_f"], writes=["bank7"])
                P.op("act", lambda e, g2=g2: e.copy(out=u2Tf[:, g2 * 4:(g2 + 1) * 4, :], in_=banks[7][:, :].rearrange("p (a b) -> p a b", b=128)), reads=["bank7", "u2Tf"], writes=["u2Tf"])

            def mml(e):
                for kt in range(8):
                    r = e.matmul(banks[6][:, 0:NE], lhsT=u2Tf[:, kt, :], rhs=rwt[:, kt, :], start=(kt == 0), stop=(kt == 7))
                return r
            P.op("pe", mml, reads=["u2Tf", "rwt"], writes=["bank6"])
            P.op("dve", lambda e: e.tensor_tensor(out=lg, in0=banks[6][:, 0:NE], in1=rbt, op=ALU.add), reads=["bank6", "rbt"], writes=["lg"])
            P.op("dve", lambda e: e.max(out=mx8, in_=lg), reads=["lg"], writes=["mx8"])
            P.op("dve", lambda e: e.tensor_scalar(out=msk, in0=lg, scalar1=mx8[:, 3:4], scalar2=None, op0=ALU.is_ge), reads=["lg", "mx8"], writes=["msk"])
            P.op("dve", lambda e: e.tensor_scalar(out=nmx, in0=mx8[:, 0:1], scalar1=-1.0, scalar2=None, op0=ALU.mult), reads=["mx8"], writes=["nmx"])
            P.op("act", lambda e: e.activation(out=em, in_=lg, func=AF.Exp, bias=nmx[:, 0:1], scale=1.0), reads=["lg", "nmx"], writes=["em"])
            P.op("dve", lambda e: e.tensor_tensor(out=em, in0=em, in1=msk, op=ALU.mult), reads=["em", "msk"], writes=["em"])
            P.op("dve", lambda e: e.reduce_sum(out=ssum, in_=em, axis=AX.X), reads=["em"], writes=["ssum"])
            P.op("dve", lambda e: e.reciprocal(out=rsum, in_=ssum), reads=["ssum"], writes=["rsum"])
            P.op("dve", lambda e, ti=ti: e.tensor_scalar(out=cmb[:, ti, :], in0=em, scalar1=rsum[:, 0:1], scalar2=None, op0=ALU.mult), reads=["em", "rsum"], writes=["cmb_%d" % ti])
            P.op("pe", lambda e, ti=ti: e.transpose(banks[7][0:NE, 0:128], cmb[:, ti, :], ident_f), reads=["cmb_%d" % ti, "ident_f"], writes=["bank7"])
            P.op("act", lambda e: e.copy(out=cT[0:NE, :], in_=banks[7][0:NE, 0:128]), reads=["bank7"], writes=["cT"])
            P.dma("sp", dm_combT[:, ti * 128:(ti + 1) * 128], cT[0:NE, :], "st_cT", reads=["cT"], writes=["dm_combT"])
    P.dma("sp", dm_comb, cmb, "st_cmb", reads=["cmb_%d" % t for t in range(16)], writes=["dm_comb"])
    P.barrier(keep=["dm_mod", "dm_x1", "dm_u2t", "dm_comb", "dm_combT"])
    A.release(m_pre_ut)
    if upto <= 6:
        return finish(P, nc)

    G2 = A.alloc((D,), F32)
    P.dma("sp", G2, dm_mod[:, 5 * D:6 * D], "ld_c", reads=["dm_mod"], writes=["G2"])
    bu = A.alloc((NE * 16,), F32)
    P.dma("sp", bu, bupT_d, "ld_c", writes=["bu"])
    bdn = A.alloc((D,), F32)
    P.dma("sp", bdn[0:NE, :], bdn_d, "ld_c", writes=["bdn"])
    cmb7 = A.alloc((16, NE), F32)
    P.dma("sp", cmb7, dm_comb, "ld_c", reads=["dm_comb"], writes=["cmb7"])
    P.op("dve", lambda e: e.tensor_scalar(out=cmb7, in0=cmb7, scalar1=1.0 / 1.702, scalar2=None, op0=ALU.mult), reads=["cmb7"], writes=["cmb7"])
    bu1 = A.alloc((NE * 16,), F32)
    P.op("dve", lambda e: e.tensor_scalar(out=bu1, in0=bu, scalar1=1.0, scalar2=None, op0=ALU.add), reads=["bu"], writes=["bu1"])
    cT2 = A.alloc((1024,), F32)
    u2T = A.alloc((8, 1024), BF16)
    acc = A.alloc((8, D), F32)
    wu = [A.alloc((8, 2 * D), BF16) for _ in range(2)]
    wd = [A.alloc((8, D), BF16) for _ in range(2)]
    aTraw = [A.alloc((4096,), BF16) for _ in range(2)]
    aT = [a.rearrange("p (a b) -> p a b", b=512) for a in aTraw]
    gc = [A.alloc((512,), F32) for _ in range(2)]
    sgm = [A.alloc((512,), F32) for _ in range(2)]
    lc = [A.alloc((512,), F32) for _ in range(2)]
    xt7 = [aTraw[0][:, 0:2048].bitcast(F32)] * 2
    tmp7 = aTraw[0][:, 2048:4096].bitcast(F32)
    ot7 = [aTraw[1][:, 0:2048].bitcast(F32)] * 2
    junk7 = aTraw[1][:, 2048:3072]
    ss7 = A.alloc((16,), F32)
    ln7 = A.alloc((16,), F32)
    rs7 = A.alloc((16,), F32)
    P.op("pool", lambda e: e.memset(ss7, 0.0), writes=["ss7"])
    ecount = 0
    for half in range(2):
        hc = slice(half * 1024, (half + 1) * 1024)
        P.dma("sp", u2T, dm_u2t[:, :, hc], "ld_u2t", reads=["dm_u2t"], writes=["u2T"])
        P.dma("sp", cT2[0:NE, :], dm_combT[:, hc], "ld_cT2", reads=["dm_combT"], writes=["cT2"])
        for tt in range(8):
            for dh in range(2):
                bk = 4 + (tt * 2 + dh) % 4
                P.op("pe", lambda e, tt=tt, dh=dh, bk=bk: e.matmul(banks[bk][:, :], lhsT=cT2[0:NE, tt * 128:(tt + 1) * 128], rhs=bdn[0:NE, dh * 512:(dh + 1) * 512], start=True, stop=True),
                     reads=["cT2", "bdn"], writes=["bank%d" % bk])
                P.op("act", lambda e, tt=tt, dh=dh, bk=bk: e.copy(out=acc[:, tt, dh * 512:(dh + 1) * 512], in_=banks[bk][:, :]), reads=["bank%d" % bk], writes=["acc_%d_%d" % (tt, dh)])
        for ex in range(NE):
            ws = ecount % 2
            ecount += 1
            P.dma("pool", wu[ws], wup_d[ex].rearrange("(kt p) n -> p kt n", p=128), "ld_wu%d" % ws, writes=["wu%d" % ws])
            P.dma("pool", wd[ws], wdn_d[ex].rearrange("(kt p) n -> p kt n", p=128), "ld_wd%d" % ws, writes=["wd%d" % ws])
            for blk in range(2):
                bs = slice(blk * 512, (blk + 1) * 512)
                ab = aT[blk % 2]
                abk = "aT%d" % (blk % 2)
                for g in range(8):
                    s = g % 2
                    bg, bl = g % 2, 2 + g % 2

                    def mmu(e, g=g, bg=bg, bl=bl, ws=ws, bs=bs):
                        for kt in range(8):
                            e.matmul(banks[bg][:, :], lhsT=wu[ws][:, kt, g * 128:(g + 1) * 128], rhs=u2T[:, kt, bs], start=(kt == 0), stop=(kt == 7))
                        for kt in range(8):
                            r = e.matmul(banks[bl][:, :], lhsT=wu[ws][:, kt, 1024 + g * 128:1024 + (g + 1) * 128], rhs=u2T[:, kt, bs], start=(kt == 0), stop=(kt == 7))
                        return r
                    P.op("pe", mmu, reads=["wu%d" % ws, "u2T"], writes=["bank%d" % bg, "bank%d" % bl])
                    P.op("dve", lambda e, s=s, bg=bg, ex=ex, g=g: e.tensor_scalar(out=gc[s], in0=banks[bg][:, :], scalar1=bu[:, ex * 16 + g:ex * 16 + g + 1], scalar2=7.0, op0=ALU.add, op1=ALU.min),
                         reads=["bank%d" % bg, "bu"], writes=["gc%d" % s])
                    P.op("act", lambda e, s=s: e.activation(out=sgm[s], in_=gc[s], func=AF.Silu, scale=1.702), reads=["gc%d" % s], writes=["sgm%d" % s])
                    P.op("dve", lambda e, s=s, bl=bl, ex=ex, g=g: e.tensor_scalar(out=lc[s], in0=banks[bl][:, :], scalar1=bu1[:, ex * 16 + 8 + g:ex * 16 + 8 + g + 1], scalar2=8.0, op0=ALU.add, op1=ALU.min),
                         reads=["bank%d" % bl, "bu1"], writes=["lc%d" % s])
                    P.op("dve", lambda e, s=s, g=g, ab=ab: e.scalar_tensor_tensor(out=ab[:, g, :], in0=lc[s], scalar=-6.0, in1=sgm[s], op0=ALU.max, op1=ALU.mult),
                         reads=["sgm%d" % s, "lc%d" % s], writes=[abk])
                for tt in range(4):
                    til = blk * 4 + tt
                    for dh in range(2):
                        bk = 4 + (tt * 2 + dh) % 4

                        def mmd(e, tt=tt, dh=dh, bk=bk, ws=ws, ab=ab):
                            for fk in range(8):
                                r = e.matmul(banks[bk][:, :], lhsT=ab[:, fk, tt * 128:(tt + 1) * 128], rhs=wd[ws][:, fk, dh * 512:(dh + 1) * 512], start=(fk == 0), stop=(fk == 7))
                            return r
                        P.op("pe", mmd, reads=[abk, "wd%d" % ws], writes=["bank%d" % bk])
                        ak = "acc_%d_%d" % (til, dh)
                        P.op("dve", lambda e, til=til, dh=dh, bk=bk, ex=ex, half=half: e.scalar_tensor_tensor(
                            out=acc[:, til, dh * 512:(dh + 1) * 512], in0=banks[bk][:, :], scalar=cmb7[:, half * 8 + til, ex:ex + 1], in1=acc[:, til, dh * 512:(dh + 1) * 512], op0=ALU.mult, op1=ALU.add),
                            reads=["bank%d" % bk, "cmb7", ak], writes=[ak])
        P.barrier(keep=["dm_x1", "dm_u2t", "dm_combT"])
        for tt in range(8):
            ti = half * 8 + tt
            s = 0
            P.dma("sp", xt7[s], dm_x1[ti * 128:(ti + 1) * 128, :], "ld_x7%d" % s, reads=["dm_x1"], writes=["xt7%d" % s])
            P.op("act", lambda e, tt=tt, ti=ti: e.activation(out=junk7, in_=acc[:, tt, :], func=AF.Square, accum_out=ss7[:, ti:ti + 1]),
                 reads=["acc_%d_0" % tt, "acc_%d_1" % tt, "ss7"], writes=["junk7", "ss7_%d" % ti])
            rstd_from_ss(ss7[:, ti:ti + 1], D, rs7[:, ti:ti + 1], ln7[:, ti:ti + 1], ["ss7_%d" % ti], "rs7_%d" % ti)
            P.op("dve", lambda e, tt=tt, ti=ti: e.scalar_tensor_tensor(out=tmp7, in0=acc[:, tt, :], scalar=rs7[:, ti:ti + 1], in1=G2, op0=ALU.mult, op1=ALU.mult),
                 reads=["acc_%d_0" % tt, "acc_%d_1" % tt, "rs7_%d" % ti, "G2"], writes=["tmp7"])
            P.op("dve", lambda e, s=s: e.tensor_tensor(out=ot7[s], in0=tmp7, in1=xt7[s], op=ALU.add), reads=["tmp7", "xt7%d" % s], writes=["ot7%d" % s])
            P.dma("sp", out_d[ti * 128:(ti + 1) * 128, :], ot7[s], "st_out%d" % s, reads=["ot7%d" % s], writes=["out"])
        P.barrier(keep=["dm_x1", "dm_u2t", "dm_combT"])

    finish(P, nc)
    return nc


def finish(P, nc):
    P.wait_all("sp")
    P.build()
    P.close()
    return nc


def _rope_table(j):
    t = np.arange(NT) + j * NT
    rows = (t // 64).astype(np.float32)
    cols = (t % 64).astype(np.float32)
    inv = (10000.0 ** (-np.arange(0, 64, 2, dtype=np.float32) / 64)).astype(np.float32)
    ar = rows[:, None] * inv[None, :]
    ac = cols[:, None] * inv[None, :]
    cr, sr, cc, sc = np.cos(ar), np.sin(ar), np.cos(ac), np.sin(ac)
    return np.concatenate([cr, cr, cc, cc, -sr, sr, -sc, sc], axis=1).astype(np.float32)


def make_in_maps(inp, small=False):
    f = lambda a: np.ascontiguousarray(np.asarray(a, dtype=np.float32))
    x, c, ctx, c_ctx = f(inp["x"]), f(inp["c"]), f(inp["ctx"]), f(inp["c_ctx"])
    shared = {
        "w_mod": f(inp["w_mod"][0]), "b_mod": f(inp["b_mod"][0]).reshape(1, -1),
        "norm_g": f(inp["norm_g"][0]).reshape(1, -1), "w_in": f(inp["w_in"][0]),
        "lbv": f(np.asarray(inp["hgrn_lb"]).reshape(2, 2, 8, 128).transpose(3, 0, 1, 2).reshape(128, 32)),
        "hng": f(inp["hgrn_norm_g"][0]).reshape(128, 1), "qkg": f(inp["qk_norm_g"][0]).reshape(1, 256),
        "w_branch": f(inp["w_branch"][0]), "w_out": f(inp["w_out"][0]),
        "router_w": f(inp["router_w"][0]), "router_b": f(inp["router_b"][0]).reshape(1, -1),
        "w_up": f(inp["w_up"][0]), "b_upT": f(np.asarray(inp["b_up"][0]).reshape(32, 16, 128).transpose(2, 0, 1).reshape(128, 512)),
        "w_down": f(inp["w_down"][0]), "b_down": f(inp["b_down"][0]),
    }
    ropes = [_rope_table(j) for j in range(4)]
    maps = []
    for core in range(8):
        b, j = core // 4, core % 4
        cvec = np.concatenate([c[b].reshape(8, 128).T, c_ctx.reshape(8, 128).T], axis=1)
        sel = np.zeros((128, 8), np.float32)
        for r in range(4):
            sel[:, r] = 1.0 if r < j else 0.0
            sel[:, 4 + r] = 1.0 if r > j else 0.0
        m = dict(shared)
        if small:
            m["w_up"] = m["w_up"][0:1]
            m["w_down"] = m["w_down"][0:1]
        m.update({"x": f(x[b, j * NT:(j + 1) * NT]), "ctx": f(ctx[b]), "cvec": f(cvec), "rope": ropes[j], "sel": sel})
        maps.append(m)
    return maps


_NC_CACHE = {}


def kernel(**inputs):
    if "nc" not in _NC_CACHE:
        _NC_CACHE["nc"] = build()
    nc = _NC_CACHE["nc"]
    maps = make_in_maps(inputs)
    res = run_bass_kernel_spmd(nc, maps, core_ids=list(range(8)))
    out = np.empty((2, 8192, D), np.float32)
    for core in range(8):
        b, j = core // 4, core % 4
        out[b, j * NT:(j + 1) * NT] = res.results[core]["out"]
    return out
```

```python
from contextlib import ExitStack
import numpy as np
import concourse.bass as bass
import concourse.mybir as mybir
from concourse.bass_utils import run_bass_kernel_spmd

F32 = mybir.dt.float32
BF16 = mybir.dt.bfloat16
ALU = mybir.AluOpType
AF = mybir.ActivationFunctionType
AX = mybir.AxisListType

ENGS = ("pe", "act", "dve", "pool", "sp")
EPOCH = 16000
EPS = 1e-6


def _freeze(fn, memo=None):
    import types
    if memo is None:
        memo = {}
    if not isinstance(fn, types.FunctionType) or fn.__closure__ is None:
        return fn
    if id(fn) in memo:
        return memo[id(fn)]
    cells = []
    for c in fn.__closure__:
        try:
            v = c.cell_contents
        except ValueError:
            cells.append(c)
            continue
        if isinstance(v, types.FunctionType) and v.__closure__ is not None and v is not fn:
            v = _freeze(v, memo)
        cells.append(types.CellType(v))
    new = types.FunctionType(fn.__code__, fn.__globals__, fn.__name__, fn.__defaults__, tuple(cells))
    new.__kwdefaults__ = fn.__kwdefaults__
    memo[id(fn)] = new
    return new


class Prog:
    def __init__(self, nc, same_engine_sync=True):
        self.nc = nc
        self.es = ExitStack()
        self.q = {e: [] for e in ENGS}
        self.cnt = {e: 0 for e in ENGS}
        self.waited = {}
        self.buf = {}
        self.sems = {}
        self.dma_cnt = {}
        self.same_engine_sync = same_engine_sync
        self.n_sem = 0

    def sem(self, key):
        if key not in self.sems:
            self.n_sem += 1
            self.sems[key] = self.es.enter_context(self.nc.semaphore("s%d" % self.n_sem))
        return self.sems[key]

    def sbuf(self, name, shape, dtype):
        return self.es.enter_context(self.nc.sbuf_tensor(name, list(shape), dtype))

    def psum(self, name, shape, dtype=F32):
        return self.es.enter_context(self.nc.psum_tensor(name, list(shape), dtype))

    def _semkey_for(self, prod):
        kind, name, count = prod
        if kind == "e":
            ep = (count - 1) // EPOCH
            return ("e", name, ep), count - ep * EPOCH
        return ("d", name), count

    def _need(self, eng, prod, waits):
        if prod is None:
            return
        kind, name, count = prod
        if kind == "e" and name == eng and (eng in ("pe", "sp") or not self.same_engine_sync):
            return
        sk, val = self._semkey_for(prod)
        wk = (eng, kind, name)
        if self.waited.get(wk, 0) >= count:
            return
        self.waited[wk] = count
        waits.append((sk, val))

    def _deps(self, eng, reads, writes):
        waits = []
        for k in reads:
            b = self.buf.get(k)
            if b is not None:
                self._need(eng, b["w"], waits)
        for k in writes:
            b = self.buf.get(k)
            if b is not None:
                self._need(eng, b["w"], waits)
                for r in b["r"].values():
                    self._need(eng, r, waits)
        return waits

    def _record(self, prod, reads, writes):
        for k in reads:
            b = self.buf.setdefault(k, {"w": None, "r": {}})
            b["r"][(prod[0], prod[1])] = prod
        for k in writes:
            self.buf[k] = {"w": prod, "r": {}}

    def op(self, eng, fn, reads=(), writes=()):
        fn = _freeze(fn)
        waits = self._deps(eng, reads, writes)
        self.cnt[eng] += 1
        prod = ("e", eng, self.cnt[eng])
        sk, _ = self._semkey_for(prod)
        self.q[eng].append((fn, waits, (sk, 1)))
        self._record(prod, reads, writes)
        return prod

    def dma(self, eng, out, in_, semname, reads=(), writes=(), **kw):
        if writes:
            semname = semname + ":" + writes[0]
        waits = self._deps(eng, reads, writes)
        self.dma_cnt[semname] = self.dma_cnt.get(semname, 0) + 16
        prod = ("d", semname, self.dma_cnt[semname])
        self.q[eng].append((lambda e: e.dma_start(out=out, in_=in_, **kw), waits, (("d", semname), 16)))
        self._record(prod, reads, writes)
        return prod

    def custom(self, eng, fn, semname, inc, reads=(), writes=()):
        fn = _freeze(fn)
        waits = self._deps(eng, reads, writes)
        self.dma_cnt[semname] = self.dma_cnt.get(semname, 0) + inc
        prod = ("d", semname, self.dma_cnt[semname])
        self.q[eng].append((fn, waits, (("d", semname), inc)))
        self._record(prod, reads, writes)
        return prod

    def wait_all(self, eng):
        waits = []
        for e in ENGS:
            if self.cnt[e] > 0 and e != eng:
                self._need(eng, ("e", e, self.cnt[e]), waits)
        for name, c in self.dma_cnt.items():
            self._need(eng, ("d", name, c), waits)
        self.q[eng].append((None, waits, None))

    def barrier(self, keep=()):
        for e in ENGS:
            self.wait_all(e)
        self.buf = {k: v for k, v in self.buf.items() if k in keep}

    def build(self):
        nc = self.nc
        keys = []
        for e in ENGS:
            for (_, w, inc) in self.q[e]:
                for x in w:
                    keys.append(x[0])
                if inc:
                    keys.append(inc[0])
        for sk in dict.fromkeys(keys):
            self.sem(sk)
        engmap = {"pe": "tensor", "act": "scalar", "dve": "vector", "pool": "gpsimd", "sp": "sync"}
        with nc.Block() as block:
            for e in ENGS:
                items = self.q[e]

                def body(eng, items=items):
                    for fn, waits, inc in items:
                        for sk, val in waits:
                            eng.wait_ge(self.sems[sk], val)
                        if fn is not None:
                            ins = fn(eng)
                            if inc is not None:
                                ins.then_inc(self.sems[inc[0]], inc[1])

                getattr(block, engmap[e])(body)

    def close(self):
        self.es.close()


class Arena:
    def __init__(self, P, nbytes):
        self.t = P.sbuf("arena", [128, nbytes // 2], BF16)
        self.nbytes = nbytes
        self.off = 0

    def alloc(self, free_shape, dtype):
        n = int(np.prod(free_shape))
        size = n * (4 if dtype == F32 else 2)
        size = (size + 63) // 64 * 64
        assert self.off + size <= self.nbytes, ("SBUF arena overflow", self.off, size)
        v = self.t[:, self.off // 2:(self.off + size) // 2]
        if dtype == F32:
            v = v.bitcast(F32)
        v = v[:, 0:n]
        self.off += size
        if len(free_shape) == 2:
            v = v.rearrange("p (a b) -> p a b", b=free_shape[1])
        elif len(free_shape) == 3:
            v = v.rearrange("p (a b c) -> p a b c", b=free_shape[1], c=free_shape[2])
        elif len(free_shape) == 4:
            v = v.rearrange("p (a b c d) -> p a b c d", b=free_shape[1], c=free_shape[2], d=free_shape[3])
        return v

    def mark(self):
        return self.off

    def release(self, m):
        self.off = m


def bc(ap, axis, shape):
    return ap.unsqueeze(axis).to_broadcast(list(shape))


NT = 2048
NTT = 16
NCTX = 256
NALL = NT + NCTX
D = 1024
NE = 32
LAST_PHASE = 99


def build(upto=LAST_PHASE, debug=False):
    nc = bass.Bass("TRN2", target_bir_lowering=False)

    def din(name, shape, dt=F32):
        return nc.dram_tensor(name, list(shape), dt, kind="ExternalInput").ap()

    x_d = din("x", [NT, D])
    ctx_d = din("ctx", [NCTX, D])
    cvec_d = din("cvec", [128, 16])
    wmod_d = din("w_mod", [D, 6 * D])
    bmod_d = din("b_mod", [1, 6 * D])
    ng_d = din("norm_g", [1, 4 * D])
    win_d = din("w_in", [D, 8704])
    lbv_d = din("lbv", [128, 32])
    hng_d = din("hng", [128, 1])
    qkg_d = din("qkg", [1, 256])
    wbr_d = din("w_branch", [2, D, D])
    wout_d = din("w_out", [D, D])
    rw_d = din("router_w", [D, NE])
    rb_d = din("router_b", [1, NE])
    NEW = NE if upto >= 7 else 1
    wup_d = din("w_up", [NEW, D, 2 * D])
    bupT_d = din("b_upT", [128, NE * 16])
    wdn_d = din("w_down", [NEW, D, D])
    bdn_d = din("b_down", [NE, D])
    rope_d = din("rope", [NT, 256])
    sel_d = din("sel", [128, 8])
    out_d = nc.dram_tensor("out", [NT, D], F32, kind="ExternalOutput").ap()

    def dscr(name, shape, dt):
        if debug:
            return nc.dram_tensor(name, list(shape), dt, kind="ExternalOutput").ap()
        return nc.dram_tensor(name, list(shape), dt).ap()

    dm_mod = dscr("dm_mod", [128, 6 * D], F32)
    dm_ut = dscr("dm_ut", [128, 8, NALL], BF16)
    ag1_ins = [nc.dram_tensor("ag1_in%d" % q, [128, 2048], BF16).ap() for q in range(4)]
    ag1_outs = [nc.dram_tensor("ag1_out%d" % q, [4 * 128, 2048], BF16).ap() for q in range(4)]
    dm_kvctx = dscr("dm_kvctx", [512, NCTX], BF16)
    dm_oloc = dscr("dm_oloc", [8, 128, NT], F32)
    dm_qb = dscr("dm_qb", [2, 8, 128, NT], BF16)
    dm_gs = dscr("dm_gs", [8, 128, NT], BF16)
    ag2_ins = [nc.dram_tensor("ag2_in%d" % d, [128, 1032], F32).ap() for d in range(2)]
    ag2_outs = [nc.dram_tensor("ag2_out%d" % d, [4 * 128, 1032], F32).ap() for d in range(2)]
    dm_oth = dscr("dm_oth", [8, 128, NT], BF16)
    dm_ota = dscr("dm_ota", [8, 128, NT], BF16)
    dm_x1 = dscr("dm_x1", [NT, D], F32)
    dm_u2t = dscr("dm_u2t", [128, 8, NT], BF16)
    dm_comb = dscr("dm_comb", [128, NTT, NE], F32)
    dm_combT = dscr("dm_combT", [NE, NT], F32)

    P = Prog(nc)
    A = Arena(P, 207 * 1024)
    banks = [P.psum("bank%d" % i, [128, 512], F32) for i in range(8)]

    def bank_bf(i):
        return banks[i][:, :].bitcast(BF16)

    ident_f = A.alloc((128,), F32)
    ident_b = A.alloc((128,), BF16)
    ones_b = A.alloc((128,), BF16)
    ones_f = A.alloc((128,), F32)
    P.op("pool", lambda e: e.memset(ident_f, 0.0), writes=["ident_f"])
    P.op("pool", lambda e: e.affine_select(out=ident_f, in_=ident_f, pattern=[[-1, 128]], compare_op=ALU.not_equal,
                                           fill=1.0, base=0, channel_multiplier=1), reads=["ident_f"], writes=["ident_f"])
    P.op("dve", lambda e: e.tensor_copy(out=ident_b, in_=ident_f), reads=["ident_f"], writes=["ident_b"])
    P.op("pool", lambda e: e.memset(ones_f, 1.0), writes=["ones_f"])
    P.op("dve", lambda e: e.tensor_copy(out=ones_b, in_=ones_f), reads=["ones_f"], writes=["ones_b"])

    m_pre_ut = A.mark()
    uT = A.alloc((8, NALL), BF16)

    def rstd_from_ss(ss_ap, n, out_ap, tmp_ap, rk, wk):
        P.op("act", lambda e: e.activation(out=tmp_ap, in_=ss_ap, func=AF.Ln, scale=1.0 / n, bias=EPS), reads=rk, writes=[wk + "_ln"])
        P.op("act", lambda e: e.activation(out=out_ap, in_=tmp_ap, func=AF.Exp, scale=-0.5), reads=[wk + "_ln"], writes=[wk])

    m0 = A.mark()
    cv = A.alloc((16,), F32)
    scv = A.alloc((16,), F32)
    scb = A.alloc((16, 128), F32)
    bmod = A.alloc((6 * D,), F32)
    ng = A.alloc((4, D), F32)
    modl = A.alloc((6 * D,), F32)
    modc = A.alloc((2 * D,), F32)
    wm = [A.alloc((8, 512), F32) for _ in range(2)]
    P.dma("sp", cv, cvec_d, "ld_c", writes=["cv"])
    P.dma("sp", bmod, bmod_d[0].partition_broadcast(128), "ld_c", writes=["bmod"])
    P.dma("sp", ng, ng_d[0].partition_broadcast(128).rearrange("p (a b) -> p a b", b=D), "ld_c", writes=["ng"])
    P.op("act", lambda e: e.activation(out=scv, in_=cv, func=AF.Silu), reads=["cv"], writes=["scv"])
    for k in range(16):
        P.op("dve", lambda e, k=k: e.tensor_copy(out=scb[:, k, :], in_=scv[:, k:k + 1].to_broadcast([128, 128])),
             reads=["scv"], writes=["scb"])
    for s in range(12):
        w = wm[s % 2]
        wk = "wm%d" % (s % 2)
        P.dma("sp", w, wmod_d[:, s * 512:(s + 1) * 512].rearrange("(kt p) n -> p kt n", p=128), "ld_" + wk, writes=[wk])

        def mm(e, w=w, off=0, bk=0):
            for kt in range(8):
                r = e.matmul(banks[bk][:, :], lhsT=scb[:, off + kt, :], rhs=w[:, kt, :], start=(kt == 0), stop=(kt == 7))
            return r
        P.op("pe", lambda e, w=w: mm(e, w, 0, 0), reads=["scb", wk], writes=["bank0"])
        P.op("dve", lambda e, s=s: e.tensor_tensor(out=modl[:, s * 512:(s + 1) * 512], in0=banks[0][:, :], in1=bmod[:, s * 512:(s + 1) * 512], op=ALU.add),
             reads=["bank0", "bmod"], writes=["modl"])
        if s < 4:
            P.op("pe", lambda e, w=w: mm(e, w, 8, 1), reads=["scb", wk], writes=["bank1"])
            P.op("dve", lambda e, s=s: e.tensor_tensor(out=modc[:, s * 512:(s + 1) * 512], in0=banks[1][:, :], in1=bmod[:, s * 512:(s + 1) * 512], op=ALU.add),
                 reads=["bank1", "bmod"], writes=["modc"])
    P.op("dve", lambda e: e.scalar_tensor_tensor(out=modl[:, D:2 * D], in0=modl[:, D:2 * D], scalar=1.0, in1=ng[:, 0, :], op0=ALU.add, op1=ALU.mult),
         reads=["modl", "ng"], writes=["modl"])
    P.op("dve", lambda e: e.scalar_tensor_tensor(out=modc[:, D:2 * D], in0=modc[:, D:2 * D], scalar=1.0, in1=ng[:, 0, :], op0=ALU.add, op1=ALU.mult),
         reads=["modc", "ng"], writes=["modc"])
    P.op("dve", lambda e: e.tensor_tensor(out=modl[:, 2 * D:3 * D], in0=modl[:, 2 * D:3 * D], in1=ng[:, 1, :], op=ALU.mult), reads=["modl", "ng"], writes=["modl"])
    P.op("dve", lambda e: e.scalar_tensor_tensor(out=modl[:, 4 * D:5 * D], in0=modl[:, 4 * D:5 * D], scalar=1.0, in1=ng[:, 2, :], op0=ALU.add, op1=ALU.mult),
         reads=["modl", "ng"], writes=["modl"])
    P.op("dve", lambda e: e.tensor_tensor(out=modl[:, 5 * D:6 * D], in0=modl[:, 5 * D:6 * D], in1=ng[:, 3, :], op=ALU.mult), reads=["modl", "ng"], writes=["modl"])
    P.dma("sp", dm_mod, modl, "st_mod", reads=["modl"], writes=["dm_mod"])

    xt = [A.alloc((D,), F32) for _ in range(2)]
    junk = A.alloc((D,), BF16)
    tmpf = A.alloc((D,), F32)
    ub = [A.alloc((D,), BF16) for _ in range(2)]
    ss1 = A.alloc((18,), F32)
    ln1 = A.alloc((18,), F32)
    rs1 = A.alloc((18,), F32)
    P.op("pool", lambda e: e.memset(ss1, 0.0), writes=["ss1"])
    for ti in range(18):
        s = ti % 2
        src = x_d[ti * 128:(ti + 1) * 128, :] if ti < NTT else ctx_d[(ti - NTT) * 128:(ti - NTT + 1) * 128, :]
        Am = modl if ti < NTT else modc
        amk = "modl" if ti < NTT else "modc"
        P.dma("sp", xt[s], src, "ld_xt%d" % s, writes=["xt%d" % s])
        P.op("act", lambda e, s=s, ti=ti: e.activation(out=junk, in_=xt[s], func=AF.Square, accum_out=ss1[:, ti:ti + 1]),
             reads=["xt%d" % s, "ss1"], writes=["junk", "ss1_%d" % ti])
        rstd_from_ss(ss1[:, ti:ti + 1], D, rs1[:, ti:ti + 1], ln1[:, ti:ti + 1], ["ss1_%d" % ti], "rs1_%d" % ti)
        P.op("dve", lambda e, s=s, ti=ti, Am=Am: e.scalar_tensor_tensor(out=tmpf, in0=xt[s], scalar=rs1[:, ti:ti + 1], in1=Am[:, D:2 * D], op0=ALU.mult, op1=ALU.mult),
             reads=["xt%d" % s, "rs1_%d" % ti, amk], writes=["tmpf"])
        P.op("dve", lambda e, s=s, Am=Am: e.tensor_tensor(out=ub[s], in0=tmpf, in1=Am[:, 0:D], op=ALU.add), reads=["tmpf", amk], writes=["ub%d" % s])
        bk = 2 + s

        def tr(e, s=s, bk=bk):
            for kt in range(8):
                r = e.transpose(bank_bf(bk)[:, kt * 128:(kt + 1) * 128], ub[s][:, kt * 128:(kt + 1) * 128], ident_b)
            return r
        P.op("pe", tr, reads=["ub%d" % s, "ident_b"], writes=["bank%d" % bk])
        P.op("act", lambda e, ti=ti, bk=bk: e.copy(out=uT[:, :, ti * 128:(ti + 1) * 128], in_=bank_bf(bk).rearrange("p (a b) -> p a b", b=128)),
             reads=["bank%d" % bk], writes=["uT_%d" % ti])
    UT_KEYS = ["uT_%d" % ti for ti in range(18)]
    if debug:
        P.dma("sp", dm_ut, uT, "st_dbg", reads=UT_KEYS, writes=["dm_ut"])
    P.barrier()
    A.release(m0)
    if upto <= 1:
        return finish(P, nc)


    m2 = A.mark()
    wkv = A.alloc((8, 512), BF16)
    P.dma("pool", wkv, win_d[:, 6144:6656].rearrange("(kt p) n -> p kt n", p=128), "ld_wkv", writes=["wkv"])
    ropeT = A.alloc((NTT, 256), F32)
    P.dma("sp", ropeT, rope_d.rearrange("(t p) n -> p t n", p=128), "ld_rope", writes=["ropeT"])
    gqk = A.alloc((256,), F32)
    P.dma("sp", gqk, qkg_d[0].partition_broadcast(128), "ld_c", writes=["gqk"])
    KTl = A.alloc((2, NALL), BF16)
    Vl = A.alloc((18, 256), BF16)
    ssk = A.alloc((18, 2), F32)
    lnk = A.alloc((18, 2), F32)
    rsk = A.alloc((18, 2), F32)
    knb = [A.alloc((2, 128), F32) for _ in range(2)]
    t1b = A.alloc((2, 128), F32)
    t2b = A.alloc((2, 128), F32)
    kbf = [A.alloc((256,), BF16) for _ in range(2)]
    junk2 = A.alloc((128,), BF16)
    P.op("pool", lambda e: e.memset(ssk, 0.0), writes=["ssk"])

    def rope_apply(xn, nh, ti, outbf, pfx, eng2="dve"):
        cosv = ropeT[:, ti, 0:128]
        sinv = ropeT[:, ti, 128:256].rearrange("p (r x d) -> p r x d", r=2, x=2, d=32)
        t1 = t1b if nh == 2 else t1q
        t2 = t2b if nh == 2 else t2q
        P.op("dve", lambda e: e.tensor_tensor(out=t1, in0=xn, in1=bc(cosv, 1, [128, nh, 128]), op=ALU.mult),
             reads=[pfx + "xn", "ropeT"], writes=[pfx + "t1"])
        x6 = xn.rearrange("p h (r x d) -> p h r x d", r=2, x=2, d=32)
        t6 = t2.rearrange("p h (r x d) -> p h r x d", r=2, x=2, d=32)
        for xo in range(2):
            P.op(eng2, lambda e, xo=xo: e.tensor_tensor(out=t6[:, :, :, xo, :], in0=x6[:, :, :, 1 - xo, :],
                                                        in1=bc(sinv[:, :, xo, :], 1, [128, nh, 2, 32]), op=ALU.mult),
                 reads=[pfx + "xn", "ropeT"], writes=[pfx + "t2_%d" % xo])
        P.op("dve", lambda e: e.tensor_tensor(out=outbf.rearrange("p (h d) -> p h d", d=128), in0=t1, in1=t2, op=ALU.add),
             reads=[pfx + "t1", pfx + "t2_0", pfx + "t2_1"], writes=[pfx + "bf"])

    import os
    BIS = int(os.environ.get("BIS", "99"))
    for ti in range(18):
        s = ti % 2
        bk = s
        kn = knb[s]

        def mmkv(e, ti=ti, bk=bk):
            for kt in range(8):
                r = e.matmul(banks[bk][:, :], lhsT=uT[:, kt, ti * 128:(ti + 1) * 128], rhs=wkv[:, kt, :], start=(kt == 0), stop=(kt == 7))
            return r
        P.op("pe", mmkv, reads=["uT_%d" % ti, "wkv"], writes=["bank%d" % bk])
        kps = banks[bk][:, 0:256].rearrange("p (h d) -> p h d", d=128)
        P.op("act", lambda e, ti=ti, bk=bk: e.copy(out=Vl[:, ti, :], in_=banks[bk][:, 256:512]), reads=["bank%d" % bk], writes=["Vl_%d" % ti])
        if BIS < 2:
            continue
        for h in range(2):
            P.op("act", lambda e, h=h, ti=ti, kps=kps: e.activation(out=junk2, in_=kps[:, h, :], func=AF.Square, accum_out=ssk[:, ti, h:h + 1]),
                 reads=["bank%d" % bk, "ssk"], writes=["junk2", "ssk_%d_%d" % (ti, h)])
        rstd_from_ss(ssk[:, ti, :], 128, rsk[:, ti, :], lnk[:, ti, :], ["ssk_%d_0" % ti, "ssk_%d_1" % ti], "rsk_%d" % ti)
        P.op("dve", lambda e, kn=kn, kps=kps, ti=ti: e.tensor_tensor(out=kn, in0=kps, in1=bc(rsk[:, ti, :], 2, [128, 2, 128]), op=ALU.mult),
             reads=["bank%d" % bk, "rsk_%d" % ti], writes=["k%dxn" % s])
        P.op("dve", lambda e, kn=kn: e.tensor_tensor(out=kn, in0=kn, in1=bc(gqk[:, 128:256], 1, [128, 2, 128]), op=ALU.mult),
             reads=["k%dxn" % s, "gqk"], writes=["k%dxn" % s])
        if BIS < 3:
            continue
        if ti < NTT and BIS != 3:
            rope_apply(kn, 2, ti, kbf[s], "k%d" % s)
        else:
            P.op("dve", lambda e, kn=kn, s=s: e.tensor_copy(out=kbf[s].rearrange("p (h d) -> p h d", d=128), in_=kn), reads=["k%dxn" % s], writes=["k%dbf" % s])
        bk2 = 2 + s
        if BIS < 5:
            continue

        def trk(e, s=s, bk2=bk2):
            for h in range(2):
                r = e.transpose(bank_bf(bk2)[:, h * 128:(h + 1) * 128], kbf[s][:, h * 128:(h + 1) * 128], ident_b)
            return r
        P.op("pe", trk, reads=["k%dbf" % s, "ident_b"], writes=["bank%d" % bk2])
        P.op("act", lambda e, ti=ti, bk2=bk2: e.copy(out=KTl[:, :, ti * 128:(ti + 1) * 128], in_=bank_bf(bk2)[:, 0:256].rearrange("p (a b) -> p a b", b=128)),
             reads=["bank%d" % bk2], writes=["KTl_%d" % ti])
    for h in range(2 if BIS >= 6 else 0):
        P.dma("sp", ag1_ins[h], KTl[:, h, 0:NT], "st_ag1", reads=["KTl_%d" % t for t in range(16)], writes=["ag1_in"])
        P.dma("sp", ag1_ins[2 + h].rearrange("p (ti c) -> p ti c", c=256), Vl[:, 8 * h:8 * h + 8, :], "st_ag1", reads=["Vl_%d" % t for t in range(16)], writes=["ag1_in"])
    if BIS >= 6:
        P.dma("sp", dm_kvctx[0:256, :].rearrange("(h p) t -> p h t", p=128), KTl[:, :, NT:NALL], "st_kvc", reads=["KTl_16", "KTl_17"], writes=["dm_kvctx"])
        P.dma("sp", dm_kvctx[256:512, :].rearrange("(ti p) c -> p ti c", p=128), Vl[:, 16:18, :], "st_kvc", reads=["Vl_16", "Vl_17"], writes=["dm_kvctx"])
    for q in range(4 if BIS >= 7 else 0):
        P.custom("pool", lambda e, q=q: e.collective_compute("AllGather", ALU.bypass, replica_groups=[[0, 1, 2, 3], [4, 5, 6, 7]], ins=[ag1_ins[q]], outs=[ag1_outs[q]]),
                 "cc1", 1, reads=["ag1_in"] + (["ag1_out"] if q > 0 else []), writes=["ag1_out"])
    P.barrier(keep=["ag1_out", "dm_kvctx", "dm_mod"] + UT_KEYS)
    A.release(m2)
    if upto <= 2:
        return finish(P, nc)

    m3 = A.mark()
    rst = A.alloc((NALL,), F32)
    maskF = A.alloc((128,), F32)
    maskB = A.alloc((128,), F32)
    lbt = A.alloc((2, 2, 8), F32)
    lbd = A.alloc((2, 8), F32)
    lb = A.alloc((2, 8), F32)
    oml = A.alloc((2, 8), F32)
    hng = A.alloc((1,), F32)
    stage = A.alloc((2, 8, 128), F32)
    sctx = A.alloc((2, 8, 128), F32)
    Dv = A.alloc((2, 8), F32)
    m3h = A.mark()
    whb = [A.alloc((8, 5, 128), BF16)] * 2
    qT = A.alloc((NT,), F32)
    gsT = A.alloc((NT,), BF16)
    v_tm = A.alloc((18, 128), BF16)
    fa = A.alloc((NALL,), F32)
    lf = A.alloc((NALL,), F32)
    kk = A.alloc((NALL,), F32)
    bb = A.alloc((NALL,), F32)
    xx = A.alloc((NALL,), F32)
    ee = A.alloc((NALL,), F32)
    gtmp = ee[:, 0:NT]
    qe = [A.alloc((NT,), BF16) for _ in range(2)]
    ke = [A.alloc((NT,), BF16) for _ in range(2)]
    kd = [A.alloc((NALL,), BF16) for _ in range(2)]
    kd_tm = [A.alloc((18, 128), BF16) for _ in range(2)]
    qB = [A.alloc((NT,), BF16) for _ in range(2)]
    tot = [A.alloc((36,), F32) for _ in range(2)]
    etot = [A.alloc((36,), F32) for _ in range(2)]
    ipf = [A.alloc((32,), F32) for _ in range(2)]
    gg = [A.alloc((32,), F32) for _ in range(2)]
    eg = [A.alloc((32,), F32) for _ in range(2)]
    attm = [A.alloc((4, 128), BF16) for _ in range(2)]
    Sst = [[A.alloc((128,), F32) for _ in range(2)] for _ in range(2)]
    Sbf = [[A.alloc((128,), BF16) for _ in range(3)] for _ in range(2)]
    Scx = [A.alloc((128,), F32) for _ in range(2)]
    o_acc = A.alloc((NT,), F32)

    P.op("pool", lambda e: e.memset(rst, 1.0), writes=["rst"])
    P.op("pool", lambda e: e.memset(rst.rearrange("p (c t) -> p c t", t=64)[:, :, 0:1], 0.0), reads=["rst"], writes=["rst"])
    P.op("pool", lambda e: e.memset(maskF, 1.0), writes=["maskF"])
    P.op("pool", lambda e: e.affine_select(out=maskF, in_=maskF, pattern=[[1, 128]], compare_op=ALU.is_ge, fill=0.0, base=0, channel_multiplier=-1),
         reads=["maskF"], writes=["maskF"])
    P.op("pool", lambda e: e.memset(maskF[0:64, 64:128], 0.0), reads=["maskF"], writes=["maskF"])
    P.op("pool", lambda e: e.memset(maskB, 1.0), writes=["maskB"])
    P.op("pool", lambda e: e.affine_select(out=maskB, in_=maskB, pattern=[[-1, 128]], compare_op=ALU.is_ge, fill=0.0, base=0, channel_multiplier=1),
         reads=["maskB"], writes=["maskB"])
    P.op("pool", lambda e: e.memset(maskB[64:128, 0:64], 0.0), reads=["maskB"], writes=["maskB"])
    masks = [maskF, maskB]
    P.dma("sp", lbt, lbv_d.rearrange("p (a b c) -> p a b c", a=2, b=2), "ld_c", writes=["lbt"])
    P.dma("sp", hng, hng_d, "ld_c", writes=["hng"])
    P.op("dve", lambda e: e.tensor_tensor(out=lbd, in0=lbt[:, :, 0, :], in1=lbt[:, :, 1, :], op=ALU.subtract), reads=["lbt"], writes=["lbd"])
    P.op("act", lambda e: e.activation(out=lb, in_=lbd, func=AF.Sigmoid), reads=["lbd"], writes=["lb"])
    P.op("dve", lambda e: e.tensor_scalar(out=oml, in0=lb, scalar1=-1.0, scalar2=1.0, op0=ALU.mult, op1=ALU.add), reads=["lb"], writes=["oml"])

    win_v = win_d.rearrange("(kt p) (s n) -> p kt s n", p=128, n=128)
    BLK5 = [(0, 512), (512, 512), (1024, 512), (1536, 512), (2048, 256)]
    PB = 6
    pbc = [0]

    def proj_fm(wh, whk, sidx, c0, n, evac):
        bk = PB + (pbc[0] % 2)
        pbc[0] += 1

        def mm(e):
            for kt in range(8):
                r = e.matmul(banks[bk][:, 0:n], lhsT=wh[:, kt, sidx, :], rhs=uT[:, kt, c0:c0 + n], start=(kt == 0), stop=(kt == 7))
            return r
        P.op("pe", mm, reads=[whk] + ["uT_%d" % t for t in range(c0 // 128, (c0 + n) // 128)], writes=["bank%d" % bk])
        evac(banks[bk][:, 0:n], "bank%d" % bk)

    def hgrn_dir_prep(h, d):
        wh = whb[h % 2]
        whk = "wh0"
        dk = "d%d" % d
        for (c0, n) in BLK5:
            proj_fm(wh, whk, 2 + d, c0, n, lambda bap, bkey, c0=c0, n=n: P.op(
                "act", lambda e: e.activation(out=fa[:, c0:c0 + n], in_=bap, func=AF.Sigmoid), reads=[bkey], writes=["fa"]))
        fak = ["fa"]
        P.op("dve", lambda e: e.tensor_scalar(out=fa, in0=fa, scalar1=oml[:, d, h:h + 1], scalar2=lb[:, d, h:h + 1], op0=ALU.mult, op1=ALU.add),
             reads=fak + ["oml", "lb"], writes=["fa"])
        P.op("act", lambda e: e.activation(out=lf, in_=fa, func=AF.Ln), reads=["fa"], writes=["lf"])
        P.op("dve", lambda e: e.tensor_scalar(out=kk, in0=fa, scalar1=-1.0, scalar2=1.0, op0=ALU.mult, op1=ALU.add), reads=["fa"], writes=["kk"])
        P.op("dve", lambda e: e.tensor_tensor_scan(out=bb, data0=rst, data1=lf, initial=0.0, op0=ALU.mult, op1=ALU.add), reads=["rst", "lf"], writes=["bb"])
        b3 = bb.rearrange("p (c t) -> p c t", t=64)
        P.op("dve", lambda e: e.tensor_copy(out=tot[d], in_=b3[:, :, 63]), reads=["bb"], writes=["tot" + dk])
        P.op("act", lambda e: e.activation(out=etot[d], in_=tot[d], func=AF.Exp), reads=["tot" + dk], writes=["etot" + dk])
        x3 = xx.rearrange("p (c t) -> p c t", t=64)
        P.op("dve", lambda e: e.tensor_tensor(out=x3, in0=bc(tot[d], 2, [128, 36, 64]), in1=b3, op=ALU.subtract), reads=["tot" + dk, "bb"], writes=["xx"])
        if d == 0:
            bu, dd = bb, xx
            bk_, ddk = "bb", "xx"
        else:
            P.op("dve", lambda e: e.tensor_tensor(out=xx, in0=xx, in1=lf, op=ALU.add), reads=["xx", "lf"], writes=["xx"])
            P.op("dve", lambda e: e.tensor_tensor(out=bb, in0=bb, in1=lf, op=ALU.subtract), reads=["bb", "lf"], writes=["bb"])
            bu, dd = xx, bb
            bk_, ddk = "xx", "bb"
        P.op("act", lambda e: e.activation(out=ee[:, 0:NT], in_=bu[:, 0:NT], func=AF.Exp), reads=[bk_], writes=["ee"])
        P.op("dve", lambda e: e.tensor_tensor(out=qe[d], in0=qT, in1=ee[:, 0:NT], op=ALU.mult), reads=["ee", "qT"], writes=["qe" + dk])
        P.op("act", lambda e: e.activation(out=ee[:, 0:NT], in_=bu[:, 0:NT], func=AF.Exp, scale=-1.0), reads=[bk_, "ee"], writes=["ee"])
        P.op("dve", lambda e: e.tensor_tensor(out=ke[d], in0=kk[:, 0:NT], in1=ee[:, 0:NT], op=ALU.mult), reads=["ee", "kk"], writes=["ke" + dk])
        P.op("act", lambda e: e.activation(out=ee, in_=dd, func=AF.Exp), reads=[ddk, "ee"], writes=["ee"])
        P.op("dve", lambda e: e.tensor_tensor(out=kd[d], in0=kk, in1=ee, op=ALU.mult), reads=["ee", "kk"], writes=["kd" + dk])
        P.op("dve", lambda e: e.tensor_tensor_scan(out=ipf[d], data0=ones_f[:, 0:32], data1=tot[d][:, 0:32], initial=0.0, op0=ALU.mult, op1=ALU.add),
             reads=["tot" + dk, "ones_f"], writes=["ipf" + dk])
        if d == 0:
            P.op("dve", lambda e: e.tensor_tensor(out=gg[d], in0=ipf[d], in1=tot[d][:, 0:32], op=ALU.subtract), reads=["ipf" + dk, "tot" + dk], writes=["gg" + dk])
        else:
            P.op("dve", lambda e: e.tensor_tensor(out=gg[d], in0=ipf[d][:, 31:32].to_broadcast([128, 32]), in1=ipf[d], op=ALU.subtract),
                 reads=["ipf" + dk], writes=["gg" + dk])
        P.op("act", lambda e: e.activation(out=eg[d], in_=gg[d], func=AF.Exp), reads=["gg" + dk], writes=["eg" + dk])
        P.op("act", lambda e: e.activation(out=Dv[:, d, h:h + 1], in_=ipf[d][:, 31:32], func=AF.Exp), reads=["ipf" + dk], writes=["Dv_%d_%d" % (d, h)])
        P.op("dve", lambda e: e.tensor_tensor(out=qB[d].rearrange("p (c t) -> p c t", t=64), in0=qe[d].rearrange("p (c t) -> p c t", t=64),
                                               in1=bc(eg[d], 2, [128, 32, 64]), op=ALU.mult), reads=["qe" + dk, "eg" + dk], writes=["qB" + dk])
        P.dma("sp", dm_qb[d, h], qB[d], "st_qb", reads=["qB" + dk], writes=["dm_qb"])
        for g3 in range(3):
            bk = PB + (pbc[0] % 2)
            pbc[0] += 1

            def trd(e, g3=g3, bk=bk):
                for i in range(6):
                    ti = g3 * 6 + i
                    r = e.transpose(bank_bf(bk)[:, i * 128:(i + 1) * 128], kd[d][:, ti * 128:(ti + 1) * 128], ident_b)
                return r
            P.op("pe", trd, reads=["kd" + dk, "ident_b"], writes=["bank%d" % bk])
            P.op("act", lambda e, g3=g3, bk=bk: e.copy(out=kd_tm[d][:, g3 * 6:(g3 + 1) * 6, :], in_=bank_bf(bk)[:, 0:768].rearrange("p (a b) -> p a b", b=128)),
                 reads=["bank%d" % bk], writes=["kdtm%s_%d" % (dk, g3)])

    def hgrn_dir_scan(h, d):
        dk = "d%d" % d
        kdk = ["kdtm%s_%d" % (dk, g3) for g3 in range(3)]
        bA, bO, bS = 3 * d, 3 * d + 1, 3 * d + 2
        Sc = Scx[d]
        Sbufs, Sbb = Sst[d], Sbf[d]
        slot = [0]

        def delta_mm(c):
            sl = slot[0] % 2
            slot[0] += 1
            ti, half = c // 2, c % 2
            ps = slice(half * 64, half * 64 + 64)
            bidx = (bS, PB + d)[sl]
            bap = banks[bidx][:, 0:128]
            bkey = "bank%d" % bidx
            P.op("pe", lambda e: e.matmul(bap, lhsT=kd_tm[d][ps, ti, :], rhs=v_tm[ps, ti, :], start=True, stop=True),
                 reads=kdk + ["v_tm"], writes=[bkey])
            return bap, bkey
        corder = [32, 33, 34, 35] if d == 0 else [35, 34, 33, 32]
        for i, c in enumerate(corder):
            bap, bkey = delta_mm(c)
            if i == 0:
                P.op("dve", lambda e: e.tensor_copy(out=Sc, in_=bap), reads=[bkey], writes=["Sc" + dk])
            else:
                P.op("dve", lambda e: e.scalar_tensor_tensor(out=Sc, in0=Sc, scalar=etot[d][:, c:c + 1], in1=bap, op0=ALU.mult, op1=ALU.add),
                     reads=[bkey, "Sc" + dk, "etot" + dk], writes=["Sc" + dk])
            yield
        P.op("dve", lambda e: e.tensor_copy(out=sctx[:, d, h, :], in_=Sc), reads=["Sc" + dk], writes=["sctx_%d_%d" % (d, h)])
        P.op("pool", lambda e: e.memset(Sbb[0], 0.0), reads=["Sb%s_0" % dk], writes=["Sb%s_0" % dk])
        si, bi, nstate = 0, 0, 0
        groups = [0, 1, 2, 3] if d == 0 else [3, 2, 1, 0]
        pis = [0, 1, 2, 3] if d == 0 else [3, 2, 1, 0]
        seq = []
        for g in groups:
            for pi in pis:
                p = g * 4 + pi
                for c in ([2 * p, 2 * p + 1] if d == 0 else [2 * p + 1, 2 * p]):
                    seq.append(c)
        dl = {}
        for i in range(min(2, len(seq))):
            dl[i] = delta_mm(seq[i])
        step = 0
        for g in groups:
            def att(e, g=g):
                for pi in range(4):
                    p = g * 4 + pi
                    r = e.matmul(banks[bA][:, pi * 128:(pi + 1) * 128], lhsT=ke[d][:, p * 128:(p + 1) * 128], rhs=qe[d][:, p * 128:(p + 1) * 128], start=True, stop=True)
                return r
            P.op("pe", att, reads=["ke" + dk, "qe" + dk], writes=["bank%d" % bA])
            P.op("dve", lambda e: e.tensor_tensor(out=attm[d], in0=banks[bA][:, :].rearrange("p (a b) -> p a b", b=128), in1=bc(masks[d], 1, [128, 4, 128]), op=ALU.mult),
                 reads=["bank%d" % bA, "mask"], writes=["attm" + dk])
            for pi in pis:
                p = g * 4 + pi
                P.op("pe", lambda e, pi=pi, p=p: e.matmul(banks[bO][:, pi * 128:(pi + 1) * 128], lhsT=v_tm[:, p, :], rhs=attm[d][:, pi, :], start=True, stop=False),
                     reads=["v_tm", "attm" + dk], writes=["bank%d" % bO])
                chunks = [2 * p, 2 * p + 1] if d == 0 else [2 * p + 1, 2 * p]
                for ci, c in enumerate(chunks):
                    col = pi * 128 + (c % 2) * 64
                    sbc = Sbb[bi]
                    P.op("pe", lambda e, c=c, col=col, ci=ci, sbc=sbc: e.matmul(banks[bO][:, col:col + 64], lhsT=sbc, rhs=qe[d][:, c * 64:(c + 1) * 64], start=False, stop=(ci == 1)),
                         reads=["Sb%s_%d" % (dk, bi), "qe" + dk], writes=["bank%d" % bO])
                    assert seq[step] == c
                    bap, bkey = dl.pop(step)
                    nsi = (si + 1) % 2
                    So, Sn = Sbufs[si], Sbufs[nsi]
                    if nstate == 0:
                        P.op("dve", lambda e, Sn=Sn, bap=bap: e.tensor_copy(out=Sn, in_=bap), reads=[bkey], writes=["S%s_%d" % (dk, nsi)])
                    else:
                        P.op("dve", lambda e, So=So, Sn=Sn, bap=bap, c=c: e.scalar_tensor_tensor(out=Sn, in0=So, scalar=etot[d][:, c:c + 1], in1=bap, op0=ALU.mult, op1=ALU.add),
                             reads=[bkey, "S%s_%d" % (dk, si), "etot" + dk], writes=["S%s_%d" % (dk, nsi)])
                    si = nsi
                    nstate += 1
                    nbi = (bi + 1) % 3
                    sbn = Sbb[nbi]
                    P.op("act", lambda e, Sn=Sn, sbn=sbn: e.copy(out=sbn, in_=Sn), reads=["S%s_%d" % (dk, si)], writes=["Sb%s_%d" % (dk, nbi)])
                    bi = nbi
                    if step + 2 < len(seq):
                        dl[step + 2] = delta_mm(seq[step + 2])
                    step += 1
                    yield
            cs = slice(g * 512, (g + 1) * 512)
            if (d == 0 and g < 2) or (d == 1 and g >= 2):
                P.op("act", lambda e, cs=cs: e.copy(out=o_acc[:, cs], in_=banks[bO][:, :]), reads=["bank%d" % bO, "oacc_%d" % g], writes=["oacc_%d" % g])
            else:
                P.op("dve", lambda e, cs=cs: e.tensor_tensor(out=o_acc[:, cs], in0=o_acc[:, cs], in1=banks[bO][:, :], op=ALU.add),
                     reads=["bank%d" % bO, "oacc_%d" % g], writes=["oacc_%d" % g])
        Sl = Sbufs[si]
        P.op("dve", lambda e, Sl=Sl: e.tensor_copy(out=stage[:, d, h, :], in_=Sl), reads=["S%s_%d" % (dk, si)], writes=["stage_%d_%d" % (d, h)])

    P.buf["mask"] = {"w": ("e", "pool", P.cnt["pool"]), "r": {}}
    for h in range(8):
        wh = whb[h % 2]
        whk = "wh0"
        for s5 in range(5):
            P.dma("pool", wh[:, :, s5, :], win_v[:, :, h + 8 * s5, :], "ld_" + whk, writes=[whk])
        for blk in range(4):
            proj_fm(wh, whk, 0, blk * 512, 512, lambda bap, bkey, blk=blk: P.op(
                "act", lambda e: e.copy(out=qT[:, blk * 512:(blk + 1) * 512], in_=bap), reads=[bkey, "qT"], writes=["qT"]))
        for blk in range(4):
            proj_fm(wh, whk, 4, blk * 512, 512, lambda bap, bkey, blk=blk: P.op(
                "act", lambda e: e.activation(out=gtmp[:, blk * 512:(blk + 1) * 512], in_=bap, func=AF.Silu), reads=[bkey, "ee"], writes=["ee"]))
        P.op("dve", lambda e: e.tensor_scalar(out=gsT, in0=gtmp, scalar1=hng[:, 0:1], scalar2=None, op0=ALU.mult), reads=["ee", "hng"], writes=["gsT"])
        P.dma("sp", dm_gs[h], gsT, "st_gs", reads=["gsT"], writes=["dm_gs"])
        for g4 in range(5):
            tiles = list(range(g4 * 4, min(18, g4 * 4 + 4)))
            bk = PB + (pbc[0] % 2)
            pbc[0] += 1

            def mmv(e, tiles=tiles, bk=bk, wh=wh):
                for i, ti in enumerate(tiles):
                    for kt in range(8):
                        r = e.matmul(banks[bk][:, i * 128:(i + 1) * 128], lhsT=uT[:, kt, ti * 128:(ti + 1) * 128], rhs=wh[:, kt, 1, :], start=(kt == 0), stop=(kt == 7))
                return r
            P.op("pe", mmv, reads=[whk] + ["uT_%d" % t for t in tiles], writes=["bank%d" % bk])
            nt_ = len(tiles)
            P.op("act", lambda e, tiles=tiles, bk=bk, nt_=nt_: e.copy(out=v_tm[:, tiles[0]:tiles[0] + nt_, :], in_=banks[bk][:, 0:nt_ * 128].rearrange("p (a b) -> p a b", b=128)),
                 reads=["bank%d" % bk, "v_tm"], writes=["v_tm"])
        hgrn_dir_prep(h, 0)
        hgrn_dir_prep(h, 1)
        gens = [hgrn_dir_scan(h, 0), hgrn_dir_scan(h, 1)]
        alive = [True, True]
        while any(alive):
            for i in range(2):
                if alive[i]:
                    try:
                        next(gens[i])
                    except StopIteration:
                        alive[i] = False
        P.dma("sp", dm_oloc[h], o_acc, "st_oloc", reads=["oacc_%d" % g for g in range(4)], writes=["dm_oloc"])
    for d in range(2):
        P.dma("sp", ag2_ins[d][:, 0:1024], stage[:, d, :, :].rearrange("p b c -> p (b c)"), "st_ag2", reads=["stage_%d_%d" % (d, h) for h in range(8)], writes=["ag2_in"])
        P.dma("sp", ag2_ins[d][:, 1024:1032], Dv[:, d, :], "st_ag2", reads=["Dv_%d_%d" % (d, h) for h in range(8)], writes=["ag2_in"])
    for d in range(2):
        P.custom("pool", lambda e, d=d: e.collective_compute("AllGather", ALU.bypass, replica_groups=[[0, 1, 2, 3], [4, 5, 6, 7]], ins=[ag2_ins[d]], outs=[ag2_outs[d]]),
                 "cc2", 1, reads=["ag2_in"] + (["ag2_out"] if d > 0 else []), writes=["ag2_out"])
    P.barrier(keep=["ag1_out", "dm_kvctx", "dm_mod", "ag2_out", "dm_oloc", "dm_qb", "dm_gs"] + UT_KEYS)
    A.release(m3h)
    gath = A.alloc((2, 4, 1032), F32)
    Rr = A.alloc((2, 8, 128), F32)
    Tt = A.alloc((8, 128), F32)
    selv = A.alloc((8,), F32)
    Sin = A.alloc((2, 8, 128), BF16)
    for d in range(2):
        P.dma("sp", gath[:, d, :, :], ag2_outs[d].rearrange("(r p) n -> p r n", p=128), "ld_gath", reads=["ag2_out"], writes=["gath"])
    P.dma("sp", selv, sel_d, "ld_c", writes=["selv"])
    SCK = ["sctx_%d_%d" % (d, h) for d in range(2) for h in range(8)]
    P.op("dve", lambda e: e.tensor_copy(out=Rr.rearrange("p a b c -> p (a b c)"), in_=sctx.rearrange("p a b c -> p (a b c)")), writes=["Rr"])
    for d in range(2):
        order = [0, 1, 2, 3] if d == 0 else [3, 2, 1, 0]
        for r in order:
            Sl = gath[:, d, r, 0:1024].rearrange("p (h v) -> p h v", v=128)
            Dr = gath[:, d, r, 1024:1032]
            P.op("dve", lambda e, d=d, Dr=Dr: e.tensor_tensor(out=Tt, in0=Rr[:, d, :, :], in1=bc(Dr, 2, [128, 8, 128]), op=ALU.mult), reads=["Rr", "gath"], writes=["Tt"])
            P.op("dve", lambda e, Sl=Sl: e.tensor_tensor(out=Tt, in0=Tt, in1=Sl, op=ALU.add), reads=["Tt", "gath"], writes=["Tt"])
            P.op("dve", lambda e, d=d: e.tensor_tensor(out=Tt, in0=Tt, in1=Rr[:, d, :, :], op=ALU.subtract), reads=["Tt", "Rr"], writes=["Tt"])
            P.op("dve", lambda e, d=d, r=r: e.scalar_tensor_tensor(out=Rr[:, d, :, :], in0=Tt, scalar=selv[:, d * 4 + r:d * 4 + r + 1], in1=Rr[:, d, :, :], op0=ALU.mult, op1=ALU.add),
                 reads=["Tt", "Rr", "selv"], writes=["Rr"])
    P.op("act", lambda e: e.copy(out=Sin.rearrange("p a b c -> p (a b c)"), in_=Rr.rearrange("p a b c -> p (a b c)")), reads=["Rr"], writes=["Sin"])
    ol = A.alloc((NT,), F32)
    qbf_ = A.alloc((NT,), BF16)
    qbb_ = A.alloc((NT,), BF16)
    gsl = A.alloc((NT,), BF16)
    sqb = A.alloc((NT,), BF16)
    lnr = A.alloc((NT,), F32)
    rsr = A.alloc((NT,), F32)
    for h in range(8):
        P.dma("sp", ol, dm_oloc[h], "ld_ol", reads=["dm_oloc"], writes=["ol"] + ["ol_%d" % b_ for b_ in range(4)])
        P.dma("sp", qbf_, dm_qb[0, h], "ld_qb0", reads=["dm_qb"], writes=["qbf_"])
        P.dma("sp", qbb_, dm_qb[1, h], "ld_qb1", reads=["dm_qb"], writes=["qbb_"])
        P.dma("sp", gsl, dm_gs[h], "ld_gs", reads=["dm_gs"], writes=["gsl"])
        for blk in range(4):
            cs = slice(blk * 512, (blk + 1) * 512)
            bk = blk % 2

            def corr(e, h=h, cs=cs, bk=bk):
                e.matmul(banks[bk][:, :], lhsT=Sin[:, 0, h, :], rhs=qbf_[:, cs], start=True, stop=False)
                return e.matmul(banks[bk][:, :], lhsT=Sin[:, 1, h, :], rhs=qbb_[:, cs], start=False, stop=True)
            P.op("pe", corr, reads=["Sin", "qbf_", "qbb_"], writes=["bank%d" % bk])
            P.op("dve", lambda e, cs=cs, bk=bk: e.tensor_tensor(out=ol[:, cs], in0=ol[:, cs], in1=banks[bk][:, :], op=ALU.add), reads=["bank%d" % bk, "ol", "ol_%d" % blk], writes=["ol_%d" % blk])
            P.op("act", lambda e, cs=cs: e.activation(out=sqb[:, cs], in_=ol[:, cs], func=AF.Square), reads=["ol_%d" % blk], writes=["sqb_%d" % blk])
            bk2 = 2 + blk % 2
            P.op("pe", lambda e, cs=cs, bk2=bk2: e.matmul(banks[bk2][:, :], lhsT=ones_b, rhs=sqb[:, cs], start=True, stop=True), reads=["sqb_%d" % blk, "ones_b"], writes=["bank%d" % bk2])
            P.op("act", lambda e, cs=cs, bk2=bk2: e.activation(out=lnr[:, cs], in_=banks[bk2][:, :], func=AF.Ln, scale=1.0 / 128, bias=EPS), reads=["bank%d" % bk2], writes=["lnr_%d" % blk])
            P.op("act", lambda e, cs=cs: e.activation(out=rsr[:, cs], in_=lnr[:, cs], func=AF.Exp, scale=-0.5), reads=["lnr_%d" % blk], writes=["rsr_%d" % blk])
            P.op("dve", lambda e, cs=cs: e.tensor_tensor(out=ol[:, cs], in0=ol[:, cs], in1=rsr[:, cs], op=ALU.mult), reads=["ol_%d" % blk, "rsr_%d" % blk], writes=["ol_%d" % blk])
            P.op("dve", lambda e, cs=cs: e.tensor_tensor(out=sqb[:, cs], in0=ol[:, cs], in1=gsl[:, cs], op=ALU.mult), reads=["ol_%d" % blk, "gsl"], writes=["sqb_%d" % blk])
        P.dma("sp", dm_oth[h], sqb, "st_oth", reads=["sqb_%d" % b_ for b_ in range(4)], writes=["dm_oth"])
        for b_ in range(4):
            for nm in ("ol_%d", "sqb_%d", "lnr_%d", "rsr_%d", "oth_%d"):
                pass
    P.barrier(keep=["ag1_out", "dm_kvctx", "dm_mod", "dm_oth"] + UT_KEYS)
    A.release(m3)
    if upto <= 3:
        return finish(P, nc)


    m4 = A.mark()
    QT = A.alloc((8, NT), BF16)
    KTa = A.alloc((2, 8448), BF16)
    Va = A.alloc((66, 256), BF16)
    for r in range(4):
        for h in range(2):
            P.dma("sp", KTa[:, h, r * NT:(r + 1) * NT], ag1_outs[h][r * 128:(r + 1) * 128, :], "ld_kta", reads=["ag1_out"], writes=["KTa"])
            P.dma("sp", Va[:, r * 16 + 8 * h:r * 16 + 8 * h + 8, :], ag1_outs[2 + h][r * 128:(r + 1) * 128, :].rearrange("p (ti c) -> p ti c", c=256), "ld_va", reads=["ag1_out"], writes=["Va"])
    P.dma("sp", KTa[:, :, 8192:8448], dm_kvctx[0:256, :].rearrange("(h p) t -> p h t", p=128), "ld_kta", reads=["dm_kvctx"], writes=["KTa"])
    P.dma("sp", Va[:, 64:66, :], dm_kvctx[256:512, :].rearrange("(ti p) c -> p ti c", p=128), "ld_va", reads=["dm_kvctx"], writes=["Va"])
    m4b = A.mark()
    wq = A.alloc((8, 1024), BF16)
    P.dma("pool", wq, win_d[:, 5120:6144].rearrange("(kt p) n -> p kt n", p=128), "ld_wq", writes=["wq"])
    ropeT = A.alloc((NTT, 256), F32)
    P.dma("sp", ropeT, rope_d.rearrange("(t p) n -> p t n", p=128), "ld_rope", writes=["ropeT"])
    gqk = A.alloc((256,), F32)
    P.dma("sp", gqk, qkg_d[0].partition_broadcast(128), "ld_c", writes=["gqk"])
    ssq = A.alloc((16, 8), F32)
    lnq = A.alloc((16, 8), F32)
    rsq = A.alloc((16, 8), F32)
    qn = A.alloc((8, 128), F32)
    t1q = A.alloc((8, 128), F32)
    t2q = A.alloc((8, 128), F32)
    qbf = A.alloc((1024,), BF16)
    junk2 = A.alloc((128,), BF16)
    P.op("pool", lambda e: e.memset(ssq, 0.0), writes=["ssq"])
    for ti in range(NTT):
        s = ti % 2
        for half in range(2):
            def mmq(e, ti=ti, half=half):
                for kt in range(8):
                    r = e.matmul(banks[half][:, :], lhsT=uT[:, kt, ti * 128:(ti + 1) * 128], rhs=wq[:, kt, half * 512:(half + 1) * 512], start=(kt == 0), stop=(kt == 7))
                return r
            P.op("pe", mmq, reads=["uT_%d" % ti, "wq"], writes=["bank%d" % half])
        for h in range(8):
            P.op("act", lambda e, h=h, ti=ti: e.activation(out=junk2, in_=banks[h // 4][:, (h % 4) * 128:(h % 4 + 1) * 128], func=AF.Square, accum_out=ssq[:, ti, h:h + 1]),
                 reads=["bank%d" % (h // 4), "ssq"], writes=["junk2", "ssq_%d" % ti])
        rstd_from_ss(ssq[:, ti, :], 128, rsq[:, ti, :], lnq[:, ti, :], ["ssq_%d" % ti], "rsq_%d" % ti)
        for half in range(2):
            P.op("dve", lambda e, half=half, ti=ti: e.tensor_tensor(out=qn[:, half * 4:(half + 1) * 4, :], in0=banks[half][:, :].rearrange("p (h d) -> p h d", d=128),
                                                                in1=bc(rsq[:, ti, half * 4:(half + 1) * 4], 2, [128, 4, 128]), op=ALU.mult),
                 reads=["bank%d" % half, "rsq_%d" % ti, "qxn"], writes=["qxn"])
        P.op("dve", lambda e: e.tensor_tensor(out=qn, in0=qn, in1=bc(gqk[:, 0:128], 1, [128, 8, 128]), op=ALU.mult), reads=["qxn", "gqk"], writes=["qxn"])
        rope_apply(qn, 8, ti, qbf, "q")
        bk2 = 2 + s

        def trq(e, bk2=bk2):
            for h in range(8):
                r = e.transpose(bank_bf(bk2)[:, h * 128:(h + 1) * 128], qbf[:, h * 128:(h + 1) * 128], ident_b)
            return r
        P.op("pe", trq, reads=["qbf", "ident_b"], writes=["bank%d" % bk2])
        P.op("act", lambda e, ti=ti, bk2=bk2: e.copy(out=QT[:, :, ti * 128:(ti + 1) * 128], in_=bank_bf(bk2).rearrange("p (a b) -> p a b", b=128)),
             reads=["bank%d" % bk2], writes=["QT"])
    P.barrier(keep=["dm_mod", "dm_oth", "KTa", "Va"] + UT_KEYS)
    A.release(m4b)
    if upto <= 4:
        return finish(P, nc)

    pT = [A.alloc((512,), BF16) for _ in range(3)]
    rden = A.alloc((512,), F32)
    ob = [A.alloc((512,), BF16) for _ in range(2)]
    dacc = [A.alloc((512,), F32) for _ in range(2)]
    SCALE = float(128 ** -0.5)
    NKT = 66
    it = 0
    for kvh in range(2):
        for qb in range(4):
            for g in range(4):
                head = kvh * 4 + g
                bo, bd = 4 + it % 2, 6 + it % 2
                qs = slice(qb * 512, (qb + 1) * 512)

                def s_mm(kt, kvh=kvh, head=head, qs=qs):
                    P.op("pe", lambda e: e.matmul(banks[kt % 3][:, :], lhsT=KTa[:, kvh, kt * 128:(kt + 1) * 128], rhs=QT[:, head, qs], start=True, stop=True),
                         reads=["KTa", "QT"], writes=["bank%d" % (kt % 3)])
                s_mm(0)
                s_mm(1)
                for kt in range(NKT):
                    if kt + 2 < NKT:
                        s_mm(kt + 2)
                    P.op("act", lambda e, kt=kt: e.activation(out=pT[kt % 3], in_=banks[kt % 3][:, :], func=AF.Exp, scale=SCALE),
                         reads=["bank%d" % (kt % 3)], writes=["pT%d" % (kt % 3)])

                    P.op("pe", lambda e, kt=kt, kvh=kvh, bo=bo: e.matmul(banks[bo][:, :], lhsT=Va[:, kt, kvh * 128:(kvh + 1) * 128], rhs=pT[kt % 3], start=(kt == 0), stop=(kt == NKT - 1)),
                         reads=["pT%d" % (kt % 3), "Va"], writes=["bank%d" % bo])
                    da = dacc[0]
                    if kt % 3 == 2:
                        P.op("pe", lambda e, kt=kt, bd=bd: e.matmul(banks[bd][:, :], lhsT=ones_b, rhs=pT[kt % 3], start=(kt == 2), stop=False),
                             reads=["pT%d" % (kt % 3), "ones_b"], writes=["bank%d" % bd])
                    elif kt == 0:
                        P.op("dve", lambda e, kt=kt, da=da: e.tensor_copy(out=da, in_=pT[kt % 3]), reads=["pT%d" % (kt % 3)], writes=["dacc0"])
                    else:
                        P.op("dve", lambda e, kt=kt, da=da: e.tensor_tensor(out=da, in0=da, in1=pT[kt % 3], op=ALU.add), reads=["pT%d" % (kt % 3), "dacc0"], writes=["dacc0"])

                def dsum(e, bd=bd):
                    return e.matmul(banks[bd][:, :], lhsT=ones_f, rhs=dacc[0], start=False, stop=True)
                P.op("pe", dsum, reads=["dacc0", "ones_f"], writes=["bank%d" % bd])
                P.op("dve", lambda e, bd=bd: e.reciprocal(out=rden, in_=banks[bd][:, :]), reads=["bank%d" % bd], writes=["rden"])
                P.op("dve", lambda e, bo=bo, it=it: e.tensor_tensor(out=ob[it % 2], in0=banks[bo][:, :], in1=rden, op=ALU.mult), reads=["bank%d" % bo, "rden"], writes=["ob%d" % (it % 2)])
                P.dma("sp", dm_ota[head][:, qs], ob[it % 2], "st_ota%d" % (it % 2), reads=["ob%d" % (it % 2)], writes=["dm_ota"])
                it += 1
    P.barrier(keep=["dm_mod", "dm_oth", "dm_ota"] + UT_KEYS)
    A.release(m4)
    if upto <= 5:
        return finish(P, nc)

    m6 = A.mark()
    wg = A.alloc((8, 2048), BF16)
    wb0 = A.alloc((8, 1024), BF16)
    wb1 = A.alloc((8, 1024), BF16)
    wo = A.alloc((8, 1024), BF16)
    P.dma("pool", wg, win_d[:, 6656:8704].rearrange("(kt p) n -> p kt n", p=128), "ld_w6", writes=["wg"])
    P.dma("pool", wb0, wbr_d[0].rearrange("(kt p) n -> p kt n", p=128), "ld_w6", writes=["wb0"])
    P.dma("pool", wb1, wbr_d[1].rearrange("(kt p) n -> p kt n", p=128), "ld_w6", writes=["wb1"])
    P.dma("pool", wo, wout_d.rearrange("(kt p) n -> p kt n", p=128), "ld_w6", writes=["wo"])
    G1 = A.alloc((D,), F32)
    A2 = A.alloc((D,), F32)
    B2 = A.alloc((D,), F32)
    P.dma("sp", G1, dm_mod[:, 2 * D:3 * D], "ld_c", reads=["dm_mod"], writes=["G1"])
    P.dma("sp", A2, dm_mod[:, 4 * D:5 * D], "ld_c", reads=["dm_mod"], writes=["A2"])
    P.dma("sp", B2, dm_mod[:, 3 * D:4 * D], "ld_c", reads=["dm_mod"], writes=["B2"])
    rwt = A.alloc((8, NE), F32)
    rbt = A.alloc((NE,), F32)
    P.dma("sp", rwt, rw_d.rearrange("(kt p) e -> p kt e", p=128), "ld_c", writes=["rwt"])
    P.dma("sp", rbt, rb_d[0].partition_broadcast(128), "ld_c", writes=["rbt"])
    othb = A.alloc((8, 512), BF16)
    otab = A.alloc((8, 512), BF16)
    y1T = A.alloc((8, 512), BF16)
    sgh = [A.alloc((512,), F32)] * 2
    sga = [A.alloc((512,), F32)] * 2
    tA = [A.alloc((512,), F32)] * 2
    tB = [A.alloc((512,), F32)] * 2
    xt6 = [A.alloc((D,), F32) for _ in range(2)]
    tmp6 = A.alloc((D,), F32)
    x1t = [A.alloc((D,), F32)] * 2
    u2f = A.alloc((D,), F32)
    u2b = A.alloc((D,), BF16)
    junk6 = A.alloc((D,), BF16)
    ssy = A.alloc((16, 2), F32)
    ssy1 = A.alloc((16,), F32)
    lny = A.alloc((16,), F32)
    rsy = A.alloc((16,), F32)
    ssx = A.alloc((16,), F32)
    lnx = A.alloc((16,), F32)
    rsx = A.alloc((16,), F32)
    u2Tf = A.alloc((8, 128), F32)
    u2Tb = [A.alloc((8, 128), BF16) for _ in range(2)]
    lg = A.alloc((NE,), F32)
    mx8 = A.alloc((8,), F32)
    msk = A.alloc((NE,), F32)
    em = A.alloc((NE,), F32)
    nmx = A.alloc((1,), F32)
    ssum = A.alloc((1,), F32)
    rsum = A.alloc((1,), F32)
    cmb = A.alloc((16, NE), F32)
    cT = A.alloc((128,), F32)
    P.op("pool", lambda e: e.memset(ssy, 0.0), writes=["ssy"])
    P.op("pool", lambda e: e.memset(ssx, 0.0), writes=["ssx"])
    oth_v = dm_oth.rearrange("h p t -> p h t")
    ota_v = dm_ota.rearrange("h p t -> p h t")
    for blk in range(4):
        cs = slice(blk * 512, (blk + 1) * 512)
        P.dma("sp", othb, oth_v[:, :, cs], "ld_oth", reads=["dm_oth"], writes=["othb"])
        P.dma("sp", otab, ota_v[:, :, cs], "ld_ota", reads=["dm_ota"], writes=["otab"])
        utk = ["uT_%d" % t for t in range(blk * 4, blk * 4 + 4)]
        for dt in range(8):
            s = 0
            ds = slice(dt * 128, (dt + 1) * 128)

            b0 = 4 * (dt % 2)

            def mm4(e, ds=ds, dt=dt, cs=cs, b0=b0):
                for kt in range(8):
                    e.matmul(banks[b0][:, :], lhsT=wg[:, kt, dt * 128:(dt + 1) * 128], rhs=uT[:, kt, cs], start=(kt == 0), stop=(kt == 7))
                for kt in range(8):
                    e.matmul(banks[b0 + 1][:, :], lhsT=wg[:, kt, 1024 + dt * 128:1024 + (dt + 1) * 128], rhs=uT[:, kt, cs], start=(kt == 0), stop=(kt == 7))
                for kt in range(8):
                    e.matmul(banks[b0 + 2][:, :], lhsT=wb0[:, kt, ds], rhs=othb[:, kt, :], start=(kt == 0), stop=(kt == 7))
                for kt in range(8):
                    r = e.matmul(banks[b0 + 3][:, :], lhsT=wb1[:, kt, ds], rhs=otab[:, kt, :], start=(kt == 0), stop=(kt == 7))
                return r
            P.op("pe", mm4, reads=["wg", "wb0", "wb1", "othb", "otab"] + utk, writes=["bank%d" % (b0 + i) for i in range(4)])
            P.op("act", lambda e, s=s, b0=b0: e.activation(out=sgh[s], in_=banks[b0][:, :], func=AF.Sigmoid), reads=["bank%d" % b0], writes=["sgh%d" % s])
            P.op("act", lambda e, s=s, b0=b0: e.activation(out=sga[s], in_=banks[b0 + 1][:, :], func=AF.Sigmoid), reads=["bank%d" % (b0 + 1)], writes=["sga%d" % s])
            P.op("dve", lambda e, s=s, b0=b0: e.tensor_tensor(out=tA[s], in0=sgh[s], in1=banks[b0 + 2][:, :], op=ALU.mult), reads=["sgh%d" % s, "bank%d" % (b0 + 2)], writes=["tA%d" % s])
            P.op("dve", lambda e, s=s, b0=b0: e.tensor_tensor(out=tB[s], in0=sga[s], in1=banks[b0 + 3][:, :], op=ALU.mult), reads=["sga%d" % s, "bank%d" % (b0 + 3)], writes=["tB%d" % s])
            P.op("dve", lambda e, s=s, dt=dt: e.tensor_tensor(out=y1T[:, dt, :], in0=tA[s], in1=tB[s], op=ALU.add), reads=["tA%d" % s, "tB%d" % s], writes=["y1T"])
        for tt in range(4):
            ti = blk * 4 + tt
            s = ti % 2
            ts_ = slice(tt * 128, (tt + 1) * 128)
            P.dma("sp", xt6[s], x_d[ti * 128:(ti + 1) * 128, :], "ld_x6%d" % s, writes=["xt6%d" % s])
            for half in range(2):
                def mmy(e, half=half, ts_=ts_):
                    for kt in range(8):
                        r = e.matmul(banks[4 + half][:, :], lhsT=y1T[:, kt, ts_], rhs=wo[:, kt, half * 512:(half + 1) * 512], start=(kt == 0), stop=(kt == 7))
                    return r
                P.op("pe", mmy, reads=["y1T", "wo"], writes=["bank%d" % (4 + half)])
                P.op("act", lambda e, half=half, ti=ti: e.activation(out=junk6[:, 0:512], in_=banks[4 + half][:, :], func=AF.Square, accum_out=ssy[:, ti, half:half + 1]),
                     reads=["bank%d" % (4 + half), "ssy"], writes=["junk6", "ssy_%d_%d" % (ti, half)])
            P.op("dve", lambda e, ti=ti: e.tensor_tensor(out=ssy1[:, ti:ti + 1], in0=ssy[:, ti, 0:1], in1=ssy[:, ti, 1:2], op=ALU.add),
                 reads=["ssy_%d_0" % ti, "ssy_%d_1" % ti], writes=["ssy1_%d" % ti])
            rstd_from_ss(ssy1[:, ti:ti + 1], D, rsy[:, ti:ti + 1], lny[:, ti:ti + 1], ["ssy1_%d" % ti], "rsy_%d" % ti)
            for half in range(2):
                hs = slice(half * 512, (half + 1) * 512)
                P.op("dve", lambda e, half=half, hs=hs, ti=ti: e.scalar_tensor_tensor(out=tmp6[:, hs], in0=banks[4 + half][:, :], scalar=rsy[:, ti:ti + 1], in1=G1[:, hs], op0=ALU.mult, op1=ALU.mult),
                     reads=["bank%d" % (4 + half), "rsy_%d" % ti, "G1", "tmp6"], writes=["tmp6"])
            P.op("dve", lambda e, s=s: e.tensor_tensor(out=x1t[s], in0=tmp6, in1=xt6[s], op=ALU.add), reads=["tmp6", "xt6%d" % s], writes=["x1t"])
            P.dma("sp", dm_x1[ti * 128:(ti + 1) * 128, :], x1t[s], "st_x1%d" % s, reads=["x1t"], writes=["dm_x1"])
            P.op("act", lambda e, s=s, ti=ti: e.activation(out=junk6, in_=x1t[s], func=AF.Square, accum_out=ssx[:, ti:ti + 1]), reads=["x1t", "ssx"], writes=["junk6", "ssx_%d" % ti])
            rstd_from_ss(ssx[:, ti:ti + 1], D, rsx[:, ti:ti + 1], lnx[:, ti:ti + 1], ["ssx_%d" % ti], "rsx_%d" % ti)
            P.op("dve", lambda e, s=s, ti=ti: e.scalar_tensor_tensor(out=tmp6, in0=x1t[s], scalar=rsx[:, ti:ti + 1], in1=A2, op0=ALU.mult, op1=ALU.mult),
                 reads=["x1t", "rsx_%d" % ti, "A2", "tmp6"], writes=["tmp6"])
            P.op("dve", lambda e: e.tensor_tensor(out=u2f, in0=tmp6, in1=B2, op=ALU.add), reads=["tmp6", "B2"], writes=["u2f"])
            P.op("act", lambda e: e.copy(out=u2b, in_=u2f), reads=["u2f"], writes=["u2b"])

            def tru(e):
                for kt in range(8):
                    r = e.transpose(bank_bf(6)[:, kt * 128:(kt + 1) * 128], u2b[:, kt * 128:(kt + 1) * 128], ident_b)
                return r
            P.op("pe", tru, reads=["u2b", "ident_b"], writes=["bank6"])
            P.op("act", lambda e, s=s: e.copy(out=u2Tb[s], in_=bank_bf(6).rearrange("p (a b) -> p a b", b=128)), reads=["bank6"], writes=["u2Tb%d" % s])
            P.dma("sp", dm_u2t[:, :, ti * 128:(ti + 1) * 128], u2Tb[s], "st_u2t%d" % s, reads=["u2Tb%d" % s], writes=["dm_u2t"])
            for g2 in range(2):
                def truf(e, g2=g2):
                    for i in range(4):
                        kt = g2 * 4 + i
                        r = e.transpose(banks[7][:, i * 128:(i + 1) * 128], u2f[:, kt * 128:(kt + 1) * 128], ident_f)
                    return r
                P.op("pe", truf, reads=["u2f", "ident_f"], writes=["bank7"])
                P.op("act", lambda e, g2=g2: e.copy(out=u2Tf[:, g2 * 4:(g2 + 1) * 4, :], in_=banks[7][:, :].rearrange("p (a b) -> p a b", b=128)), reads=["bank7", "u2Tf"], writes=["u2Tf"])

            def mml(e):
                for kt in range(8):
                    r = e.matmul(banks[6][:, 0:NE], lhsT=u2Tf[:, kt, :], rhs=rwt[:, kt, :], start=(kt == 0), stop=(kt == 7))
                return r
            P.op("pe", mml, reads=["u2Tf", "rwt"], writes=["bank6"])
            P.op("dve", lambda e: e.tensor_tensor(out=lg, in0=banks[6][:, 0:NE], in1=rbt, op=ALU.add), reads=["bank6", "rbt"], writes=["lg"])
            P.op("dve", lambda e: e.max(out=mx8, in_=lg), reads=["lg"], writes=["mx8"])
            P.op("dve", lambda e: e.tensor_scalar(out=msk, in0=lg, scalar1=mx8[:, 3:4], scalar2=None, op0=ALU.is_ge), reads=["lg", "mx8"], writes=["msk"])
            P.op("dve", lambda e: e.tensor_scalar(out=nmx, in0=mx8[:, 0:1], scalar1=-1.0, scalar2=None, op0=ALU.mult), reads=["mx8"], writes=["nmx"])
            P.op("act", lambda e: e.activation(out=em, in_=lg, func=AF.Exp, bias=nmx[:, 0:1], scale=1.0), reads=["lg", "nmx"], writes=["em"])
            P.op("dve", lambda e: e.tensor_tensor(out=em, in0=em, in1=msk, op=ALU.mult), reads=["em", "msk"], writes=["em"])
            P.op("dve", lambda e: e.reduce_sum(out=ssum, in_=em, axis=AX.X), reads=["em"], writes=["ssum"])
            P.op("dve", lambda e: e.reciprocal(out=rsum, in_=ssum), reads=["ssum"], writes=["rsum"])
            P.op("dve", lambda e, ti=ti: e.tensor_scalar(out=cmb[:, ti, :], in0=em, scalar1=rsum[:, 0:1], scalar2=None, op0=ALU.mult), reads=["em", "rsum"], writes=["cmb_%d" % ti])
            P.op("pe", lambda e, ti=ti: e.transpose(banks[7][0:NE, 0:128], cmb[:, ti, :], ident_f), reads=["cmb_%d" % ti, "ident_f"], writes=["bank7"])
            P.op("act", lambda e: e.copy(out=cT[0:NE, :], in_=banks[7][0:NE, 0:128]), reads=["bank7"], writes=["cT"])
            P.dma("sp", dm_combT[:, ti * 128:(ti + 1) * 128], cT[0:NE, :], "st_cT", reads=["cT"], writes=["dm_combT"])
    P.dma("sp", dm_comb, cmb, "st_cmb", reads=["cmb_%d" % t for t in range(16)], writes=["dm_comb"])
    P.barrier(keep=["dm_mod", "dm_x1", "dm_u2t", "dm_comb", "dm_combT"])
    A.release(m_pre_ut)
    if upto <= 6:
        return finish(P, nc)

    G2 = A.alloc((D,), F32)
    P.dma("sp", G2, dm_mod[:, 5 * D:6 * D], "ld_c", reads=["dm_mod"], writes=["G2"])
    bu = A.alloc((NE * 16,), F32)
    P.dma("sp", bu, bupT_d, "ld_c", writes=["bu"])
    bdn = A.alloc((D,), F32)
    P.dma("sp", bdn[0:NE, :], bdn_d, "ld_c", writes=["bdn"])
    cmb7 = A.alloc((16, NE), F32)
    P.dma("sp", cmb7, dm_comb, "ld_c", reads=["dm_comb"], writes=["cmb7"])
    P.op("dve", lambda e: e.tensor_scalar(out=cmb7, in0=cmb7, scalar1=1.0 / 1.702, scalar2=None, op0=ALU.mult), reads=["cmb7"], writes=["cmb7"])
    bu1 = A.alloc((NE * 16,), F32)
    P.op("dve", lambda e: e.tensor_scalar(out=bu1, in0=bu, scalar1=1.0, scalar2=None, op0=ALU.add), reads=["bu"], writes=["bu1"])
    cT2 = A.alloc((1024,), F32)
    u2T = A.alloc((8, 1024), BF16)
    acc = A.alloc((8, D), F32)
    wu = [A.alloc((8, 2 * D), BF16) for _ in range(2)]
    wd = [A.alloc((8, D), BF16) for _ in range(2)]
    aTraw = [A.alloc((4096,), BF16) for _ in range(2)]
    aT = [a.rearrange("p (a b) -> p a b", b=512) for a in aTraw]
    gc = [A.alloc((512,), F32) for _ in range(2)]
    sgm = [A.alloc((512,), F32) for _ in range(2)]
    lc = [A.alloc((512,), F32) for _ in range(2)]
    xt7 = [aTraw[0][:, 0:2048].bitcast(F32)] * 2
    tmp7 = aTraw[0][:, 2048:4096].bitcast(F32)
    ot7 = [aTraw[1][:, 0:2048].bitcast(F32)] * 2
    junk7 = aTraw[1][:, 2048:3072]
    ss7 = A.alloc((16,), F32)
    ln7 = A.alloc((16,), F32)
    rs7 = A.alloc((16,), F32)
    P.op("pool", lambda e: e.memset(ss7, 0.0), writes=["ss7"])
    ecount = 0
    for half in range(2):
        hc = slice(half * 1024, (half + 1) * 1024)
        P.dma("sp", u2T, dm_u2t[:, :, hc], "ld_u2t", reads=["dm_u2t"], writes=["u2T"])
        P.dma("sp", cT2[0:NE, :], dm_combT[:, hc], "ld_cT2", reads=["dm_combT"], writes=["cT2"])
        for tt in range(8):
            for dh in range(2):
                bk = 4 + (tt * 2 + dh) % 4
                P.op("pe", lambda e, tt=tt, dh=dh, bk=bk: e.matmul(banks[bk][:, :], lhsT=cT2[0:NE, tt * 128:(tt + 1) * 128], rhs=bdn[0:NE, dh * 512:(dh + 1) * 512], start=True, stop=True),
                     reads=["cT2", "bdn"], writes=["bank%d" % bk])
                P.op("act", lambda e, tt=tt, dh=dh, bk=bk: e.copy(out=acc[:, tt, dh * 512:(dh + 1) * 512], in_=banks[bk][:, :]), reads=["bank%d" % bk], writes=["acc_%d_%d" % (tt, dh)])
        for ex in range(NE):
            ws = ecount % 2
            ecount += 1
            P.dma("pool", wu[ws], wup_d[ex].rearrange("(kt p) n -> p kt n", p=128), "ld_wu%d" % ws, writes=["wu%d" % ws])
            P.dma("pool", wd[ws], wdn_d[ex].rearrange("(kt p) n -> p kt n", p=128), "ld_wd%d" % ws, writes=["wd%d" % ws])
            for blk in range(2):
                bs = slice(blk * 512, (blk + 1) * 512)
                ab = aT[blk % 2]
                abk = "aT%d" % (blk % 2)
                for g in range(8):
                    s = g % 2
                    bg, bl = g % 2, 2 + g % 2

                    def mmu(e, g=g, bg=bg, bl=bl, ws=ws, bs=bs):
                        for kt in range(8):
                            e.matmul(banks[bg][:, :], lhsT=wu[ws][:, kt, g * 128:(g + 1) * 128], rhs=u2T[:, kt, bs], start=(kt == 0), stop=(kt == 7))
                        for kt in range(8):
                            r = e.matmul(banks[bl][:, :], lhsT=wu[ws][:, kt, 1024 + g * 128:1024 + (g + 1) * 128], rhs=u2T[:, kt, bs], start=(kt == 0), stop=(kt == 7))
                        return r
                    P.op("pe", mmu, reads=["wu%d" % ws, "u2T"], writes=["bank%d" % bg, "bank%d" % bl])
                    P.op("dve", lambda e, s=s, bg=bg, ex=ex, g=g: e.tensor_scalar(out=gc[s], in0=banks[bg][:, :], scalar1=bu[:, ex * 16 + g:ex * 16 + g + 1], scalar2=7.0, op0=ALU.add, op1=ALU.min),
                         reads=["bank%d" % bg, "bu"], writes=["gc%d" % s])
                    P.op("act", lambda e, s=s: e.activation(out=sgm[s], in_=gc[s], func=AF.Silu, scale=1.702), reads=["gc%d" % s], writes=["sgm%d" % s])
                    P.op("dve", lambda e, s=s, bl=bl, ex=ex, g=g: e.tensor_scalar(out=lc[s], in0=banks[bl][:, :], scalar1=bu1[:, ex * 16 + 8 + g:ex * 16 + 8 + g + 1], scalar2=8.0, op0=ALU.add, op1=ALU.min),
                         reads=["bank%d" % bl, "bu1"], writes=["lc%d" % s])
                    P.op("dve", lambda e, s=s, g=g, ab=ab: e.scalar_tensor_tensor(out=ab[:, g, :], in0=lc[s], scalar=-6.0, in1=sgm[s], op0=ALU.max, op1=ALU.mult),
                         reads=["sgm%d" % s, "lc%d" % s], writes=[abk])
                for tt in range(4):
                    til = blk * 4 + tt
                    for dh in range(2):
                        bk = 4 + (tt * 2 + dh) % 4

                        def mmd(e, tt=tt, dh=dh, bk=bk, ws=ws, ab=ab):
                            for fk in range(8):
                                r = e.matmul(banks[bk][:, :], lhsT=ab[:, fk, tt * 128:(tt + 1) * 128], rhs=wd[ws][:, fk, dh * 512:(dh + 1) * 512], start=(fk == 0), stop=(fk == 7))
                            return r
                        P.op("pe", mmd, reads=[abk, "wd%d" % ws], writes=["bank%d" % bk])
                        ak = "acc_%d_%d" % (til, dh)
                        P.op("dve", lambda e, til=til, dh=dh, bk=bk, ex=ex, half=half: e.scalar_tensor_tensor(
                            out=acc[:, til, dh * 512:(dh + 1) * 512], in0=banks[bk][:, :], scalar=cmb7[:, half * 8 + til, ex:ex + 1], in1=acc[:, til, dh * 512:(dh + 1) * 512], op0=ALU.mult, op1=ALU.add),
                            reads=["bank%d" % bk, "cmb7", ak], writes=[ak])
        P.barrier(keep=["dm_x1", "dm_u2t", "dm_combT"])
        for tt in range(8):
            ti = half * 8 + tt
            s = 0
            P.dma("sp", xt7[s], dm_x1[ti * 128:(ti + 1) * 128, :], "ld_x7%d" % s, reads=["dm_x1"], writes=["xt7%d" % s])
            P.op("act", lambda e, tt=tt, ti=ti: e.activation(out=junk7, in_=acc[:, tt, :], func=AF.Square, accum_out=ss7[:, ti:ti + 1]),
                 reads=["acc_%d_0" % tt, "acc_%d_1" % tt, "ss7"], writes=["junk7", "ss7_%d" % ti])
            rstd_from_ss(ss7[:, ti:ti + 1], D, rs7[:, ti:ti + 1], ln7[:, ti:ti + 1], ["ss7_%d" % ti], "rs7_%d" % ti)
            P.op("dve", lambda e, tt=tt, ti=ti: e.scalar_tensor_tensor(out=tmp7, in0=acc[:, tt, :], scalar=rs7[:, ti:ti + 1], in1=G2, op0=ALU.mult, op1=ALU.mult),
                 reads=["acc_%d_0" % tt, "acc_%d_1" % tt, "rs7_%d" % ti, "G2"], writes=["tmp7"])
            P.op("dve", lambda e, s=s: e.tensor_tensor(out=ot7[s], in0=tmp7, in1=xt7[s], op=ALU.add), reads=["tmp7", "xt7%d" % s], writes=["ot7%d" % s])
            P.dma("sp", out_d[ti * 128:(ti + 1) * 128, :], ot7[s], "st_out%d" % s, reads=["ot7%d" % s], writes=["out"])
        P.barrier(keep=["dm_x1", "dm_u2t", "dm_combT"])

    finish(P, nc)
    return nc


def finish(P, nc):
    P.wait_all("sp")
    P.build()
    P.close()
    return nc


def _rope_table(j):
    t = np.arange(NT) + j * NT
    rows = (t // 64).astype(np.float32)
    cols = (t % 64).astype(np.float32)
    inv = (10000.0 ** (-np.arange(0, 64, 2, dtype=np.float32) / 64)).astype(np.float32)
    ar = rows[:, None] * inv[None, :]
    ac = cols[:, None] * inv[None, :]
    cr, sr, cc, sc = np.cos(ar), np.sin(ar), np.cos(ac), np.sin(ac)
    return np.concatenate([cr, cr, cc, cc, -sr, sr, -sc, sc], axis=1).astype(np.float32)


def make_in_maps(inp, small=False):
    f = lambda a: np.ascontiguousarray(np.asarray(a, dtype=np.float32))
    x, c, ctx, c_ctx = f(inp["x"]), f(inp["c"]), f(inp["ctx"]), f(inp["c_ctx"])
    shared = {
        "w_mod": f(inp["w_mod"][0]), "b_mod": f(inp["b_mod"][0]).reshape(1, -1),
        "norm_g": f(inp["norm_g"][0]).reshape(1, -1), "w_in": f(inp["w_in"][0]),
        "lbv": f(np.asarray(inp["hgrn_lb"]).reshape(2, 2, 8, 128).transpose(3, 0, 1, 2).reshape(128, 32)),
        "hng": f(inp["hgrn_norm_g"][0]).reshape(128, 1), "qkg": f(inp["qk_norm_g"][0]).reshape(1, 256),
        "w_branch": f(inp["w_branch"][0]), "w_out": f(inp["w_out"][0]),
        "router_w": f(inp["router_w"][0]), "router_b": f(inp["router_b"][0]).reshape(1, -1),
        "w_up": f(inp["w_up"][0]), "b_upT": f(np.asarray(inp["b_up"][0]).reshape(32, 16, 128).transpose(2, 0, 1).reshape(128, 512)),
        "w_down": f(inp["w_down"][0]), "b_down": f(inp["b_down"][0]),
    }
    ropes = [_rope_table(j) for j in range(4)]
    maps = []
    for core in range(8):
        b, j = core // 4, core % 4
        cvec = np.concatenate([c[b].reshape(8, 128).T, c_ctx.reshape(8, 128).T], axis=1)
        sel = np.zeros((128, 8), np.float32)
        for r in range(4):
            sel[:, r] = 1.0 if r < j else 0.0
            sel[:, 4 + r] = 1.0 if r > j else 0.0
        m = dict(shared)
        if small:
            m["w_up"] = m["w_up"][0:1]
            m["w_down"] = m["w_down"][0:1]
        m.update({"x": f(x[b, j * NT:(j + 1) * NT]), "ctx": f(ctx[b]), "cvec": f(cvec), "rope": ropes[j], "sel": sel})
        maps.append(m)
    return maps


_NC_CACHE = {}


def kernel(**inputs):
    if "nc" not in _NC_CACHE:
        _NC_CACHE["nc"] = build()
    nc = _NC_CACHE["nc"]
    maps = make_in_maps(inputs)
    res = run_bass_kernel_spmd(nc, maps, core_ids=list(range(8)))
    out = np.empty((2, 8192, D), np.float32)
    for core in range(8):
        b, j = core // 4, core % 4
        out[b, j * NT:(j + 1) * NT] = res.results[core]["out"]
    return out
```

```python
from contextlib import ExitStack
import numpy as np
import concourse.bass as bass
import concourse.mybir as mybir
from concourse.bass_utils import run_bass_kernel_spmd

F32 = mybir.dt.float32
BF16 = mybir.dt.bfloat16
ALU = mybir.AluOpType
AF = mybir.ActivationFunctionType
AX = mybir.AxisListType

ENGS = ("pe", "act", "dve", "pool", "sp")
EPOCH = 16000
EPS = 1e-6


def _freeze(fn, memo=None):
    import types
    if memo is None:
        memo = {}
    if not isinstance(fn, types.FunctionType) or fn.__closure__ is None:
        return fn
    if id(fn) in memo:
        return memo[id(fn)]
    cells = []
    for c in fn.__closure__:
        try:
            v = c.cell_contents
        except ValueError:
            cells.append(c)
            continue
        if isinstance(v, types.FunctionType) and v.__closure__ is not None and v is not fn:
            v = _freeze(v, memo)
        cells.append(types.CellType(v))
    new = types.FunctionType(fn.__code__, fn.__globals__, fn.__name__, fn.__defaults__, tuple(cells))
    new.__kwdefaults__ = fn.__kwdefaults__
    memo[id(fn)] = new
    return new


class Prog:
    def __init__(self, nc, same_engine_sync=True):
        self.nc = nc
        self.es = ExitStack()
        self.q = {e: [] for e in ENGS}
        self.cnt = {e: 0 for e in ENGS}
        self.waited = {}
        self.buf = {}
        self.sems = {}
        self.dma_cnt = {}
        self.same_engine_sync = same_engine_sync
        self.n_sem = 0

    def sem(self, key):
        if key not in self.sems:
            self.n_sem += 1
            self.sems[key] = self.es.enter_context(self.nc.semaphore("s%d" % self.n_sem))
        return self.sems[key]

    def sbuf(self, name, shape, dtype):
        return self.es.enter_context(self.nc.sbuf_tensor(name, list(shape), dtype))

    def psum(self, name, shape, dtype=F32):
        return self.es.enter_context(self.nc.psum_tensor(name, list(shape), dtype))

    def _semkey_for(self, prod):
        kind, name, count = prod
        if kind == "e":
            ep = (count - 1) // EPOCH
            return ("e", name, ep), count - ep * EPOCH
        return ("d", name), count

    def _need(self, eng, prod, waits):
        if prod is None:
            return
        kind, name, count = prod
        if kind == "e" and name == eng and (eng in ("pe", "sp") or not self.same_engine_sync):
            return
        sk, val = self._semkey_for(prod)
        wk = (eng, kind, name)
        if self.waited.get(wk, 0) >= count:
            return
        self.waited[wk] = count
        waits.append((sk, val))

    def _deps(self, eng, reads, writes):
        waits = []
        for k in reads:
            b = self.buf.get(k)
            if b is not None:
                self._need(eng, b["w"], waits)
        for k in writes:
            b = self.buf.get(k)
            if b is not None:
                self._need(eng, b["w"], waits)
                for r in b["r"].values():
                    self._need(eng, r, waits)
        return waits

    def _record(self, prod, reads, writes):
        for k in reads:
            b = self.buf.setdefault(k, {"w": None, "r": {}})
            b["r"][(prod[0], prod[1])] = prod
        for k in writes:
            self.buf[k] = {"w": prod, "r": {}}

    def op(self, eng, fn, reads=(), writes=()):
        fn = _freeze(fn)
        waits = self._deps(eng, reads, writes)
        self.cnt[eng] += 1
        prod = ("e", eng, self.cnt[eng])
        sk, _ = self._semkey_for(prod)
        self.q[eng].append((fn, waits, (sk, 1)))
        self._record(prod, reads, writes)
        return prod

    def dma(self, eng, out, in_, semname, reads=(), writes=(), **kw):
        if writes:
            semname = semname + ":" + writes[0]
        waits = self._deps(eng, reads, writes)
        self.dma_cnt[semname] = self.dma_cnt.get(semname, 0) + 16
        prod = ("d", semname, self.dma_cnt[semname])
        self.q[eng].append((lambda e: e.dma_start(out=out, in_=in_, **kw), waits, (("d", semname), 16)))
        self._record(prod, reads, writes)
        return prod

    def custom(self, eng, fn, semname, inc, reads=(), writes=()):
        fn = _freeze(fn)
        waits = self._deps(eng, reads, writes)
        self.dma_cnt[semname] = self.dma_cnt.get(semname, 0) + inc
        prod = ("d", semname, self.dma_cnt[semname])
        self.q[eng].append((fn, waits, (("d", semname), inc)))
        self._record(prod, reads, writes)
        return prod

    def wait_all(self, eng):
        waits = []
        for e in ENGS:
            if self.cnt[e] > 0 and e != eng:
                self._need(eng, ("e", e, self.cnt[e]), waits)
        for name, c in self.dma_cnt.items():
            self._need(eng, ("d", name, c), waits)
        self.q[eng].append((None, waits, None))

    def barrier(self, keep=()):
        for e in ENGS:
            self.wait_all(e)
        self.buf = {k: v for k, v in self.buf.items() if k in keep}

    def build(self):
        nc = self.nc
        keys = []
        for e in ENGS:
            for (_, w, inc) in self.q[e]:
                for x in w:
                    keys.append(x[0])
                if inc:
                    keys.append(inc[0])
        for sk in dict.fromkeys(keys):
            self.sem(sk)
        engmap = {"pe": "tensor", "act": "scalar", "dve": "vector", "pool": "gpsimd", "sp": "sync"}
        with nc.Block() as block:
            for e in ENGS:
                items = self.q[e]

                def body(eng, items=items):
                    for fn, waits, inc in items:
                        for sk, val in waits:
                            eng.wait_ge(self.sems[sk], val)
                        if fn is not None:
                            ins = fn(eng)
                            if inc is not None:
                                ins.then_inc(self.sems[inc[0]], inc[1])

                getattr(block, engmap[e])(body)

    def close(self):
        self.es.close()


class Arena:
    def __init__(self, P, nbytes):
        self.t = P.sbuf("arena", [128, nbytes // 2], BF16)
        self.nbytes = nbytes
        self.off = 0

    def alloc(self, free_shape, dtype):
        n = int(np.prod(free_shape))
        size = n * (4 if dtype == F32 else 2)
        size = (size + 63) // 64 * 64
        assert self.off + size <= self.nbytes, ("SBUF arena overflow", self.off, size)
        v = self.t[:, self.off // 2:(self.off + size) // 2]
        if dtype == F32:
            v = v.bitcast(F32)
        v = v[:, 0:n]
        self.off += size
        if len(free_shape) == 2:
            v = v.rearrange("p (a b) -> p a b", b=free_shape[1])
        elif len(free_shape) == 3:
            v = v.rearrange("p (a b c) -> p a b c", b=free_shape[1], c=free_shape[2])
        elif len(free_shape) == 4:
            v = v.rearrange("p (a b c d) -> p a b c d", b=free_shape[1], c=free_shape[2], d=free_shape[3])
        return v

    def mark(self):
        return self.off

    def release(self, m):
        self.off = m


def bc(ap, axis, shape):
    return ap.unsqueeze(axis).to_broadcast(list(shape))


NT = 2048
NTT = 16
NCTX = 256
NALL = NT + NCTX
D = 1024
NE = 32
LAST_PHASE = 99


def build(upto=LAST_PHASE, debug=False):
    nc = bass.Bass("TRN2", target_bir_lowering=False)

    def din(name, shape, dt=F32):
        return nc.dram_tensor(name, list(shape), dt, kind="ExternalInput").ap()

    x_d = din("x", [NT, D])
    ctx_d = din("ctx", [NCTX, D])
    cvec_d = din("cvec", [128, 16])
    wmod_d = din("w_mod", [D, 6 * D])
    bmod_d = din("b_mod", [1, 6 * D])
    ng_d = din("norm_g", [1, 4 * D])
    win_d = din("w_in", [D, 8704])
    lbv_d = din("lbv", [128, 32])
    hng_d = din("hng", [128, 1])
    qkg_d = din("qkg", [1, 256])
    wbr_d = din("w_branch", [2, D, D])
    wout_d = din("w_out", [D, D])
    rw_d = din("router_w", [D, NE])
    rb_d = din("router_b", [1, NE])
    NEW = NE if upto >= 7 else 1
    wup_d = din("w_up", [NEW, D, 2 * D])
    bupT_d = din("b_upT", [128, NE * 16])
    wdn_d = din("w_down", [NEW, D, D])
    bdn_d = din("b_down", [NE, D])
    rope_d = din("rope", [NT, 256])
    sel_d = din("sel", [128, 8])
    out_d = nc.dram_tensor("out", [NT, D], F32, kind="ExternalOutput").ap()

    def dscr(name, shape, dt):
        if debug:
            return nc.dram_tensor(name, list(shape), dt, kind="ExternalOutput").ap()
        return nc.dram_tensor(name, list(shape), dt).ap()

    dm_mod = dscr("dm_mod", [128, 6 * D], F32)
    dm_ut = dscr("dm_ut", [128, 8, NALL], BF16)
    ag1_ins = [nc.dram_tensor("ag1_in%d" % q, [128, 2048], BF16).ap() for q in range(4)]
    ag1_outs = [nc.dram_tensor("ag1_out%d" % q, [4 * 128, 2048], BF16).ap() for q in range(4)]
    dm_kvctx = dscr("dm_kvctx", [512, NCTX], BF16)
    dm_oloc = dscr("dm_oloc", [8, 128, NT], F32)
    dm_qb = dscr("dm_qb", [2, 8, 128, NT], BF16)
    dm_gs = dscr("dm_gs", [8, 128, NT], BF16)
    ag2_ins = [nc.dram_tensor("ag2_in%d" % d, [128, 1032], F32).ap() for d in range(2)]
    ag2_outs = [nc.dram_tensor("ag2_out%d" % d, [4 * 128, 1032], F32).ap() for d in range(2)]
    dm_oth = dscr("dm_oth", [8, 128, NT], BF16)
    dm_ota = dscr("dm_ota", [8, 128, NT], BF16)
    dm_x1 = dscr("dm_x1", [NT, D], F32)
    dm_u2t = dscr("dm_u2t", [128, 8, NT], BF16)
    dm_comb = dscr("dm_comb", [128, NTT, NE], F32)
    dm_combT = dscr("dm_combT", [NE, NT], F32)

    P = Prog(nc)
    A = Arena(P, 207 * 1024)
    banks = [P.psum("bank%d" % i, [128, 512], F32) for i in range(8)]

    def bank_bf(i):
        return banks[i][:, :].bitcast(BF16)

    ident_f = A.alloc((128,), F32)
    ident_b = A.alloc((128,), BF16)
    ones_b = A.alloc((128,), BF16)
    ones_f = A.alloc((128,), F32)
    P.op("pool", lambda e: e.memset(ident_f, 0.0), writes=["ident_f"])
    P.op("pool", lambda e: e.affine_select(out=ident_f, in_=ident_f, pattern=[[-1, 128]], compare_op=ALU.not_equal,
                                           fill=1.0, base=0, channel_multiplier=1), reads=["ident_f"], writes=["ident_f"])
    P.op("dve", lambda e: e.tensor_copy(out=ident_b, in_=ident_f), reads=["ident_f"], writes=["ident_b"])
    P.op("pool", lambda e: e.memset(ones_f, 1.0), writes=["ones_f"])
    P.op("dve", lambda e: e.tensor_copy(out=ones_b, in_=ones_f), reads=["ones_f"], writes=["ones_b"])

    m_pre_ut = A.mark()
    uT = A.alloc((8, NALL), BF16)

    def rstd_from_ss(ss_ap, n, out_ap, tmp_ap, rk, wk):
        P.op("act", lambda e: e.activation(out=tmp_ap, in_=ss_ap, func=AF.Ln, scale=1.0 / n, bias=EPS), reads=rk, writes=[wk + "_ln"])
        P.op("act", lambda e: e.activation(out=out_ap, in_=tmp_ap, func=AF.Exp, scale=-0.5), reads=[wk + "_ln"], writes=[wk])

    m0 = A.mark()
    cv = A.alloc((16,), F32)
    scv = A.alloc((16,), F32)
    scb = A.alloc((16, 128), F32)
    bmod = A.alloc((6 * D,), F32)
    ng = A.alloc((4, D), F32)
    modl = A.alloc((6 * D,), F32)
    modc = A.alloc((2 * D,), F32)
    wm = [A.alloc((8, 512), F32) for _ in range(2)]
    P.dma("sp", cv, cvec_d, "ld_c", writes=["cv"])
    P.dma("sp", bmod, bmod_d[0].partition_broadcast(128), "ld_c", writes=["bmod"])
    P.dma("sp", ng, ng_d[0].partition_broadcast(128).rearrange("p (a b) -> p a b", b=D), "ld_c", writes=["ng"])
    P.op("act", lambda e: e.activation(out=scv, in_=cv, func=AF.Silu), reads=["cv"], writes=["scv"])
    for k in range(16):
        P.op("dve", lambda e, k=k: e.tensor_copy(out=scb[:, k, :], in_=scv[:, k:k + 1].to_broadcast([128, 128])),
             reads=["scv"], writes=["scb"])
    for s in range(12):
        w = wm[s % 2]
        wk = "wm%d" % (s % 2)
        P.dma("sp", w, wmod_d[:, s * 512:(s + 1) * 512].rearrange("(kt p) n -> p kt n", p=128), "ld_" + wk, writes=[wk])

        def mm(e, w=w, off=0, bk=0):
            for kt in range(8):
                r = e.matmul(banks[bk][:, :], lhsT=scb[:, off + kt, :], rhs=w[:, kt, :], start=(kt == 0), stop=(kt == 7))
            return r
        P.op("pe", lambda e, w=w: mm(e, w, 0, 0), reads=["scb", wk], writes=["bank0"])
        P.op("dve", lambda e, s=s: e.tensor_tensor(out=modl[:, s * 512:(s + 1) * 512], in0=banks[0][:, :], in1=bmod[:, s * 512:(s + 1) * 512], op=ALU.add),
             reads=["bank0", "bmod"], writes=["modl"])
        if s < 4:
            P.op("pe", lambda e, w=w: mm(e, w, 8, 1), reads=["scb", wk], writes=["bank1"])
            P.op("dve", lambda e, s=s: e.tensor_tensor(out=modc[:, s * 512:(s + 1) * 512], in0=banks[1][:, :], in1=bmod[:, s * 512:(s + 1) * 512], op=ALU.add),
                 reads=["bank1", "bmod"], writes=["modc"])
    P.op("dve", lambda e: e.scalar_tensor_tensor(out=modl[:, D:2 * D], in0=modl[:, D:2 * D], scalar=1.0, in1=ng[:, 0, :], op0=ALU.add, op1=ALU.mult),
         reads=["modl", "ng"], writes=["modl"])
    P.op("dve", lambda e: e.scalar_tensor_tensor(out=modc[:, D:2 * D], in0=modc[:, D:2 * D], scalar=1.0, in1=ng[:, 0, :], op0=ALU.add, op1=ALU.mult),
         reads=["modc", "ng"], writes=["modc"])
    P.op("dve", lambda e: e.tensor_tensor(out=modl[:, 2 * D:3 * D], in0=modl[:, 2 * D:3 * D], in1=ng[:, 1, :], op=ALU.mult), reads=["modl", "ng"], writes=["modl"])
    P.op("dve", lambda e: e.scalar_tensor_tensor(out=modl[:, 4 * D:5 * D], in0=modl[:, 4 * D:5 * D], scalar=1.0, in1=ng[:, 2, :], op0=ALU.add, op1=ALU.mult),
         reads=["modl", "ng"], writes=["modl"])
    P.op("dve", lambda e: e.tensor_tensor(out=modl[:, 5 * D:6 * D], in0=modl[:, 5 * D:6 * D], in1=ng[:, 3, :], op=ALU.mult), reads=["modl", "ng"], writes=["modl"])
    P.dma("sp", dm_mod, modl, "st_mod", reads=["modl"], writes=["dm_mod"])

    xt = [A.alloc((D,), F32) for _ in range(2)]
    junk = A.alloc((D,), BF16)
    tmpf = A.alloc((D,), F32)
    ub = [A.alloc((D,), BF16) for _ in range(2)]
    ss1 = A.alloc((18,), F32)
    ln1 = A.alloc((18,), F32)
    rs1 = A.alloc((18,), F32)
    P.op("pool", lambda e: e.memset(ss1, 0.0), writes=["ss1"])
    for ti in range(18):
        s = ti % 2
        src = x_d[ti * 128:(ti + 1) * 128, :] if ti < NTT else ctx_d[(ti - NTT) * 128:(ti - NTT + 1) * 128, :]
        Am = modl if ti < NTT else modc
        amk = "modl" if ti < NTT else "modc"
        P.dma("sp", xt[s], src, "ld_xt%d" % s, writes=["xt%d" % s])
        P.op("act", lambda e, s=s, ti=ti: e.activation(out=junk, in_=xt[s], func=AF.Square, accum_out=ss1[:, ti:ti + 1]),
             reads=["xt%d" % s, "ss1"], writes=["junk", "ss1_%d" % ti])
        rstd_from_ss(ss1[:, ti:ti + 1], D, rs1[:, ti:ti + 1], ln1[:, ti:ti + 1], ["ss1_%d" % ti], "rs1_%d" % ti)
        P.op("dve", lambda e, s=s, ti=ti, Am=Am: e.scalar_tensor_tensor(out=tmpf, in0=xt[s], scalar=rs1[:, ti:ti + 1], in1=Am[:, D:2 * D], op0=ALU.mult, op1=ALU.mult),
             reads=["xt%d" % s, "rs1_%d" % ti, amk], writes=["tmpf"])
        P.op("dve", lambda e, s=s, Am=Am: e.tensor_tensor(out=ub[s], in0=tmpf, in1=Am[:, 0:D], op=ALU.add), reads=["tmpf", amk], writes=["ub%d" % s])
        bk = 2 + s

        def tr(e, s=s, bk=bk):
            for kt in range(8):
                r = e.transpose(bank_bf(bk)[:, kt * 128:(kt + 1) * 128], ub[s][:, kt * 128:(kt + 1) * 128], ident_b)
            return r
        P.op("pe", tr, reads=["ub%d" % s, "ident_b"], writes=["bank%d" % bk])
        P.op("act", lambda e, ti=ti, bk=bk: e.copy(out=uT[:, :, ti * 128:(ti + 1) * 128], in_=bank_bf(bk).rearrange("p (a b) -> p a b", b=128)),
             reads=["bank%d" % bk], writes=["uT_%d" % ti])
    UT_KEYS = ["uT_%d" % ti for ti in range(18)]
    if debug:
        P.dma("sp", dm_ut, uT, "st_dbg", reads=UT_KEYS, writes=["dm_ut"])
    P.barrier()
    A.release(m0)
    if upto <= 1:
        return finish(P, nc)


    m2 = A.mark()
    wkv = A.alloc((8, 512), BF16)
    P.dma("pool", wkv, win_d[:, 6144:6656].rearrange("(kt p) n -> p kt n", p=128), "ld_wkv", writes=["wkv"])
    ropeT = A.alloc((NTT, 256), F32)
    P.dma("sp", ropeT, rope_d.rearrange("(t p) n -> p t n", p=128), "ld_rope", writes=["ropeT"])
    gqk = A.alloc((256,), F32)
    P.dma("sp", gqk, qkg_d[0].partition_broadcast(128), "ld_c", writes=["gqk"])
    KTl = A.alloc((2, NALL), BF16)
    Vl = A.alloc((18, 256), BF16)
    ssk = A.alloc((18, 2), F32)
    lnk = A.alloc((18, 2), F32)
    rsk = A.alloc((18, 2), F32)
    knb = [A.alloc((2, 128), F32) for _ in range(2)]
    t1b = A.alloc((2, 128), F32)
    t2b = A.alloc((2, 128), F32)
    kbf = [A.alloc((256,), BF16) for _ in range(2)]
    junk2 = A.alloc((128,), BF16)
    P.op("pool", lambda e: e.memset(ssk, 0.0), writes=["ssk"])

    def rope_apply(xn, nh, ti, outbf, pfx, eng2="dve"):
        cosv = ropeT[:, ti, 0:128]
        sinv = ropeT[:, ti, 128:256].rearrange("p (r x d) -> p r x d", r=2, x=2, d=32)
        t1 = t1b if nh == 2 else t1q
        t2 = t2b if nh == 2 else t2q
        P.op("dve", lambda e: e.tensor_tensor(out=t1, in0=xn, in1=bc(cosv, 1, [128, nh, 128]), op=ALU.mult),
             reads=[pfx + "xn", "ropeT"], writes=[pfx + "t1"])
        x6 = xn.rearrange("p h (r x d) -> p h r x d", r=2, x=2, d=32)
        t6 = t2.rearrange("p h (r x d) -> p h r x d", r=2, x=2, d=32)
        for xo in range(2):
            P.op(eng2, lambda e, xo=xo: e.tensor_tensor(out=t6[:, :, :, xo, :], in0=x6[:, :, :, 1 - xo, :],
                                                        in1=bc(sinv[:, :, xo, :], 1, [128, nh, 2, 32]), op=ALU.mult),
                 reads=[pfx + "xn", "ropeT"], writes=[pfx + "t2_%d" % xo])
        P.op("dve", lambda e: e.tensor_tensor(out=outbf.rearrange("p (h d) -> p h d", d=128), in0=t1, in1=t2, op=ALU.add),
             reads=[pfx + "t1", pfx + "t2_0", pfx + "t2_1"], writes=[pfx + "bf"])

    import os
    BIS = int(os.environ.get("BIS", "99"))
    for ti in range(18):
        s = ti % 2
        bk = s
        kn = knb[s]

        def mmkv(e, ti=ti, bk=bk):
            for kt in range(8):
                r = e.matmul(banks[bk][:, :], lhsT=uT[:, kt, ti * 128:(ti + 1) * 128], rhs=wkv[:, kt, :], start=(kt == 0), stop=(kt == 7))
            return r
        P.op("pe", mmkv, reads=["uT_%d" % ti, "wkv"], writes=["bank%d" % bk])
        kps = banks[bk][:, 0:256].rearrange("p (h d) -> p h d", d=128)
        P.op("act", lambda e, ti=ti, bk=bk: e.copy(out=Vl[:, ti, :], in_=banks[bk][:, 256:512]), reads=["bank%d" % bk], writes=["Vl_%d" % ti])
        if BIS < 2:
            continue
        for h in range(2):
            P.op("act", lambda e, h=h, ti=ti, kps=kps: e.activation(out=junk2, in_=kps[:, h, :], func=AF.Square, accum_out=ssk[:, ti, h:h + 1]),
                 reads=["bank%d" % bk, "ssk"], writes=["junk2", "ssk_%d_%d" % (ti, h)])
        rstd_from_ss(ssk[:, ti, :], 128, rsk[:, ti, :], lnk[:, ti, :], ["ssk_%d_0" % ti, "ssk_%d_1" % ti], "rsk_%d" % ti)
        P.op("dve", lambda e, kn=kn, kps=kps, ti=ti: e.tensor_tensor(out=kn, in0=kps, in1=bc(rsk[:, ti, :], 2, [128, 2, 128]), op=ALU.mult),
             reads=["bank%d" % bk, "rsk_%d" % ti], writes=["k%dxn" % s])
        P.op("dve", lambda e, kn=kn: e.tensor_tensor(out=kn, in0=kn, in1=bc(gqk[:, 128:256], 1, [128, 2, 128]), op=ALU.mult),
             reads=["k%dxn" % s, "gqk"], writes=["k%dxn" % s])
        if BIS < 3:
            continue
        if ti < NTT and BIS != 3:
            rope_apply(kn, 2, ti, kbf[s], "k%d" % s)
        else:
            P.op("dve", lambda e, kn=kn, s=s: e.tensor_copy(out=kbf[s].rearrange("p (h d) -> p h d", d=128), in_=kn), reads=["k%dxn" % s], writes=["k%dbf" % s])
        bk2 = 2 + s
        if BIS < 5:
            continue

        def trk(e, s=s, bk2=bk2):
            for h in range(2):
                r = e.transpose(bank_bf(bk2)[:, h * 128:(h + 1) * 128], kbf[s][:, h * 128:(h + 1) * 128], ident_b)
            return r
        P.op("pe", trk, reads=["k%dbf" % s, "ident_b"], writes=["bank%d" % bk2])
        P.op("act", lambda e, ti=ti, bk2=bk2: e.copy(out=KTl[:, :, ti * 128:(ti + 1) * 128], in_=bank_bf(bk2)[:, 0:256].rearrange("p (a b) -> p a b", b=128)),
             reads=["bank%d" % bk2], writes=["KTl_%d" % ti])
    for h in range(2 if BIS >= 6 else 0):
        P.dma("sp", ag1_ins[h], KTl[:, h, 0:NT], "st_ag1", reads=["KTl_%d" % t for t in range(16)], writes=["ag1_in"])
        P.dma("sp", ag1_ins[2 + h].rearrange("p (ti c) -> p ti c", c=256), Vl[:, 8 * h:8 * h + 8, :], "st_ag1", reads=["Vl_%d" % t for t in range(16)], writes=["ag1_in"])
    if BIS >= 6:
        P.dma("sp", dm_kvctx[0:256, :].rearrange("(h p) t -> p h t", p=128), KTl[:, :, NT:NALL], "st_kvc", reads=["KTl_16", "KTl_17"], writes=["dm_kvctx"])
        P.dma("sp", dm_kvctx[256:512, :].rearrange("(ti p) c -> p ti c", p=128), Vl[:, 16:18, :], "st_kvc", reads=["Vl_16", "Vl_17"], writes=["dm_kvctx"])
    for q in range(4 if BIS >= 7 else 0):
        P.custom("pool", lambda e, q=q: e.collective_compute("AllGather", ALU.bypass, replica_groups=[[0, 1, 2, 3], [4, 5, 6, 7]], ins=[ag1_ins[q]], outs=[ag1_outs[q]]),
                 "cc1", 1, reads=["ag1_in"] + (["ag1_out"] if q > 0 else []), writes=["ag1_out"])
    P.barrier(keep=["ag1_out", "dm_kvctx", "dm_mod"] + UT_KEYS)
    A.release(m2)
    if upto <= 2:
        return finish(P, nc)

    m3 = A.mark()
    rst = A.alloc((NALL,), F32)
    maskF = A.alloc((128,), F32)
    maskB = A.alloc((128,), F32)
    lbt = A.alloc((2, 2, 8), F32)
    lbd = A.alloc((2, 8), F32)
    lb = A.alloc((2, 8), F32)
    oml = A.alloc((2, 8), F32)
    hng = A.alloc((1,), F32)
    stage = A.alloc((2, 8, 128), F32)
    sctx = A.alloc((2, 8, 128), F32)
    Dv = A.alloc((2, 8), F32)
    m3h = A.mark()
    whb = [A.alloc((8, 5, 128), BF16)] * 2
    qT = A.alloc((NT,), F32)
    gsT = A.alloc((NT,), BF16)
    v_tm = A.alloc((18, 128), BF16)
    fa = A.alloc((NALL,), F32)
    lf = A.alloc((NALL,), F32)
    kk = A.alloc((NALL,), F32)
    bb = A.alloc((NALL,), F32)
    xx = A.alloc((NALL,), F32)
    ee = A.alloc((NALL,), F32)
    gtmp = ee[:, 0:NT]
    qe = [A.alloc((NT,), BF16) for _ in range(2)]
    ke = [A.alloc((NT,), BF16) for _ in range(2)]
    kd = [A.alloc((NALL,), BF16) for _ in range(2)]
    kd_tm = [A.alloc((18, 128), BF16) for _ in range(2)]
    qB = [A.alloc((NT,), BF16) for _ in range(2)]
    tot = [A.alloc((36,), F32) for _ in range(2)]
    etot = [A.alloc((36,), F32) for _ in range(2)]
    ipf = [A.alloc((32,), F32) for _ in range(2)]
    gg = [A.alloc((32,), F32) for _ in range(2)]
    eg = [A.alloc((32,), F32) for _ in range(2)]
    attm = [A.alloc((4, 128), BF16) for _ in range(2)]
    Sst = [[A.alloc((128,), F32) for _ in range(2)] for _ in range(2)]
    Sbf = [[A.alloc((128,), BF16) for _ in range(3)] for _ in range(2)]
    Scx = [A.alloc((128,), F32) for _ in range(2)]
    o_acc = A.alloc((NT,), F32)

    P.op("pool", lambda e: e.memset(rst, 1.0), writes=["rst"])
    P.op("pool", lambda e: e.memset(rst.rearrange("p (c t) -> p c t", t=64)[:, :, 0:1], 0.0), reads=["rst"], writes=["rst"])
    P.op("pool", lambda e: e.memset(maskF, 1.0), writes=["maskF"])
    P.op("pool", lambda e: e.affine_select(out=maskF, in_=maskF, pattern=[[1, 128]], compare_op=ALU.is_ge, fill=0.0, base=0, channel_multiplier=-1),
         reads=["maskF"], writes=["maskF"])
    P.op("pool", lambda e: e.memset(maskF[0:64, 64:128], 0.0), reads=["maskF"], writes=["maskF"])
    P.op("pool", lambda e: e.memset(maskB, 1.0), writes=["maskB"])
    P.op("pool", lambda e: e.affine_select(out=maskB, in_=maskB, pattern=[[-1, 128]], compare_op=ALU.is_ge, fill=0.0, base=0, channel_multiplier=1),
         reads=["maskB"], writes=["maskB"])
    P.op("pool", lambda e: e.memset(maskB[64:128, 0:64], 0.0), reads=["maskB"], writes=["maskB"])
    masks = [maskF, maskB]
    P.dma("sp", lbt, lbv_d.rearrange("p (a b c) -> p a b c", a=2, b=2), "ld_c", writes=["lbt"])
    P.dma("sp", hng, hng_d, "ld_c", writes=["hng"])
    P.op("dve", lambda e: e.tensor_tensor(out=lbd, in0=lbt[:, :, 0, :], in1=lbt[:, :, 1, :], op=ALU.subtract), reads=["lbt"], writes=["lbd"])
    P.op("act", lambda e: e.activation(out=lb, in_=lbd, func=AF.Sigmoid), reads=["lbd"], writes=["lb"])
    P.op("dve", lambda e: e.tensor_scalar(out=oml, in0=lb, scalar1=-1.0, scalar2=1.0, op0=ALU.mult, op1=ALU.add), reads=["lb"], writes=["oml"])

    win_v = win_d.rearrange("(kt p) (s n) -> p kt s n", p=128, n=128)
    BLK5 = [(0, 512), (512, 512), (1024, 512), (1536, 512), (2048, 256)]
    PB = 6
    pbc = [0]

    def proj_fm(wh, whk, sidx, c0, n, evac):
        bk = PB + (pbc[0] % 2)
        pbc[0] += 1

        def mm(e):
            for kt in range(8):
                r = e.matmul(banks[bk][:, 0:n], lhsT=wh[:, kt, sidx, :], rhs=uT[:, kt, c0:c0 + n], start=(kt == 0), stop=(kt == 7))
            return r
        P.op("pe", mm, reads=[whk] + ["uT_%d" % t for t in range(c0 // 128, (c0 + n) // 128)], writes=["bank%d" % bk])
        evac(banks[bk][:, 0:n], "bank%d" % bk)

    def hgrn_dir_prep(h, d):
        wh = whb[h % 2]
        whk = "wh0"
        dk = "d%d" % d
        for (c0, n) in BLK5:
            proj_fm(wh, whk, 2 + d, c0, n, lambda bap, bkey, c0=c0, n=n: P.op(
                "act", lambda e: e.activation(out=fa[:, c0:c0 + n], in_=bap, func=AF.Sigmoid), reads=[bkey], writes=["fa"]))
        fak = ["fa"]
        P.op("dve", lambda e: e.tensor_scalar(out=fa, in0=fa, scalar1=oml[:, d, h:h + 1], scalar2=lb[:, d, h:h + 1], op0=ALU.mult, op1=ALU.add),
             reads=fak + ["oml", "lb"], writes=["fa"])
        P.op("act", lambda e: e.activation(out=lf, in_=fa, func=AF.Ln), reads=["fa"], writes=["lf"])
        P.op("dve", lambda e: e.tensor_scalar(out=kk, in0=fa, scalar1=-1.0, scalar2=1.0, op0=ALU.mult, op1=ALU.add), reads=["fa"], writes=["kk"])
        P.op("dve", lambda e: e.tensor_tensor_scan(out=bb, data0=rst, data1=lf, initial=0.0, op0=ALU.mult, op1=ALU.add), reads=["rst", "lf"], writes=["bb"])
        b3 = bb.rearrange("p (c t) -> p c t", t=64)
        P.op("dve", lambda e: e.tensor_copy(out=tot[d], in_=b3[:, :, 63]), reads=["bb"], writes=["tot" + dk])
        P.op("act", lambda e: e.activation(out=etot[d], in_=tot[d], func=AF.Exp), reads=["tot" + dk], writes=["etot" + dk])
        x3 = xx.rearrange("p (c t) -> p c t", t=64)
        P.op("dve", lambda e: e.tensor_tensor(out=x3, in0=bc(tot[d], 2, [128, 36, 64]), in1=b3, op=ALU.subtract), reads=["tot" + dk, "bb"], writes=["xx"])
        if d == 0:
            bu, dd = bb, xx
            bk_, ddk = "bb", "xx"
        else:
            P.op("dve", lambda e: e.tensor_tensor(out=xx, in0=xx, in1=lf, op=ALU.add), reads=["xx", "lf"], writes=["xx"])
            P.op("dve", lambda e: e.tensor_tensor(out=bb, in0=bb, in1=lf, op=ALU.subtract), reads=["bb", "lf"], writes=["bb"])
            bu, dd = xx, bb
            bk_, ddk = "xx", "bb"
        P.op("act", lambda e: e.activation(out=ee[:, 0:NT], in_=bu[:, 0:NT], func=AF.Exp), reads=[bk_], writes=["ee"])
        P.op("dve", lambda e: e.tensor_tensor(out=qe[d], in0=qT, in1=ee[:, 0:NT], op=ALU.mult), reads=["ee", "qT"], writes=["qe" + dk])
        P.op("act", lambda e: e.activation(out=ee[:, 0:NT], in_=bu[:, 0:NT], func=AF.Exp, scale=-1.0), reads=[bk_, "ee"], writes=["ee"])
        P.op("dve", lambda e: e.tensor_tensor(out=ke[d], in0=kk[:, 0:NT], in1=ee[:, 0:NT], op=ALU.mult), reads=["ee", "kk"], writes=["ke" + dk])
        P.op("act", lambda e: e.activation(out=ee, in_=dd, func=AF.Exp), reads=[ddk, "ee"], writes=["ee"])
        P.op("dve", lambda e: e.tensor_tensor(out=kd[d], in0=kk, in1=ee, op=ALU.mult), reads=["ee", "kk"], writes=["kd" + dk])
        P.op("dve", lambda e: e.tensor_tensor_scan(out=ipf[d], data0=ones_f[:, 0:32], data1=tot[d][:, 0:32], initial=0.0, op0=ALU.mult, op1=ALU.add),
             reads=["tot" + dk, "ones_f"], writes=["ipf" + dk])
        if d == 0:
            P.op("dve", lambda e: e.tensor_tensor(out=gg[d], in0=ipf[d], in1=tot[d][:, 0:32], op=ALU.subtract), reads=["ipf" + dk, "tot" + dk], writes=["gg" + dk])
        else:
            P.op("dve", lambda e: e.tensor_tensor(out=gg[d], in0=ipf[d][:, 31:32].to_broadcast([128, 32]), in1=ipf[d], op=ALU.subtract),
                 reads=["ipf" + dk], writes=["gg" + dk])
        P.op("act", lambda e: e.activation(out=eg[d], in_=gg[d], func=AF.Exp), reads=["gg" + dk], writes=["eg" + dk])
        P.op("act", lambda e: e.activation(out=Dv[:, d, h:h + 1], in_=ipf[d][:, 31:32], func=AF.Exp), reads=["ipf" + dk], writes=["Dv_%d_%d" % (d, h)])
        P.op("dve", lambda e: e.tensor_tensor(out=qB[d].rearrange("p (c t) -> p c t", t=64), in0=qe[d].rearrange("p (c t) -> p c t", t=64),
                                               in1=bc(eg[d], 2, [128, 32, 64]), op=ALU.mult), reads=["qe" + dk, "eg" + dk], writes=["qB" + dk])
        P.dma("sp", dm_qb[d, h], qB[d], "st_qb", reads=["qB" + dk], writes=["dm_qb"])
        for g3 in range(3):
            bk = PB + (pbc[0] % 2)
            pbc[0] += 1

            def trd(e, g3=g3, bk=bk):
                for i in range(6):
                    ti = g3 * 6 + i
                    r = e.transpose(bank_bf(bk)[:, i * 128:(i + 1) * 128], kd[d][:, ti * 128:(ti + 1) * 128], ident_b)
                return r
            P.op("pe", trd, reads=["kd" + dk, "ident_b"], writes=["bank%d" % bk])
            P.op("act", lambda e, g3=g3, bk=bk: e.copy(out=kd_tm[d][:, g3 * 6:(g3 + 1) * 6, :], in_=bank_bf(bk)[:, 0:768].rearrange("p (a b) -> p a b", b=128)),
                 reads=["bank%d" % bk], writes=["kdtm%s_%d" % (dk, g3)])

    def hgrn_dir_scan(h, d):
        dk = "d%d" % d
        kdk = ["kdtm%s_%d" % (dk, g3) for g3 in range(3)]
        bA, bO, bS = 3 * d, 3 * d + 1, 3 * d + 2
        Sc = Scx[d]
        Sbufs, Sbb = Sst[d], Sbf[d]
        slot = [0]

        def delta_mm(c):
            sl = slot[0] % 2
            slot[0] += 1
            ti, half = c // 2, c % 2
            ps = slice(half * 64, half * 64 + 64)
            bidx = (bS, PB + d)[sl]
            bap = banks[bidx][:, 0:128]
            bkey = "bank%d" % bidx
            P.op("pe", lambda e: e.matmul(bap, lhsT=kd_tm[d][ps, ti, :], rhs=v_tm[ps, ti, :], start=True, stop=True),
                 reads=kdk + ["v_tm"], writes=[bkey])
            return bap, bkey
        corder = [32, 33, 34, 35] if d == 0 else [35, 34, 33, 32]
        for i, c in enumerate(corder):
            bap, bkey = delta_mm(c)
            if i == 0:
                P.op("dve", lambda e: e.tensor_copy(out=Sc, in_=bap), reads=[bkey], writes=["Sc" + dk])
            else:
                P.op("dve", lambda e: e.scalar_tensor_tensor(out=Sc, in0=Sc, scalar=etot[d][:, c:c + 1], in1=bap, op0=ALU.mult, op1=ALU.add),
                     reads=[bkey, "Sc" + dk, "etot" + dk], writes=["Sc" + dk])
            yield
        P.op("dve", lambda e: e.tensor_copy(out=sctx[:, d, h, :], in_=Sc), reads=["Sc" + dk], writes=["sctx_%d_%d" % (d, h)])
        P.op("pool", lambda e: e.memset(Sbb[0], 0.0), reads=["Sb%s_0" % dk], writes=["Sb%s_0" % dk])
        si, bi, nstate = 0, 0, 0
        groups = [0, 1, 2, 3] if d == 0 else [3, 2, 1, 0]
        pis = [0, 1, 2, 3] if d == 0 else [3, 2, 1, 0]
        seq = []
        for g in groups:
            for pi in pis:
                p = g * 4 + pi
                for c in ([2 * p, 2 * p + 1] if d == 0 else [2 * p + 1, 2 * p]):
                    seq.append(c)
        dl = {}
        for i in range(min(2, len(seq))):
            dl[i] = delta_mm(seq[i])
        step = 0
        for g in groups:
            def att(e, g=g):
                for pi in range(4):
                    p = g * 4 + pi
                    r = e.matmul(banks[bA][:, pi * 128:(pi + 1) * 128], lhsT=ke[d][:, p * 128:(p + 1) * 128], rhs=qe[d][:, p * 128:(p + 1) * 128], start=True, stop=True)
                return r
            P.op("pe", att, reads=["ke" + dk, "qe" + dk], writes=["bank%d" % bA])
            P.op("dve", lambda e: e.tensor_tensor(out=attm[d], in0=banks[bA][:, :].rearrange("p (a b) -> p a b", b=128), in1=bc(masks[d], 1, [128, 4, 128]), op=ALU.mult),
                 reads=["bank%d" % bA, "mask"], writes=["attm" + dk])
            for pi in pis:
                p = g * 4 + pi
                P.op("pe", lambda e, pi=pi, p=p: e.matmul(banks[bO][:, pi * 128:(pi + 1) * 128], lhsT=v_tm[:, p, :], rhs=attm[d][:, pi, :], start=True, stop=False),
                     reads=["v_tm", "attm" + dk], writes=["bank%d" % bO])
                chunks = [2 * p, 2 * p + 1] if d == 0 else [2 * p + 1, 2 * p]
                for ci, c in enumerate(chunks):
                    col = pi * 128 + (c % 2) * 64
                    sbc = Sbb[bi]
                    P.op("pe", lambda e, c=c, col=col, ci=ci, sbc=sbc: e.matmul(banks[bO][:, col:col + 64], lhsT=sbc, rhs=qe[d][:, c * 64:(c + 1) * 64], start=False, stop=(ci == 1)),
                         reads=["Sb%s_%d" % (dk, bi), "qe" + dk], writes=["bank%d" % bO])
                    assert seq[step] == c
                    bap, bkey = dl.pop(step)
                    nsi = (si + 1) % 2
                    So, Sn = Sbufs[si], Sbufs[nsi]
                    if nstate == 0:
                        P.op("dve", lambda e, Sn=Sn, bap=bap: e.tensor_copy(out=Sn, in_=bap), reads=[bkey], writes=["S%s_%d" % (dk, nsi)])
                    else:
                        P.op("dve", lambda e, So=So, Sn=Sn, bap=bap, c=c: e.scalar_tensor_tensor(out=Sn, in0=So, scalar=etot[d][:, c:c + 1], in1=bap, op0=ALU.mult, op1=ALU.add),
                             reads=[bkey, "S%s_%d" % (dk, si), "etot" + dk], writes=["S%s_%d" % (dk, nsi)])
                    si = nsi
                    nstate += 1
                    nbi = (bi + 1) % 3
                    sbn = Sbb[nbi]
                    P.op("act", lambda e, Sn=Sn, sbn=sbn: e.copy(out=sbn, in_=Sn), reads=["S%s_%d" % (dk, si)], writes=["Sb%s_%d" % (dk, nbi)])
                    bi = nbi
                    if step + 2 < len(seq):
                        dl[step + 2] = delta_mm(seq[step + 2])
                    step += 1
                    yield
            cs = slice(g * 512, (g + 1) * 512)
            if (d == 0 and g < 2) or (d == 1 and g >= 2):
                P.op("act", lambda e, cs=cs: e.copy(out=o_acc[:, cs], in_=banks[bO][:, :]), reads=["bank%d" % bO, "oacc_%d" % g], writes=["oacc_%d" % g])
            else:
                P.op("dve", lambda e, cs=cs: e.tensor_tensor(out=o_acc[:, cs], in0=o_acc[:, cs], in1=banks[bO][:, :], op=ALU.add),
                     reads=["bank%d" % bO, "oacc_%d" % g], writes=["oacc_%d" % g])
        Sl = Sbufs[si]
        P.op("dve", lambda e, Sl=Sl: e.tensor_copy(out=stage[:, d, h, :], in_=Sl), reads=["S%s_%d" % (dk, si)], writes=["stage_%d_%d" % (d, h)])

    P.buf["mask"] = {"w": ("e", "pool", P.cnt["pool"]), "r": {}}
    for h in range(8):
        wh = whb[h % 2]
        whk = "wh0"
        for s5 in range(5):
            P.dma("pool", wh[:, :, s5, :], win_v[:, :, h + 8 * s5, :], "ld_" + whk, writes=[whk])
        for blk in range(4):
            proj_fm(wh, whk, 0, blk * 512, 512, lambda bap, bkey, blk=blk: P.op(
                "act", lambda e: e.copy(out=qT[:, blk * 512:(blk + 1) * 512], in_=bap), reads=[bkey, "qT"], writes=["qT"]))
        for blk in range(4):
            proj_fm(wh, whk, 4, blk * 512, 512, lambda bap, bkey, blk=blk: P.op(
                "act", lambda e: e.activation(out=gtmp[:, blk * 512:(blk + 1) * 512], in_=bap, func=AF.Silu), reads=[bkey, "ee"], writes=["ee"]))
        P.op("dve", lambda e: e.tensor_scalar(out=gsT, in0=gtmp, scalar1=hng[:, 0:1], scalar2=None, op0=ALU.mult), reads=["ee", "hng"], writes=["gsT"])
        P.dma("sp", dm_gs[h], gsT, "st_gs", reads=["gsT"], writes=["dm_gs"])
        for g4 in range(5):
            tiles = list(range(g4 * 4, min(18, g4 * 4 + 4)))
            bk = PB + (pbc[0] % 2)
            pbc[0] += 1

            def mmv(e, tiles=tiles, bk=bk, wh=wh):
                for i, ti in enumerate(tiles):
                    for kt in range(8):
                        r = e.matmul(banks[bk][:, i * 128:(i + 1) * 128], lhsT=uT[:, kt, ti * 128:(ti + 1) * 128], rhs=wh[:, kt, 1, :], start=(kt == 0), stop=(kt == 7))
                return r
            P.op("pe", mmv, reads=[whk] + ["uT_%d" % t for t in tiles], writes=["bank%d" % bk])
            nt_ = len(tiles)
            P.op("act", lambda e, tiles=tiles, bk=bk, nt_=nt_: e.copy(out=v_tm[:, tiles[0]:tiles[0] + nt_, :], in_=banks[bk][:, 0:nt_ * 128].rearrange("p (a b) -> p a b", b=128)),
                 reads=["bank%d" % bk, "v_tm"], writes=["v_tm"])
        hgrn_dir_prep(h, 0)
        hgrn_dir_prep(h, 1)
        gens = [hgrn_dir_scan(h, 0), hgrn_dir_scan(h, 1)]
        alive = [True, True]
        while any(alive):
            for i in range(2):
                if alive[i]:
                    try:
                        next(gens[i])
                    except StopIteration:
                        alive[i] = False
        P.dma("sp", dm_oloc[h], o_acc, "st_oloc", reads=["oacc_%d" % g for g in range(4)], writes=["dm_oloc"])
    for d in range(2):
        P.dma("sp", ag2_ins[d][:, 0:1024], stage[:, d, :, :].rearrange("p b c -> p (b c)"), "st_ag2", reads=["stage_%d_%d" % (d, h) for h in range(8)], writes=["ag2_in"])
        P.dma("sp", ag2_ins[d][:, 1024:1032], Dv[:, d, :], "st_ag2", reads=["Dv_%d_%d" % (d, h) for h in range(8)], writes=["ag2_in"])
    for d in range(2):
        P.custom("pool", lambda e, d=d: e.collective_compute("AllGather", ALU.bypass, replica_groups=[[0, 1, 2, 3], [4, 5, 6, 7]], ins=[ag2_ins[d]], outs=[ag2_outs[d]]),
                 "cc2", 1, reads=["ag2_in"] + (["ag2_out"] if d > 0 else []), writes=["ag2_out"])
    P.barrier(keep=["ag1_out", "dm_kvctx", "dm_mod", "ag2_out", "dm_oloc", "dm_qb", "dm_gs"] + UT_KEYS)
    A.release(m3h)
    gath = A.alloc((2, 4, 1032), F32)
    Rr = A.alloc((2, 8, 128), F32)
    Tt = A.alloc((8, 128), F32)
    selv = A.alloc((8,), F32)
    Sin = A.alloc((2, 8, 128), BF16)
    for d in range(2):
        P.dma("sp", gath[:, d, :, :], ag2_outs[d].rearrange("(r p) n -> p r n", p=128), "ld_gath", reads=["ag2_out"], writes=["gath"])
    P.dma("sp", selv, sel_d, "ld_c", writes=["selv"])
    SCK = ["sctx_%d_%d" % (d, h) for d in range(2) for h in range(8)]
    P.op("dve", lambda e: e.tensor_copy(out=Rr.rearrange("p a b c -> p (a b c)"), in_=sctx.rearrange("p a b c -> p (a b c)")), writes=["Rr"])
    for d in range(2):
        order = [0, 1, 2, 3] if d == 0 else [3, 2, 1, 0]
        for r in order:
            Sl = gath[:, d, r, 0:1024].rearrange("p (h v) -> p h v", v=128)
            Dr = gath[:, d, r, 1024:1032]
            P.op("dve", lambda e, d=d, Dr=Dr: e.tensor_tensor(out=Tt, in0=Rr[:, d, :, :], in1=bc(Dr, 2, [128, 8, 128]), op=ALU.mult), reads=["Rr", "gath"], writes=["Tt"])
            P.op("dve", lambda e, Sl=Sl: e.tensor_tensor(out=Tt, in0=Tt, in1=Sl, op=ALU.add), reads=["Tt", "gath"], writes=["Tt"])
            P.op("dve", lambda e, d=d: e.tensor_tensor(out=Tt, in0=Tt, in1=Rr[:, d, :, :], op=ALU.subtract), reads=["Tt", "Rr"], writes=["Tt"])
            P.op("dve", lambda e, d=d, r=r: e.scalar_tensor_tensor(out=Rr[:, d, :, :], in0=Tt, scalar=selv[:, d * 4 + r:d * 4 + r + 1], in1=Rr[:, d, :, :], op0=ALU.mult, op1=ALU.add),
                 reads=["Tt", "Rr", "selv"], writes=["Rr"])
    P.op("act", lambda e: e.copy(out=Sin.rearrange("p a b c -> p (a b c)"), in_=Rr.rearrange("p a b c -> p (a b c)")), reads=["Rr"], writes=["Sin"])
    ol = A.alloc((NT,), F32)
    qbf_ = A.alloc((NT,), BF16)
    qbb_ = A.alloc((NT,), BF16)
    gsl = A.alloc((NT,), BF16)
    sqb = A.alloc((NT,), BF16)
    lnr = A.alloc((NT,), F32)
    rsr = A.alloc((NT,), F32)
    for h in range(8):
        P.dma("sp", ol, dm_oloc[h], "ld_ol", reads=["dm_oloc"], writes=["ol"] + ["ol_%d" % b_ for b_ in range(4)])
        P.dma("sp", qbf_, dm_qb[0, h], "ld_qb0", reads=["dm_qb"], writes=["qbf_"])
        P.dma("sp", qbb_, dm_qb[1, h], "ld_qb1", reads=["dm_qb"], writes=["qbb_"])
        P.dma("sp", gsl, dm_gs[h], "ld_gs", reads=["dm_gs"], writes=["gsl"])
        for blk in range(4):
            cs = slice(blk * 512, (blk + 1) * 512)
            bk = blk % 2

            def corr(e, h=h, cs=cs, bk=bk):
                e.matmul(banks[bk][:, :], lhsT=Sin[:, 0, h, :], rhs=qbf_[:, cs], start=True, stop=False)
                return e.matmul(banks[bk][:, :], lhsT=Sin[:, 1, h, :], rhs=qbb_[:, cs], start=False, stop=True)
            P.op("pe", corr, reads=["Sin", "qbf_", "qbb_"], writes=["bank%d" % bk])
            P.op("dve", lambda e, cs=cs, bk=bk: e.tensor_tensor(out=ol[:, cs], in0=ol[:, cs], in1=banks[bk][:, :], op=ALU.add), reads=["bank%d" % bk, "ol", "ol_%d" % blk], writes=["ol_%d" % blk])
            P.op("act", lambda e, cs=cs: e.activation(out=sqb[:, cs], in_=ol[:, cs], func=AF.Square), reads=["ol_%d" % blk], writes=["sqb_%d" % blk])
            bk2 = 2 + blk % 2
            P.op("pe", lambda e, cs=cs, bk2=bk2: e.matmul(banks[bk2][:, :], lhsT=ones_b, rhs=sqb[:, cs], start=True, stop=True), reads=["sqb_%d" % blk, "ones_b"], writes=["bank%d" % bk2])
            P.op("act", lambda e, cs=cs, bk2=bk2: e.activation(out=lnr[:, cs], in_=banks[bk2][:, :], func=AF.Ln, scale=1.0 / 128, bias=EPS), reads=["bank%d" % bk2], writes=["lnr_%d" % blk])
            P.op("act", lambda e, cs=cs: e.activation(out=rsr[:, cs], in_=lnr[:, cs], func=AF.Exp, scale=-0.5), reads=["lnr_%d" % blk], writes=["rsr_%d" % blk])
            P.op("dve", lambda e, cs=cs: e.tensor_tensor(out=ol[:, cs], in0=ol[:, cs], in1=rsr[:, cs], op=ALU.mult), reads=["ol_%d" % blk, "rsr_%d" % blk], writes=["ol_%d" % blk])
            P.op("dve", lambda e, cs=cs: e.tensor_tensor(out=sqb[:, cs], in0=ol[:, cs], in1=gsl[:, cs], op=ALU.mult), reads=["ol_%d" % blk, "gsl"], writes=["sqb_%d" % blk])
        P.dma("sp", dm_oth[h], sqb, "st_oth", reads=["sqb_%d" % b_ for b_ in range(4)], writes=["dm_oth"])
        for b_ in range(4):
            for nm in ("ol_%d", "sqb_%d", "lnr_%d", "rsr_%d", "oth_%d"):
                pass
    P.barrier(keep=["ag1_out", "dm_kvctx", "dm_mod", "dm_oth"] + UT_KEYS)
    A.release(m3)
    if upto <= 3:
        return finish(P, nc)


    m4 = A.mark()
    QT = A.alloc((8, NT), BF16)
    KTa = A.alloc((2, 8448), BF16)
    Va = A.alloc((66, 256), BF16)
    for r in range(4):
        for h in range(2):
            P.dma("sp", KTa[:, h, r * NT:(r + 1) * NT], ag1_outs[h][r * 128:(r + 1) * 128, :], "ld_kta", reads=["ag1_out"], writes=["KTa"])
            P.dma("sp", Va[:, r * 16 + 8 * h:r * 16 + 8 * h + 8, :], ag1_outs[2 + h][r * 128:(r + 1) * 128, :].rearrange("p (ti c) -> p ti c", c=256), "ld_va", reads=["ag1_out"], writes=["Va"])
    P.dma("sp", KTa[:, :, 8192:8448], dm_kvctx[0:256, :].rearrange("(h p) t -> p h t", p=128), "ld_kta", reads=["dm_kvctx"], writes=["KTa"])
    P.dma("sp", Va[:, 64:66, :], dm_kvctx[256:512, :].rearrange("(ti p) c -> p ti c", p=128), "ld_va", reads=["dm_kvctx"], writes=["Va"])
    m4b = A.mark()
    wq = A.alloc((8, 1024), BF16)
    P.dma("pool", wq, win_d[:, 5120:6144].rearrange("(kt p) n -> p kt n", p=128), "ld_wq", writes=["wq"])
    ropeT = A.alloc((NTT, 256), F32)
    P.dma("sp", ropeT, rope_d.rearrange("(t p) n -> p t n", p=128), "ld_rope", writes=["ropeT"])
    gqk = A.alloc((256,), F32)
    P.dma("sp", gqk, qkg_d[0].partition_broadcast(128), "ld_c", writes=["gqk"])
    ssq = A.alloc((16, 8), F32)
    lnq = A.alloc((16, 8), F32)
    rsq = A.alloc((16, 8), F32)
    qn = A.alloc((8, 128), F32)
    t1q = A.alloc((8, 128), F32)
    t2q = A.alloc((8, 128), F32)
    qbf = A.alloc((1024,), BF16)
    junk2 = A.alloc((128,), BF16)
    P.op("pool", lambda e: e.memset(ssq, 0.0), writes=["ssq"])
    for ti in range(NTT):
        s = ti % 2
        for half in range(2):
            def mmq(e, ti=ti, half=half):
                for kt in range(8):
                    r = e.matmul(banks[half][:, :], lhsT=uT[:, kt, ti * 128:(ti + 1) * 128], rhs=wq[:, kt, half * 512:(half + 1) * 512], start=(kt == 0), stop=(kt == 7))
                return r
            P.op("pe", mmq, reads=["uT_%d" % ti, "wq"], writes=["bank%d" % half])
        for h in range(8):
            P.op("act", lambda e, h=h, ti=ti: e.activation(out=junk2, in_=banks[h // 4][:, (h % 4) * 128:(h % 4 + 1) * 128], func=AF.Square, accum_out=ssq[:, ti, h:h + 1]),
                 reads=["bank%d" % (h // 4), "ssq"], writes=["junk2", "ssq_%d" % ti])
        rstd_from_ss(ssq[:, ti, :], 128, rsq[:, ti, :], lnq[:, ti, :], ["ssq_%d" % ti], "rsq_%d" % ti)
        for half in range(2):
            P.op("dve", lambda e, half=half, ti=ti: e.tensor_tensor(out=qn[:, half * 4:(half + 1) * 4, :], in0=banks[half][:, :].rearrange("p (h d) -> p h d", d=128),
                                                                in1=bc(rsq[:, ti, half * 4:(half + 1) * 4], 2, [128, 4, 128]), op=ALU.mult),
                 reads=["bank%d" % half, "rsq_%d" % ti, "qxn"], writes=["qxn"])
        P.op("dve", lambda e: e.tensor_tensor(out=qn, in0=qn, in1=bc(gqk[:, 0:128], 1, [128, 8, 128]), op=ALU.mult), reads=["qxn", "gqk"], writes=["qxn"])
        rope_apply(qn, 8, ti, qbf, "q")
        bk2 = 2 + s

        def trq(e, bk2=bk2):
            for h in range(8):
                r = e.transpose(bank_bf(bk2)[:, h * 128:(h + 1) * 128], qbf[:, h * 128:(h + 1) * 128], ident_b)
            return r
        P.op("pe", trq, reads=["qbf", "ident_b"], writes=["bank%d" % bk2])
        P.op("act", lambda e, ti=ti, bk2=bk2: e.copy(out=QT[:, :, ti * 128:(ti + 1) * 128], in_=bank_bf(bk2).rearrange("p (a b) -> p a b", b=128)),
             reads=["bank%d" % bk2], writes=["QT"])
    P.barrier(keep=["dm_mod", "dm_oth", "KTa", "Va"] + UT_KEYS)
    A.release(m4b)
    if upto <= 4:
        return finish(P, nc)

    pT = [A.alloc((512,), BF16) for _ in range(4)]
    rden = A.alloc((512,), F32)
    ob = [A.alloc((512,), BF16) for _ in range(2)]
    dacc = [A.alloc((512,), F32) for _ in range(2)]
    SCALE = float(128 ** -0.5)
    NKT = 66
    it = 0
    for kvh in range(2):
        for qb in range(4):
            for g in range(4):
                head = kvh * 4 + g
                bo, bd = 4 + it % 2, 6 + it % 2
                qs = slice(qb * 512, (qb + 1) * 512)

                def s_mm(kt, kvh=kvh, head=head, qs=qs):
                    P.op("pe", lambda e: e.matmul(banks[kt % 4][:, :], lhsT=KTa[:, kvh, kt * 128:(kt + 1) * 128], rhs=QT[:, head, qs], start=True, stop=True),
                         reads=["KTa", "QT"], writes=["bank%d" % (kt % 4)])
                s_mm(0)
                s_mm(1)
                s_mm(2)
                for kt in range(NKT):
                    if kt + 3 < NKT:
                        s_mm(kt + 3)
                    P.op("act", lambda e, kt=kt: e.activation(out=pT[kt % 4], in_=banks[kt % 4][:, :], func=AF.Exp, scale=SCALE),
                         reads=["bank%d" % (kt % 4)], writes=["pT%d" % (kt % 4)])

                    P.op("pe", lambda e, kt=kt, kvh=kvh, bo=bo: e.matmul(banks[bo][:, :], lhsT=Va[:, kt, kvh * 128:(kvh + 1) * 128], rhs=pT[kt % 4], start=(kt == 0), stop=(kt == NKT - 1)),
                         reads=["pT%d" % (kt % 4), "Va"], writes=["bank%d" % bo])
                    da = dacc[0]
                    if kt % 3 == 2:
                        P.op("pe", lambda e, kt=kt, bd=bd: e.matmul(banks[bd][:, :], lhsT=ones_b, rhs=pT[kt % 4], start=(kt == 2), stop=False),
                             reads=["pT%d" % (kt % 4), "ones_b"], writes=["bank%d" % bd])
                    elif kt == 0:
                        P.op("dve", lambda e, kt=kt, da=da: e.tensor_copy(out=da, in_=pT[kt % 4]), reads=["pT%d" % (kt % 4)], writes=["dacc0"])
                    else:
                        P.op("dve", lambda e, kt=kt, da=da: e.tensor_tensor(out=da, in0=da, in1=pT[kt % 4], op=ALU.add), reads=["pT%d" % (kt % 4), "dacc0"], writes=["dacc0"])

                def dsum(e, bd=bd):
                    return e.matmul(banks[bd][:, :], lhsT=ones_f, rhs=dacc[0], start=False, stop=True)
                P.op("pe", dsum, reads=["dacc0", "ones_f"], writes=["bank%d" % bd])
                P.op("dve", lambda e, bd=bd: e.reciprocal(out=rden, in_=banks[bd][:, :]), reads=["bank%d" % bd], writes=["rden"])
                P.op("dve", lambda e, bo=bo, it=it: e.tensor_tensor(out=ob[it % 2], in0=banks[bo][:, :], in1=rden, op=ALU.mult), reads=["bank%d" % bo, "rden"], writes=["ob%d" % (it % 2)])
                P.dma("sp", dm_ota[head][:, qs], ob[it % 2], "st_ota%d" % (it % 2), reads=["ob%d" % (it % 2)], writes=["dm_ota"])
                it += 1
    P.barrier(keep=["dm_mod", "dm_oth", "dm_ota"] + UT_KEYS)
    A.release(m4)
    if upto <= 5:
        return finish(P, nc)

    m6 = A.mark()
    wg = A.alloc((8, 2048), BF16)
    wb0 = A.alloc((8, 1024), BF16)
    wb1 = A.alloc((8, 1024), BF16)
    wo = A.alloc((8, 1024), BF16)
    P.dma("pool", wg, win_d[:, 6656:8704].rearrange("(kt p) n -> p kt n", p=128), "ld_w6", writes=["wg"])
    P.dma("pool", wb0, wbr_d[0].rearrange("(kt p) n -> p kt n", p=128), "ld_w6", writes=["wb0"])
    P.dma("pool", wb1, wbr_d[1].rearrange("(kt p) n -> p kt n", p=128), "ld_w6", writes=["wb1"])
    P.dma("pool", wo, wout_d.rearrange("(kt p) n -> p kt n", p=128), "ld_w6", writes=["wo"])
    G1 = A.alloc((D,), F32)
    A2 = A.alloc((D,), F32)
    B2 = A.alloc((D,), F32)
    P.dma("sp", G1, dm_mod[:, 2 * D:3 * D], "ld_c", reads=["dm_mod"], writes=["G1"])
    P.dma("sp", A2, dm_mod[:, 4 * D:5 * D], "ld_c", reads=["dm_mod"], writes=["A2"])
    P.dma("sp", B2, dm_mod[:, 3 * D:4 * D], "ld_c", reads=["dm_mod"], writes=["B2"])
    rwt = A.alloc((8, NE), F32)
    rbt = A.alloc((NE,), F32)
    P.dma("sp", rwt, rw_d.rearrange("(kt p) e -> p kt e", p=128), "ld_c", writes=["rwt"])
    P.dma("sp", rbt, rb_d[0].partition_broadcast(128), "ld_c", writes=["rbt"])
    othb = A.alloc((8, 512), BF16)
    otab = A.alloc((8, 512), BF16)
    y1T = A.alloc((8, 512), BF16)
    sgh = [A.alloc((512,), F32)] * 2
    sga = [A.alloc((512,), F32)] * 2
    tA = [A.alloc((512,), F32)] * 2
    tB = [A.alloc((512,), F32)] * 2
    xt6 = [A.alloc((D,), F32) for _ in range(2)]
    tmp6 = A.alloc((D,), F32)
    x1t = [A.alloc((D,), F32)] * 2
    u2f = A.alloc((D,), F32)
    u2b = A.alloc((D,), BF16)
    junk6 = A.alloc((D,), BF16)
    ssy = A.alloc((16, 2), F32)
    ssy1 = A.alloc((16,), F32)
    lny = A.alloc((16,), F32)
    rsy = A.alloc((16,), F32)
    ssx = A.alloc((16,), F32)
    lnx = A.alloc((16,), F32)
    rsx = A.alloc((16,), F32)
    u2Tf = A.alloc((8, 128), F32)
    u2Tb = [A.alloc((8, 128), BF16) for _ in range(2)]
    lg = A.alloc((NE,), F32)
    mx8 = A.alloc((8,), F32)
    msk = A.alloc((NE,), F32)
    em = A.alloc((NE,), F32)
    nmx = A.alloc((1,), F32)
    ssum = A.alloc((1,), F32)
    rsum = A.alloc((1,), F32)
    cmb = A.alloc((16, NE), F32)
    cT = A.alloc((128,), F32)
    P.op("pool", lambda e: e.memset(ssy, 0.0), writes=["ssy"])
    P.op("pool", lambda e: e.memset(ssx, 0.0), writes=["ssx"])
    oth_v = dm_oth.rearrange("h p t -> p h t")
    ota_v = dm_ota.rearrange("h p t -> p h t")
    for blk in range(4):
        cs = slice(blk * 512, (blk + 1) * 512)
        P.dma("sp", othb, oth_v[:, :, cs], "ld_oth", reads=["dm_oth"], writes=["othb"])
        P.dma("sp", otab, ota_v[:, :, cs], "ld_ota", reads=["dm_ota"], writes=["otab"])
        utk = ["uT_%d" % t for t in range(blk * 4, blk * 4 + 4)]
        for dt in range(8):
            s = 0
            ds = slice(dt * 128, (dt + 1) * 128)

            b0 = 4 * (dt % 2)

            def mm4(e, ds=ds, dt=dt, cs=cs, b0=b0):
                for kt in range(8):
                    e.matmul(banks[b0][:, :], lhsT=wg[:, kt, dt * 128:(dt + 1) * 128], rhs=uT[:, kt, cs], start=(kt == 0), stop=(kt == 7))
                for kt in range(8):
                    e.matmul(banks[b0 + 1][:, :], lhsT=wg[:, kt, 1024 + dt * 128:1024 + (dt + 1) * 128], rhs=uT[:, kt, cs], start=(kt == 0), stop=(kt == 7))
                for kt in range(8):
                    e.matmul(banks[b0 + 2][:, :], lhsT=wb0[:, kt, ds], rhs=othb[:, kt, :], start=(kt == 0), stop=(kt == 7))
                for kt in range(8):
                    r = e.matmul(banks[b0 + 3][:, :], lhsT=wb1[:, kt, ds], rhs=otab[:, kt, :], start=(kt == 0), stop=(kt == 7))
                return r
            P.op("pe", mm4, reads=["wg", "wb0", "wb1", "othb", "otab"] + utk, writes=["bank%d" % (b0 + i) for i in range(4)])
            P.op("act", lambda e, s=s, b0=b0: e.activation(out=sgh[s], in_=banks[b0][:, :], func=AF.Sigmoid), reads=["bank%d" % b0], writes=["sgh%d" % s])
            P.op("act", lambda e, s=s, b0=b0: e.activation(out=sga[s], in_=banks[b0 + 1][:, :], func=AF.Sigmoid), reads=["bank%d" % (b0 + 1)], writes=["sga%d" % s])
            P.op("dve", lambda e, s=s, b0=b0: e.tensor_tensor(out=tA[s], in0=sgh[s], in1=banks[b0 + 2][:, :], op=ALU.mult), reads=["sgh%d" % s, "bank%d" % (b0 + 2)], writes=["tA%d" % s])
            P.op("dve", lambda e, s=s, b0=b0: e.tensor_tensor(out=tB[s], in0=sga[s], in1=banks[b0 + 3][:, :], op=ALU.mult), reads=["sga%d" % s, "bank%d" % (b0 + 3)], writes=["tB%d" % s])
            P.op("dve", lambda e, s=s, dt=dt: e.tensor_tensor(out=y1T[:, dt, :], in0=tA[s], in1=tB[s], op=ALU.add), reads=["tA%d" % s, "tB%d" % s], writes=["y1T"])
        for tt in range(4):
            ti = blk * 4 + tt
            s = ti % 2
            ts_ = slice(tt * 128, (tt + 1) * 128)
            P.dma("sp", xt6[s], x_d[ti * 128:(ti + 1) * 128, :], "ld_x6%d" % s, writes=["xt6%d" % s])
            for half in range(2):
                def mmy(e, half=half, ts_=ts_):
                    for kt in range(8):
                        r = e.matmul(banks[4 + half][:, :], lhsT=y1T[:, kt, ts_], rhs=wo[:, kt, half * 512:(half + 1) * 512], start=(kt == 0), stop=(kt == 7))
                    return r
                P.op("pe", mmy, reads=["y1T", "wo"], writes=["bank%d" % (4 + half)])
                P.op("act", lambda e, half=half, ti=ti: e.activation(out=junk6[:, 0:512], in_=banks[4 + half][:, :], func=AF.Square, accum_out=ssy[:, ti, half:half + 1]),
                     reads=["bank%d" % (4 + half), "ssy"], writes=["junk6", "ssy_%d_%d" % (ti, half)])
            P.op("dve", lambda e, ti=ti: e.tensor_tensor(out=ssy1[:, ti:ti + 1], in0=ssy[:, ti, 0:1], in1=ssy[:, ti, 1:2], op=ALU.add),
                 reads=["ssy_%d_0" % ti, "ssy_%d_1" % ti], writes=["ssy1_%d" % ti])
            rstd_from_ss(ssy1[:, ti:ti + 1], D, rsy[:, ti:ti + 1], lny[:, ti:ti + 1], ["ssy1_%d" % ti], "rsy_%d" % ti)
            for half in range(2):
                hs = slice(half * 512, (half + 1) * 512)
                P.op("dve", lambda e, half=half, hs=hs, ti=ti: e.scalar_tensor_tensor(out=tmp6[:, hs], in0=banks[4 + half][:, :], scalar=rsy[:, ti:ti + 1], in1=G1[:, hs], op0=ALU.mult, op1=ALU.mult),
                     reads=["bank%d" % (4 + half), "rsy_%d" % ti, "G1", "tmp6"], writes=["tmp6"])
            P.op("dve", lambda e, s=s: e.tensor_tensor(out=x1t[s], in0=tmp6, in1=xt6[s], op=ALU.add), reads=["tmp6", "xt6%d" % s], writes=["x1t"])
            P.dma("sp", dm_x1[ti * 128:(ti + 1) * 128, :], x1t[s], "st_x1%d" % s, reads=["x1t"], writes=["dm_x1"])
            P.op("act", lambda e, s=s, ti=ti: e.activation(out=junk6, in_=x1t[s], func=AF.Square, accum_out=ssx[:, ti:ti + 1]), reads=["x1t", "ssx"], writes=["junk6", "ssx_%d" % ti])
            rstd_from_ss(ssx[:, ti:ti + 1], D, rsx[:, ti:ti + 1], lnx[:, ti:ti + 1], ["ssx_%d" % ti], "rsx_%d" % ti)
            P.op("dve", lambda e, s=s, ti=ti: e.scalar_tensor_tensor(out=tmp6, in0=x1t[s], scalar=rsx[:, ti:ti + 1], in1=A2, op0=ALU.mult, op1=ALU.mult),
                 reads=["x1t", "rsx_%d" % ti, "A2", "tmp6"], writes=["tmp6"])
            P.op("dve", lambda e: e.tensor_tensor(out=u2f, in0=tmp6, in1=B2, op=ALU.add), reads=["tmp6", "B2"], writes=["u2f"])
            P.op("act", lambda e: e.copy(out=u2b, in_=u2f), reads=["u2f"], writes=["u2b"])

            def tru(e):
                for kt in range(8):
                    r = e.transpose(bank_bf(6)[:, kt * 128:(kt + 1) * 128], u2b[:, kt * 128:(kt + 1) * 128], ident_b)
                return r
            P.op("pe", tru, reads=["u2b", "ident_b"], writes=["bank6"])
            P.op("act", lambda e, s=s: e.copy(out=u2Tb[s], in_=bank_bf(6).rearrange("p (a b) -> p a b", b=128)), reads=["bank6"], writes=["u2Tb%d" % s])
            P.dma("sp", dm_u2t[:, :, ti * 128:(ti + 1) * 128], u2Tb[s], "st_u2t%d" % s, reads=["u2Tb%d" % s], writes=["dm_u2t"])
            for g2 in range(2):
                def truf(e, g2=g2):
                    for i in range(4):
                        kt = g2 * 4 + i
                        r = e.transpose(banks[7][:, i * 128:(i + 1) * 128], u2f[:, kt * 128:(kt + 1) * 128], ident_f)
                    return r
                P.op("pe", truf, reads=["u2f", "ident_f"], writes=["bank7"])
                P.op("act", lambda e, g2=g2: e.copy(out=u2Tf[:, g2 * 4:(g2 + 1) * 4, :], in_=banks[7][:, :].rearrange("p (a b) -> p a b", b=128)), reads=["bank7", "u2Tf"], writes=["u2Tf"])

            def mml(e):
                for kt in range(8):
                    r = e.matmul(banks[6][:, 0:NE], lhsT=u2Tf[:, kt, :], rhs=rwt[:, kt, :], start=(kt == 0), stop=(kt == 7))
                return r
            P.op("pe", mml, reads=["u2Tf", "rwt"], writes=["bank6"])
            P.op("dve", lambda e: e.tensor_tensor(out=lg, in0=banks[6][:, 0:NE], in1=rbt, op=ALU.add), reads=["bank6", "rbt"], writes=["lg"])
            P.op("dve", lambda e: e.max(out=mx8, in_=lg), reads=["lg"], writes=["mx8"])
            P.op("dve", lambda e: e.tensor_scalar(out=msk, in0=lg, scalar1=mx8[:, 3:4], scalar2=None, op0=ALU.is_ge), reads=["lg", "mx8"], writes=["msk"])
            P.op("dve", lambda e: e.tensor_scalar(out=nmx, in0=mx8[:, 0:1], scalar1=-1.0, scalar2=None, op0=ALU.mult), reads=["mx8"], writes=["nmx"])
            P.op("act", lambda e: e.activation(out=em, in_=lg, func=AF.Exp, bias=nmx[:, 0:1], scale=1.0), reads=["lg", "nmx"], writes=["em"])
            P.op("dve", lambda e: e.tensor_tensor(out=em, in0=em, in1=msk, op=ALU.mult), reads=["em", "msk"], writes=["em"])
            P.op("dve", lambda e: e.reduce_sum(out=ssum, in_=em, axis=AX.X), reads=["em"], writes=["ssum"])
            P.op("dve", lambda e: e.reciprocal(out=rsum, in_=ssum), reads=["ssum"], writes=["rsum"])
            P.op("dve", lambda e, ti=ti: e.tensor_scalar(out=cmb[:, ti, :], in0=em, scalar1=rsum[:, 0:1], scalar2=None, op0=ALU.mult), reads=["em", "rsum"], writes=["cmb_%d" % ti])
            P.op("pe", lambda e, ti=ti: e.transpose(banks[7][0:NE, 0:128], cmb[:, ti, :], ident_f), reads=["cmb_%d" % ti, "ident_f"], writes=["bank7"])
            P.op("act", lambda e: e.copy(out=cT[0:NE, :], in_=banks[7][0:NE, 0:128]), reads=["bank7"], writes=["cT"])
            P.dma("sp", dm_combT[:, ti * 128:(ti + 1) * 128], cT[0:NE, :], "st_cT", reads=["cT"], writes=["dm_combT"])
    P.dma("sp", dm_comb, cmb, "st_cmb", reads=["cmb_%d" % t for t in range(16)], writes=["dm_comb"])
    P.barrier(keep=["dm_mod", "dm_x1", "dm_u2t", "dm_comb", "dm_combT"])
    A.release(m_pre_ut)
    if upto <= 6:
        return finish(P, nc)

    G2 = A.alloc((D,), F32)
    P.dma("sp", G2, dm_mod[:, 5 * D:6 * D], "ld_c", reads=["dm_mod"], writes=["G2"])
    bu = A.alloc((NE * 16,), F32)
    P.dma("sp", bu, bupT_d, "ld_c", writes=["bu"])
    bdn = A.alloc((D,), F32)
    P.dma("sp", bdn[0:NE, :], bdn_d, "ld_c", writes=["bdn"])
    cmb7 = A.alloc((16, NE), F32)
    P.dma("sp", cmb7, dm_comb, "ld_c", reads=["dm_comb"], writes=["cmb7"])
    P.op("dve", lambda e: e.tensor_scalar(out=cmb7, in0=cmb7, scalar1=1.0 / 1.702, scalar2=None, op0=ALU.mult), reads=["cmb7"], writes=["cmb7"])
    bu1 = A.alloc((NE * 16,), F32)
    P.op("dve", lambda e: e.tensor_scalar(out=bu1, in0=bu, scalar1=1.0, scalar2=None, op0=ALU.add), reads=["bu"], writes=["bu1"])
    cT2 = A.alloc((1024,), F32)
    u2T = A.alloc((8, 1024), BF16)
    acc = A.alloc((8, D), F32)
    wu = [A.alloc((8, 2 * D), BF16) for _ in range(2)]
    wd = [A.alloc((8, D), BF16) for _ in range(2)]
    aTraw = [A.alloc((4096,), BF16) for _ in range(2)]
    aT = [a.rearrange("p (a b) -> p a b", b=512) for a in aTraw]
    gc = [A.alloc((512,), F32) for _ in range(2)]
    sgm = [A.alloc((512,), F32) for _ in range(2)]
    lc = [A.alloc((512,), F32) for _ in range(2)]
    xt7 = [aTraw[0][:, 0:2048].bitcast(F32)] * 2
    tmp7 = aTraw[0][:, 2048:4096].bitcast(F32)
    ot7 = [aTraw[1][:, 0:2048].bitcast(F32)] * 2
    junk7 = aTraw[1][:, 2048:3072]
    ss7 = A.alloc((16,), F32)
    ln7 = A.alloc((16,), F32)
    rs7 = A.alloc((16,), F32)
    P.op("pool", lambda e: e.memset(ss7, 0.0), writes=["ss7"])
    ecount = 0
    for half in range(2):
        hc = slice(half * 1024, (half + 1) * 1024)
        P.dma("sp", u2T, dm_u2t[:, :, hc], "ld_u2t", reads=["dm_u2t"], writes=["u2T"])
        P.dma("sp", cT2[0:NE, :], dm_combT[:, hc], "ld_cT2", reads=["dm_combT"], writes=["cT2"])
        for tt in range(8):
            for dh in range(2):
                bk = 4 + (tt * 2 + dh) % 4
                P.op("pe", lambda e, tt=tt, dh=dh, bk=bk: e.matmul(banks[bk][:, :], lhsT=cT2[0:NE, tt * 128:(tt + 1) * 128], rhs=bdn[0:NE, dh * 512:(dh + 1) * 512], start=True, stop=True),
                     reads=["cT2", "bdn"], writes=["bank%d" % bk])
                P.op("act", lambda e, tt=tt, dh=dh, bk=bk: e.copy(out=acc[:, tt, dh * 512:(dh + 1) * 512], in_=banks[bk][:, :]), reads=["bank%d" % bk], writes=["acc_%d_%d" % (tt, dh)])
        for ex in range(NE):
            ws = ecount % 2
            ecount += 1
            P.dma("pool", wu[ws], wup_d[ex].rearrange("(kt p) n -> p kt n", p=128), "ld_wu%d" % ws, writes=["wu%d" % ws])
            P.dma("pool", wd[ws], wdn_d[ex].rearrange("(kt p) n -> p kt n", p=128), "ld_wd%d" % ws, writes=["wd%d" % ws])
            for blk in range(2):
                bs = slice(blk * 512, (blk + 1) * 512)
                ab = aT[blk % 2]
                abk = "aT%d" % (blk % 2)
                for g in range(8):
                    s = g % 2
                    bg, bl = g % 2, 2 + g % 2

                    def mmu(e, g=g, bg=bg, bl=bl, ws=ws, bs=bs):
                        for kt in range(8):
                            e.matmul(banks[bg][:, :], lhsT=wu[ws][:, kt, g * 128:(g + 1) * 128], rhs=u2T[:, kt, bs], start=(kt == 0), stop=(kt == 7))
                        for kt in range(8):
                            r = e.matmul(banks[bl][:, :], lhsT=wu[ws][:, kt, 1024 + g * 128:1024 + (g + 1) * 128], rhs=u2T[:, kt, bs], start=(kt == 0), stop=(kt == 7))
                        return r
                    P.op("pe", mmu, reads=["wu%d" % ws, "u2T"], writes=["bank%d" % bg, "bank%d" % bl])
                    P.op("dve", lambda e, s=s, bg=bg, ex=ex, g=g: e.tensor_scalar(out=gc[s], in0=banks[bg][:, :], scalar1=bu[:, ex * 16 + g:ex * 16 + g + 1], scalar2=7.0, op0=ALU.add, op1=ALU.min),
                         reads=["bank%d" % bg, "bu"], writes=["gc%d" % s])
                    P.op("act", lambda e, s=s: e.activation(out=sgm[s], in_=gc[s], func=AF.Silu, scale=1.702), reads=["gc%d" % s], writes=["sgm%d" % s])
                    P.op("dve", lambda e, s=s, bl=bl, ex=ex, g=g: e.tensor_scalar(out=lc[s], in0=banks[bl][:, :], scalar1=bu1[:, ex * 16 + 8 + g:ex * 16 + 8 + g + 1], scalar2=8.0, op0=ALU.add, op1=ALU.min),
                         reads=["bank%d" % bl, "bu1"], writes=["lc%d" % s])
                    P.op("dve", lambda e, s=s, g=g, ab=ab: e.scalar_tensor_tensor(out=ab[:, g, :], in0=lc[s], scalar=-6.0, in1=sgm[s], op0=ALU.max, op1=ALU.mult),
                         reads=["sgm%d" % s, "lc%d" % s], writes=[abk])
                for tt in range(4):
                    til = blk * 4 + tt
                    for dh in range(2):
                        bk = 4 + (tt * 2 + dh) % 4

                        def mmd(e, tt=tt, dh=dh, bk=bk, ws=ws, ab=ab):
                            for fk in range(8):
                                r = e.matmul(banks[bk][:, :], lhsT=ab[:, fk, tt * 128:(tt + 1) * 128], rhs=wd[ws][:, fk, dh * 512:(dh + 1) * 512], start=(fk == 0), stop=(fk == 7))
                            return r
                        P.op("pe", mmd, reads=[abk, "wd%d" % ws], writes=["bank%d" % bk])
                        ak = "acc_%d_%d" % (til, dh)
                        P.op("dve", lambda e, til=til, dh=dh, bk=bk, ex=ex, half=half: e.scalar_tensor_tensor(
                            out=acc[:, til, dh * 512:(dh + 1) * 512], in0=banks[bk][:, :], scalar=cmb7[:, half * 8 + til, ex:ex + 1], in1=acc[:, til, dh * 512:(dh + 1) * 512], op0=ALU.mult, op1=ALU.add),
                            reads=["bank%d" % bk, "cmb7", ak], writes=[ak])
        P.barrier(keep=["dm_x1", "dm_u2t", "dm_combT"])
        for tt in range(8):
            ti = half * 8 + tt
            s = 0
            P.dma("sp", xt7[s], dm_x1[ti * 128:(ti + 1) * 128, :], "ld_x7%d" % s, reads=["dm_x1"], writes=["xt7%d" % s])
            P.op("act", lambda e, tt=tt, ti=ti: e.activation(out=junk7, in_=acc[:, tt, :], func=AF.Square, accum_out=ss7[:, ti:ti + 1]),
                 reads=["acc_%d_0" % tt, "acc_%d_1" % tt, "ss7"], writes=["junk7", "ss7_%d" % ti])
            rstd_from_ss(ss7[:, ti:ti + 1], D, rs7[:, ti:ti + 1], ln7[:, ti:ti + 1], ["ss7_%d" % ti], "rs7_%d" % ti)
            P.op("dve", lambda e, tt=tt, ti=ti: e.scalar_tensor_tensor(out=tmp7, in0=acc[:, tt, :], scalar=rs7[:, ti:ti + 1], in1=G2, op0=ALU.mult, op1=ALU.mult),
                 reads=["acc_%d_0" % tt, "acc_%d_1" % tt, "rs7_%d" % ti, "G2"], writes=["tmp7"])
            P.op("dve", lambda e, s=s: e.tensor_tensor(out=ot7[s], in0=tmp7, in1=xt7[s], op=ALU.add), reads=["tmp7", "xt7%d" % s], writes=["ot7%d" % s])
            P.dma("sp", out_d[ti * 128:(ti + 1) * 128, :], ot7[s], "st_out%d" % s, reads=["ot7%d" % s], writes=["out"])
        P.barrier(keep=["dm_x1", "dm_u2t", "dm_combT"])

    finish(P, nc)
    return nc


def finish(P, nc):
    P.wait_all("sp")
    P.build()
    P.close()
    return nc


def _rope_table(j):
    t = np.arange(NT) + j * NT
    rows = (t // 64).astype(np.float32)
    cols = (t % 64).astype(np.float32)
    inv = (10000.0 ** (-np.arange(0, 64, 2, dtype=np.float32) / 64)).astype(np.float32)
    ar = rows[:, None] * inv[None, :]
    ac = cols[:, None] * inv[None, :]
    cr, sr, cc, sc = np.cos(ar), np.sin(ar), np.cos(ac), np.sin(ac)
    return np.concatenate([cr, cr, cc, cc, -sr, sr, -sc, sc], axis=1).astype(np.float32)


def make_in_maps(inp, small=False):
    f = lambda a: np.ascontiguousarray(np.asarray(a, dtype=np.float32))
    x, c, ctx, c_ctx = f(inp["x"]), f(inp["c"]), f(inp["ctx"]), f(inp["c_ctx"])
    shared = {
        "w_mod": f(inp["w_mod"][0]), "b_mod": f(inp["b_mod"][0]).reshape(1, -1),
        "norm_g": f(inp["norm_g"][0]).reshape(1, -1), "w_in": f(inp["w_in"][0]),
        "lbv": f(np.asarray(inp["hgrn_lb"]).reshape(2, 2, 8, 128).transpose(3, 0, 1, 2).reshape(128, 32)),
        "hng": f(inp["hgrn_norm_g"][0]).reshape(128, 1), "qkg": f(inp["qk_norm_g"][0]).reshape(1, 256),
        "w_branch": f(inp["w_branch"][0]), "w_out": f(inp["w_out"][0]),
        "router_w": f(inp["router_w"][0]), "router_b": f(inp["router_b"][0]).reshape(1, -1),
        "w_up": f(inp["w_up"][0]), "b_upT": f(np.asarray(inp["b_up"][0]).reshape(32, 16, 128).transpose(2, 0, 1).reshape(128, 512)),
        "w_down": f(inp["w_down"][0]), "b_down": f(inp["b_down"][0]),
    }
    ropes = [_rope_table(j) for j in range(4)]
    maps = []
    for core in range(8):
        b, j = core // 4, core % 4
        cvec = np.concatenate([c[b].reshape(8, 128).T, c_ctx.reshape(8, 128).T], axis=1)
        sel = np.zeros((128, 8), np.float32)
        for r in range(4):
            sel[:, r] = 1.0 if r < j else 0.0
            sel[:, 4 + r] = 1.0 if r > j else 0.0
        m = dict(shared)
        if small:
            m["w_up"] = m["w_up"][0:1]
            m["w_down"] = m["w_down"][0:1]
        m.update({"x": f(x[b, j * NT:(j + 1) * NT]), "ctx": f(ctx[b]), "cvec": f(cvec), "rope": ropes[j], "sel": sel})
        maps.append(m)
    return maps


_NC_CACHE = {}


def kernel(**inputs):
    if "nc" not in _NC_CACHE:
        _NC_CACHE["nc"] = build()
    nc = _NC_CACHE["nc"]
    maps = make_in_maps(inputs)
    res = run_bass_kernel_spmd(nc, maps, core_ids=list(range(8)))
    out = np.empty((2, 8192, D), np.float32)
    for core in range(8):
        b, j = core // 4, core % 4
        out[b, j * NT:(j + 1) * NT] = res.results[core]["out"]
    return out
```
